# Optimizing a Trainium2 kernel written in Bass

```python
import numpy as np
import jax
import jax.numpy as jnp
from jax import lax

D_MODEL = 1024
BATCH = 2
SEQ = 8192
DEPTH = 2

HEAD_DIM = 64
N_HEADS_MIX = 4
W_MIX = N_HEADS_MIX * HEAD_DIM
N_MIXERS = 4
QBLK = 128
ROPE_THETA = 500000.0
ROPE_DIMS = HEAD_DIM // 4
EPS = 1e-6
NEG = -1e30
BIG = 1e30

NSA_CMP_LEN = 32
NSA_CMP_STRIDE = 16
NSA_CMP_HIDDEN = 2 * HEAD_DIM
NSA_SEL_LEN = 64
NSA_TOPN = 16
NSA_WINDOW = 512

DIL_CONFIGS = ((128, 1), (512, 4), (2048, 16))

FOX_BIAS_INIT = 2.0

D_FF = 2816
N_EXPERTS = 8
TOP_K = 2
N_DENSE_LAYERS = (DEPTH + 1) // 2
N_MOE_LAYERS = DEPTH // 2

IN_SIZES = (W_MIX, 6 * HEAD_DIM, 3 * N_HEADS_MIX, 3 * W_MIX, 3 * W_MIX, 3 * W_MIX, N_HEADS_MIX, N_MIXERS * D_MODEL)
IN_COLS = sum(IN_SIZES)
POS_OFFSET_MAX = 4096

kernel_name = 'hybrid_nsa_dilated_stickbreak_fox_moe'


def rmsnorm(x, g):
    xf = x.astype(jnp.float32)
    y = xf * lax.rsqrt(jnp.mean(xf * xf, axis=-1, keepdims=True) + EPS)
    return (y * g).astype(x.dtype)


def rope_tables(positions):
    inv = ROPE_THETA ** (-jnp.arange(0, ROPE_DIMS, 2, dtype=jnp.float32) / ROPE_DIMS)
    ang = positions.astype(jnp.float32)[..., None] * inv
    return jnp.cos(ang), jnp.sin(ang)


def apply_rope(x, cos, sin):
    if x.ndim == 4:
        cos, sin = cos[:, :, None, :], sin[:, :, None, :]
    half = ROPE_DIMS // 2
    x1, x2, rest = x[..., :half], x[..., half:ROPE_DIMS], x[..., ROPE_DIMS:]
    rot = jnp.concatenate([x1 * cos - x2 * sin, x2 * cos + x1 * sin], axis=-1).astype(x.dtype)
    return jnp.concatenate([rot, rest], axis=-1)


def masked_softmax(s, mask):
    p = jax.nn.softmax(jnp.where(mask, s, NEG), axis=-1)
    return jnp.where(mask, p, 0.0)


def to_blocks(a):
    b, s = a.shape[:2]
    return jnp.moveaxis(a.reshape(b, s // QBLK, QBLK, *a.shape[2:]), 1, 0)


def from_blocks(a):
    a = jnp.moveaxis(a, 0, 1)
    return a.reshape(a.shape[0], -1, *a.shape[3:])


def compress(x, pe, w1, w2):
    b, s, dh = x.shape
    r = NSA_CMP_LEN // NSA_CMP_STRIDE
    ch = x.reshape(b, s // NSA_CMP_STRIDE, NSA_CMP_STRIDE, dh)
    nc = ch.shape[1] - r + 1
    blk = jnp.concatenate([ch[:, i:i + nc] for i in range(r)], axis=2) + pe
    return jax.nn.gelu(blk.reshape(b, nc, NSA_CMP_LEN * dh) @ w1) @ w2


def cmp_to_sel_overlap(nc, n_sel):
    c0 = np.arange(nc) * NSA_CMP_STRIDE
    c1 = c0 + NSA_CMP_LEN
    s0 = np.arange(n_sel) * NSA_SEL_LEN
    s1 = s0 + NSA_SEL_LEN
    return ((c0[:, None] < s1[None, :]) & (c1[:, None] > s0[None, :])).astype(np.float32)


def nsa_attention(q_nr, q_r, kc_raw, vc_raw, ks, vs, kw, vw, gate_logits, pe_k, pe_v, ck1, ck2, cv1, cv2):
    b, s, h, dh = q_r.shape
    scale = dh ** -0.5
    t_idx = jnp.arange(s)
    kc = compress(kc_raw, pe_k, ck1, ck2)
    vc = compress(vc_raw, pe_v, cv1, cv2)
    nc = kc.shape[1]
    c_end = jnp.arange(nc) * NSA_CMP_STRIDE + NSA_CMP_LEN - 1
    c_mask = c_end[None, :] <= t_idx[:, None]
    sc = jnp.einsum('bthd,bcd->bhtc', q_nr, kc).astype(jnp.float32) * scale
    p_cmp = masked_softmax(sc, c_mask)
    o_cmp = jnp.einsum('bhtc,bcd->bthd', p_cmp.astype(vc.dtype), vc)
    n_sel = s // NSA_SEL_LEN
    n_top = min(NSA_TOPN, n_sel)
    overlap = jnp.asarray(cmp_to_sel_overlap(nc, n_sel))
    imp = jnp.einsum('bhtc,cj->btj', p_cmp, overlap)
    j = jnp.arange(n_sel)[None, :]
    cur = (t_idx // NSA_SEL_LEN)[:, None]
    valid = j <= cur
    forced = (j == 0) | (j == cur) | (j == cur - 1)
    score = jnp.where(valid, jnp.where(forced, BIG, imp), NEG)
    _, sel = lax.top_k(score, n_top)
    ks_blk = ks.reshape(b, n_sel, NSA_SEL_LEN, dh)
    vs_blk = vs.reshape(b, n_sel, NSA_SEL_LEN, dh)
    gather = jax.vmap(lambda kb, ib: kb[ib])

    def sel_block(args):
        qb, ib, t0 = args
        t = t0 + jnp.arange(QBLK)
        kg = gather(ks_blk, ib)
        vg = gather(vs_blk, ib)
        sco = jnp.einsum('bqhd,bqnld->bhqnl', qb, kg).astype(jnp.float32) * scale
        kpos = ib[..., None] * NSA_SEL_LEN + jnp.arange(NSA_SEL_LEN)
        m = (kpos <= t[None, :, None, None]).reshape(b, 1, QBLK, -1)
        p = masked_softmax(sco.reshape(b, h, QBLK, -1), m).reshape(b, h, QBLK, n_top, NSA_SEL_LEN)
        return jnp.einsum('bhqnl,bqnld->bqhd', p.astype(vg.dtype), vg)

    starts = jnp.arange(s // QBLK) * QBLK
    o_sel = from_blocks(lax.map(sel_block, (to_blocks(q_r), to_blocks(sel), starts)))
    nb = s // QBLK
    wc = NSA_WINDOW // QBLK

    def band(a):
        ac = jnp.pad(a.reshape(b, nb, QBLK, dh), ((0, 0), (wc, 0), (0, 0), (0, 0)))
        return jnp.concatenate([ac[:, i:i + nb] for i in range(wc + 1)], axis=2)

    kb, vb = band(kw), band(vw)
    qpos = t_idx.reshape(nb, QBLK)
    kpos = jnp.arange(nb)[:, None] * QBLK - NSA_WINDOW + jnp.arange((wc + 1) * QBLK)[None, :]
    diff = qpos[:, :, None] - kpos[:, None, :]
    wmask = (kpos[:, None, :] >= 0) & (diff >= 0) & (diff < NSA_WINDOW)
    sw = jnp.einsum('bnqhd,bnkd->bhnqk', q_r.reshape(b, nb, QBLK, h, dh), kb).astype(jnp.float32) * scale
    pw = masked_softmax(sw, wmask)
    o_win = jnp.einsum('bhnqk,bnkd->bnqhd', pw.astype(vb.dtype), vb).reshape(b, s, h, dh)
    g = jax.nn.sigmoid(gate_logits).reshape(b, s, h, 3, 1)
    return g[:, :, :, 0] * o_cmp + g[:, :, :, 1] * o_sel + g[:, :, :, 2] * o_win


def dilated_attention(q, k, v):
    b, s, h, dh = q.shape
    scale = dh ** -0.5

    def blk(args):
        qb, t0 = args
        t = t0 + jnp.arange(QBLK)
        outs, lses = [], []
        for (w, d) in DIL_CONFIGS:
            m = jnp.arange(w // d + 1)
            idx = t[:, None] - m[None, :] * d
            valid = idx >= 0
            idx = jnp.maximum(idx, 0)
            kg, vg = k[:, idx], v[:, idx]
            sco = jnp.einsum('bqhd,bqmhd->bhqm', qb, kg).astype(jnp.float32) * scale
            sco = jnp.where(valid, sco, NEG)
            lse = jax.nn.logsumexp(sco, axis=-1)
            p = jnp.exp(sco - lse[..., None])
            outs.append(jnp.einsum('bhqm,bqmhd->bqhd', p.astype(vg.dtype), vg))
            lses.append(lse)
        wts = jax.nn.softmax(jnp.stack(lses), axis=0)
        return jnp.einsum('gbhq,gbqhd->bqhd', wts.astype(v.dtype), jnp.stack(outs))

    starts = jnp.arange(s // QBLK) * QBLK
    return from_blocks(lax.map(blk, (to_blocks(q), starts)))


def stick_breaking_attention(q, k, v):
    b, s, h, dh = q.shape
    scale = dh ** -0.5
    s_idx = jnp.arange(s)

    def blk(args):
        qb, t0 = args
        t = t0 + jnp.arange(QBLK)
        z = jnp.einsum('bqhd,bshd->bhqs', qb, k).astype(jnp.float32) * scale
        strict = s_idx[None, :] < t[:, None]
        log_1mb = jnp.where(strict, jax.nn.log_sigmoid(-z), 0.0)
        after = lax.cumsum(log_1mb, axis=3, reverse=True) - log_1mb
        a = jnp.where(strict, jnp.exp(jax.nn.log_sigmoid(z) + after), 0.0)
        return jnp.einsum('bhqs,bshd->bqhd', a.astype(v.dtype), v)

    starts = jnp.arange(s // QBLK) * QBLK
    return from_blocks(lax.map(blk, (to_blocks(q), starts)))


def forgetting_attention(q, k, v, log_f):
    b, s, h, dh = q.shape
    scale = dh ** -0.5
    fc = jnp.cumsum(log_f, axis=1)
    f_k = jnp.transpose(fc, (0, 2, 1))
    s_idx = jnp.arange(s)

    def blk(args):
        qb, fq, t0 = args
        t = t0 + jnp.arange(QBLK)
        sco = jnp.einsum('bqhd,bshd->bhqs', qb, k).astype(jnp.float32) * scale
        sco = sco + jnp.transpose(fq, (0, 2, 1))[..., None] - f_k[:, :, None, :]
        p = masked_softmax(sco, s_idx[None, :] <= t[:, None])
        return jnp.einsum('bhqs,bshd->bqhd', p.astype(v.dtype), v)

    starts = jnp.arange(s // QBLK) * QBLK
    return from_blocks(lax.map(blk, (to_blocks(q), to_blocks(fc), starts)))


def hybrid_mixer(h, cos, sin, w_in, qk_gain, pe_k, pe_v, ck1, ck2, cv1, cv2, fox_b, w_branch, w_out):
    b, s, _ = h.shape
    split_at = [int(v) for v in np.cumsum(IN_SIZES)[:-1]]
    a_q, a_kv, a_g, b_qkv, c_qkv, d_qkv, d_f, merge_logits = jnp.split(h @ w_in, split_at, axis=-1)
    heads = lambda t: t.reshape(b, s, N_HEADS_MIX, HEAD_DIM)
    q_a = rmsnorm(heads(a_q), qk_gain[0])
    kc, vc, ksl, vsl, kw, vw = jnp.split(a_kv, 6, axis=-1)
    o_a = nsa_attention(q_a, apply_rope(q_a, cos, sin), rmsnorm(kc, qk_gain[1]), vc,
                        apply_rope(rmsnorm(ksl, qk_gain[2]), cos, sin), vsl,
                        apply_rope(rmsnorm(kw, qk_gain[3]), cos, sin), vw,
                        a_g, pe_k, pe_v, ck1, ck2, cv1, cv2)
    qb, kb, vb = jnp.split(b_qkv, 3, axis=-1)
    o_b = dilated_attention(apply_rope(rmsnorm(heads(qb), qk_gain[4]), cos, sin),
                            apply_rope(rmsnorm(heads(kb), qk_gain[5]), cos, sin), heads(vb))
    qc, kc2, vc2 = jnp.split(c_qkv, 3, axis=-1)
    o_c = stick_breaking_attention(heads(qc), heads(kc2), heads(vc2))
    qd, kd, vd = jnp.split(d_qkv, 3, axis=-1)
    log_f = jax.nn.log_sigmoid((d_f + fox_b).astype(jnp.float32))
    o_d = forgetting_attention(rmsnorm(heads(qd), qk_gain[6]), rmsnorm(heads(kd), qk_gain[7]), heads(vd), log_f)
    o = jnp.stack([o_a, o_b, o_c, o_d]).reshape(N_MIXERS, b, s, W_MIX)
    y = jnp.einsum('mbsk,mkd->bsmd', o, w_branch)
    gates = jax.nn.sigmoid(merge_logits).reshape(b, s, N_MIXERS, D_MODEL)
    return jnp.sum(gates * y, axis=2) @ w_out


def swiglu(h, w1, w3, w2):
    return (jax.nn.silu(h @ w1) * (h @ w3)) @ w2


def moe_swiglu(h, router_w, w1, w3, w2):
    b, s, d = h.shape
    xt = h.reshape(-1, d)
    logits = (xt @ router_w).astype(jnp.float32)
    top_v, top_i = lax.top_k(logits, TOP_K)
    gate = jax.nn.softmax(top_v, axis=-1)
    e_flat = top_i.reshape(-1)
    order = jnp.argsort(e_flat)
    tok = order // TOP_K
    xs = xt[tok]
    sizes = jax.ops.segment_sum(jnp.ones_like(e_flat), e_flat, num_segments=N_EXPERTS).astype(jnp.int32)
    a = lax.ragged_dot(xs, w1, sizes)
    g = lax.ragged_dot(xs, w3, sizes)
    y = lax.ragged_dot(jax.nn.silu(a) * g, w2, sizes)
    y = y * gate.reshape(-1)[order][:, None].astype(y.dtype)
    return jnp.zeros_like(xt).at[tok].add(y).reshape(b, s, d)


def setup_inputs(seed: int = 0) -> dict:
    key = jax.random.key(seed)
    ks = jax.random.split(key, 28)
    f32 = jnp.float32

    def nrm(k, shape, fan_in, gain=1.0):
        return jax.random.normal(k, shape, f32) * (gain * fan_in ** -0.5)

    def gain(k, shape):
        return 1.0 + 0.02 * jax.random.normal(k, shape, f32)

    cl = NSA_CMP_LEN * HEAD_DIM
    return {
        'x': jax.random.normal(ks[0], (BATCH, SEQ, D_MODEL), f32),
        'c': jax.random.normal(ks[1], (BATCH, D_MODEL), f32),
        'positions': jnp.arange(SEQ, dtype=jnp.int32)[None, :] + jax.random.randint(ks[2], (BATCH, 1), 0, POS_OFFSET_MAX, dtype=jnp.int32),
        'w_ada': nrm(ks[3], (DEPTH, D_MODEL, 6 * D_MODEL), D_MODEL, 0.5),
        'b_ada': 0.02 * jax.random.normal(ks[4], (DEPTH, 6 * D_MODEL), f32),
        'norm_mix': gain(ks[5], (DEPTH, D_MODEL)),
        'norm_ffn': gain(ks[6], (DEPTH, D_MODEL)),
        'w_in': nrm(ks[7], (DEPTH, D_MODEL, IN_COLS), D_MODEL),
        'qk_gain': gain(ks[8], (DEPTH, 8, HEAD_DIM)),
        'nsa_pe_k': 0.02 * jax.random.normal(ks[9], (DEPTH, NSA_CMP_LEN, HEAD_DIM), f32),
        'nsa_pe_v': 0.02 * jax.random.normal(ks[10], (DEPTH, NSA_CMP_LEN, HEAD_DIM), f32),
        'nsa_ck_w1': nrm(ks[11], (DEPTH, cl, NSA_CMP_HIDDEN), cl),
        'nsa_ck_w2': nrm(ks[12], (DEPTH, NSA_CMP_HIDDEN, HEAD_DIM), NSA_CMP_HIDDEN),
        'nsa_cv_w1': nrm(ks[13], (DEPTH, cl, NSA_CMP_HIDDEN), cl),
        'nsa_cv_w2': nrm(ks[14], (DEPTH, NSA_CMP_HIDDEN, HEAD_DIM), NSA_CMP_HIDDEN),
        'fox_bias': FOX_BIAS_INIT + 0.1 * jax.random.normal(ks[15], (DEPTH, N_HEADS_MIX), f32),
        'w_branch': nrm(ks[16], (DEPTH, N_MIXERS, W_MIX, D_MODEL), W_MIX),
        'w_out': nrm(ks[17], (DEPTH, D_MODEL, D_MODEL), D_MODEL),
        'ffn_w1': nrm(ks[18], (N_DENSE_LAYERS, D_MODEL, D_FF), D_MODEL),
        'ffn_w3': nrm(ks[19], (N_DENSE_LAYERS, D_MODEL, D_FF), D_MODEL),
        'ffn_w2': nrm(ks[20], (N_DENSE_LAYERS, D_FF, D_MODEL), D_FF),
        'router_w': nrm(ks[21], (N_MOE_LAYERS, D_MODEL, N_EXPERTS), D_MODEL),
        'moe_w1': nrm(ks[22], (N_MOE_LAYERS, N_EXPERTS, D_MODEL, D_FF), D_MODEL),
        'moe_w3': nrm(ks[23], (N_MOE_LAYERS, N_EXPERTS, D_MODEL, D_FF), D_MODEL),
        'moe_w2': nrm(ks[24], (N_MOE_LAYERS, N_EXPERTS, D_FF, D_MODEL), D_FF),
    }


def reference(x, c, positions, w_ada, b_ada, norm_mix, norm_ffn, w_in, qk_gain, nsa_pe_k, nsa_pe_v,
              nsa_ck_w1, nsa_ck_w2, nsa_cv_w1, nsa_cv_w2, fox_bias, w_branch, w_out,
              ffn_w1, ffn_w3, ffn_w2, router_w, moe_w1, moe_w3, moe_w2):
    cos, sin = rope_tables(positions)
    c_act = jax.nn.silu(c)
    for l in range(DEPTH):
        mod = (c_act @ w_ada[l] + b_ada[l])[:, None, :]
        sh_a, sc_a, g_a, sh_f, sc_f, g_f = jnp.split(mod, 6, axis=-1)
        h = rmsnorm(x, norm_mix[l]) * (1.0 + sc_a) + sh_a
        x = x + g_a * hybrid_mixer(h, cos, sin, w_in[l], qk_gain[l], nsa_pe_k[l], nsa_pe_v[l],
                                   nsa_ck_w1[l], nsa_ck_w2[l], nsa_cv_w1[l], nsa_cv_w2[l],
                                   fox_bias[l], w_branch[l], w_out[l])
        h = rmsnorm(x, norm_ffn[l]) * (1.0 + sc_f) + sh_f
        if l % 2 == 0:
            f = swiglu(h, ffn_w1[l // 2], ffn_w3[l // 2], ffn_w2[l // 2])
        else:
            f = moe_swiglu(h, router_w[l // 2], moe_w1[l // 2], moe_w3[l // 2], moe_w2[l // 2])
        x = x + g_f * f
    return x
```

```python
import contextlib
import numpy as np
import concourse.bass as bass
import concourse.mybir as mybir
from concourse.bass_utils import run_bass_kernel_spmd

F32 = mybir.dt.float32
BF16 = mybir.dt.bfloat16
I32 = mybir.dt.int32
AF = mybir.ActivationFunctionType
ALU = mybir.AluOpType
AX = mybir.AxisListType


class Res:
    __slots__ = ("w", "r")

    def __init__(self):
        self.w = None
        self.r = {}


class Sched:
    NDMA = 24

    def __init__(self, nc, es):
        self.nc = nc
        self.engs = {"pe": nc.tensor, "act": nc.scalar, "dve": nc.vector,
                     "pool": nc.gpsimd, "sp": nc.sync}
        self.sem = {}
        self.cnt = {}
        for k in self.engs:
            self.sem[k] = es.enter_context(nc.semaphore("s_" + k))
            self.cnt[k] = 0
        for i in range(self.NDMA):
            k = "d%d" % i
            self.sem[k] = es.enter_context(nc.semaphore("s_" + k))
            self.cnt[k] = 0
        self.seen = {k: {} for k in self.engs}
        self.dma_rr = 0
        self.out_events = []

    def _wait(self, e, ev):
        if ev is None:
            return
        key, val = ev
        if key == e and e == "pe":
            return
        if self.seen[e].get(key, 0) >= val:
            return
        self.engs[e].wait_ge(self.sem[key], val)
        self.seen[e][key] = val

    def _deps(self, e, reads, writes):
        for r in reads:
            self._wait(e, r.w)
        for r in writes:
            self._wait(e, r.w)
            for k, v in r.r.items():
                self._wait(e, (k, v))

    def op(self, e, fn, reads=(), writes=()):
        self._deps(e, reads, writes)
        ins = fn()
        self.cnt[e] += 1
        ins.then_inc(self.sem[e], 1)
        ev = (e, self.cnt[e])
        for r in reads:
            r.r[e] = ev[1]
        for r in writes:
            r.w = ev
            r.r = {}
        return ev

    def dma(self, out, in_, reads=(), writes=(), q="sp", is_output=False, **kw):
        k = "d%d" % self.dma_rr
        self.dma_rr = (self.dma_rr + 1) % self.NDMA
        self._wait(q, (k, self.cnt[k]))
        self._deps(q, reads, writes)
        ins = self.engs[q].dma_start(out=out, in_=in_, **kw)
        self.cnt[k] += 16
        ins.then_inc(self.sem[k], 16)
        ev = (k, self.cnt[k])
        for r in reads:
            r.r[k] = ev[1]
        for r in writes:
            r.w = ev
            r.r = {}
        if is_output:
            self.out_events.append(ev)
        return ev

    def finish(self):
        for i in range(self.NDMA):
            k = "d%d" % i
            self._wait("sp", (k, self.cnt[k]))
        for k in ("pe", "act", "dve", "pool"):
            self._wait("sp", (k, self.cnt[k]))


class Tile:
    def __init__(self, t):
        self.t = t
        self.res = Res()

    def __getitem__(self, idx):
        return self.t[idx]


class Ctx:
    def __init__(self, name="k"):
        self.nc = bass.Bass("TRN2", target_bir_lowering=False)
        self.es = contextlib.ExitStack()
        self.s = Sched(self.nc, self.es)
        self.n = 0

    def sb(self, shape, dt, name=None):
        self.n += 1
        return Tile(self.es.enter_context(self.nc.sbuf_tensor(name or ("t%d" % self.n), list(shape), dt)))

    def ps(self, shape, dt=F32, name=None):
        self.n += 1
        return Tile(self.es.enter_context(self.nc.psum_tensor(name or ("p%d" % self.n), list(shape), dt)))

    pre = ""
    alias = None
    kind_override = None

    def dram(self, name, shape, dt, kind):
        full = self.pre + name
        if self.alias and full in self.alias:
            return self.alias[full]
        if self.kind_override and full in self.kind_override:
            kind = self.kind_override[full]
        ap = self.nc.dram_tensor(full, list(shape), dt, kind=kind).ap()
        if self.alias is None:
            self.alias = {}
        self.made = getattr(self, "made", {})
        self.made[full] = ap
        return ap

    def begin_phase(self):
        es = contextlib.ExitStack()
        old, self.es = self.es, es
        return (old, es)

    def end_phase(self, ph):
        barrier(self)
        self.es = ph[0]
        ph[1].close()

    def close(self):
        self.s.finish()
        self.es.close()


D = 1024
S = 8192
NB = 2
NT = 2048
TT = 512
NCH_IN = 57
EPS = 1e-6
TWO_PI = float(2 * np.pi)
C1_2PI = 6.28125
C2_2PI = TWO_PI - C1_2PI


def barrier(c):
    s = c.s
    for e in ("pe", "act", "dve", "pool", "sp"):
        for k in list(s.cnt.keys()):
            if k != e:
                s._wait(e, (k, s.cnt[k]))


def build_M():
    c = Ctx()
    nc, s = c.nc, c.s
    cT_d = c.dram("cT", [128, 8, 2], F32, "ExternalInput")
    w_d = c.dram("w", [12, 128, 8, 128], F32, "ExternalInput")
    b_d = c.dram("b", [128, 12], F32, "ExternalInput")
    o_d = c.dram("modT", [128, 12, 2], F32, "ExternalOutput")
    cT = c.sb([128, 8, 2], F32)
    ca = c.sb([128, 8, 2], F32)
    bt = c.sb([128, 12], F32)
    ot = c.sb([128, 12, 2], F32)
    s.dma(cT[:], cT_d, writes=[cT.res])
    s.dma(bt[:], b_d, writes=[bt.res])
    s.op("act", lambda: nc.scalar.activation(out=ca[:], in_=cT[:], func=AF.Silu), reads=[cT.res], writes=[ca.res])
    wts = [c.sb([128, 8, 128], F32) for _ in range(3)]
    pm = c.ps([128, 12, 2])
    for j in range(12):
        wt = wts[j % 3]
        s.dma(wt[:], w_d[j], writes=[wt.res])
        for k in range(8):
            s.op("pe", (lambda wt=wt, k=k, j=j: nc.tensor.matmul(pm[:, j, :], lhsT=wt[:, k, :], rhs=ca[:, k, :],
                                                                 start=(k == 0), stop=(k == 7))),
                 reads=[wt.res, ca.res], writes=[pm.res])
    for b in range(2):
        s.op("dve", (lambda b=b: nc.vector.tensor_tensor(out=ot[:, :, b], in0=pm[:, :, b], in1=bt[:], op=ALU.add)),
             reads=[pm.res, bt.res], writes=[ot.res])
    s.dma(o_d, ot[:], reads=[ot.res], is_output=True)
    c.close()
    return c


def run_M(inp):
    c = build_M()
    cT = np.ascontiguousarray(inp["c"].T.reshape(8, 128, 2).transpose(1, 0, 2))
    w_all = inp["w_ada"]
    b_all = inp["b_ada"]
    maps = []
    for i in range(8):
        chunks = [(g // 48, g % 48) for g in range(i * 12, i * 12 + 12)]
        w = np.stack([w_all[l][:, n * 128:(n + 1) * 128].reshape(8, 128, 128).transpose(1, 0, 2) for l, n in chunks])
        b = np.stack([b_all[l][n * 128:(n + 1) * 128] for l, n in chunks], axis=1)
        maps.append({"cT": cT, "w": np.ascontiguousarray(w), "b": np.ascontiguousarray(b)})
    res = run_bass_kernel_spmd(c.nc, maps, core_ids=list(range(8)))
    mod = np.zeros((2, 2, 128, 48), np.float32)
    for i in range(8):
        o = res.results[i]["modT"]
        for jj, g in enumerate(range(i * 12, i * 12 + 12)):
            mod[g // 48, :, :, g % 48] = o[:, jj, :].T
    return mod


def emit_modnorm(c, src, hT, ntok, ones, Acol, Bcol, epsb, tmp_ring, pbank, rs, after_h=None):
    nc, s = c.nc, c.s
    ntt = ntok // TT
    for tt in range(ntt):
        sl = slice(tt * TT, (tt + 1) * TT)
        for j in range(8):
            sq = tmp_ring[j % len(tmp_ring)]
            s.op("act", (lambda sq=sq, j=j: nc.scalar.activation(out=sq[:], in_=src[:, j, sl], func=AF.Square)),
                 reads=[src.res], writes=[sq.res])
            s.op("pe", (lambda sq=sq, j=j: nc.tensor.matmul(pbank[:], lhsT=ones[:], rhs=sq[:], start=(j == 0), stop=(j == 7))),
                 reads=[sq.res, ones.res], writes=[pbank.res])
        sd = tmp_ring[0]
        s.op("act", (lambda sd=sd: nc.scalar.activation(out=sd[:], in_=pbank[:], func=AF.Sqrt, scale=1.0 / D, bias=epsb[:])),
             reads=[pbank.res, epsb.res], writes=[sd.res])
        s.op("dve", (lambda sd=sd: nc.vector.reciprocal(out=rs[:, sl], in_=sd[:])), reads=[sd.res], writes=[rs.res])
        for j in range(8):
            tmp = tmp_ring[1 + (j % (len(tmp_ring) - 1))]
            s.op("dve", (lambda tmp=tmp, j=j: nc.vector.tensor_tensor(out=tmp[:], in0=src[:, j, sl], in1=rs[:, sl], op=ALU.mult)),
                 reads=[src.res, rs.res], writes=[tmp.res])
            if after_h is None:
                s.op("act", (lambda tmp=tmp, j=j: nc.scalar.activation(out=hT[:, j, sl], in_=tmp[:], func=AF.Identity,
                                                                      scale=Acol[:, j:j + 1], bias=Bcol[:, j:j + 1])),
                     reads=[tmp.res, Acol.res, Bcol.res], writes=[hT.res])
            else:
                after_h(tmp, j, tt, sl)


def emit_AB(c, gn, mod, sh_off, sc_off, Acol, Bcol):
    nc, s = c.nc, c.s
    s.op("dve", lambda: nc.vector.scalar_tensor_tensor(out=Acol[:], in0=mod[:, sc_off:sc_off + 8], scalar=1.0, in1=gn[:],
                                                       op0=ALU.add, op1=ALU.mult),
         reads=[mod.res, gn.res], writes=[Acol.res])
    s.op("dve", lambda: nc.vector.tensor_copy(out=Bcol[:], in_=mod[:, sh_off:sh_off + 8]), reads=[mod.res], writes=[Bcol.res])


ROPE_CH = (0, 1, 2, 4, 5, 6, 7)
NORM_CH = (3, 8, 9, 10, 11)
RAW_CH = tuple(range(12, 24))
MISC_CH = 24


def build_A(c=None):
    own = c is None
    if own:
        c = Ctx()
    ph = c.begin_phase()
    nc, s = c.nc, c.s
    xT_d = c.dram("xT", [128, 8, NT], F32, "ExternalInput")
    mod_d = c.dram("modA", [128, 16], F32, "ExternalInput")
    gn_d = c.dram("gn", [128, 8], F32, "ExternalInput")
    pos_d = c.dram("pos", [1, NT], I32, "ExternalInput")
    invf_d = c.dram("invf", [128, 1], F32, "ExternalInput")
    w_d = c.dram("w", [NCH_IN, 128, 8, 128], F32, "ExternalInput")
    gain_d = c.dram("gain", [128, 12], F32, "ExternalInput")
    osc_d = c.dram("osc", [128, 12], F32, "ExternalInput")
    foxb_d = c.dram("foxb", [128, 1], F32, "ExternalInput")
    pm_d = c.dram("pm", [128, 128], F32, "ExternalInput")
    bones_d = c.dram("bones", [128, 128], F32, "ExternalInput")
    proj_d = c.dram("proj", [26, 128, NT], BF16, "ExternalOutput")
    gates_d = c.dram("gates", [32, 128, NT], F32, "ExternalOutput")
    misc_d = c.dram("misc", [64, NT], F32, "ExternalOutput")

    hT = c.sb([128, 8, NT], BF16)
    rs = c.sb([128, NT], F32)
    COS = c.sb([128, NT], F32)
    SIN = c.sb([128, NT], F32)
    ones = c.sb([128, 128], F32)
    bones = c.sb([128, 128], F32)
    pm = c.sb([128, 128], F32)
    gain = c.sb([128, 12], F32)
    osc = c.sb([128, 12], F32)
    foxb = c.sb([128, 1], F32)
    epsb = c.sb([128, 1], F32)
    negpi = c.sb([128, 1], F32)
    invf = c.sb([128, 1], F32)
    mod = c.sb([128, 16], F32)
    gn = c.sb([128, 8], F32)
    Acol = c.sb([128, 8], F32)
    Bcol = c.sb([128, 8], F32)
    pbank = c.ps([128, TT])

    s.op("pool", lambda: nc.gpsimd.memset(ones[:], 1.0), writes=[ones.res])
    s.op("pool", lambda: nc.gpsimd.memset(epsb[:], EPS), writes=[epsb.res])
    s.op("pool", lambda: nc.gpsimd.memset(negpi[:], -float(np.pi)), writes=[negpi.res])
    for t, d in ((bones, bones_d), (pm, pm_d), (gain, gain_d), (osc, osc_d), (foxb, foxb_d), (invf, invf_d), (mod, mod_d), (gn, gn_d)):
        s.dma(t[:], d, writes=[t.res])
    s.op("dve", lambda: nc.vector.tensor_tensor(out=gain[:], in0=gain[:], in1=osc[:], op=ALU.mult), reads=[gain.res, osc.res], writes=[gain.res])
    emit_AB(c, gn, mod, 0, 8, Acol, Bcol)

    with contextlib.ExitStack() as es1:
        old_es, c.es = c.es, es1
        xT = c.sb([128, 8, NT], F32)
        for j in range(8):
            s.dma(xT[:, j, :], xT_d[:, j, :], writes=[xT.res])
        posi = c.sb([128, NT], I32)
        ang = c.sb([128, NT], F32)
        tq = c.sb([128, NT], F32)
        ki = c.sb([128, NT], I32)
        kf = c.sb([128, NT], F32)
        s.dma(posi[:], pos_d[0, :].partition_broadcast(128), writes=[posi.res])
        s.op("dve", lambda: nc.vector.tensor_copy(out=tq[:], in_=posi[:]), reads=[posi.res], writes=[tq.res])
        s.op("dve", lambda: nc.vector.tensor_scalar(out=ang[:], in0=tq[:], scalar1=invf[:, 0:1], scalar2=None, op0=ALU.mult),
             reads=[tq.res, invf.res], writes=[ang.res])
        for dst, shift in ((SIN, 0.0), (COS, 0.25)):
            s.op("dve", lambda shift=shift: nc.vector.tensor_scalar(out=tq[:], in0=ang[:], scalar1=1.0 / TWO_PI, scalar2=0.5 + shift,
                                                                    op0=ALU.mult, op1=ALU.add), reads=[ang.res], writes=[tq.res])
            s.op("dve", lambda: nc.vector.tensor_copy(out=ki[:], in_=tq[:]), reads=[tq.res], writes=[ki.res])
            s.op("dve", lambda: nc.vector.tensor_copy(out=kf[:], in_=ki[:]), reads=[ki.res], writes=[kf.res])
            s.op("dve", lambda shift=shift: nc.vector.tensor_scalar(out=tq[:], in0=ang[:], scalar1=float(np.pi) + shift * TWO_PI, scalar2=None,
                                                                    op0=ALU.add), reads=[ang.res], writes=[tq.res])
            s.op("dve", lambda: nc.vector.scalar_tensor_tensor(out=tq[:], in0=kf[:], scalar=-C1_2PI, in1=tq[:], op0=ALU.mult, op1=ALU.add),
                 reads=[kf.res, tq.res], writes=[tq.res])
            s.op("dve", lambda: nc.vector.scalar_tensor_tensor(out=tq[:], in0=kf[:], scalar=-C2_2PI, in1=tq[:], op0=ALU.mult, op1=ALU.add),
                 reads=[kf.res, tq.res], writes=[tq.res])
            s.op("dve", lambda: nc.vector.tensor_scalar(out=kf[:], in0=tq[:], scalar1=0.0, scalar2=TWO_PI, op0=ALU.is_lt, op1=ALU.mult),
                 reads=[tq.res], writes=[kf.res])
            s.op("dve", lambda: nc.vector.tensor_tensor(out=tq[:], in0=tq[:], in1=kf[:], op=ALU.add), reads=[tq.res, kf.res], writes=[tq.res])
            s.op("dve", lambda: nc.vector.tensor_scalar(out=tq[:], in0=tq[:], scalar1=0.0, scalar2=TWO_PI, op0=ALU.max, op1=ALU.min),
                 reads=[tq.res], writes=[tq.res])
            s.op("act", lambda dst=dst: nc.scalar.activation(out=dst[:], in_=tq[:], func=AF.Sin, bias=negpi[:], scale=1.0),
                 reads=[tq.res, negpi.res], writes=[dst.res])
        ring = [c.sb([128, TT], F32) for _ in range(4)]
        emit_modnorm(c, xT, hT, NT, ones, Acol, Bcol, epsb, ring, pbank, rs)
        barrier(c)
        c.es = old_es

    R = 4
    wst = [c.sb([128, 8, 128], F32) for _ in range(3)]
    wbf = [c.sb([128, 8, 128], BF16) for _ in range(3)]
    pp = [c.ps([128, TT]) for _ in range(3)]
    psq = [c.ps([128, TT]) for _ in range(2)]
    pq = [pbank, c.ps([128, TT])]
    sq_r = [c.sb([128, TT], F32) for _ in range(R)]
    rstd_r = [c.sb([128, TT], F32) for _ in range(R)]
    y_r = [c.sb([128, TT], F32) for _ in range(R)]
    y2_r = [c.sb([128, TT], F32) for _ in range(R)]
    t1_r = [c.sb([128, TT], F32) for _ in range(R)]
    ob_r = [c.sb([128, TT], BF16) for _ in range(R)]
    ob2_r = [c.sb([128, TT], BF16) for _ in range(R)]
    of_r = [c.sb([128, TT], F32) for _ in range(R)]

    items = [(ch, tt) for ch in range(NCH_IN) for tt in range(NT // TT)]

    def kind(ch):
        if ch in ROPE_CH:
            return "rope"
        if ch in NORM_CH:
            return "norm"
        if ch in RAW_CH:
            return "raw"
        if ch == MISC_CH:
            return "misc"
        return "sig"

    def stl(n):
        ch, tt = items[n]
        if tt == 0:
            ws = wst[ch % 3]
            s.dma(ws[:], w_d[ch], writes=[ws.res], q="act")

    def stc(n):
        ch, tt = items[n]
        if tt == 0:
            ws, wb = wst[ch % 3], wbf[ch % 3]
            s.op("pool", lambda: nc.gpsimd.tensor_copy(out=wb[:], in_=ws[:]), reads=[ws.res], writes=[wb.res])

    def st0(n):
        ch, tt = items[n]
        wb = wbf[ch % 3]
        p = pp[n % 3]
        for j in range(8):
            s.op("pe", (lambda j=j: nc.tensor.matmul(p[:], lhsT=wb[:, j, :], rhs=hT[:, j, tt * TT:(tt + 1) * TT],
                                                     start=(j == 0), stop=(j == 7))),
                 reads=[wb.res, hT.res], writes=[p.res])

    def st1(n):
        ch, tt = items[n]
        k = kind(ch)
        p = pp[n % 3]
        sl = slice(tt * TT, (tt + 1) * TT)
        if k in ("rope", "norm"):
            sq = sq_r[n % R]
            s.op("act", lambda: nc.scalar.activation(out=sq[:], in_=p[:], func=AF.Square), reads=[p.res], writes=[sq.res])
            s.op("pe", lambda: nc.tensor.matmul(psq[n % 2][:], lhsT=bones[:], rhs=sq[:], start=True, stop=True),
                 reads=[bones.res, sq.res], writes=[psq[n % 2].res])
        elif k == "raw":
            ob = ob_r[n % R]
            sc = 0.125 if ch in (16, 17) else 1.0
            s.op("act", lambda: nc.scalar.activation(out=ob[:], in_=p[:], func=AF.Copy, scale=sc), reads=[p.res], writes=[ob.res])
            s.dma(proj_d[ch, :, sl], ob[:], reads=[ob.res], is_output=True)
        elif k == "sig":
            of = of_r[n % R]
            s.op("act", lambda: nc.scalar.activation(out=of[:], in_=p[:], func=AF.Sigmoid), reads=[p.res], writes=[of.res])
            s.dma(gates_d[ch - 25, :, sl], of[:], reads=[of.res], is_output=True)
        else:
            of = of_r[n % R]
            s.op("act", lambda: nc.scalar.activation(out=of[0:64, :], in_=p[0:64, :], func=AF.Sigmoid, bias=foxb[0:64, :], scale=1.0),
                 reads=[p.res, foxb.res], writes=[of.res])
            s.op("act", lambda: nc.scalar.activation(out=of[32:64, :], in_=of[32:64, :], func=AF.Ln), reads=[of.res], writes=[of.res])
            s.dma(misc_d[:, sl], of[0:64, :], reads=[of.res], is_output=True)

    def st2(n):
        ch, tt = items[n]
        k = kind(ch)
        if k not in ("rope", "norm"):
            return
        p = pp[n % 3]
        sl = slice(tt * TT, (tt + 1) * TT)
        rstd = rstd_r[n % R]
        y = y_r[n % R]
        s.op("act", lambda: nc.scalar.activation(out=rstd[:], in_=psq[n % 2][:], func=AF.Sqrt, scale=1.0 / 64, bias=epsb[:]),
             reads=[psq[n % 2].res, epsb.res], writes=[rstd.res])
        s.op("dve", lambda: nc.vector.reciprocal(out=rstd[:], in_=rstd[:]), reads=[rstd.res], writes=[rstd.res])
        s.op("dve", lambda: nc.vector.tensor_tensor(out=y[:], in0=p[:], in1=rstd[:], op=ALU.mult), reads=[p.res, rstd.res], writes=[y.res])
        if k == "norm":
            ob = ob_r[n % R]
            s.op("act", lambda: nc.scalar.activation(out=ob[:], in_=y[:], func=AF.Copy, scale=gain[:, ch:ch + 1]),
                 reads=[y.res, gain.res], writes=[ob.res])
            s.dma(proj_d[ch, :, sl], ob[:], reads=[ob.res], is_output=True)
        else:
            y2 = y2_r[n % R]
            s.op("act", lambda: nc.scalar.activation(out=y2[:], in_=y[:], func=AF.Copy, scale=gain[:, ch:ch + 1]),
                 reads=[y.res, gain.res], writes=[y2.res])
            s.op("pe", lambda: nc.tensor.matmul(pq[n % 2][:], lhsT=pm[:], rhs=y2[:], start=True, stop=True),
                 reads=[pm.res, y2.res], writes=[pq[n % 2].res])
            if ch in (0, 1):
                ob2 = ob2_r[n % R]
                s.op("pool", lambda: nc.gpsimd.tensor_copy(out=ob2[:], in_=y2[:]), reads=[y2.res], writes=[ob2.res])
                s.dma(proj_d[24 + ch, :, sl], ob2[:], reads=[ob2.res], is_output=True)

    def st3(n):
        ch, tt = items[n]
        if kind(ch) != "rope":
            return
        sl = slice(tt * TT, (tt + 1) * TT)
        y2 = y2_r[n % R]
        t1 = t1_r[n % R]
        y = y_r[n % R]
        ob = ob_r[n % R]
        s.op("pool", lambda: nc.gpsimd.tensor_tensor(out=t1[:], in0=y2[:], in1=COS[:, sl], op=ALU.mult), reads=[y2.res, COS.res], writes=[t1.res])
        s.op("dve", lambda: nc.vector.tensor_tensor(out=y[:], in0=pq[n % 2][:], in1=SIN[:, sl], op=ALU.mult),
             reads=[pq[n % 2].res, SIN.res], writes=[y.res])
        s.op("dve", lambda: nc.vector.tensor_tensor(out=ob[:], in0=t1[:], in1=y[:], op=ALU.add), reads=[t1.res, y.res], writes=[ob.res])
        s.dma(proj_d[ch, :, sl], ob[:], reads=[ob.res], is_output=True)

    pipeline(len(items), [stl, stc, st0, st1, st2, st3])
    c.end_phase(ph)
    if own:
        c.close()
    return c


def in_perm():
    Z = [-1] * 64
    r = lambda a, n: list(range(a, a + n))
    ch = []
    ch.append(r(0, 128)); ch.append(r(128, 128))
    ch.append(r(384, 64) + r(512, 64))
    ch.append(r(256, 64) + Z)
    ch.append(r(652, 128)); ch.append(r(780, 128))
    ch.append(r(908, 128)); ch.append(r(1036, 128))
    ch.append(r(2188, 128)); ch.append(r(2316, 128))
    ch.append(r(2444, 128)); ch.append(r(2572, 128))
    ch.append(r(320, 64) + r(448, 64))
    ch.append(r(576, 64) + Z)
    ch.append(r(1164, 128)); ch.append(r(1292, 128))
    ch.append(r(1420, 128)); ch.append(r(1548, 128))
    ch.append(r(1676, 128)); ch.append(r(1804, 128))
    ch.append(r(1932, 128)); ch.append(r(2060, 128))
    ch.append(r(2700, 128)); ch.append(r(2828, 128))
    ch.append(r(640, 12) + [-1] * 20 + r(2956, 4) + [-1] * 92)
    for g in range(32):
        ch.append(r(2960 + g * 128, 128))
    return np.array(ch, np.int64)


def consts_A():
    inv = (500000.0 ** (-np.arange(0, 16, 2, dtype=np.float32) / 16)).astype(np.float32)
    invf = np.zeros((128, 1), np.float32)
    pm = np.zeros((128, 128), np.float32)
    bones = np.zeros((128, 128), np.float32)
    for hb in (0, 64):
        bones[hb:hb + 64, hb:hb + 64] = 1.0
        for i in range(8):
            invf[hb + i, 0] = inv[i]
            invf[hb + 8 + i, 0] = inv[i]
            pm[hb + i + 8, hb + i] = -1.0
            pm[hb + i, hb + i + 8] = 1.0
    osc = np.ones((128, 12), np.float32)
    for chn in (0, 1, 4, 5, 8, 9):
        osc[:, chn] = 0.125
    return invf, pm, bones, osc


def maps_A(inp, l, xT_all, mod):
    perm = in_perm()
    w = inp["w_in"][l]
    wz = np.concatenate([w, np.zeros((D, 1), np.float32)], axis=1)
    wp = wz[:, perm.reshape(-1)].reshape(D, NCH_IN, 128)
    wp = np.ascontiguousarray(wp.reshape(8, 128, NCH_IN, 128).transpose(2, 1, 0, 3))
    invf, pm, bones, osc = consts_A()
    g = inp["qk_gain"][l]
    t2 = lambda a: np.concatenate([a, a])
    z64 = np.zeros(64, np.float32)
    gain = np.stack([t2(g[0]), t2(g[0]), np.concatenate([g[2], g[3]]), np.concatenate([g[1], z64]),
                     t2(g[4]), t2(g[4]), t2(g[5]), t2(g[5]), t2(g[6]), t2(g[6]), t2(g[7]), t2(g[7])], axis=1).astype(np.float32)
    foxb = np.zeros((128, 1), np.float32)
    foxb[32:36, 0] = inp["fox_bias"][l]
    gn = np.ascontiguousarray(inp["norm_mix"][l].reshape(8, 128).T)
    maps = []
    for i in range(8):
        b, q = i // 4, i % 4
        m = {"modA": np.ascontiguousarray(mod[l, b][:, 0:16]), "gn": gn,
             "pos": np.ascontiguousarray(inp["positions"][b:b + 1, q * NT:(q + 1) * NT]).astype(np.int32),
             "invf": invf, "w": wp, "gain": gain, "osc": osc, "foxb": foxb, "pm": pm, "bones": bones}
        if xT_all is not None:
            xs = xT_all[b][:, q * NT:(q + 1) * NT].reshape(8, 128, NT).transpose(1, 0, 2)
            m["xT"] = np.ascontiguousarray(xs)
        maps.append(m)
    return maps


def collect_A(results, pre=""):
    proj = [np.concatenate([results[b * 4 + q][pre + "proj"] for q in range(4)], axis=2) for b in range(2)]
    gates = [np.concatenate([results[b * 4 + q][pre + "gates"] for q in range(4)], axis=2) for b in range(2)]
    misc = [np.concatenate([results[b * 4 + q][pre + "misc"] for q in range(4)], axis=1) for b in range(2)]
    return proj, gates, misc


def run_A(cA, inp, l, xT_all, mod):
    maps = maps_A(inp, l, xT_all, mod)
    res = run_bass_kernel_spmd(cA.nc, maps, core_ids=list(range(8)))
    return collect_A(res.results)


B_MIXERS = ("dil", "fox", "sb", "nsa")
NKB = S // 128
NQT = S // TT
M_CAUSAL, M_STRICT, M_WIN, M_DIL, M_CMP, M_NEGC = 0, 4, 8, 16, 36, 41
N_MASKS = 45


def consts_B():
    import ml_dtypes
    k = np.arange(128)[:, None]
    cc = np.arange(512)[None, :]
    masks = np.zeros((N_MASKS, 128, 512), np.float32)
    for i in range(4):
        masks[M_CAUSAL + i] = (cc - k >= 128 * i)
        masks[M_STRICT + i] = (cc - k > 128 * i)
    for w in range(8):
        diff = cc - k + 512 - 128 * w
        masks[M_WIN + w] = (diff >= 0) & (diff < 512)
    for w in range(20):
        diff = cc - k + 2048 - 128 * w
        m = np.zeros((128, 512), np.float32)
        for (ww, d) in ((128, 1), (512, 4), (2048, 16)):
            m += ((diff % d == 0) & (diff >= 0) & (diff <= ww))
        masks[M_DIL + w] = m
    for u in range(5):
        masks[M_CMP + u] = (16 * k + 31 <= 512 * u + cc)
    for i in range(4):
        masks[M_NEGC + i] = -30000.0 * (cc - k < 128 * i)
    G = (np.arange(S)[None, :] // 64 == np.arange(128)[:, None]).astype(np.float32)
    c0 = np.arange(511) * 16
    s0 = np.arange(128) * 64
    ov = ((c0[:, None] < s0[None, :] + 64) & (c0[:, None] + 32 > s0[None, :])).astype(np.float32)
    ov = np.concatenate([ov, np.zeros((1, 128), np.float32)], 0).reshape(4, 128, 128).transpose(1, 0, 2)
    onesc = np.ones((128, 4, 1), np.float32)
    onesc[127, 3, 0] = 0.0
    Rconst = np.concatenate([ov, onesc], axis=2)
    add = np.zeros((128, 254), np.float32)
    jj = np.arange(254)[None, :] - 126
    cr = (np.arange(128) // 64)[:, None]
    add[(jj == cr) | (jj == cr - 1)] = 1e30
    add[jj > cr] = -1e30
    jn = np.arange(128)[:, None]
    kn = np.arange(128)[None, :]
    nti = -(jn >= kn).astype(np.float32)
    ntc = -(jn < kn).astype(np.float32)
    bf = ml_dtypes.bfloat16
    return dict(masks=masks.astype(bf), G=G.astype(bf), Rconst=Rconst.astype(bf), add=add,
                nti=nti.astype(bf), ntc=ntc.astype(bf), ident=np.eye(128, dtype=np.float32),
                tri64=(np.arange(64)[:, None] < np.arange(64)[None, :]).astype(np.float32))


def pipeline(n_items, stages):
    K = len(stages)
    for i in range(n_items + K - 1):
        for k, st in enumerate(stages):
            n = i - k
            if 0 <= n < n_items:
                st(n)


def build_B():
    c = Ctx()
    nc, s = c.nc, c.s
    di = lambda name, shape, dt: c.dram(name, shape, dt, "ExternalInput")
    qnr_d = di("qnr", [4, 64, S], BF16)
    qr_d = di("qr", [64, S], BF16)
    kcT_d = di("kcT", [64, S], BF16)
    vcT_d = di("vcT", [64, S], BF16)
    kslT_d = di("kslT", [64, S], BF16)
    kwT_d = di("kwT", [64, S], BF16)
    vsl_d = di("vsl", [128, NKB, 65], BF16)
    vw_d = di("vw", [128, NKB, 65], BF16)
    ag_d = di("ag", [128, NKB, 3], F32)
    dq_d = di("dq", [64, S], BF16)
    dk_d = di("dk", [64, S], BF16)
    dv_d = di("dv", [128, NKB, 65], BF16)
    sq_d = di("sq", [64, S], BF16)
    sk_d = di("sk", [64, S], BF16)
    sv_d = di("sv", [128, NKB, 65], BF16)
    fq_d = di("fq", [64, S], BF16)
    fk_d = di("fk", [64, S], BF16)
    fv_d = di("fv", [128, NKB, 65], BF16)
    lf_d = di("logf", [1, S], F32)
    w1k_d = di("w1k", [64, 32, 128], F32)
    w1v_d = di("w1v", [64, 32, 128], F32)
    w2k_d = di("w2k", [128, 64], F32)
    w2v_d = di("w2v", [128, 64], F32)
    pek_d = di("pek", [64, 32], F32)
    pev_d = di("pev", [64, 32], F32)
    masks_d = di("masks", [N_MASKS, 128, 512], BF16)
    G_d = di("G", [128, S], BF16)
    Rc_d = di("Rconst", [128, 4, 129], BF16)
    add_d = di("add", [128, 254], F32)
    nti_d = di("nti", [128, 128], BF16)
    ntc_d = di("ntc", [128, 128], BF16)
    ident_d = di("ident", [128, 128], F32)
    tri_d = di("tri64", [64, 64], F32)
    o_d = c.dram("o", [4, 128, NKB, 64], BF16, "ExternalOutput")

    ps_ring = [c.ps([128, 512]) for _ in range(4)]
    po_ring = [c.ps([128, 512]) for _ in range(2)]
    pX = c.ps([128, 512])
    pY = c.ps([128, 512])
    e_ring = [c.sb([128, 512], BF16) for _ in range(4)]
    p_ring = [c.sb([128, 512], BF16) for _ in range(4)]
    pre_ring = [c.sb([128, 512], F32) for _ in range(4)]
    rz_ring = [c.sb([128, 4], F32) for _ in range(2)]
    fac_ring = [c.sb([128, 4], F32) for _ in range(2)]
    ost = c.sb([128, NKB, 64], BF16)

    class Scope:
        def __enter__(self):
            self.es = contextlib.ExitStack()
            self.old = c.es
            c.es = self.es
            return self

        def __exit__(self, *a):
            barrier(c)
            c.es = self.old
            self.es.close()

    def load_masks(lo, n, order=None):
        t = c.sb([128, n, 512], BF16)
        t.parts = [Res() for _ in range(n)]
        for ii, i in enumerate(order if order is not None else range(n)):
            s.dma(t[:, i, :], masks_d[lo + i], writes=[t.parts[i]], q=("sp", "act")[ii % 2])
        return t

    def split_load(QT, KT, V, qT_d, kT_d, v_d):
        qs = ("sp", "act")
        for t in (QT, KT, V):
            if t is not None:
                t.parts = [Res() for _ in range(4)]
        for h in range(4):
            sl = slice(h * 2048, (h + 1) * 2048)
            if QT is not None:
                s.dma(QT[0:64, sl], qT_d[:, sl], reads=[QT.res], writes=[QT.parts[h]], q=qs[h % 2])
            s.dma(KT[0:64, sl], kT_d[:, sl], reads=[KT.res], writes=[KT.parts[h]], q=qs[(h + 1) % 2])
            s.dma(V[:, 16 * h:16 * (h + 1), :], v_d[:, 16 * h:16 * (h + 1), :], reads=[V.res], writes=[V.parts[h]], q=qs[h % 2])

    def rQ(QT, qt):
        return [QT.res, QT.parts[qt // 4]]

    def rK(KT, kb):
        return [KT.res, KT.parts[kb // 16]]

    def attn(items, qk, pv, fin, mask_of, alt=[0], nps=4):
        def st0(n):
            qk(n, ps_ring[n % nps])

        def st1(n):
            it = items[n]
            ps, e = ps_ring[n % nps], e_ring[n % 4]
            if it["mi"] is not None and it["mi"] >= M_NEGC:
                mt, mi = mask_of(it["mi"])
                pre = pre_ring[n % 4]
                s.op("dve", lambda: nc.vector.tensor_tensor(out=pre[:], in0=ps[:], in1=mt[:, mi, :], op=ALU.add),
                     reads=[ps.res, mt.parts[mi]], writes=[pre.res])
                p = p_ring[n % 4]
                s.op("act", lambda: nc.scalar.activation(out=p[:], in_=pre[:], func=AF.Exp), reads=[pre.res], writes=[p.res])
                return
            s.op("act", lambda: nc.scalar.activation(out=e[:], in_=ps[:], func=AF.Exp), reads=[ps.res], writes=[e.res])
            if it["mi"] is not None:
                p = p_ring[n % 4]
                mt, mi = mask_of(it["mi"])
                alt[0] = 1
                if alt[0]:
                    s.op("dve", lambda: nc.vector.tensor_tensor(out=p[:], in0=e[:], in1=mt[:, mi, :], op=ALU.mult),
                         reads=[e.res, mt.parts[mi]], writes=[p.res])
                else:
                    s.op("pool", lambda: nc.gpsimd.tensor_tensor(out=p[:], in0=e[:], in1=mt[:, mi, :], op=ALU.mult),
                         reads=[e.res, mt.parts[mi]], writes=[p.res])

        def st2(n):
            it = items[n]
            pt = p_ring[n % 4] if it["mi"] is not None else e_ring[n % 4]
            pv(n, pt)
            if it["last"]:
                fin(n)
        pipeline(len(items), [st0, st1, (lambda n: None), st2])

    def std_pv(items, V, ncol=65):
        def pv(n, pt):
            it = items[n]
            po = po_ring[it["qt"] % 2]
            for qb in range(4):
                s.op("pe", (lambda qb=qb: nc.tensor.matmul(po[:, qb * 65:qb * 65 + ncol], lhsT=pt[:, qb * 128:(qb + 1) * 128],
                                                           rhs=V[:, it["kb"], 0:ncol], start=(it["first"] and qb == 0), stop=it["last"],
                                                           skip_group_check=True)),
                     reads=[pt.res, V.res, V.parts[it["kb"] // 16]], writes=[po.res])
        return pv

    def rz_of(qt, po, zoff=64, stride=65):
        rz = rz_ring[qt % 2]
        for qb in range(4):
            s.op("dve", (lambda qb=qb: nc.vector.tensor_scalar(out=rz[:, qb:qb + 1], in0=po[:, qb * stride + zoff:qb * stride + zoff + 1],
                                                               scalar1=1e-30, scalar2=None, op0=ALU.max)),
                 reads=[po.res], writes=[rz.res])
        s.op("dve", lambda: nc.vector.reciprocal(out=rz[:], in_=rz[:]), reads=[rz.res], writes=[rz.res])
        return rz

    def std_items(blocks_of):
        items = []
        for qt in range(NQT):
            bl = blocks_of(qt)
            for ii, (kb, mi) in enumerate(bl):
                items.append(dict(qt=qt, kb=kb, mi=mi, first=(ii == 0), last=(ii == len(bl) - 1)))
        return items

    def simple_mixer(m, qT_d, kT_d, v_d, blocks_of, mask_lo, mask_n, kdim=64, prep=None, mask_order=None):
        with Scope():
            QT = c.sb([128, S], BF16)
            KT = c.sb([128, S], BF16)
            V = c.sb([128, NKB, 65], BF16)
            if prep is not None:
                prep(QT, KT)
            split_load(QT, KT, V, qT_d, kT_d, v_d)
            mt = load_masks(mask_lo, mask_n, order=mask_order)
            items = std_items(blocks_of)

            def qk(n, ps):
                it = items[n]
                s.op("pe", lambda: nc.tensor.matmul(ps[:], lhsT=KT[0:kdim, it["kb"] * 128:(it["kb"] + 1) * 128],
                                                    rhs=QT[0:kdim, it["qt"] * 512:(it["qt"] + 1) * 512], start=True, stop=True),
                     reads=rK(KT, it["kb"]) + rQ(QT, it["qt"]), writes=[ps.res])

            def fin(n):
                qt = items[n]["qt"]
                po = po_ring[qt % 2]
                rz = rz_of(qt, po)
                for qb in range(4):
                    s.op("dve", (lambda qb=qb: nc.vector.tensor_scalar(out=ost[:, 4 * qt + qb, :], in0=po[:, qb * 65:qb * 65 + 64],
                                                                       scalar1=rz[:, qb:qb + 1], scalar2=None, op0=ALU.mult)),
                         reads=[po.res, rz.res], writes=[ost.res])
            attn(items, qk, std_pv(items, V), fin, lambda mi: (mt, mi - mask_lo))
            s.dma(o_d[m], ost[:], reads=[ost.res], is_output=True)

    def dil_blocks(qt):
        return [(4 * qt - 16 + w, M_DIL + w) for w in range(20) if 4 * qt - 16 + w >= 0]
    if "dil" in B_MIXERS:
        simple_mixer(1, dq_d, dk_d, dv_d, dil_blocks, M_DIL, 20, mask_order=[16, 17, 18, 19, 12, 13, 14, 15, 8, 9, 10, 11, 4, 5, 6, 7, 0, 1, 2, 3])

    fs_d = c.dram("fsplit", [6, S], BF16, "Internal")
    fs_res = Res()

    def fox_prep(QT, KT):
        lf = c.sb([64, 128], F32)
        F = c.sb([64, 128], F32)
        zr = c.sb([64, 128], F32)
        r1 = c.sb([64, 128], F32)
        off = c.sb([64, 1], F32)
        U = c.sb([64, 64], F32)
        sp3 = [c.sb([64, 128], BF16) for _ in range(3)]
        ng3 = [c.sb([64, 128], BF16) for _ in range(3)]
        s.dma(lf[:], lf_d.rearrange("o (p j) -> (o p) j", j=128), writes=[lf.res])
        s.dma(U[:], tri_d, writes=[U.res])
        s.op("pool", lambda: nc.gpsimd.memset(zr[:], 0.0), writes=[zr.res])
        s.op("pool", lambda: nc.gpsimd.memset(QT[:], 0.0), writes=[QT.res])
        s.op("pool", lambda: nc.gpsimd.memset(KT[:], 0.0), writes=[KT.res])
        s.op("pool", lambda: nc.gpsimd.memset(QT[96:99, :], 1.0), writes=[QT.res])
        s.op("pool", lambda: nc.gpsimd.memset(KT[64:67, :], 1.0), writes=[KT.res])
        s.op("dve", lambda: nc.vector.tensor_tensor_scan(out=F[:], data0=lf[:], data1=zr[:], initial=0.0, op0=ALU.add, op1=ALU.add),
             reads=[lf.res, zr.res], writes=[F.res])
        s.op("pe", lambda: nc.tensor.matmul(pY[0:64, 0:1], lhsT=U[:], rhs=F[:, 127:128], start=True, stop=True),
             reads=[U.res, F.res], writes=[pY.res])
        s.op("act", lambda: nc.scalar.copy(out=off[:], in_=pY[0:64, 0:1]), reads=[pY.res], writes=[off.res])
        s.op("dve", lambda: nc.vector.tensor_scalar(out=F[:], in0=F[:], scalar1=off[:, 0:1], scalar2=None, op0=ALU.add),
             reads=[F.res, off.res], writes=[F.res])
        cur = F
        for i in range(3):
            s.op("dve", (lambda i=i, cur=cur: nc.vector.tensor_copy(out=sp3[i][:], in_=cur[:])), reads=[cur.res], writes=[sp3[i].res])
            s.op("dve", (lambda i=i: nc.vector.tensor_scalar(out=ng3[i][:], in0=sp3[i][:], scalar1=-1.0, scalar2=None, op0=ALU.mult)),
                 reads=[sp3[i].res], writes=[ng3[i].res])
            if i < 2:
                s.op("dve", (lambda i=i, cur=cur: nc.vector.tensor_tensor(out=r1[:], in0=cur[:], in1=sp3[i][:], op=ALU.subtract)),
                     reads=[cur.res, sp3[i].res], writes=[r1.res])
                cur = r1
            s.dma(fs_d[i, :].rearrange("(p j) -> p j", j=128), sp3[i][:], reads=[sp3[i].res], writes=[fs_res])
            s.dma(fs_d[3 + i, :].rearrange("(p j) -> p j", j=128), ng3[i][:], reads=[ng3[i].res], writes=[fs_res])
        s.dma(QT[64:67, :], fs_d[0:3, :], reads=[fs_res], writes=[QT.res])
        s.dma(KT[96:99, :], fs_d[3:6, :], reads=[fs_res], writes=[KT.res])

    def causal_blocks(qt):
        return [(kb, None) for kb in range(4 * qt)] + [(4 * qt + i, M_CAUSAL + i) for i in range(4)]

    def negc_blocks(qt):
        return [(kb, None) for kb in range(4 * qt)] + [(4 * qt + i, M_NEGC + i) for i in range(4)]
    if "fox" in B_MIXERS:
        simple_mixer(3, fq_d, fk_d, fv_d, negc_blocks, M_NEGC, 4, kdim=99, prep=fox_prep)

    with (Scope() if "sb" in B_MIXERS else contextlib.nullcontext()):
      if "sb" in B_MIXERS:
        QT = c.sb([64, S], BF16)
        KT = c.sb([64, S], BF16)
        V = c.sb([128, NKB, 65], BF16)
        nti = c.sb([128, 128], BF16)
        ntc = c.sb([128, 128], BF16)
        split_load(QT, KT, V, sq_d, sk_d, sv_d)
        s.dma(nti[:], nti_d, writes=[nti.res])
        s.dma(ntc[:], ntc_d, writes=[ntc.res])
        mt = load_masks(M_STRICT, 4)
        E_r = [c.sb([128, 512], F32) for _ in range(4)]
        L_r = [c.sb([128, 512], BF16) for _ in range(4)]
        X_r = [c.sb([128, 512], F32) for _ in range(4)]
        A_r = [c.sb([128, 512], BF16) for _ in range(4)]
        def sb_blocks(qt):
            bl = [(4 * qt + i, M_STRICT + i) for i in (3, 2, 1, 0)] + [(kb, None) for kb in range(4 * qt - 1, -1, -1)]
            return [dict(qt=qt, kb=kb, mi=mi, first=(ii == 0), last=(ii == len(bl) - 1)) for ii, (kb, mi) in enumerate(bl)]
        items = []
        for pr in range(NQT // 2):
            la, lb = sb_blocks(2 * pr), sb_blocks(2 * pr + 1)
            for ii in range(len(lb)):
                if ii < len(la):
                    items.append(la[ii])
                items.append(lb[ii])
        pXs = (pX, pY)

        def sb0(n):
            it = items[n]
            ps = ps_ring[n % 4]
            s.op("pe", lambda: nc.tensor.matmul(ps[:], lhsT=KT[:, it["kb"] * 128:(it["kb"] + 1) * 128],
                                                rhs=QT[:, it["qt"] * 512:(it["qt"] + 1) * 512], start=True, stop=True),
                 reads=rK(KT, it["kb"]) + rQ(QT, it["qt"]), writes=[ps.res])

        def sb1(n):
            it = items[n]
            ps, E, L = ps_ring[n % 4], E_r[n % 4], L_r[n % 4]
            s.op("act", lambda: nc.scalar.activation(out=E[:], in_=ps[:], func=AF.Exp), reads=[ps.res], writes=[E.res])
            s.op("act", lambda: nc.scalar.activation(out=L[:], in_=E[:], func=AF.Ln, bias=1.0, scale=1.0), reads=[E.res], writes=[L.res])
            if it["mi"] is not None:
                mi = it["mi"] - M_STRICT
                s.op("pool", lambda: nc.gpsimd.tensor_tensor(out=L[:], in0=L[:], in1=mt[:, mi, :], op=ALU.mult),
                     reads=[L.res, mt.parts[mi]], writes=[L.res])
                s.op("dve", lambda: nc.vector.tensor_tensor(out=E[:], in0=E[:], in1=mt[:, mi, :], op=ALU.mult),
                     reads=[E.res, mt.parts[mi]], writes=[E.res])

        def sb2(n):
            it = items[n]
            L, X = L_r[n % 4], X_r[n % 4]
            pXq = pXs[it["qt"] % 2]
            s.op("pe", lambda: nc.tensor.matmul(pXq[:], lhsT=nti[:], rhs=L[:], start=it["first"], stop=False, skip_group_check=True),
                 reads=[nti.res, L.res], writes=[pXq.res])
            s.op("act", lambda: nc.scalar.activation(out=X[:], in_=pXq[:], func=AF.Exp), reads=[pXq.res], writes=[X.res])

        def sb3(n):
            it = items[n]
            L, X, E, A = L_r[n % 4], X_r[n % 4], E_r[n % 4], A_r[n % 4]
            pXq = pXs[it["qt"] % 2]
            s.op("pe", lambda: nc.tensor.matmul(pXq[:], lhsT=ntc[:], rhs=L[:], start=False, stop=it["last"], skip_group_check=True),
                 reads=[ntc.res, L.res], writes=[pXq.res])
            s.op("dve", lambda: nc.vector.tensor_tensor(out=A[:], in0=E[:], in1=X[:], op=ALU.mult), reads=[E.res, X.res], writes=[A.res])

        def sb4(n):
            it = items[n]
            A = A_r[n % 4]
            qt = it["qt"]
            po = po_ring[qt % 2]
            for qb in range(4):
                s.op("pe", (lambda qb=qb: nc.tensor.matmul(po[:, qb * 65:qb * 65 + 64], lhsT=A[:, qb * 128:(qb + 1) * 128],
                                                           rhs=V[:, it["kb"], 0:64], start=(it["first"] and qb == 0), stop=it["last"],
                                                           skip_group_check=True)),
                     reads=[A.res, V.res, V.parts[it["kb"] // 16]], writes=[po.res])
            if it["last"]:
                for qb in range(4):
                    s.op("act", (lambda qb=qb: nc.scalar.copy(out=ost[:, 4 * qt + qb, :], in_=po[:, qb * 65:qb * 65 + 64])),
                         reads=[po.res], writes=[ost.res])
        K_ = len(items)
        for i in range(K_ + 4):
            if i < K_:
                sb0(i)
            if 0 <= i - 1 < K_:
                sb1(i - 1)
            if 0 <= i - 3 < K_:
                sb3(i - 3)
            if 0 <= i - 2 < K_:
                sb2(i - 2)
            if 0 <= i - 4 < K_:
                sb4(i - 4)
        s.dma(o_d[2], ost[:], reads=[ost.res], is_output=True)

    hsel_d = di("hsel", [128, 4], F32)
    with (Scope() if "nsa" in B_MIXERS else contextlib.nullcontext()):
      if "nsa" in B_MIXERS:
        G = c.sb([128, S], BF16)
        selbT = c.sb([128, S], BF16)
        oa = c.sb([128, NKB, 64], F32)
        ag = c.sb([128, NKB, 3], F32)
        ident = c.sb([128, 128], F32)
        addt = c.sb([128, 254], F32)
        hsel = c.sb([128, 4], F32)
        for h in range(4):
            sl = slice(h * 2048, (h + 1) * 2048)
            s.dma(G[:, sl], G_d[:, sl], writes=[G.res])
        for t, d in ((ag, ag_d), (ident, ident_d), (addt, add_d), (hsel, hsel_d)):
            s.dma(t[:], d, writes=[t.res])
        with Scope():
            kcT = c.sb([64, S], BF16)
            vcT = c.sb([64, S], BF16)
            for h in range(4):
                sl = slice(h * 2048, (h + 1) * 2048)
                s.dma(kcT[:, sl], kcT_d[:, sl], writes=[kcT.res])
                s.dma(vcT[:, sl], vcT_d[:, sl], writes=[vcT.res])
            kccT = c.sb([64, 512], BF16)
            Rt = c.sb([128, 4, 193], BF16)
            s.dma(Rt[:, :, 0:129], Rc_d, writes=[Rt.res])
            w1f = c.sb([64, 32, 128], F32)
            w1b = c.sb([64, 32, 128], BF16)
            w2f = c.sb([128, 64], F32)
            w2b = c.sb([128, 64], BF16)
            pef = c.sb([64, 32], F32)
            peb = c.sb([128, 1], F32)
            xg = c.sb([128, 512], F32)
            x2 = c.sb([128, 512], F32)
            gT = c.sb([128, 512], BF16)
            for which in ("k", "v"):
                src = kcT if which == "k" else vcT
                s.dma(w1f[:], w1k_d if which == "k" else w1v_d, writes=[w1f.res])
                s.dma(w2f[:], w2k_d if which == "k" else w2v_d, writes=[w2f.res])
                s.dma(pef[:], pek_d if which == "k" else pev_d, writes=[pef.res])
                s.op("pool", lambda: nc.gpsimd.tensor_copy(out=w1b[:], in_=w1f[:]), reads=[w1f.res], writes=[w1b.res])
                s.op("pool", lambda: nc.gpsimd.tensor_copy(out=w2b[:], in_=w2f[:]), reads=[w2f.res], writes=[w2b.res])
                for l in range(32):
                    s.op("pe", (lambda l=l: nc.tensor.matmul(pY[:, 0:1], lhsT=w1f[:, l, :], rhs=pef[:, l:l + 1], start=(l == 0), stop=(l == 31))),
                         reads=[w1f.res, pef.res], writes=[pY.res])
                s.op("act", lambda: nc.scalar.copy(out=peb[:], in_=pY[:, 0:1]), reads=[pY.res], writes=[peb.res])
                srcv = src[:].rearrange("p (c s) -> p c s", s=16)
                for l in range(32):
                    rhs = srcv[:, 0:511, l] if l < 16 else srcv[:, 1:512, l - 16]
                    s.op("pe", (lambda l=l, rhs=rhs: nc.tensor.matmul(pX[:, 0:511], lhsT=w1b[:, l, :], rhs=rhs, start=(l == 0), stop=(l == 31))),
                         reads=[w1b.res, src.res], writes=[pX.res])
                s.op("act", lambda: nc.scalar.activation(out=xg[:, 0:511], in_=pX[:, 0:511], func=AF.Identity, bias=peb[:], scale=1.0),
                     reads=[pX.res, peb.res], writes=[xg.res])
                s.op("dve", lambda: nc.vector.tensor_tensor(out=x2[:, 0:511], in0=xg[:, 0:511], in1=xg[:, 0:511], op=ALU.mult), reads=[xg.res], writes=[x2.res])
                s.op("dve", lambda: nc.vector.tensor_scalar(out=x2[:, 0:511], in0=x2[:, 0:511], scalar1=0.044715, scalar2=1.0, op0=ALU.mult, op1=ALU.add),
                     reads=[x2.res], writes=[x2.res])
                s.op("dve", lambda: nc.vector.tensor_tensor(out=x2[:, 0:511], in0=x2[:, 0:511], in1=xg[:, 0:511], op=ALU.mult), reads=[x2.res, xg.res], writes=[x2.res])
                s.op("act", lambda: nc.scalar.activation(out=x2[:, 0:511], in_=x2[:, 0:511], func=AF.Sigmoid, scale=1.5957691216057308),
                     reads=[x2.res], writes=[x2.res])
                s.op("pool", lambda: nc.gpsimd.memset(gT[:], 0.0), writes=[gT.res])
                s.op("dve", lambda: nc.vector.tensor_tensor(out=gT[:, 0:511], in0=xg[:, 0:511], in1=x2[:, 0:511], op=ALU.mult), reads=[xg.res, x2.res, gT.res], writes=[gT.res])
                if which == "k":
                    s.op("pe", lambda: nc.tensor.matmul(pY[0:64, :], lhsT=w2b[:], rhs=gT[:], start=True, stop=True), reads=[w2b.res, gT.res], writes=[pY.res])
                    s.op("act", lambda: nc.scalar.copy(out=kccT[:], in_=pY[0:64, :]), reads=[pY.res], writes=[kccT.res])
                else:
                    for cc in range(4):
                        s.op("pe", (lambda cc=cc: nc.tensor.matmul(pY[:, cc * 64:(cc + 1) * 64], lhsT=gT[:, cc * 128:(cc + 1) * 128], rhs=w2b[:],
                                                                   start=True, stop=True)),
                             reads=[w2b.res, gT.res], writes=[pY.res])
                    s.op("act", lambda: nc.scalar.copy(out=Rt[:, :, 129:193], in_=pY[:, 0:256].rearrange("p (a b) -> p a b", b=64)),
                         reads=[pY.res], writes=[Rt.res])
            mt = load_masks(M_CMP, 5)
            qring = [c.sb([64, 512], BF16) for _ in range(4)]
            imp = [c.sb([128, 4, 128], F32) for _ in range(2)]
            sc_r = [c.sb([128, 128], F32) for _ in range(4)]
            sc2_r = [c.sb([128, 128], F32) for _ in range(4)]
            sb_r = [c.sb([128, 128], F32) for _ in range(4)]
            m8_r = [c.sb([128, 16], F32) for _ in range(4)]
            pT = ps_ring[3]
            usets = [(po_ring[0], po_ring[1]), (pX, pY)]
            items = []
            for qt in range(NQT):
                for h in range(4):
                    ccs = [cc for cc in range(4) if qt - 4 * cc >= 0]
                    for ii, cc in enumerate(ccs):
                        u = qt - 4 * cc
                        items.append(dict(qt=qt, h=h, kb=cc, mi=(M_CMP + u if u <= 4 else None), first=(ii == 0), last=(ii == len(ccs) - 1)))

            def qk_c(n, ps):
                it = items[n]
                qtile = qring[(it["qt"] * 4 + it["h"]) % 4]
                if it["first"]:
                    s.dma(qtile[:], qnr_d[it["h"], :, it["qt"] * 512:(it["qt"] + 1) * 512], writes=[qtile.res])
                s.op("pe", lambda: nc.tensor.matmul(ps[:], lhsT=kccT[:, it["kb"] * 128:(it["kb"] + 1) * 128], rhs=qtile[:], start=True, stop=True),
                     reads=[kccT.res, qtile.res], writes=[ps.res])

            def pv_c(n, pt):
                it = items[n]
                us = usets[(it["qt"] * 4 + it["h"]) % 2]
                for qb in range(4):
                    tl = us[qb // 2]
                    off = (qb % 2) * 193
                    s.op("pe", (lambda qb=qb, tl=tl, off=off: nc.tensor.matmul(tl[:, off:off + 193], lhsT=pt[:, qb * 128:(qb + 1) * 128], rhs=Rt[:, it["kb"], :],
                                                                             start=(it["first"] and qb % 2 == 0), stop=it["last"], skip_group_check=True)),
                         reads=[pt.res, Rt.res], writes=[tl.res])

            def fin_c(n):
                it = items[n]
                qt, h = it["qt"], it["h"]
                us = usets[(qt * 4 + h) % 2]
                rz = rz_ring[h % 2]
                fac = fac_ring[h % 2]
                imp_t = imp[qt % 2]
                for qb in range(4):
                    tl, off = us[qb // 2], (qb % 2) * 193
                    s.op("dve", (lambda qb=qb, tl=tl, off=off: nc.vector.tensor_scalar(out=rz[:, qb:qb + 1], in0=tl[:, off + 128:off + 129], scalar1=1e-30,
                                                                                     scalar2=None, op0=ALU.max)), reads=[tl.res], writes=[rz.res])
                s.op("dve", lambda: nc.vector.reciprocal(out=rz[:], in_=rz[:]), reads=[rz.res], writes=[rz.res])
                s.op("dve", lambda: nc.vector.tensor_tensor(out=fac[:], in0=rz[:], in1=ag[:, 4 * qt:4 * qt + 4, 0], op=ALU.mult), reads=[rz.res, ag.res], writes=[fac.res])
                s.op("dve", lambda: nc.vector.tensor_scalar(out=fac[:], in0=fac[:], scalar1=hsel[:, h:h + 1], scalar2=None, op0=ALU.mult),
                     reads=[fac.res, hsel.res], writes=[fac.res])
                for qb in range(4):
                    tl, off = us[qb // 2], (qb % 2) * 193
                    tb = 4 * qt + qb
                    if h == 0:
                        s.op("dve", (lambda qb=qb, tl=tl, off=off: nc.vector.tensor_scalar(out=imp_t[:, qb, :], in0=tl[:, off:off + 128], scalar1=rz[:, qb:qb + 1],
                                                                                         scalar2=None, op0=ALU.mult)), reads=[tl.res, rz.res], writes=[imp_t.res])
                        s.op("dve", (lambda qb=qb, tl=tl, off=off, tb=tb: nc.vector.tensor_scalar(out=oa[:, tb, :], in0=tl[:, off + 129:off + 193], scalar1=fac[:, qb:qb + 1],
                                                                                                scalar2=None, op0=ALU.mult)), reads=[tl.res, fac.res], writes=[oa.res])
                    else:
                        s.op("dve", (lambda qb=qb, tl=tl, off=off: nc.vector.scalar_tensor_tensor(out=imp_t[:, qb, :], in0=tl[:, off:off + 128], scalar=rz[:, qb:qb + 1],
                                                                                                in1=imp_t[:, qb, :], op0=ALU.mult, op1=ALU.add)),
                             reads=[tl.res, rz.res, imp_t.res], writes=[imp_t.res])
                        s.op("dve", (lambda qb=qb, tl=tl, off=off, tb=tb: nc.vector.scalar_tensor_tensor(out=oa[:, tb, :], in0=tl[:, off + 129:off + 193], scalar=fac[:, qb:qb + 1],
                                                                                                       in1=oa[:, tb, :], op0=ALU.mult, op1=ALU.add)),
                             reads=[tl.res, fac.res, oa.res], writes=[oa.res])
                if h != 3:
                    return
                for qb in range(4):
                    tb = 4 * qt + qb
                    sc, sc2, sbt, m8 = sc_r[qb], sc2_r[qb], sb_r[qb], m8_r[qb]
                    s.op("dve", (lambda qb=qb, sc=sc, tb=tb: nc.vector.tensor_tensor(out=sc[:], in0=imp_t[:, qb, :], in1=addt[:, 126 - 2 * tb:254 - 2 * tb], op=ALU.add)),
                         reads=[imp_t.res, addt.res], writes=[sc.res])
                    s.op("dve", (lambda sc=sc: nc.vector.memset(sc[:, 0:1], 1e30)), reads=[sc.res], writes=[sc.res])
                    s.op("dve", (lambda sc=sc, m8=m8: nc.vector.max(out=m8[:, 0:8], in_=sc[:])), reads=[sc.res], writes=[m8.res])
                    s.op("dve", (lambda sc=sc, sc2=sc2, m8=m8: nc.vector.match_replace(out=sc2[:], in_to_replace=m8[:, 0:8], in_values=sc[:], imm_value=-3e38)),
                         reads=[sc.res, m8.res], writes=[sc2.res])
                    s.op("dve", (lambda sc2=sc2, m8=m8: nc.vector.max(out=m8[:, 8:16], in_=sc2[:])), reads=[sc2.res, m8.res], writes=[m8.res])
                    s.op("dve", (lambda sc=sc, sbt=sbt, m8=m8: nc.vector.tensor_scalar(out=sbt[:], in0=sc[:], scalar1=m8[:, 15:16], scalar2=-30000.0,
                                                                                   op0=ALU.is_lt, op1=ALU.mult)), reads=[sc.res, m8.res], writes=[sbt.res])
                    s.op("pe", (lambda qb=qb, sbt=sbt: nc.tensor.transpose(pT[:, qb * 128:(qb + 1) * 128], sbt[:], ident[:])),
                         reads=[sbt.res, ident.res], writes=[pT.res])
                s.op("act", lambda: nc.scalar.copy(out=selbT[:, qt * 512:(qt + 1) * 512], in_=pT[:]), reads=[pT.res], writes=[selbT.res])
            attn(items, qk_c, pv_c, fin_c, lambda mi: (mt, mi - M_CMP), nps=3)

        with Scope():
            QT = c.sb([64, S], BF16)
            KS = c.sb([64, S], BF16)
            KW = c.sb([64, S], BF16)
            VS = c.sb([128, NKB, 65], BF16)
            VW = c.sb([128, NKB, 65], BF16)
            mtc = load_masks(M_CAUSAL, 4)
            split_load(QT, KS, VS, qr_d, kslT_d, vsl_d)
            split_load(None, KW, VW, None, kwT_d, vw_d)
            mtw = load_masks(M_WIN, 8)

            def branch(KT, V, blocks_of, mt, mask_lo, br, with_sel):
                items = std_items(blocks_of)

                def qk(n, ps):
                    it = items[n]
                    s.op("pe", lambda: nc.tensor.matmul(ps[:], lhsT=KT[:, it["kb"] * 128:(it["kb"] + 1) * 128],
                                                        rhs=QT[:, it["qt"] * 512:(it["qt"] + 1) * 512], start=True, stop=not with_sel),
                         reads=rK(KT, it["kb"]) + rQ(QT, it["qt"]), writes=[ps.res])
                    if with_sel:
                        s.op("pe", lambda: nc.tensor.matmul(ps[:], lhsT=G[:, it["kb"] * 128:(it["kb"] + 1) * 128],
                                                            rhs=selbT[:, it["qt"] * 512:(it["qt"] + 1) * 512], start=False, stop=True),
                             reads=[G.res, selbT.res], writes=[ps.res])

                def fin(n):
                    qt = items[n]["qt"]
                    po = po_ring[qt % 2]
                    rz = rz_of(qt, po)
                    fac = fac_ring[qt % 2]
                    s.op("dve", lambda: nc.vector.tensor_tensor(out=fac[:], in0=rz[:], in1=ag[:, 4 * qt:4 * qt + 4, br], op=ALU.mult),
                         reads=[rz.res, ag.res], writes=[fac.res])
                    for qb in range(4):
                        tb = 4 * qt + qb
                        s.op("dve", (lambda qb=qb, tb=tb: nc.vector.scalar_tensor_tensor(out=oa[:, tb, :], in0=po[:, qb * 65:qb * 65 + 64], scalar=fac[:, qb:qb + 1],
                                                                                         in1=oa[:, tb, :], op0=ALU.mult, op1=ALU.add)),
                             reads=[po.res, fac.res, oa.res], writes=[oa.res])
                attn(items, qk, std_pv(items, V), fin, lambda mi: (mt, mi - mask_lo))
            branch(KS, VS, causal_blocks, mtc, M_CAUSAL, 1, True)
            branch(KW, VW, lambda qt: [(4 * qt - 4 + w, M_WIN + w) for w in range(8) if 4 * qt - 4 + w >= 0], mtw, M_WIN, 2, False)
        s.op("act", lambda: nc.scalar.copy(out=ost[:], in_=oa[:]), reads=[oa.res], writes=[ost.res])
        s.dma(o_d[0], ost[:], reads=[ost.res], is_output=True)
    c.close()
    return c


def run_B(cB, inp, l, proj, misc):
    import ml_dtypes
    bf = ml_dtypes.bfloat16
    cst = consts_B()

    def head_rows(P, ch0, i):
        return np.ascontiguousarray(P[ch0 + i // 2][(i % 2) * 64:(i % 2) * 64 + 64])

    def vaug(vT):
        v = vT.T.reshape(NKB, 128, 64).transpose(1, 0, 2)
        return np.ascontiguousarray(np.concatenate([v, np.ones((128, NKB, 1), bf)], axis=2))
    cl = 32 * 64
    w1k = np.ascontiguousarray(inp["nsa_ck_w1"][l].reshape(32, 64, 128).transpose(1, 0, 2))
    w1v = np.ascontiguousarray(inp["nsa_cv_w1"][l].reshape(32, 64, 128).transpose(1, 0, 2))
    pek = np.ascontiguousarray(inp["nsa_pe_k"][l].T)
    pev = np.ascontiguousarray(inp["nsa_pe_v"][l].T)
    maps = []
    for core in range(8):
        b, i = core // 4, core % 4
        P = proj[b]
        m = dict(cst)
        m["qnr"] = np.ascontiguousarray(np.stack([head_rows(P, 24, h) for h in range(4)]))
        m["qr"] = head_rows(P, 0, i)
        m["kcT"] = np.ascontiguousarray(P[3][0:64]); m["vcT"] = np.ascontiguousarray(P[12][0:64])
        m["kslT"] = np.ascontiguousarray(P[2][0:64]); m["kwT"] = np.ascontiguousarray(P[2][64:128])
        m["vsl"] = vaug(P[12][64:128]); m["vw"] = vaug(P[13][0:64])
        ag = misc[b][3 * i:3 * i + 3]
        m["ag"] = np.ascontiguousarray(ag.T.reshape(NKB, 128, 3).transpose(1, 0, 2))
        m["dq"] = head_rows(P, 4, i); m["dk"] = head_rows(P, 6, i); m["dv"] = vaug(head_rows(P, 14, i))
        m["sq"] = head_rows(P, 16, i); m["sk"] = head_rows(P, 18, i); m["sv"] = vaug(head_rows(P, 20, i))
        m["fq"] = head_rows(P, 8, i); m["fk"] = head_rows(P, 10, i); m["fv"] = vaug(head_rows(P, 22, i))
        m["logf"] = np.ascontiguousarray(misc[b][32 + i:33 + i])
        m["w1k"] = w1k; m["w1v"] = w1v; m["w2k"] = inp["nsa_ck_w2"][l]; m["w2v"] = inp["nsa_cv_w2"][l]
        m["pek"] = pek; m["pev"] = pev
        hs = np.zeros((128, 4), np.float32); hs[:, i] = 1.0
        m["hsel"] = hs
        maps.append(m)
    res = run_bass_kernel_spmd(cB.nc, maps, core_ids=list(range(8)))
    outs = []
    for b in range(2):
        ob = np.zeros((4, 256, S), bf)
        for i in range(4):
            o = res.results[b * 4 + i]["o"]
            for mm in range(4):
                tok = o[mm].transpose(1, 0, 2).reshape(S, 64)
                ob[mm, i * 64:(i + 1) * 64, :] = tok.T
        outs.append(ob)
    return outs


def build_C1(c=None):
    own = c is None
    if own:
        c = Ctx()
    ph = c.begin_phase()
    nc, s = c.nc, c.s
    oT_d = c.dram("oT", [4, 2, 128, NT], BF16, "ExternalInput")
    gates_d = c.dram("gates", [32, 128, NT], F32, "ExternalInput")
    xT_d = c.dram("xT", [128, 8, NT], F32, "ExternalInput")
    wbr_d = c.dram("wbr", [128, 8, 1024], F32, "ExternalInput")
    wout_d = c.dram("wout", [128, 8, 1024], F32, "ExternalInput")
    ga_d = c.dram("ga", [128, 8], F32, "ExternalInput")
    x1_d = c.dram("x1T", [128, 8, NT], F32, "ExternalOutput")

    xT = c.sb([128, 8, NT], F32)
    zT = c.sb([128, 8, TT], BF16)
    wbr = c.sb([128, 8, 1024], BF16)
    wout = c.sb([128, 8, 1024], BF16)
    ga = c.sb([128, 8], F32)
    stg = [c.sb([128, 2, 1024], F32) for _ in range(2)]
    s.dma(ga[:], ga_d, writes=[ga.res])
    for j in range(8):
        s.dma(xT[:, j, :], xT_d[:, j, :], writes=[xT.res])
    k = 0
    for (dst, src) in ((wbr, wbr_d), (wout, wout_d)):
        for q in range(4):
            st = stg[k % 2]
            k += 1
            s.dma(st[:], src[:, 2 * q:2 * q + 2, :], writes=[st.res])
            s.op("pool", (lambda st=st, dst=dst, q=q: nc.gpsimd.tensor_copy(out=dst[:, 2 * q:2 * q + 2, :], in_=st[:])), reads=[st.res], writes=[dst.res])
    ot_r = [c.sb([128, 8, TT], BF16) for _ in range(2)]
    gt_r = [[c.sb([128, TT], F32) for _ in range(4)] for _ in range(2)]
    pm_r = [c.ps([128, TT]) for _ in range(4)]
    pmix = [c.ps([128, TT]) for _ in range(2)]
    t_r = [c.sb([128, TT], F32) for _ in range(4)]
    for tt in range(NT // TT):
        sl = slice(tt * TT, (tt + 1) * TT)
        ot = ot_r[tt % 2]
        for m in range(4):
            for kc in range(2):
                s.dma(ot[:, m * 2 + kc, :], oT_d[m, kc, :, sl], writes=[ot.res])
        for dc in range(8):
            gts = gt_r[dc % 2]
            for m in range(4):
                s.dma(gts[m][:], gates_d[m * 8 + dc, :, sl], writes=[gts[m].res])
            for m in range(4):
                for kc in range(2):
                    s.op("pe", (lambda m=m, kc=kc: nc.tensor.matmul(pm_r[m][:], lhsT=wbr[:, m * 2 + kc, dc * 128:(dc + 1) * 128], rhs=ot[:, m * 2 + kc, :],
                                                                    start=(kc == 0), stop=(kc == 1))),
                         reads=[wbr.res, ot.res], writes=[pm_r[m].res])
            for m in range(4):
                s.op("dve", (lambda m=m: nc.vector.tensor_tensor(out=t_r[m][:], in0=pm_r[m][:], in1=gts[m][:], op=ALU.mult)),
                     reads=[pm_r[m].res, gts[m].res], writes=[t_r[m].res])
            s.op("pool", lambda: nc.gpsimd.tensor_tensor(out=t_r[0][:], in0=t_r[0][:], in1=t_r[1][:], op=ALU.add), reads=[t_r[0].res, t_r[1].res], writes=[t_r[0].res])
            s.op("pool", lambda: nc.gpsimd.tensor_tensor(out=t_r[2][:], in0=t_r[2][:], in1=t_r[3][:], op=ALU.add), reads=[t_r[2].res, t_r[3].res], writes=[t_r[2].res])
            s.op("pool", (lambda dc=dc: nc.gpsimd.tensor_tensor(out=zT[:, dc, :], in0=t_r[0][:], in1=t_r[2][:], op=ALU.add)),
                 reads=[t_r[0].res, t_r[2].res], writes=[zT.res])
        for ec in range(8):
            pmx = pmix[ec % 2]
            for j in range(8):
                s.op("pe", (lambda j=j, ec=ec, pmx=pmx: nc.tensor.matmul(pmx[:], lhsT=wout[:, j, ec * 128:(ec + 1) * 128], rhs=zT[:, j, :],
                                                                         start=(j == 0), stop=(j == 7))),
                     reads=[wout.res, zT.res], writes=[pmx.res])
            s.op("dve", (lambda ec=ec, pmx=pmx: nc.vector.scalar_tensor_tensor(out=xT[:, ec, sl], in0=pmx[:], scalar=ga[:, ec:ec + 1], in1=xT[:, ec, sl],
                                                                              op0=ALU.mult, op1=ALU.add)),
                 reads=[pmx.res, ga.res, xT.res], writes=[xT.res])
    for j in range(8):
        s.dma(x1_d[:, j, :], xT[:, j, :], reads=[xT.res], is_output=True)
    c.end_phase(ph)
    if own:
        c.close()
    return c


def maps_C1(inp, l, o, gates, xT_all, mod):
    wbr = np.ascontiguousarray(inp["w_branch"][l].reshape(4, 2, 128, D).transpose(2, 0, 1, 3).reshape(128, 8, D))
    wout = np.ascontiguousarray(inp["w_out"][l].reshape(8, 128, D).transpose(1, 0, 2))
    maps = []
    for core in range(8):
        b, q = core // 4, core % 4
        tsl = slice(q * NT, (q + 1) * NT)
        maps.append({"oT": np.ascontiguousarray(o[b][:, :, tsl].reshape(4, 2, 128, NT)),
                     "gates": np.ascontiguousarray(gates[b][:, :, tsl]),
                     "xT": np.ascontiguousarray(xT_all[b][:, tsl].reshape(8, 128, NT).transpose(1, 0, 2)),
                     "wbr": wbr, "wout": wout, "ga": np.ascontiguousarray(mod[l, b][:, 16:24])})
    return maps


def collect_x(results, key):
    out = np.zeros((2, D, S), np.float32)
    for core in range(8):
        b, q = core // 4, core % 4
        out[b][:, q * NT:(q + 1) * NT] = results[core][key].transpose(1, 0, 2).reshape(D, NT)
    return out


def run_C1(cC1, inp, l, o, gates, xT_all, mod):
    maps = maps_C1(inp, l, o, gates, xT_all, mod)
    res = run_bass_kernel_spmd(cC1.nc, maps, core_ids=list(range(8)))
    return collect_x(res.results, "x1T")


HT = 1024
NFC = 22


def build_C2(n_exp, moe, c=None):
    own = c is None
    if own:
        c = Ctx()
    ph = c.begin_phase()
    nc, s = c.nc, c.s
    x1_d = c.dram("x1T", [128, 8, NT], F32, "ExternalInput")
    mod_d = c.dram("modF", [128, 24], F32, "ExternalInput")
    gn_d = c.dram("gn", [128, 8], F32, "ExternalInput")
    w1_d = c.dram("w1", [n_exp, NFC, 128, 8, 128], F32, "ExternalInput")
    w3_d = c.dram("w3", [n_exp, NFC, 128, 8, 128], F32, "ExternalInput")
    w2_d = c.dram("w2", [n_exp, 8, 128, NFC, 128], F32, "ExternalInput")
    if moe:
        rw_d = c.dram("rw", [128, 8, 8], F32, "ExternalInput")
        oh_d = c.dram("onehot", [8, 8, 128], F32, "ExternalInput")
        id_d = c.dram("ident", [128, 128], F32, "ExternalInput")
    x2_d = c.dram("x2T", [128, 8, NT], F32, "ExternalOutput")

    acc = c.sb([128, 8, HT], F32)
    hT = c.sb([128, 8, HT], BF16)
    act = c.sb([128, NFC, HT], BF16)
    rs = c.sb([128, HT], F32)
    ones = c.sb([128, 128], F32)
    epsb = c.sb([128, 1], F32)
    mod = c.sb([128, 24], F32)
    gn = c.sb([128, 8], F32)
    Acol = c.sb([128, 8], F32)
    Bcol = c.sb([128, 8], F32)
    ring = [c.sb([128, TT], F32) for _ in range(4)]
    w1s = [c.sb([128, 8, 128], F32) for _ in range(2)]
    w3s = [c.sb([128, 8, 128], F32) for _ in range(2)]
    w1b = [c.sb([128, 8, 128], BF16) for _ in range(3)]
    w3b = [c.sb([128, 8, 128], BF16) for _ in range(3)]
    w2s = [c.sb([128, NFC, 128], F32) for _ in range(2)]
    w2b = [c.sb([128, NFC, 128], BF16) for _ in range(3)]
    sa_r = [c.sb([128, TT], F32) for _ in range(3)]
    u_r = [c.sb([128, TT], F32) for _ in range(3)]
    pa_r = [c.ps([128, TT]) for _ in range(2)]
    pg_r = [c.ps([128, TT]) for _ in range(2)]
    po_r = [c.ps([128, TT]) for _ in range(2)]
    pbank = c.ps([128, TT])
    pmisc = c.ps([128, TT])
    s.op("pool", lambda: nc.gpsimd.memset(ones[:], 1.0), writes=[ones.res])
    s.op("pool", lambda: nc.gpsimd.memset(epsb[:], EPS), writes=[epsb.res])
    s.dma(mod[:], mod_d, writes=[mod.res])
    s.dma(gn[:], gn_d, writes=[gn.res])
    emit_AB(c, gn, mod, 0, 8, Acol, Bcol)
    if moe:
        rw = c.sb([128, 8, 8], F32)
        oh = c.sb([8, 8, 128], F32)
        ident = c.sb([128, 128], F32)
        GT = c.sb([8, HT], F32)
        gb_r = [c.sb([128, HT], F32) for _ in range(2)]
        h32_r = [c.sb([128, TT], F32) for _ in range(2)]
        lg = c.sb([128, 8, 8], F32)
        m8 = c.sb([128, 8], F32)
        nt1 = c.sb([128, 1], F32)
        e2 = c.sb([128, 1], F32)
        ex = c.sb([128, 8], F32)
        selm = c.sb([128, 8], F32)
        Gt = c.sb([128, 8], F32)
        for t, d in ((rw, rw_d), (oh, oh_d), (ident, id_d)):
            s.dma(t[:], d, writes=[t.res])

    for hf in range(NT // HT):
        hsl = slice(hf * HT, (hf + 1) * HT)
        for j in range(8):
            s.dma(acc[:, j, :], x1_d[:, j, hsl], writes=[acc.res])
        if not moe:
            emit_modnorm(c, acc, hT, HT, ones, Acol, Bcol, epsb, ring, pbank, rs)
        else:
            def after_h(tmp, j, tt, sl):
                h32 = h32_r[j % 2]
                s.op("act", lambda: nc.scalar.activation(out=h32[:], in_=tmp[:], func=AF.Identity, scale=Acol[:, j:j + 1], bias=Bcol[:, j:j + 1]),
                     reads=[tmp.res, Acol.res, Bcol.res], writes=[h32.res])
                s.op("pool", lambda: nc.gpsimd.tensor_copy(out=hT[:, j, sl], in_=h32[:]), reads=[h32.res], writes=[hT.res])
                for tb in range(4):
                    col = (tt * 4 + tb) * 8
                    s.op("pe", (lambda tb=tb, col=col: nc.tensor.matmul(pmisc[:, col:col + 8], lhsT=h32[:, tb * 128:(tb + 1) * 128], rhs=rw[:, j, :],
                                                                        start=(j == 0 and tb == 0 and tt == 0), stop=(j == 7), skip_group_check=True)),
                         reads=[h32.res, rw.res], writes=[pmisc.res])
            emit_modnorm(c, acc, hT, HT, ones, Acol, Bcol, epsb, ring, pbank, rs, after_h=after_h)
            s.op("act", lambda: nc.scalar.copy(out=lg[:], in_=pmisc[:, 0:64].rearrange("p (a b) -> p a b", b=8)), reads=[pmisc.res], writes=[lg.res])
            for tb in range(8):
                s.op("dve", (lambda tb=tb: nc.vector.max(out=m8[:], in_=lg[:, tb, :])), reads=[lg.res], writes=[m8.res])
                s.op("dve", lambda: nc.vector.tensor_scalar(out=nt1[:], in0=m8[:, 0:1], scalar1=-1.0, scalar2=None, op0=ALU.mult), reads=[m8.res], writes=[nt1.res])
                s.op("act", (lambda tb=tb: nc.scalar.activation(out=ex[:], in_=lg[:, tb, :], func=AF.Exp, bias=nt1[:], scale=1.0)),
                     reads=[lg.res, nt1.res], writes=[ex.res])
                s.op("act", lambda: nc.scalar.activation(out=e2[:], in_=m8[:, 1:2], func=AF.Exp, bias=nt1[:], scale=1.0), reads=[m8.res, nt1.res], writes=[e2.res])
                s.op("dve", lambda: nc.vector.tensor_scalar(out=e2[:], in0=e2[:], scalar1=1.0, scalar2=None, op0=ALU.add), reads=[e2.res], writes=[e2.res])
                s.op("dve", lambda: nc.vector.reciprocal(out=e2[:], in_=e2[:]), reads=[e2.res], writes=[e2.res])
                s.op("dve", (lambda tb=tb: nc.vector.tensor_scalar(out=selm[:], in0=lg[:, tb, :], scalar1=m8[:, 1:2], scalar2=None, op0=ALU.is_ge)),
                     reads=[lg.res, m8.res], writes=[selm.res])
                s.op("dve", lambda: nc.vector.scalar_tensor_tensor(out=Gt[:], in0=ex[:], scalar=e2[:, 0:1], in1=selm[:], op0=ALU.mult, op1=ALU.mult),
                     reads=[ex.res, e2.res, selm.res], writes=[Gt.res])
                s.op("pe", (lambda tb=tb: nc.tensor.transpose(pbank[0:8, (tb % 4) * 128:(tb % 4 + 1) * 128], Gt[:], ident[:])), reads=[Gt.res, ident.res], writes=[pbank.res])
                if tb % 4 == 3:
                    q4 = tb // 4
                    s.op("act", (lambda q4=q4: nc.scalar.copy(out=GT[:, q4 * 512:(q4 + 1) * 512], in_=pbank[0:8, 0:512])), reads=[pbank.res], writes=[GT.res])

        items = []
        kf = kg = 0
        for e in range(n_exp):
            for fc in range(NFC):
                for tt in range(2):
                    items.append(("f", e, fc, tt, kf))
                kf += 1
            for ec in range(8):
                for tt in range(2):
                    items.append(("g", e, ec, tt, kg))
                kg += 1

        def s_load(n):
            kind, e, ci, tt, k = items[n]
            if tt != 0:
                return
            if kind == "f":
                if moe and ci == 0:
                    g_b = gb_r[e % 2]
                    for t2 in range(2):
                        s.op("pe", (lambda t2=t2, e=e: nc.tensor.matmul(pmisc[:], lhsT=oh[:, e, :], rhs=GT[:, t2 * TT:(t2 + 1) * TT], start=True, stop=True)),
                             reads=[oh.res, GT.res], writes=[pmisc.res])
                        s.op("act", (lambda t2=t2, g_b=g_b: nc.scalar.copy(out=g_b[:, t2 * TT:(t2 + 1) * TT], in_=pmisc[:])), reads=[pmisc.res], writes=[g_b.res])
                s.dma(w1s[k % 2][:], w1_d[e, ci], writes=[w1s[k % 2].res], q="act")
                s.dma(w3s[k % 2][:], w3_d[e, ci], writes=[w3s[k % 2].res], q="act")
            else:
                s.dma(w2s[k % 2][:], w2_d[e, ci], writes=[w2s[k % 2].res], q="act")

        def s_cast(n):
            kind, e, ci, tt, k = items[n]
            if tt != 0:
                return
            if kind == "f":
                s.op("pool", lambda: nc.gpsimd.tensor_copy(out=w1b[k % 3][:], in_=w1s[k % 2][:]), reads=[w1s[k % 2].res], writes=[w1b[k % 3].res])
                s.op("dve", lambda: nc.vector.tensor_copy(out=w3b[k % 3][:], in_=w3s[k % 2][:]), reads=[w3s[k % 2].res], writes=[w3b[k % 3].res])
            else:
                half = NFC // 2
                s.op("pool", lambda: nc.gpsimd.tensor_copy(out=w2b[k % 3][:, 0:half, :], in_=w2s[k % 2][:, 0:half, :]), reads=[w2s[k % 2].res], writes=[w2b[k % 3].res])
                s.op("dve", lambda: nc.vector.tensor_copy(out=w2b[k % 3][:, half:NFC, :], in_=w2s[k % 2][:, half:NFC, :]), reads=[w2s[k % 2].res], writes=[w2b[k % 3].res])

        def s_mm(n):
            kind, e, ci, tt, k = items[n]
            sl = slice(tt * TT, (tt + 1) * TT)
            if kind == "f":
                pa, pg = pa_r[n % 2], pg_r[n % 2]
                for j in range(8):
                    s.op("pe", (lambda j=j: nc.tensor.matmul(pa[:], lhsT=w1b[k % 3][:, j, :], rhs=hT[:, j, sl], start=(j == 0), stop=(j == 7))),
                         reads=[w1b[k % 3].res, hT.res], writes=[pa.res])
                for j in range(8):
                    s.op("pe", (lambda j=j: nc.tensor.matmul(pg[:], lhsT=w3b[k % 3][:, j, :], rhs=hT[:, j, sl], start=(j == 0), stop=(j == 7))),
                         reads=[w3b[k % 3].res, hT.res], writes=[pg.res])
            else:
                po = po_r[n % 2]
                for fc in range(NFC):
                    s.op("pe", (lambda fc=fc: nc.tensor.matmul(po[:], lhsT=w2b[k % 3][:, fc, :], rhs=act[:, fc, sl], start=(fc == 0), stop=(fc == NFC - 1))),
                         reads=[w2b[k % 3].res, act.res], writes=[po.res])

        def s_post(n):
            kind, e, ci, tt, k = items[n]
            sl = slice(tt * TT, (tt + 1) * TT)
            if kind == "f":
                pa, pg = pa_r[n % 2], pg_r[n % 2]
                sa = sa_r[n % 3]
                s.op("act", lambda: nc.scalar.activation(out=sa[:], in_=pa[:], func=AF.Silu), reads=[pa.res], writes=[sa.res])
                if not moe:
                    s.op("dve", lambda: nc.vector.tensor_tensor(out=act[:, ci, sl], in0=sa[:], in1=pg[:], op=ALU.mult), reads=[sa.res, pg.res], writes=[act.res])
                else:
                    u = u_r[n % 3]
                    g_b = gb_r[e % 2]
                    s.op("dve", lambda: nc.vector.tensor_tensor(out=u[:], in0=pg[:], in1=g_b[:, sl], op=ALU.mult), reads=[pg.res, g_b.res], writes=[u.res])
                    s.op("pool", lambda: nc.gpsimd.tensor_tensor(out=act[:, ci, sl], in0=sa[:], in1=u[:], op=ALU.mult), reads=[sa.res, u.res], writes=[act.res])
            else:
                po = po_r[n % 2]
                s.op("dve", lambda: nc.vector.scalar_tensor_tensor(out=acc[:, ci, sl], in0=po[:], scalar=mod[:, 16 + ci:17 + ci], in1=acc[:, ci, sl],
                                                                   op0=ALU.mult, op1=ALU.add), reads=[po.res, mod.res, acc.res], writes=[acc.res])
        pipeline(len(items), [s_load, s_cast, s_mm, s_post])
        for j in range(8):
            s.dma(x2_d[:, j, hsl], acc[:, j, :], reads=[acc.res], is_output=True)
    c.end_phase(ph)
    if own:
        c.close()
    return c


def maps_C2(inp, l, x1T_all, mod, moe):
    if moe:
        w1, w3, w2 = inp["moe_w1"][l // 2], inp["moe_w3"][l // 2], inp["moe_w2"][l // 2]
    else:
        w1, w3, w2 = inp["ffn_w1"][l // 2][None], inp["ffn_w3"][l // 2][None], inp["ffn_w2"][l // 2][None]
    E = w1.shape[0]
    lay1 = lambda w: np.ascontiguousarray(w.reshape(E, 8, 128, NFC, 128).transpose(0, 3, 2, 1, 4))
    lay2 = lambda w: np.ascontiguousarray(w.reshape(E, NFC, 128, 8, 128).transpose(0, 3, 2, 1, 4))
    w1l, w3l, w2l = lay1(w1), lay1(w3), lay2(w2)
    gn = np.ascontiguousarray(inp["norm_ffn"][l].reshape(8, 128).T)
    maps = []
    for core in range(8):
        b, q = core // 4, core % 4
        m = {"modF": np.ascontiguousarray(mod[l, b][:, 24:48]), "gn": gn, "w1": w1l, "w3": w3l, "w2": w2l}
        if x1T_all is not None:
            m["x1T"] = np.ascontiguousarray(x1T_all[b][:, q * NT:(q + 1) * NT].reshape(8, 128, NT).transpose(1, 0, 2))
        if moe:
            m["rw"] = np.ascontiguousarray(inp["router_w"][l // 2].reshape(8, 128, 8).transpose(1, 0, 2))
            oh = np.zeros((8, 8, 128), np.float32)
            for e in range(8):
                oh[e, e, :] = 1.0
            m["onehot"] = oh
            m["ident"] = np.eye(128, dtype=np.float32)
        maps.append(m)
    return maps


def run_C2(cC2, inp, l, x1T_all, mod, moe):
    maps = maps_C2(inp, l, x1T_all, mod, moe)
    res = run_bass_kernel_spmd(cC2.nc, maps, core_ids=list(range(8)))
    return collect_x(res.results, "x2T")


def build_CA(moe, with_A):
    c = Ctx()
    c.alias = {}
    c.pre = "c1_"
    c.kind_override = {"c1_x1T": "Internal"}
    build_C1(c)
    c.pre = "c2_"
    c.alias["c2_x1T"] = c.made["c1_x1T"]
    build_C2(8 if moe else 1, moe, c)
    if with_A:
        c.pre = "a_"
        c.alias["a_xT"] = c.made["c2_x2T"]
        build_A(c)
    c.close()
    return c


def run_CA(cCA, inp, l, o, gates, xT_all, mod, moe, with_A):
    m1 = maps_C1(inp, l, o, gates, xT_all, mod)
    m2 = maps_C2(inp, l, None, mod, moe)
    m3 = maps_A(inp, l + 1, None, mod) if with_A else [dict() for _ in range(8)]
    maps = []
    for i in range(8):
        m = {"c1_" + k: v for k, v in m1[i].items()}
        m.update({"c2_" + k: v for k, v in m2[i].items()})
        m.update({"a_" + k: v for k, v in m3[i].items()})
        maps.append(m)
    res = run_bass_kernel_spmd(cCA.nc, maps, core_ids=list(range(8)))
    x2T = collect_x(res.results, "c2_x2T")
    nxt = collect_A(res.results, "a_") if with_A else None
    return x2T, nxt


_PROGS = {}


def _prog(name, fn):
    if name not in _PROGS:
        _PROGS[name] = fn()
    return _PROGS[name]


def kernel(**inp):
    inp = {k: np.asarray(v) for k, v in inp.items()}
    mod = run_M(inp)
    xT_all = np.ascontiguousarray(inp["x"].astype(np.float32).transpose(0, 2, 1))
    cA = _prog("A", build_A)
    proj, gates, misc = run_A(cA, inp, 0, xT_all, mod)
    cB = _prog("B", build_B)
    for l in range(2):
        o = run_B(cB, inp, l, proj, misc)
        moe = (l % 2 == 1)
        with_A = (l == 0)
        cCA = _prog("CA%d" % l, lambda: build_CA(moe, with_A))
        xT_all, nxt = run_CA(cCA, inp, l, o, gates, xT_all, mod, moe, with_A)
        if with_A:
            proj, gates, misc = nxt
    return np.ascontiguousarray(xT_all.transpose(0, 2, 1)).astype(np.float32)
```

```python
import contextlib
import numpy as np
import concourse.bass as bass
import concourse.mybir as mybir
from concourse.bass_utils import run_bass_kernel_spmd

F32 = mybir.dt.float32
BF16 = mybir.dt.bfloat16
I32 = mybir.dt.int32
AF = mybir.ActivationFunctionType
ALU = mybir.AluOpType
AX = mybir.AxisListType


class Res:
    __slots__ = ("w", "r")

    def __init__(self):
        self.w = None
        self.r = {}


class Sched:
    NDMA = 24

    def __init__(self, nc, es):
        self.nc = nc
        self.engs = {"pe": nc.tensor, "act": nc.scalar, "dve": nc.vector,
                     "pool": nc.gpsimd, "sp": nc.sync}
        self.sem = {}
        self.cnt = {}
        for k in self.engs:
            self.sem[k] = es.enter_context(nc.semaphore("s_" + k))
            self.cnt[k] = 0
        for i in range(self.NDMA):
            k = "d%d" % i
            self.sem[k] = es.enter_context(nc.semaphore("s_" + k))
            self.cnt[k] = 0
        self.seen = {k: {} for k in self.engs}
        self.dma_rr = 0
        self.out_events = []

    def _wait(self, e, ev):
        if ev is None:
            return
        key, val = ev
        if key == e and e == "pe":
            return
        if self.seen[e].get(key, 0) >= val:
            return
        self.engs[e].wait_ge(self.sem[key], val)
        self.seen[e][key] = val

    def _deps(self, e, reads, writes):
        for r in reads:
            self._wait(e, r.w)
        for r in writes:
            self._wait(e, r.w)
            for k, v in r.r.items():
                self._wait(e, (k, v))

    def op(self, e, fn, reads=(), writes=()):
        self._deps(e, reads, writes)
        ins = fn()
        self.cnt[e] += 1
        ins.then_inc(self.sem[e], 1)
        ev = (e, self.cnt[e])
        for r in reads:
            r.r[e] = ev[1]
        for r in writes:
            r.w = ev
            r.r = {}
        return ev

    def dma(self, out, in_, reads=(), writes=(), q="sp", is_output=False, **kw):
        k = "d%d" % self.dma_rr
        self.dma_rr = (self.dma_rr + 1) % self.NDMA
        self._wait(q, (k, self.cnt[k]))
        self._deps(q, reads, writes)
        ins = self.engs[q].dma_start(out=out, in_=in_, **kw)
        self.cnt[k] += 16
        ins.then_inc(self.sem[k], 16)
        ev = (k, self.cnt[k])
        for r in reads:
            r.r[k] = ev[1]
        for r in writes:
            r.w = ev
            r.r = {}
        if is_output:
            self.out_events.append(ev)
        return ev

    def finish(self):
        for i in range(self.NDMA):
            k = "d%d" % i
            self._wait("sp", (k, self.cnt[k]))
        for k in ("pe", "act", "dve", "pool"):
            self._wait("sp", (k, self.cnt[k]))


class Tile:
    def __init__(self, t):
        self.t = t
        self.res = Res()

    def __getitem__(self, idx):
        return self.t[idx]


class Ctx:
    def __init__(self, name="k"):
        self.nc = bass.Bass("TRN2", target_bir_lowering=False)
        self.es = contextlib.ExitStack()
        self.s = Sched(self.nc, self.es)
        self.n = 0

    def sb(self, shape, dt, name=None):
        self.n += 1
        return Tile(self.es.enter_context(self.nc.sbuf_tensor(name or ("t%d" % self.n), list(shape), dt)))

    def ps(self, shape, dt=F32, name=None):
        self.n += 1
        return Tile(self.es.enter_context(self.nc.psum_tensor(name or ("p%d" % self.n), list(shape), dt)))

    pre = ""
    alias = None
    kind_override = None

    def dram(self, name, shape, dt, kind):
        full = self.pre + name
        if self.alias and full in self.alias:
            return self.alias[full]
        if self.kind_override and full in self.kind_override:
            kind = self.kind_override[full]
        ap = self.nc.dram_tensor(full, list(shape), dt, kind=kind).ap()
        if self.alias is None:
            self.alias = {}
        self.made = getattr(self, "made", {})
        self.made[full] = ap
        return ap

    def begin_phase(self):
        es = contextlib.ExitStack()
        old, self.es = self.es, es
        return (old, es)

    def end_phase(self, ph):
        barrier(self)
        self.es = ph[0]
        ph[1].close()

    def close(self):
        self.s.finish()
        self.es.close()


D = 1024
S = 8192
NB = 2
NT = 2048
TT = 512
NCH_IN = 57
A_MAXCH = NCH_IN
EPS = 1e-6
TWO_PI = float(2 * np.pi)
C1_2PI = 6.28125
C2_2PI = TWO_PI - C1_2PI


def barrier(c):
    s = c.s
    for e in ("pe", "act", "dve", "pool", "sp"):
        for k in list(s.cnt.keys()):
            if k != e:
                s._wait(e, (k, s.cnt[k]))


def build_M():
    c = Ctx()
    nc, s = c.nc, c.s
    cT_d = c.dram("cT", [128, 8, 2], F32, "ExternalInput")
    w_d = c.dram("w", [12, 128, 8, 128], F32, "ExternalInput")
    b_d = c.dram("b", [128, 12], F32, "ExternalInput")
    o_d = c.dram("modT", [128, 12, 2], F32, "ExternalOutput")
    cT = c.sb([128, 8, 2], F32)
    ca = c.sb([128, 8, 2], F32)
    bt = c.sb([128, 12], F32)
    ot = c.sb([128, 12, 2], F32)
    s.dma(cT[:], cT_d, writes=[cT.res])
    s.dma(bt[:], b_d, writes=[bt.res])
    s.op("act", lambda: nc.scalar.activation(out=ca[:], in_=cT[:], func=AF.Silu), reads=[cT.res], writes=[ca.res])
    wts = [c.sb([128, 8, 128], F32) for _ in range(3)]
    pm = c.ps([128, 12, 2])
    for j in range(12):
        wt = wts[j % 3]
        s.dma(wt[:], w_d[j], writes=[wt.res])
        for k in range(8):
            s.op("pe", (lambda wt=wt, k=k, j=j: nc.tensor.matmul(pm[:, j, :], lhsT=wt[:, k, :], rhs=ca[:, k, :],
                                                                 start=(k == 0), stop=(k == 7))),
                 reads=[wt.res, ca.res], writes=[pm.res])
    for b in range(2):
        s.op("dve", (lambda b=b: nc.vector.tensor_tensor(out=ot[:, :, b], in0=pm[:, :, b], in1=bt[:], op=ALU.add)),
             reads=[pm.res, bt.res], writes=[ot.res])
    s.dma(o_d, ot[:], reads=[ot.res], is_output=True)
    c.close()
    return c


def run_M(inp):
    c = build_M()
    cT = np.ascontiguousarray(inp["c"].T.reshape(8, 128, 2).transpose(1, 0, 2))
    w_all = inp["w_ada"]
    b_all = inp["b_ada"]
    maps = []
    for i in range(8):
        chunks = [(g // 48, g % 48) for g in range(i * 12, i * 12 + 12)]
        w = np.stack([w_all[l][:, n * 128:(n + 1) * 128].reshape(8, 128, 128).transpose(1, 0, 2) for l, n in chunks])
        b = np.stack([b_all[l][n * 128:(n + 1) * 128] for l, n in chunks], axis=1)
        maps.append({"cT": cT, "w": np.ascontiguousarray(w), "b": np.ascontiguousarray(b)})
    res = run_bass_kernel_spmd(c.nc, maps, core_ids=list(range(8)))
    mod = np.zeros((2, 2, 128, 48), np.float32)
    for i in range(8):
        o = res.results[i]["modT"]
        for jj, g in enumerate(range(i * 12, i * 12 + 12)):
            mod[g // 48, :, :, g % 48] = o[:, jj, :].T
    return mod


def emit_modnorm(c, src, hT, ntok, ones, Acol, Bcol, epsb, tmp_ring, pbank, rs, after_h=None, sq_ring=None):
    nc, s = c.nc, c.s
    ntt = ntok // TT
    if sq_ring is None:
        sq_ring = tmp_ring
    for tt in range(ntt):
        sl = slice(tt * TT, (tt + 1) * TT)
        for j in range(8):
            sq = sq_ring[j % len(sq_ring)]
            s.op("act", (lambda sq=sq, j=j: nc.scalar.activation(out=sq[:], in_=src[:, j, sl], func=AF.Square)),
                 reads=[src.res], writes=[sq.res])
            s.op("pe", (lambda sq=sq, j=j: nc.tensor.matmul(pbank[:], lhsT=ones[:], rhs=sq[:], start=(j == 0), stop=(j == 7))),
                 reads=[sq.res, ones.res], writes=[pbank.res])
        sd = tmp_ring[0]
        s.op("act", (lambda sd=sd: nc.scalar.activation(out=sd[:], in_=pbank[:], func=AF.Sqrt, scale=1.0 / D, bias=epsb[:])),
             reads=[pbank.res, epsb.res], writes=[sd.res])
        s.op("dve", (lambda sd=sd: nc.vector.reciprocal(out=rs[:, sl], in_=sd[:])), reads=[sd.res], writes=[rs.res])
        for j in range(8):
            tmp = tmp_ring[1 + (j % (len(tmp_ring) - 1))]
            s.op("dve", (lambda tmp=tmp, j=j: nc.vector.tensor_tensor(out=tmp[:], in0=src[:, j, sl], in1=rs[:, sl], op=ALU.mult)),
                 reads=[src.res, rs.res], writes=[tmp.res])
            if after_h is None:
                s.op("act", (lambda tmp=tmp, j=j: nc.scalar.activation(out=hT[:, j, sl], in_=tmp[:], func=AF.Identity,
                                                                      scale=Acol[:, j:j + 1], bias=Bcol[:, j:j + 1])),
                     reads=[tmp.res, Acol.res, Bcol.res], writes=[hT.res])
            else:
                after_h(tmp, j, tt, sl)


def emit_AB(c, gn, mod, sh_off, sc_off, Acol, Bcol):
    nc, s = c.nc, c.s
    s.op("dve", lambda: nc.vector.scalar_tensor_tensor(out=Acol[:], in0=mod[:, sc_off:sc_off + 8], scalar=1.0, in1=gn[:],
                                                       op0=ALU.add, op1=ALU.mult),
         reads=[mod.res, gn.res], writes=[Acol.res])
    s.op("dve", lambda: nc.vector.tensor_copy(out=Bcol[:], in_=mod[:, sh_off:sh_off + 8]), reads=[mod.res], writes=[Bcol.res])


ROPE_CH = (0, 1, 2, 4, 5, 6, 7)
NORM_CH = (3, 8, 9, 10, 11)
RAW_CH = tuple(range(12, 24))
MISC_CH = 24


def build_A(c=None):
    own = c is None
    if own:
        c = Ctx()
    ph = c.begin_phase()
    nc, s = c.nc, c.s
    xT_d = c.dram("xT", [128, 8, NT], F32, "ExternalInput")
    mod_d = c.dram("modA", [128, 16], F32, "ExternalInput")
    gn_d = c.dram("gn", [128, 8], F32, "ExternalInput")
    pos_d = c.dram("pos", [1, NT], I32, "ExternalInput")
    invf_d = c.dram("invf", [128, 1], F32, "ExternalInput")
    w_d = c.dram("w", [NCH_IN, 128, 8, 128], F32, "ExternalInput")
    gain_d = c.dram("gain", [128, 12], F32, "ExternalInput")
    osc_d = c.dram("osc", [128, 12], F32, "ExternalInput")
    foxb_d = c.dram("foxb", [128, 1], F32, "ExternalInput")
    pm_d = c.dram("pm", [128, 128], F32, "ExternalInput")
    bones_d = c.dram("bones", [128, 128], F32, "ExternalInput")
    proj_d = c.dram("proj", [26, 128, NT], BF16, "ExternalOutput")
    gates_d = c.dram("gates", [32, 128, NT], F32, "ExternalOutput")
    misc_d = c.dram("misc", [64, NT], F32, "ExternalOutput")

    hT = c.sb([128, 8, NT], BF16)
    rs = c.sb([128, NT], F32)
    COS = c.sb([128, NT], F32)
    SIN = c.sb([128, NT], F32)
    ones = c.sb([128, 128], BF16)
    bones_f = c.sb([128, 128], F32)
    pm_f = c.sb([128, 128], F32)
    bones = c.sb([128, 128], BF16)
    pm = c.sb([128, 128], BF16)
    gain = c.sb([128, 12], F32)
    osc = c.sb([128, 12], F32)
    foxb = c.sb([128, 1], F32)
    epsb = c.sb([128, 1], F32)
    negpi = c.sb([128, 1], F32)
    invf = c.sb([128, 1], F32)
    mod = c.sb([128, 16], F32)
    gn = c.sb([128, 8], F32)
    Acol = c.sb([128, 8], F32)
    Bcol = c.sb([128, 8], F32)
    pbank = c.ps([128, TT])

    s.op("pool", lambda: nc.gpsimd.memset(ones[:], 1.0), writes=[ones.res])
    s.op("pool", lambda: nc.gpsimd.memset(epsb[:], EPS), writes=[epsb.res])
    s.op("pool", lambda: nc.gpsimd.memset(negpi[:], -float(np.pi)), writes=[negpi.res])
    for t, d in ((bones_f, bones_d), (pm_f, pm_d), (gain, gain_d), (osc, osc_d), (foxb, foxb_d), (invf, invf_d), (mod, mod_d), (gn, gn_d)):
        s.dma(t[:], d, writes=[t.res])
    s.op("pool", lambda: nc.gpsimd.tensor_copy(out=bones[:], in_=bones_f[:]), reads=[bones_f.res], writes=[bones.res])
    s.op("pool", lambda: nc.gpsimd.tensor_copy(out=pm[:], in_=pm_f[:]), reads=[pm_f.res], writes=[pm.res])
    s.op("dve", lambda: nc.vector.tensor_tensor(out=gain[:], in0=gain[:], in1=osc[:], op=ALU.mult), reads=[gain.res, osc.res], writes=[gain.res])
    emit_AB(c, gn, mod, 0, 8, Acol, Bcol)

    with contextlib.ExitStack() as es1:
        old_es, c.es = c.es, es1
        xT = c.sb([128, 8, NT], F32)
        for j in range(8):
            s.dma(xT[:, j, :], xT_d[:, j, :], writes=[xT.res])
        posi = c.sb([128, NT], I32)
        ang = c.sb([128, NT], F32)
        tq = c.sb([128, NT], F32)
        ki = c.sb([128, NT], I32)
        kf = c.sb([128, NT], F32)
        s.dma(posi[:], pos_d[0, :].partition_broadcast(128), writes=[posi.res])
        s.op("dve", lambda: nc.vector.tensor_copy(out=tq[:], in_=posi[:]), reads=[posi.res], writes=[tq.res])
        s.op("dve", lambda: nc.vector.tensor_scalar(out=ang[:], in0=tq[:], scalar1=invf[:, 0:1], scalar2=None, op0=ALU.mult),
             reads=[tq.res, invf.res], writes=[ang.res])
        for dst, shift in ((SIN, 0.0), (COS, 0.25)):
            s.op("dve", lambda shift=shift: nc.vector.tensor_scalar(out=tq[:], in0=ang[:], scalar1=1.0 / TWO_PI, scalar2=0.5 + shift,
                                                                    op0=ALU.mult, op1=ALU.add), reads=[ang.res], writes=[tq.res])
            s.op("dve", lambda: nc.vector.tensor_copy(out=ki[:], in_=tq[:]), reads=[tq.res], writes=[ki.res])
            s.op("dve", lambda: nc.vector.tensor_copy(out=kf[:], in_=ki[:]), reads=[ki.res], writes=[kf.res])
            s.op("dve", lambda shift=shift: nc.vector.tensor_scalar(out=tq[:], in0=ang[:], scalar1=float(np.pi) + shift * TWO_PI, scalar2=None,
                                                                    op0=ALU.add), reads=[ang.res], writes=[tq.res])
            s.op("dve", lambda: nc.vector.scalar_tensor_tensor(out=tq[:], in0=kf[:], scalar=-C1_2PI, in1=tq[:], op0=ALU.mult, op1=ALU.add),
                 reads=[kf.res, tq.res], writes=[tq.res])
            s.op("dve", lambda: nc.vector.scalar_tensor_tensor(out=tq[:], in0=kf[:], scalar=-C2_2PI, in1=tq[:], op0=ALU.mult, op1=ALU.add),
                 reads=[kf.res, tq.res], writes=[tq.res])
            s.op("dve", lambda: nc.vector.tensor_scalar(out=kf[:], in0=tq[:], scalar1=0.0, scalar2=TWO_PI, op0=ALU.is_lt, op1=ALU.mult),
                 reads=[tq.res], writes=[kf.res])
            s.op("dve", lambda: nc.vector.tensor_tensor(out=tq[:], in0=tq[:], in1=kf[:], op=ALU.add), reads=[tq.res, kf.res], writes=[tq.res])
            s.op("dve", lambda: nc.vector.tensor_scalar(out=tq[:], in0=tq[:], scalar1=0.0, scalar2=TWO_PI, op0=ALU.max, op1=ALU.min),
                 reads=[tq.res], writes=[tq.res])
            s.op("act", lambda dst=dst: nc.scalar.activation(out=dst[:], in_=tq[:], func=AF.Sin, bias=negpi[:], scale=1.0),
                 reads=[tq.res, negpi.res], writes=[dst.res])
        ring = [c.sb([128, TT], F32) for _ in range(4)]
        sqring = [c.sb([128, TT], BF16) for _ in range(4)]
        emit_modnorm(c, xT, hT, NT, ones, Acol, Bcol, epsb, ring, pbank, rs, sq_ring=sqring)
        barrier(c)
        c.es = old_es

    R = 4
    wst = [c.sb([128, 8, 128], F32) for _ in range(3)]
    wbf = [c.sb([128, 8, 128], BF16) for _ in range(3)]
    pp = [c.ps([128, TT]) for _ in range(3)]
    psq = [c.ps([128, TT]) for _ in range(2)]
    pq = [pbank, c.ps([128, TT])]
    sq_r = [c.sb([128, TT], BF16) for _ in range(R)]
    rstd_r = [c.sb([128, TT], F32) for _ in range(R)]
    y_r = [c.sb([128, TT], F32) for _ in range(R)]
    y2_r = [c.sb([128, TT], BF16) for _ in range(R)]
    t1_r = [c.sb([128, TT], F32) for _ in range(R)]
    ob_r = [c.sb([128, TT], BF16) for _ in range(R)]
    ob2_r = [c.sb([128, TT], BF16) for _ in range(R)]
    of_r = [c.sb([128, TT], F32) for _ in range(R)]

    items = [(ch, tt) for ch in range(A_MAXCH) for tt in range(NT // TT)]

    def kind(ch):
        if ch in ROPE_CH:
            return "rope"
        if ch in NORM_CH:
            return "norm"
        if ch in RAW_CH:
            return "raw"
        if ch == MISC_CH:
            return "misc"
        return "sig"

    def stl(n):
        ch, tt = items[n]
        if tt == 0:
            ws = wst[ch % 3]
            s.dma(ws[:], w_d[ch], writes=[ws.res], q="act")

    def stc(n):
        ch, tt = items[n]
        if tt == 0:
            ws, wb = wst[ch % 3], wbf[ch % 3]
            s.op("pool", lambda: nc.gpsimd.tensor_copy(out=wb[:], in_=ws[:]), reads=[ws.res], writes=[wb.res])

    def st0(n):
        ch, tt = items[n]
        wb = wbf[ch % 3]
        p = pp[n % 3]
        for j in range(8):
            s.op("pe", (lambda j=j: nc.tensor.matmul(p[:], lhsT=wb[:, j, :], rhs=hT[:, j, tt * TT:(tt + 1) * TT],
                                                     start=(j == 0), stop=(j == 7))),
                 reads=[wb.res, hT.res], writes=[p.res])

    def st1(n):
        ch, tt = items[n]
        k = kind(ch)
        p = pp[n % 3]
        sl = slice(tt * TT, (tt + 1) * TT)
        if k in ("rope", "norm"):
            sq = sq_r[n % R]
            s.op("act", lambda: nc.scalar.activation(out=sq[:], in_=p[:], func=AF.Square), reads=[p.res], writes=[sq.res])
            s.op("pe", lambda: nc.tensor.matmul(psq[n % 2][:], lhsT=bones[:], rhs=sq[:], start=True, stop=True),
                 reads=[bones.res, sq.res], writes=[psq[n % 2].res])
        elif k == "raw":
            ob = ob_r[n % R]
            sc = 0.125 if ch in (16, 17) else 1.0
            s.op("act", lambda: nc.scalar.activation(out=ob[:], in_=p[:], func=AF.Copy, scale=sc), reads=[p.res], writes=[ob.res])
            s.dma(proj_d[ch, :, sl], ob[:], reads=[ob.res], is_output=True)
        elif k == "sig":
            of = of_r[n % R]
            s.op("act", lambda: nc.scalar.activation(out=of[:], in_=p[:], func=AF.Sigmoid), reads=[p.res], writes=[of.res])
            s.dma(gates_d[ch - 25, :, sl], of[:], reads=[of.res], is_output=True, q=("sp", "act")[n % 2])
        else:
            of = of_r[n % R]
            s.op("act", lambda: nc.scalar.activation(out=of[0:64, :], in_=p[0:64, :], func=AF.Sigmoid, bias=foxb[0:64, :], scale=1.0),
                 reads=[p.res, foxb.res], writes=[of.res])
            s.op("act", lambda: nc.scalar.activation(out=of[32:64, :], in_=of[32:64, :], func=AF.Ln), reads=[of.res], writes=[of.res])
            s.dma(misc_d[:, sl], of[0:64, :], reads=[of.res], is_output=True)

    def st2(n):
        ch, tt = items[n]
        k = kind(ch)
        if k not in ("rope", "norm"):
            return
        p = pp[n % 3]
        sl = slice(tt * TT, (tt + 1) * TT)
        rstd = rstd_r[n % R]
        y = y_r[n % R]
        s.op("act", lambda: nc.scalar.activation(out=rstd[:], in_=psq[n % 2][:], func=AF.Sqrt, scale=1.0 / 64, bias=epsb[:]),
             reads=[psq[n % 2].res, epsb.res], writes=[rstd.res])
        s.op("dve", lambda: nc.vector.reciprocal(out=rstd[:], in_=rstd[:]), reads=[rstd.res], writes=[rstd.res])
        s.op("dve", lambda: nc.vector.tensor_tensor(out=y[:], in0=p[:], in1=rstd[:], op=ALU.mult), reads=[p.res, rstd.res], writes=[y.res])
        if k == "norm":
            ob = ob_r[n % R]
            s.op("act", lambda: nc.scalar.activation(out=ob[:], in_=y[:], func=AF.Copy, scale=gain[:, ch:ch + 1]),
                 reads=[y.res, gain.res], writes=[ob.res])
            s.dma(proj_d[ch, :, sl], ob[:], reads=[ob.res], is_output=True)
        else:
            y2 = y2_r[n % R]
            s.op("act", lambda: nc.scalar.activation(out=y2[:], in_=y[:], func=AF.Copy, scale=gain[:, ch:ch + 1]),
                 reads=[y.res, gain.res], writes=[y2.res])
            s.op("pe", lambda: nc.tensor.matmul(pq[n % 2][:], lhsT=pm[:], rhs=y2[:], start=True, stop=True),
                 reads=[pm.res, y2.res], writes=[pq[n % 2].res])
            if ch in (0, 1):
                s.dma(proj_d[24 + ch, :, sl], y2[:], reads=[y2.res], is_output=True)

    def st3(n):
        ch, tt = items[n]
        if kind(ch) != "rope":
            return
        sl = slice(tt * TT, (tt + 1) * TT)
        y2 = y2_r[n % R]
        t1 = t1_r[n % R]
        y = y_r[n % R]
        ob = ob_r[n % R]
        s.op("pool", lambda: nc.gpsimd.tensor_tensor(out=t1[:], in0=y2[:], in1=COS[:, sl], op=ALU.mult), reads=[y2.res, COS.res], writes=[t1.res])
        s.op("dve", lambda: nc.vector.tensor_tensor(out=y[:], in0=pq[n % 2][:], in1=SIN[:, sl], op=ALU.mult),
             reads=[pq[n % 2].res, SIN.res], writes=[y.res])
        s.op("dve", lambda: nc.vector.tensor_tensor(out=ob[:], in0=t1[:], in1=y[:], op=ALU.add), reads=[t1.res, y.res], writes=[ob.res])
        s.dma(proj_d[ch, :, sl], ob[:], reads=[ob.res], is_output=True)

    pipeline(len(items), [stl, stc, st0, st1, st2, st3])
    c.end_phase(ph)
    if own:
        c.close()
    return c


def in_perm():
    Z = [-1] * 64
    r = lambda a, n: list(range(a, a + n))
    ch = []
    ch.append(r(0, 128)); ch.append(r(128, 128))
    ch.append(r(384, 64) + r(512, 64))
    ch.append(r(256, 64) + Z)
    ch.append(r(652, 128)); ch.append(r(780, 128))
    ch.append(r(908, 128)); ch.append(r(1036, 128))
    ch.append(r(2188, 128)); ch.append(r(2316, 128))
    ch.append(r(2444, 128)); ch.append(r(2572, 128))
    ch.append(r(320, 64) + r(448, 64))
    ch.append(r(576, 64) + Z)
    ch.append(r(1164, 128)); ch.append(r(1292, 128))
    ch.append(r(1420, 128)); ch.append(r(1548, 128))
    ch.append(r(1676, 128)); ch.append(r(1804, 128))
    ch.append(r(1932, 128)); ch.append(r(2060, 128))
    ch.append(r(2700, 128)); ch.append(r(2828, 128))
    ch.append(r(640, 12) + [-1] * 20 + r(2956, 4) + [-1] * 92)
    for g in range(32):
        ch.append(r(2960 + g * 128, 128))
    return np.array(ch, np.int64)


def consts_A():
    inv = (500000.0 ** (-np.arange(0, 16, 2, dtype=np.float32) / 16)).astype(np.float32)
    invf = np.zeros((128, 1), np.float32)
    pm = np.zeros((128, 128), np.float32)
    bones = np.zeros((128, 128), np.float32)
    for hb in (0, 64):
        bones[hb:hb + 64, hb:hb + 64] = 1.0
        for i in range(8):
            invf[hb + i, 0] = inv[i]
            invf[hb + 8 + i, 0] = inv[i]
            pm[hb + i + 8, hb + i] = -1.0
            pm[hb + i, hb + i + 8] = 1.0
    osc = np.ones((128, 12), np.float32)
    for chn in (0, 1, 4, 5, 8, 9):
        osc[:, chn] = 0.125
    return invf, pm, bones, osc


def maps_A(inp, l, xT_all, mod):
    perm = in_perm()
    w = inp["w_in"][l]
    wz = np.concatenate([w, np.zeros((D, 1), np.float32)], axis=1)
    wp = wz[:, perm.reshape(-1)].reshape(D, NCH_IN, 128)
    wp = np.ascontiguousarray(wp.reshape(8, 128, NCH_IN, 128).transpose(2, 1, 0, 3))
    invf, pm, bones, osc = consts_A()
    g = inp["qk_gain"][l]
    t2 = lambda a: np.concatenate([a, a])
    z64 = np.zeros(64, np.float32)
    gain = np.stack([t2(g[0]), t2(g[0]), np.concatenate([g[2], g[3]]), np.concatenate([g[1], z64]),
                     t2(g[4]), t2(g[4]), t2(g[5]), t2(g[5]), t2(g[6]), t2(g[6]), t2(g[7]), t2(g[7])], axis=1).astype(np.float32)
    foxb = np.zeros((128, 1), np.float32)
    foxb[32:36, 0] = inp["fox_bias"][l]
    gn = np.ascontiguousarray(inp["norm_mix"][l].reshape(8, 128).T)
    maps = []
    for i in range(8):
        b, q = i // 4, i % 4
        m = {"modA": np.ascontiguousarray(mod[l, b][:, 0:16]), "gn": gn,
             "pos": np.ascontiguousarray(inp["positions"][b:b + 1, q * NT:(q + 1) * NT]).astype(np.int32),
             "invf": invf, "w": wp, "gain": gain, "osc": osc, "foxb": foxb, "pm": pm, "bones": bones}
        if xT_all is not None:
            xs = xT_all[b][:, q * NT:(q + 1) * NT].reshape(8, 128, NT).transpose(1, 0, 2)
            m["xT"] = np.ascontiguousarray(xs)
        maps.append(m)
    return maps


def collect_A(results, pre=""):
    proj = [np.concatenate([results[b * 4 + q][pre + "proj"] for q in range(4)], axis=2) for b in range(2)]
    gates = [np.concatenate([results[b * 4 + q][pre + "gates"] for q in range(4)], axis=2) for b in range(2)]
    misc = [np.concatenate([results[b * 4 + q][pre + "misc"] for q in range(4)], axis=1) for b in range(2)]
    return proj, gates, misc


def run_A(cA, inp, l, xT_all, mod):
    maps = maps_A(inp, l, xT_all, mod)
    res = run_bass_kernel_spmd(cA.nc, maps, core_ids=list(range(8)))
    return collect_A(res.results)


B_MIXERS = ("dil", "fox", "sb", "nsa")
NKB = S // 128
NQT = S // TT
M_CAUSAL, M_STRICT, M_WIN, M_DIL, M_CMP, M_NEGC = 0, 4, 8, 16, 36, 41
N_MASKS = 45


def consts_B():
    import ml_dtypes
    k = np.arange(128)[:, None]
    cc = np.arange(512)[None, :]
    masks = np.zeros((N_MASKS, 128, 512), np.float32)
    for i in range(4):
        masks[M_CAUSAL + i] = (cc - k >= 128 * i)
        masks[M_STRICT + i] = (cc - k > 128 * i)
    for w in range(8):
        diff = cc - k + 512 - 128 * w
        masks[M_WIN + w] = (diff >= 0) & (diff < 512)
    for w in range(20):
        diff = cc - k + 2048 - 128 * w
        m = np.zeros((128, 512), np.float32)
        for (ww, d) in ((128, 1), (512, 4), (2048, 16)):
            m += ((diff % d == 0) & (diff >= 0) & (diff <= ww))
        masks[M_DIL + w] = m
    for u in range(5):
        masks[M_CMP + u] = (16 * k + 31 <= 512 * u + cc)
    for i in range(4):
        masks[M_NEGC + i] = -30000.0 * (cc - k < 128 * i)
    G = (np.arange(S)[None, :] // 64 == np.arange(128)[:, None]).astype(np.float32)
    c0 = np.arange(511) * 16
    s0 = np.arange(128) * 64
    ov = ((c0[:, None] < s0[None, :] + 64) & (c0[:, None] + 32 > s0[None, :])).astype(np.float32)
    ov = np.concatenate([ov, np.zeros((1, 128), np.float32)], 0).reshape(4, 128, 128).transpose(1, 0, 2)
    onesc = np.ones((128, 4, 1), np.float32)
    onesc[127, 3, 0] = 0.0
    Rconst = np.concatenate([ov, onesc], axis=2)
    add = np.zeros((128, 254), np.float32)
    jj = np.arange(254)[None, :] - 126
    cr = (np.arange(128) // 64)[:, None]
    add[(jj == cr) | (jj == cr - 1)] = 1e30
    add[jj > cr] = -1e30
    jn = np.arange(128)[:, None]
    kn = np.arange(128)[None, :]
    nti = -(jn >= kn).astype(np.float32)
    ntc = -(jn < kn).astype(np.float32)
    bf = ml_dtypes.bfloat16
    return dict(masks=masks.astype(bf), G=G.astype(bf), Rconst=Rconst.astype(bf), add=add,
                nti=nti.astype(bf), ntc=ntc.astype(bf), ident=np.eye(128, dtype=np.float32),
                tri64=(np.arange(64)[:, None] < np.arange(64)[None, :]).astype(np.float32))


def pipeline(n_items, stages):
    K = len(stages)
    for i in range(n_items + K - 1):
        for k, st in enumerate(stages):
            n = i - k
            if 0 <= n < n_items:
                st(n)


def build_B():
    c = Ctx()
    nc, s = c.nc, c.s
    di = lambda name, shape, dt: c.dram(name, shape, dt, "ExternalInput")
    qnr_d = di("qnr", [4, 64, S], BF16)
    qr_d = di("qr", [64, S], BF16)
    kcT_d = di("kcT", [64, S], BF16)
    vcT_d = di("vcT", [64, S], BF16)
    kslT_d = di("kslT", [64, S], BF16)
    kwT_d = di("kwT", [64, S], BF16)
    vsl_d = di("vsl", [128, NKB, 65], BF16)
    vw_d = di("vw", [128, NKB, 65], BF16)
    ag_d = di("ag", [128, NKB, 3], F32)
    dq_d = di("dq", [64, S], BF16)
    dk_d = di("dk", [64, S], BF16)
    dv_d = di("dv", [128, NKB, 65], BF16)
    sq_d = di("sq", [64, S], BF16)
    sk_d = di("sk", [64, S], BF16)
    sv_d = di("sv", [128, NKB, 65], BF16)
    fq_d = di("fq", [64, S], BF16)
    fk_d = di("fk", [64, S], BF16)
    fv_d = di("fv", [128, NKB, 65], BF16)
    lf_d = di("logf", [1, S], F32)
    w1k_d = di("w1k", [64, 32, 128], F32)
    w1v_d = di("w1v", [64, 32, 128], F32)
    w2k_d = di("w2k", [128, 64], F32)
    w2v_d = di("w2v", [128, 64], F32)
    pek_d = di("pek", [64, 32], F32)
    pev_d = di("pev", [64, 32], F32)
    masks_d = di("masks", [N_MASKS, 128, 512], BF16)
    G_d = di("G", [128, S], BF16)
    Rc_d = di("Rconst", [128, 4, 129], BF16)
    add_d = di("add", [128, 254], F32)
    nti_d = di("nti", [128, 128], BF16)
    ntc_d = di("ntc", [128, 128], BF16)
    ident_d = di("ident", [128, 128], F32)
    tri_d = di("tri64", [64, 64], F32)
    o_d = c.dram("o", [4, 128, NKB, 64], BF16, "ExternalOutput")

    ps_ring = [c.ps([128, 512]) for _ in range(4)]
    po_ring = [c.ps([128, 512]) for _ in range(2)]
    pX = c.ps([128, 512])
    pY = c.ps([128, 512])
    e_ring = [c.sb([128, 512], BF16) for _ in range(4)]
    p_ring = [c.sb([128, 512], BF16) for _ in range(4)]
    pre_ring = [c.sb([128, 512], F32) for _ in range(4)]
    rz_ring = [c.sb([128, 4], F32) for _ in range(2)]
    fac_ring = [c.sb([128, 4], F32) for _ in range(2)]
    ost = c.sb([128, NKB, 64], BF16)

    class Scope:
        def __enter__(self):
            self.es = contextlib.ExitStack()
            self.old = c.es
            c.es = self.es
            return self

        def __exit__(self, *a):
            barrier(c)
            c.es = self.old
            self.es.close()

    def load_masks(lo, n, order=None):
        t = c.sb([128, n, 512], BF16)
        t.parts = [Res() for _ in range(n)]
        for ii, i in enumerate(order if order is not None else range(n)):
            s.dma(t[:, i, :], masks_d[lo + i], writes=[t.parts[i]], q=("sp", "act")[ii % 2])
        return t

    def split_load(QT, KT, V, qT_d, kT_d, v_d):
        qs = ("sp", "act")
        for t in (QT, KT, V):
            if t is not None:
                t.parts = [Res() for _ in range(4)]
        for h in range(4):
            sl = slice(h * 2048, (h + 1) * 2048)
            if QT is not None:
                s.dma(QT[0:64, sl], qT_d[:, sl], reads=[QT.res], writes=[QT.parts[h]], q=qs[h % 2])
            s.dma(KT[0:64, sl], kT_d[:, sl], reads=[KT.res], writes=[KT.parts[h]], q=qs[(h + 1) % 2])
            s.dma(V[:, 16 * h:16 * (h + 1), :], v_d[:, 16 * h:16 * (h + 1), :], reads=[V.res], writes=[V.parts[h]], q=qs[h % 2])

    def rQ(QT, qt):
        return [QT.res, QT.parts[qt // 4]]

    def rK(KT, kb):
        return [KT.res, KT.parts[kb // 16]]

    def attn(items, qk, pv, fin, mask_of, alt=[0], nps=4):
        def st0(n):
            qk(n, ps_ring[n % nps])

        def st1(n):
            it = items[n]
            ps, e = ps_ring[n % nps], e_ring[n % 4]
            if it["mi"] is not None and it["mi"] >= M_NEGC:
                mt, mi = mask_of(it["mi"])
                pre = pre_ring[n % 4]
                s.op("dve", lambda: nc.vector.tensor_tensor(out=pre[:], in0=ps[:], in1=mt[:, mi, :], op=ALU.add),
                     reads=[ps.res, mt.parts[mi]], writes=[pre.res])
                p = p_ring[n % 4]
                s.op("act", lambda: nc.scalar.activation(out=p[:], in_=pre[:], func=AF.Exp), reads=[pre.res], writes=[p.res])
                return
            s.op("act", lambda: nc.scalar.activation(out=e[:], in_=ps[:], func=AF.Exp), reads=[ps.res], writes=[e.res])
            if it["mi"] is not None:
                p = p_ring[n % 4]
                mt, mi = mask_of(it["mi"])
                alt[0] = 1
                if alt[0]:
                    s.op("dve", lambda: nc.vector.tensor_tensor(out=p[:], in0=e[:], in1=mt[:, mi, :], op=ALU.mult),
                         reads=[e.res, mt.parts[mi]], writes=[p.res])
                else:
                    s.op("pool", lambda: nc.gpsimd.tensor_tensor(out=p[:], in0=e[:], in1=mt[:, mi, :], op=ALU.mult),
                         reads=[e.res, mt.parts[mi]], writes=[p.res])

        def st2(n):
            it = items[n]
            pt = p_ring[n % 4] if it["mi"] is not None else e_ring[n % 4]
            pv(n, pt)
            if it["last"]:
                fin(n)
        pipeline(len(items), [st0, st1, (lambda n: None), st2])

    def std_pv(items, V, ncol=65):
        def pv(n, pt):
            it = items[n]
            po = po_ring[it["qt"] % 2]
            for qb in range(4):
                s.op("pe", (lambda qb=qb: nc.tensor.matmul(po[:, qb * 65:qb * 65 + ncol], lhsT=pt[:, qb * 128:(qb + 1) * 128],
                                                           rhs=V[:, it["kb"], 0:ncol], start=(it["first"] and qb == 0), stop=it["last"],
                                                           skip_group_check=True)),
                     reads=[pt.res, V.res, V.parts[it["kb"] // 16]], writes=[po.res])
        return pv

    def rz_of(qt, po, zoff=64, stride=65):
        rz = rz_ring[qt % 2]
        for qb in range(4):
            s.op("dve", (lambda qb=qb: nc.vector.tensor_scalar(out=rz[:, qb:qb + 1], in0=po[:, qb * stride + zoff:qb * stride + zoff + 1],
                                                               scalar1=1e-30, scalar2=None, op0=ALU.max)),
                 reads=[po.res], writes=[rz.res])
        s.op("dve", lambda: nc.vector.reciprocal(out=rz[:], in_=rz[:]), reads=[rz.res], writes=[rz.res])
        return rz

    def std_items(blocks_of):
        items = []
        for qt in range(NQT):
            bl = blocks_of(qt)
            for ii, (kb, mi) in enumerate(bl):
                items.append(dict(qt=qt, kb=kb, mi=mi, first=(ii == 0), last=(ii == len(bl) - 1)))
        return items

    def simple_mixer(m, qT_d, kT_d, v_d, blocks_of, mask_lo, mask_n, kdim=64, prep=None, mask_order=None):
        with Scope():
            QT = c.sb([128, S], BF16)
            KT = c.sb([128, S], BF16)
            V = c.sb([128, NKB, 65], BF16)
            if prep is not None:
                prep(QT, KT)
            split_load(QT, KT, V, qT_d, kT_d, v_d)
            mt = load_masks(mask_lo, mask_n, order=mask_order)
            items = std_items(blocks_of)

            def qk(n, ps):
                it = items[n]
                s.op("pe", lambda: nc.tensor.matmul(ps[:], lhsT=KT[0:kdim, it["kb"] * 128:(it["kb"] + 1) * 128],
                                                    rhs=QT[0:kdim, it["qt"] * 512:(it["qt"] + 1) * 512], start=True, stop=True),
                     reads=rK(KT, it["kb"]) + rQ(QT, it["qt"]), writes=[ps.res])

            def fin(n):
                qt = items[n]["qt"]
                po = po_ring[qt % 2]
                rz = rz_of(qt, po)
                for qb in range(4):
                    s.op("dve", (lambda qb=qb: nc.vector.tensor_scalar(out=ost[:, 4 * qt + qb, :], in0=po[:, qb * 65:qb * 65 + 64],
                                                                       scalar1=rz[:, qb:qb + 1], scalar2=None, op0=ALU.mult)),
                         reads=[po.res, rz.res], writes=[ost.res])
            attn(items, qk, std_pv(items, V), fin, lambda mi: (mt, mi - mask_lo))
            s.dma(o_d[m], ost[:], reads=[ost.res], is_output=True)

    def dil_blocks(qt):
        return [(4 * qt - 16 + w, M_DIL + w) for w in range(20) if 4 * qt - 16 + w >= 0]
    if "dil" in B_MIXERS:
        simple_mixer(1, dq_d, dk_d, dv_d, dil_blocks, M_DIL, 20, mask_order=[16, 17, 18, 19, 12, 13, 14, 15, 8, 9, 10, 11, 4, 5, 6, 7, 0, 1, 2, 3])

    fs_d = c.dram("fsplit", [6, S], BF16, "Internal")
    fs_res = Res()

    def fox_prep(QT, KT):
        lf = c.sb([64, 128], F32)
        F = c.sb([64, 128], F32)
        zr = c.sb([64, 128], F32)
        r1 = c.sb([64, 128], F32)
        off = c.sb([64, 1], F32)
        U = c.sb([64, 64], F32)
        sp3 = [c.sb([64, 128], BF16) for _ in range(3)]
        ng3 = [c.sb([64, 128], BF16) for _ in range(3)]
        s.dma(lf[:], lf_d.rearrange("o (p j) -> (o p) j", j=128), writes=[lf.res])
        s.dma(U[:], tri_d, writes=[U.res])
        s.op("pool", lambda: nc.gpsimd.memset(zr[:], 0.0), writes=[zr.res])
        s.op("pool", lambda: nc.gpsimd.memset(QT[:], 0.0), writes=[QT.res])
        s.op("pool", lambda: nc.gpsimd.memset(KT[:], 0.0), writes=[KT.res])
        s.op("pool", lambda: nc.gpsimd.memset(QT[96:99, :], 1.0), writes=[QT.res])
        s.op("pool", lambda: nc.gpsimd.memset(KT[64:67, :], 1.0), writes=[KT.res])
        s.op("dve", lambda: nc.vector.tensor_tensor_scan(out=F[:], data0=lf[:], data1=zr[:], initial=0.0, op0=ALU.add, op1=ALU.add),
             reads=[lf.res, zr.res], writes=[F.res])
        s.op("pe", lambda: nc.tensor.matmul(pY[0:64, 0:1], lhsT=U[:], rhs=F[:, 127:128], start=True, stop=True),
             reads=[U.res, F.res], writes=[pY.res])
        s.op("act", lambda: nc.scalar.copy(out=off[:], in_=pY[0:64, 0:1]), reads=[pY.res], writes=[off.res])
        s.op("dve", lambda: nc.vector.tensor_scalar(out=F[:], in0=F[:], scalar1=off[:, 0:1], scalar2=None, op0=ALU.add),
             reads=[F.res, off.res], writes=[F.res])
        cur = F
        for i in range(3):
            s.op("dve", (lambda i=i, cur=cur: nc.vector.tensor_copy(out=sp3[i][:], in_=cur[:])), reads=[cur.res], writes=[sp3[i].res])
            s.op("dve", (lambda i=i: nc.vector.tensor_scalar(out=ng3[i][:], in0=sp3[i][:], scalar1=-1.0, scalar2=None, op0=ALU.mult)),
                 reads=[sp3[i].res], writes=[ng3[i].res])
            if i < 2:
                s.op("dve", (lambda i=i, cur=cur: nc.vector.tensor_tensor(out=r1[:], in0=cur[:], in1=sp3[i][:], op=ALU.subtract)),
                     reads=[cur.res, sp3[i].res], writes=[r1.res])
                cur = r1
            s.dma(fs_d[i, :].rearrange("(p j) -> p j", j=128), sp3[i][:], reads=[sp3[i].res], writes=[fs_res])
            s.dma(fs_d[3 + i, :].rearrange("(p j) -> p j", j=128), ng3[i][:], reads=[ng3[i].res], writes=[fs_res])
        s.dma(QT[64:67, :], fs_d[0:3, :], reads=[fs_res], writes=[QT.res])
        s.dma(KT[96:99, :], fs_d[3:6, :], reads=[fs_res], writes=[KT.res])

    def causal_blocks(qt):
        return [(kb, None) for kb in range(4 * qt)] + [(4 * qt + i, M_CAUSAL + i) for i in range(4)]

    def negc_blocks(qt):
        return [(kb, None) for kb in range(4 * qt)] + [(4 * qt + i, M_NEGC + i) for i in range(4)]
    if "fox" in B_MIXERS:
        simple_mixer(3, fq_d, fk_d, fv_d, negc_blocks, M_NEGC, 4, kdim=99, prep=fox_prep)

    with (Scope() if "sb" in B_MIXERS else contextlib.nullcontext()):
      if "sb" in B_MIXERS:
        QT = c.sb([64, S], BF16)
        KT = c.sb([64, S], BF16)
        V = c.sb([128, NKB, 65], BF16)
        nti = c.sb([128, 128], BF16)
        ntc = c.sb([128, 128], BF16)
        split_load(QT, KT, V, sq_d, sk_d, sv_d)
        s.dma(nti[:], nti_d, writes=[nti.res])
        s.dma(ntc[:], ntc_d, writes=[ntc.res])
        mt = load_masks(M_STRICT, 4)
        E_r = [c.sb([128, 512], F32) for _ in range(4)]
        L_r = [c.sb([128, 512], BF16) for _ in range(4)]
        X_r = [c.sb([128, 512], F32) for _ in range(4)]
        A_r = [c.sb([128, 512], BF16) for _ in range(4)]
        def sb_blocks(qt):
            bl = [(4 * qt + i, M_STRICT + i) for i in (3, 2, 1, 0)] + [(kb, None) for kb in range(4 * qt - 1, -1, -1)]
            return [dict(qt=qt, kb=kb, mi=mi, first=(ii == 0), last=(ii == len(bl) - 1)) for ii, (kb, mi) in enumerate(bl)]
        items = []
        for pr in range(NQT // 2):
            la, lb = sb_blocks(2 * pr), sb_blocks(2 * pr + 1)
            for ii in range(len(lb)):
                if ii < len(la):
                    items.append(la[ii])
                items.append(lb[ii])
        pXs = (pX, pY)

        def sb0(n):
            it = items[n]
            ps = ps_ring[n % 4]
            s.op("pe", lambda: nc.tensor.matmul(ps[:], lhsT=KT[:, it["kb"] * 128:(it["kb"] + 1) * 128],
                                                rhs=QT[:, it["qt"] * 512:(it["qt"] + 1) * 512], start=True, stop=True),
                 reads=rK(KT, it["kb"]) + rQ(QT, it["qt"]), writes=[ps.res])

        def sb1(n):
            it = items[n]
            ps, E, L = ps_ring[n % 4], E_r[n % 4], L_r[n % 4]
            s.op("act", lambda: nc.scalar.activation(out=E[:], in_=ps[:], func=AF.Exp), reads=[ps.res], writes=[E.res])
            s.op("act", lambda: nc.scalar.activation(out=L[:], in_=E[:], func=AF.Ln, bias=1.0, scale=1.0), reads=[E.res], writes=[L.res])
            if it["mi"] is not None:
                mi = it["mi"] - M_STRICT
                s.op("pool", lambda: nc.gpsimd.tensor_tensor(out=L[:], in0=L[:], in1=mt[:, mi, :], op=ALU.mult),
                     reads=[L.res, mt.parts[mi]], writes=[L.res])
                s.op("dve", lambda: nc.vector.tensor_tensor(out=E[:], in0=E[:], in1=mt[:, mi, :], op=ALU.mult),
                     reads=[E.res, mt.parts[mi]], writes=[E.res])

        def sb2(n):
            it = items[n]
            L, X = L_r[n % 4], X_r[n % 4]
            pXq = pXs[it["qt"] % 2]
            s.op("pe", lambda: nc.tensor.matmul(pXq[:], lhsT=nti[:], rhs=L[:], start=it["first"], stop=False, skip_group_check=True),
                 reads=[nti.res, L.res], writes=[pXq.res])
            s.op("act", lambda: nc.scalar.activation(out=X[:], in_=pXq[:], func=AF.Exp), reads=[pXq.res], writes=[X.res])

        def sb3(n):
            it = items[n]
            L, X, E, A = L_r[n % 4], X_r[n % 4], E_r[n % 4], A_r[n % 4]
            pXq = pXs[it["qt"] % 2]
            s.op("pe", lambda: nc.tensor.matmul(pXq[:], lhsT=ntc[:], rhs=L[:], start=False, stop=it["last"], skip_group_check=True),
                 reads=[ntc.res, L.res], writes=[pXq.res])
            s.op("dve", lambda: nc.vector.tensor_tensor(out=A[:], in0=E[:], in1=X[:], op=ALU.mult), reads=[E.res, X.res], writes=[A.res])

        def sb4(n):
            it = items[n]
            A = A_r[n % 4]
            qt = it["qt"]
            po = po_ring[qt % 2]
            for qb in range(4):
                s.op("pe", (lambda qb=qb: nc.tensor.matmul(po[:, qb * 65:qb * 65 + 64], lhsT=A[:, qb * 128:(qb + 1) * 128],
                                                           rhs=V[:, it["kb"], 0:64], start=(it["first"] and qb == 0), stop=it["last"],
                                                           skip_group_check=True)),
                     reads=[A.res, V.res, V.parts[it["kb"] // 16]], writes=[po.res])
            if it["last"]:
                for qb in range(4):
                    s.op("act", (lambda qb=qb: nc.scalar.copy(out=ost[:, 4 * qt + qb, :], in_=po[:, qb * 65:qb * 65 + 64])),
                         reads=[po.res], writes=[ost.res])
        K_ = len(items)
        for i in range(K_ + 4):
            if i < K_:
                sb0(i)
            if 0 <= i - 1 < K_:
                sb1(i - 1)
            if 0 <= i - 3 < K_:
                sb3(i - 3)
            if 0 <= i - 2 < K_:
                sb2(i - 2)
            if 0 <= i - 4 < K_:
                sb4(i - 4)
        s.dma(o_d[2], ost[:], reads=[ost.res], is_output=True)

    hsel_d = di("hsel", [128, 4], F32)
    with (Scope() if "nsa" in B_MIXERS else contextlib.nullcontext()):
      if "nsa" in B_MIXERS:
        G = c.sb([128, S], BF16)
        selbT = c.sb([128, S], BF16)
        oa = c.sb([128, NKB, 64], F32)
        ag = c.sb([128, NKB, 3], F32)
        ident = c.sb([128, 128], F32)
        addt = c.sb([128, 254], F32)
        hsel = c.sb([128, 4], F32)
        for h in range(4):
            sl = slice(h * 2048, (h + 1) * 2048)
            s.dma(G[:, sl], G_d[:, sl], writes=[G.res])
        for t, d in ((ag, ag_d), (ident, ident_d), (addt, add_d), (hsel, hsel_d)):
            s.dma(t[:], d, writes=[t.res])
        with Scope():
            kcT = c.sb([64, S], BF16)
            vcT = c.sb([64, S], BF16)
            for h in range(4):
                sl = slice(h * 2048, (h + 1) * 2048)
                s.dma(kcT[:, sl], kcT_d[:, sl], writes=[kcT.res])
                s.dma(vcT[:, sl], vcT_d[:, sl], writes=[vcT.res])
            kccT = c.sb([64, 512], BF16)
            Rt = c.sb([128, 4, 193], BF16)
            s.dma(Rt[:, :, 0:129], Rc_d, writes=[Rt.res])
            w1f = c.sb([64, 32, 128], F32)
            w1b = c.sb([64, 32, 128], BF16)
            w2f = c.sb([128, 64], F32)
            w2b = c.sb([128, 64], BF16)
            pef = c.sb([64, 32], F32)
            peb = c.sb([128, 1], F32)
            xg = c.sb([128, 512], F32)
            x2 = c.sb([128, 512], F32)
            gT = c.sb([128, 512], BF16)
            for which in ("k", "v"):
                src = kcT if which == "k" else vcT
                s.dma(w1f[:], w1k_d if which == "k" else w1v_d, writes=[w1f.res])
                s.dma(w2f[:], w2k_d if which == "k" else w2v_d, writes=[w2f.res])
                s.dma(pef[:], pek_d if which == "k" else pev_d, writes=[pef.res])
                s.op("pool", lambda: nc.gpsimd.tensor_copy(out=w1b[:], in_=w1f[:]), reads=[w1f.res], writes=[w1b.res])
                s.op("pool", lambda: nc.gpsimd.tensor_copy(out=w2b[:], in_=w2f[:]), reads=[w2f.res], writes=[w2b.res])
                for l in range(32):
                    s.op("pe", (lambda l=l: nc.tensor.matmul(pY[:, 0:1], lhsT=w1f[:, l, :], rhs=pef[:, l:l + 1], start=(l == 0), stop=(l == 31))),
                         reads=[w1f.res, pef.res], writes=[pY.res])
                s.op("act", lambda: nc.scalar.copy(out=peb[:], in_=pY[:, 0:1]), reads=[pY.res], writes=[peb.res])
                srcv = src[:].rearrange("p (c s) -> p c s", s=16)
                for l in range(32):
                    rhs = srcv[:, 0:511, l] if l < 16 else srcv[:, 1:512, l - 16]
                    s.op("pe", (lambda l=l, rhs=rhs: nc.tensor.matmul(pX[:, 0:511], lhsT=w1b[:, l, :], rhs=rhs, start=(l == 0), stop=(l == 31))),
                         reads=[w1b.res, src.res], writes=[pX.res])
                s.op("act", lambda: nc.scalar.activation(out=xg[:, 0:511], in_=pX[:, 0:511], func=AF.Identity, bias=peb[:], scale=1.0),
                     reads=[pX.res, peb.res], writes=[xg.res])
                s.op("dve", lambda: nc.vector.tensor_tensor(out=x2[:, 0:511], in0=xg[:, 0:511], in1=xg[:, 0:511], op=ALU.mult), reads=[xg.res], writes=[x2.res])
                s.op("dve", lambda: nc.vector.tensor_scalar(out=x2[:, 0:511], in0=x2[:, 0:511], scalar1=0.044715, scalar2=1.0, op0=ALU.mult, op1=ALU.add),
                     reads=[x2.res], writes=[x2.res])
                s.op("dve", lambda: nc.vector.tensor_tensor(out=x2[:, 0:511], in0=x2[:, 0:511], in1=xg[:, 0:511], op=ALU.mult), reads=[x2.res, xg.res], writes=[x2.res])
                s.op("act", lambda: nc.scalar.activation(out=x2[:, 0:511], in_=x2[:, 0:511], func=AF.Sigmoid, scale=1.5957691216057308),
                     reads=[x2.res], writes=[x2.res])
                s.op("pool", lambda: nc.gpsimd.memset(gT[:], 0.0), writes=[gT.res])
                s.op("dve", lambda: nc.vector.tensor_tensor(out=gT[:, 0:511], in0=xg[:, 0:511], in1=x2[:, 0:511], op=ALU.mult), reads=[xg.res, x2.res, gT.res], writes=[gT.res])
                if which == "k":
                    s.op("pe", lambda: nc.tensor.matmul(pY[0:64, :], lhsT=w2b[:], rhs=gT[:], start=True, stop=True), reads=[w2b.res, gT.res], writes=[pY.res])
                    s.op("act", lambda: nc.scalar.copy(out=kccT[:], in_=pY[0:64, :]), reads=[pY.res], writes=[kccT.res])
                else:
                    for cc in range(4):
                        s.op("pe", (lambda cc=cc: nc.tensor.matmul(pY[:, cc * 64:(cc + 1) * 64], lhsT=gT[:, cc * 128:(cc + 1) * 128], rhs=w2b[:],
                                                                   start=True, stop=True)),
                             reads=[w2b.res, gT.res], writes=[pY.res])
                    s.op("act", lambda: nc.scalar.copy(out=Rt[:, :, 129:193], in_=pY[:, 0:256].rearrange("p (a b) -> p a b", b=64)),
                         reads=[pY.res], writes=[Rt.res])
            mt = load_masks(M_CMP, 5)
            qring = [c.sb([64, 512], BF16) for _ in range(4)]
            imp = [c.sb([128, 4, 128], F32) for _ in range(2)]
            sc_r = [c.sb([128, 128], F32) for _ in range(4)]
            sc2_r = [c.sb([128, 128], F32) for _ in range(4)]
            sb_r = [c.sb([128, 128], F32) for _ in range(4)]
            m8_r = [c.sb([128, 16], F32) for _ in range(4)]
            pT = ps_ring[3]
            usets = [(po_ring[0], po_ring[1]), (pX, pY)]
            items = []
            for qt in range(NQT):
                for h in range(4):
                    ccs = [cc for cc in range(4) if qt - 4 * cc >= 0]
                    for ii, cc in enumerate(ccs):
                        u = qt - 4 * cc
                        items.append(dict(qt=qt, h=h, kb=cc, mi=(M_CMP + u if u <= 4 else None), first=(ii == 0), last=(ii == len(ccs) - 1)))

            def qk_c(n, ps):
                it = items[n]
                qtile = qring[(it["qt"] * 4 + it["h"]) % 4]
                if it["first"]:
                    s.dma(qtile[:], qnr_d[it["h"], :, it["qt"] * 512:(it["qt"] + 1) * 512], writes=[qtile.res])
                s.op("pe", lambda: nc.tensor.matmul(ps[:], lhsT=kccT[:, it["kb"] * 128:(it["kb"] + 1) * 128], rhs=qtile[:], start=True, stop=True),
                     reads=[kccT.res, qtile.res], writes=[ps.res])

            def pv_c(n, pt):
                it = items[n]
                us = usets[(it["qt"] * 4 + it["h"]) % 2]
                for qb in range(4):
                    tl = us[qb // 2]
                    off = (qb % 2) * 193
                    s.op("pe", (lambda qb=qb, tl=tl, off=off: nc.tensor.matmul(tl[:, off:off + 193], lhsT=pt[:, qb * 128:(qb + 1) * 128], rhs=Rt[:, it["kb"], :],
                                                                             start=(it["first"] and qb % 2 == 0), stop=it["last"], skip_group_check=True)),
                         reads=[pt.res, Rt.res], writes=[tl.res])

            def fin_c(n):
                it = items[n]
                qt, h = it["qt"], it["h"]
                us = usets[(qt * 4 + h) % 2]
                rz = rz_ring[h % 2]
                fac = fac_ring[h % 2]
                imp_t = imp[qt % 2]
                for qb in range(4):
                    tl, off = us[qb // 2], (qb % 2) * 193
                    s.op("dve", (lambda qb=qb, tl=tl, off=off: nc.vector.tensor_scalar(out=rz[:, qb:qb + 1], in0=tl[:, off + 128:off + 129], scalar1=1e-30,
                                                                                     scalar2=None, op0=ALU.max)), reads=[tl.res], writes=[rz.res])
                s.op("dve", lambda: nc.vector.reciprocal(out=rz[:], in_=rz[:]), reads=[rz.res], writes=[rz.res])
                s.op("dve", lambda: nc.vector.tensor_tensor(out=fac[:], in0=rz[:], in1=ag[:, 4 * qt:4 * qt + 4, 0], op=ALU.mult), reads=[rz.res, ag.res], writes=[fac.res])
                s.op("dve", lambda: nc.vector.tensor_scalar(out=fac[:], in0=fac[:], scalar1=hsel[:, h:h + 1], scalar2=None, op0=ALU.mult),
                     reads=[fac.res, hsel.res], writes=[fac.res])
                for qb in range(4):
                    tl, off = us[qb // 2], (qb % 2) * 193
                    tb = 4 * qt + qb
                    if h == 0:
                        s.op("dve", (lambda qb=qb, tl=tl, off=off: nc.vector.tensor_scalar(out=imp_t[:, qb, :], in0=tl[:, off:off + 128], scalar1=rz[:, qb:qb + 1],
                                                                                         scalar2=None, op0=ALU.mult)), reads=[tl.res, rz.res], writes=[imp_t.res])
                        s.op("dve", (lambda qb=qb, tl=tl, off=off, tb=tb: nc.vector.tensor_scalar(out=oa[:, tb, :], in0=tl[:, off + 129:off + 193], scalar1=fac[:, qb:qb + 1],
                                                                                                scalar2=None, op0=ALU.mult)), reads=[tl.res, fac.res], writes=[oa.res])
                    else:
                        s.op("dve", (lambda qb=qb, tl=tl, off=off: nc.vector.scalar_tensor_tensor(out=imp_t[:, qb, :], in0=tl[:, off:off + 128], scalar=rz[:, qb:qb + 1],
                                                                                                in1=imp_t[:, qb, :], op0=ALU.mult, op1=ALU.add)),
                             reads=[tl.res, rz.res, imp_t.res], writes=[imp_t.res])
                        s.op("dve", (lambda qb=qb, tl=tl, off=off, tb=tb: nc.vector.scalar_tensor_tensor(out=oa[:, tb, :], in0=tl[:, off + 129:off + 193], scalar=fac[:, qb:qb + 1],
                                                                                                       in1=oa[:, tb, :], op0=ALU.mult, op1=ALU.add)),
                             reads=[tl.res, fac.res, oa.res], writes=[oa.res])
                if h != 3:
                    return
                for qb in range(4):
                    tb = 4 * qt + qb
                    sc, sc2, sbt, m8 = sc_r[qb], sc2_r[qb], sb_r[qb], m8_r[qb]
                    s.op("dve", (lambda qb=qb, sc=sc, tb=tb: nc.vector.tensor_tensor(out=sc[:], in0=imp_t[:, qb, :], in1=addt[:, 126 - 2 * tb:254 - 2 * tb], op=ALU.add)),
                         reads=[imp_t.res, addt.res], writes=[sc.res])
                    s.op("dve", (lambda sc=sc: nc.vector.memset(sc[:, 0:1], 1e30)), reads=[sc.res], writes=[sc.res])
                    s.op("dve", (lambda sc=sc, m8=m8: nc.vector.max(out=m8[:, 0:8], in_=sc[:])), reads=[sc.res], writes=[m8.res])
                    s.op("dve", (lambda sc=sc, sc2=sc2, m8=m8: nc.vector.match_replace(out=sc2[:], in_to_replace=m8[:, 0:8], in_values=sc[:], imm_value=-3e38)),
                         reads=[sc.res, m8.res], writes=[sc2.res])
                    s.op("dve", (lambda sc2=sc2, m8=m8: nc.vector.max(out=m8[:, 8:16], in_=sc2[:])), reads=[sc2.res, m8.res], writes=[m8.res])
                    s.op("dve", (lambda sc=sc, sbt=sbt, m8=m8: nc.vector.tensor_scalar(out=sbt[:], in0=sc[:], scalar1=m8[:, 15:16], scalar2=-30000.0,
                                                                                   op0=ALU.is_lt, op1=ALU.mult)), reads=[sc.res, m8.res], writes=[sbt.res])
                    s.op("pe", (lambda qb=qb, sbt=sbt: nc.tensor.transpose(pT[:, qb * 128:(qb + 1) * 128], sbt[:], ident[:])),
                         reads=[sbt.res, ident.res], writes=[pT.res])
                s.op("act", lambda: nc.scalar.copy(out=selbT[:, qt * 512:(qt + 1) * 512], in_=pT[:]), reads=[pT.res], writes=[selbT.res])
            attn(items, qk_c, pv_c, fin_c, lambda mi: (mt, mi - M_CMP), nps=3)

        with Scope():
            QT = c.sb([64, S], BF16)
            KS = c.sb([64, S], BF16)
            KW = c.sb([64, S], BF16)
            VS = c.sb([128, NKB, 65], BF16)
            VW = c.sb([128, NKB, 65], BF16)
            mtc = load_masks(M_CAUSAL, 4)
            split_load(QT, KS, VS, qr_d, kslT_d, vsl_d)
            split_load(None, KW, VW, None, kwT_d, vw_d)
            mtw = load_masks(M_WIN, 8)

            def branch(KT, V, blocks_of, mt, mask_lo, br, with_sel):
                items = std_items(blocks_of)

                def qk(n, ps):
                    it = items[n]
                    s.op("pe", lambda: nc.tensor.matmul(ps[:], lhsT=KT[:, it["kb"] * 128:(it["kb"] + 1) * 128],
                                                        rhs=QT[:, it["qt"] * 512:(it["qt"] + 1) * 512], start=True, stop=not with_sel),
                         reads=rK(KT, it["kb"]) + rQ(QT, it["qt"]), writes=[ps.res])
                    if with_sel:
                        s.op("pe", lambda: nc.tensor.matmul(ps[:], lhsT=G[:, it["kb"] * 128:(it["kb"] + 1) * 128],
                                                            rhs=selbT[:, it["qt"] * 512:(it["qt"] + 1) * 512], start=False, stop=True),
                             reads=[G.res, selbT.res], writes=[ps.res])

                def fin(n):
                    qt = items[n]["qt"]
                    po = po_ring[qt % 2]
                    rz = rz_of(qt, po)
                    fac = fac_ring[qt % 2]
                    s.op("dve", lambda: nc.vector.tensor_tensor(out=fac[:], in0=rz[:], in1=ag[:, 4 * qt:4 * qt + 4, br], op=ALU.mult),
                         reads=[rz.res, ag.res], writes=[fac.res])
                    for qb in range(4):
                        tb = 4 * qt + qb
                        s.op("dve", (lambda qb=qb, tb=tb: nc.vector.scalar_tensor_tensor(out=oa[:, tb, :], in0=po[:, qb * 65:qb * 65 + 64], scalar=fac[:, qb:qb + 1],
                                                                                         in1=oa[:, tb, :], op0=ALU.mult, op1=ALU.add)),
                             reads=[po.res, fac.res, oa.res], writes=[oa.res])
                attn(items, qk, std_pv(items, V), fin, lambda mi: (mt, mi - mask_lo))
            branch(KS, VS, causal_blocks, mtc, M_CAUSAL, 1, True)
            branch(KW, VW, lambda qt: [(4 * qt - 4 + w, M_WIN + w) for w in range(8) if 4 * qt - 4 + w >= 0], mtw, M_WIN, 2, False)
        s.op("act", lambda: nc.scalar.copy(out=ost[:], in_=oa[:]), reads=[oa.res], writes=[ost.res])
        s.dma(o_d[0], ost[:], reads=[ost.res], is_output=True)
    c.close()
    return c


def run_B(cB, inp, l, proj, misc):
    import ml_dtypes
    bf = ml_dtypes.bfloat16
    cst = consts_B()

    def head_rows(P, ch0, i):
        return np.ascontiguousarray(P[ch0 + i // 2][(i % 2) * 64:(i % 2) * 64 + 64])

    def vaug(vT):
        v = vT.T.reshape(NKB, 128, 64).transpose(1, 0, 2)
        return np.ascontiguousarray(np.concatenate([v, np.ones((128, NKB, 1), bf)], axis=2))
    cl = 32 * 64
    w1k = np.ascontiguousarray(inp["nsa_ck_w1"][l].reshape(32, 64, 128).transpose(1, 0, 2))
    w1v = np.ascontiguousarray(inp["nsa_cv_w1"][l].reshape(32, 64, 128).transpose(1, 0, 2))
    pek = np.ascontiguousarray(inp["nsa_pe_k"][l].T)
    pev = np.ascontiguousarray(inp["nsa_pe_v"][l].T)
    maps = []
    for core in range(8):
        b, i = core // 4, core % 4
        P = proj[b]
        m = dict(cst)
        m["qnr"] = np.ascontiguousarray(np.stack([head_rows(P, 24, h) for h in range(4)]))
        m["qr"] = head_rows(P, 0, i)
        m["kcT"] = np.ascontiguousarray(P[3][0:64]); m["vcT"] = np.ascontiguousarray(P[12][0:64])
        m["kslT"] = np.ascontiguousarray(P[2][0:64]); m["kwT"] = np.ascontiguousarray(P[2][64:128])
        m["vsl"] = vaug(P[12][64:128]); m["vw"] = vaug(P[13][0:64])
        ag = misc[b][3 * i:3 * i + 3]
        m["ag"] = np.ascontiguousarray(ag.T.reshape(NKB, 128, 3).transpose(1, 0, 2))
        m["dq"] = head_rows(P, 4, i); m["dk"] = head_rows(P, 6, i); m["dv"] = vaug(head_rows(P, 14, i))
        m["sq"] = head_rows(P, 16, i); m["sk"] = head_rows(P, 18, i); m["sv"] = vaug(head_rows(P, 20, i))
        m["fq"] = head_rows(P, 8, i); m["fk"] = head_rows(P, 10, i); m["fv"] = vaug(head_rows(P, 22, i))
        m["logf"] = np.ascontiguousarray(misc[b][32 + i:33 + i])
        m["w1k"] = w1k; m["w1v"] = w1v; m["w2k"] = inp["nsa_ck_w2"][l]; m["w2v"] = inp["nsa_cv_w2"][l]
        m["pek"] = pek; m["pev"] = pev
        hs = np.zeros((128, 4), np.float32); hs[:, i] = 1.0
        m["hsel"] = hs
        maps.append(m)
    res = run_bass_kernel_spmd(cB.nc, maps, core_ids=list(range(8)))
    outs = []
    for b in range(2):
        ob = np.zeros((4, 256, S), bf)
        for i in range(4):
            o = res.results[b * 4 + i]["o"]
            for mm in range(4):
                tok = o[mm].transpose(1, 0, 2).reshape(S, 64)
                ob[mm, i * 64:(i + 1) * 64, :] = tok.T
        outs.append(ob)
    return outs


def build_C1(c=None):
    own = c is None
    if own:
        c = Ctx()
    ph = c.begin_phase()
    nc, s = c.nc, c.s
    oT_d = c.dram("oT", [4, 2, 128, NT], BF16, "ExternalInput")
    gates_d = c.dram("gates", [32, 128, NT], F32, "ExternalInput")
    xT_d = c.dram("xT", [128, 8, NT], F32, "ExternalInput")
    wbr_d = c.dram("wbr", [128, 8, 1024], F32, "ExternalInput")
    wout_d = c.dram("wout", [128, 8, 1024], F32, "ExternalInput")
    ga_d = c.dram("ga", [128, 8], F32, "ExternalInput")
    x1_d = c.dram("x1T", [128, 8, NT], F32, "ExternalOutput")

    xT = c.sb([128, 8, NT], F32)
    wbr = c.sb([128, 8, 1024], BF16)
    wout = c.sb([128, 8, 1024], BF16)
    ga = c.sb([128, 8], F32)
    stg = [c.sb([128, 2, 1024], F32) for _ in range(2)]
    s.dma(ga[:], ga_d, writes=[ga.res])
    for j in range(8):
        s.dma(xT[:, j, :], xT_d[:, j, :], writes=[xT.res])
    k = 0
    for (dst, src) in ((wbr, wbr_d), (wout, wout_d)):
        for q in range(4):
            st = stg[k % 2]
            k += 1
            s.dma(st[:], src[:, 2 * q:2 * q + 2, :], writes=[st.res])
            s.op("pool", (lambda st=st, dst=dst, q=q: nc.gpsimd.tensor_copy(out=dst[:, 2 * q:2 * q + 2, :], in_=st[:])), reads=[st.res], writes=[dst.res])
    ot_r = [c.sb([128, 8, TT], BF16) for _ in range(2)]
    gt_r = [[c.sb([128, TT], F32) for _ in range(4)] for _ in range(3)]
    zT_r = [c.sb([128, 8, TT], BF16) for _ in range(2)]
    pm_r = [c.ps([128, TT]) for _ in range(4)]
    pmix = [c.ps([128, TT]) for _ in range(2)]
    t_r = [c.sb([128, TT], F32) for _ in range(4)]
    items = [(tt, dc) for tt in range(NT // TT) for dc in range(8)]

    def c_load(n):
        tt, dc = items[n]
        sl = slice(tt * TT, (tt + 1) * TT)
        if dc == 0:
            ot = ot_r[tt % 2]
            for m in range(4):
                for kc in range(2):
                    s.dma(ot[:, m * 2 + kc, :], oT_d[m, kc, :, sl], writes=[ot.res], q="act")
        gts = gt_r[n % 3]
        for m in range(4):
            s.dma(gts[m][:], gates_d[m * 8 + dc, :, sl], writes=[gts[m].res], q=("sp", "act")[m % 2])

    def c_comp(n):
        tt, dc = items[n]
        sl = slice(tt * TT, (tt + 1) * TT)
        ot = ot_r[tt % 2]
        gts = gt_r[n % 3]
        zT = zT_r[tt % 2]
        for m in range(4):
            for kc in range(2):
                s.op("pe", (lambda m=m, kc=kc: nc.tensor.matmul(pm_r[m][:], lhsT=wbr[:, m * 2 + kc, dc * 128:(dc + 1) * 128], rhs=ot[:, m * 2 + kc, :],
                                                                start=(kc == 0), stop=(kc == 1))),
                     reads=[wbr.res, ot.res], writes=[pm_r[m].res])
        for m in range(4):
            s.op("dve", (lambda m=m: nc.vector.tensor_tensor(out=t_r[m][:], in0=pm_r[m][:], in1=gts[m][:], op=ALU.mult)),
                 reads=[pm_r[m].res, gts[m].res], writes=[t_r[m].res])
        s.op("pool", lambda: nc.gpsimd.tensor_tensor(out=t_r[0][:], in0=t_r[0][:], in1=t_r[1][:], op=ALU.add), reads=[t_r[0].res, t_r[1].res], writes=[t_r[0].res])
        s.op("pool", lambda: nc.gpsimd.tensor_tensor(out=t_r[2][:], in0=t_r[2][:], in1=t_r[3][:], op=ALU.add), reads=[t_r[2].res, t_r[3].res], writes=[t_r[2].res])
        s.op("pool", lambda: nc.gpsimd.tensor_tensor(out=zT[:, dc, :], in0=t_r[0][:], in1=t_r[2][:], op=ALU.add),
             reads=[t_r[0].res, t_r[2].res], writes=[zT.res])
        if dc != 7:
            return
        for ec in range(8):
            pmx = pmix[ec % 2]
            for j in range(8):
                s.op("pe", (lambda j=j, ec=ec, pmx=pmx: nc.tensor.matmul(pmx[:], lhsT=wout[:, j, ec * 128:(ec + 1) * 128], rhs=zT[:, j, :],
                                                                         start=(j == 0), stop=(j == 7))),
                     reads=[wout.res, zT.res], writes=[pmx.res])
            s.op("dve", (lambda ec=ec, pmx=pmx: nc.vector.scalar_tensor_tensor(out=xT[:, ec, sl], in0=pmx[:], scalar=ga[:, ec:ec + 1], in1=xT[:, ec, sl],
                                                                              op0=ALU.mult, op1=ALU.add)),
                 reads=[pmx.res, ga.res, xT.res], writes=[xT.res])
    pipeline(len(items), [c_load, c_comp])
    for j in range(8):
        s.dma(x1_d[:, j, :], xT[:, j, :], reads=[xT.res], is_output=True)
    c.end_phase(ph)
    if own:
        c.close()
    return c


def maps_C1(inp, l, o, gates, xT_all, mod):
    wbr = np.ascontiguousarray(inp["w_branch"][l].reshape(4, 2, 128, D).transpose(2, 0, 1, 3).reshape(128, 8, D))
    wout = np.ascontiguousarray(inp["w_out"][l].reshape(8, 128, D).transpose(1, 0, 2))
    maps = []
    for core in range(8):
        b, q = core // 4, core % 4
        tsl = slice(q * NT, (q + 1) * NT)
        maps.append({"oT": np.ascontiguousarray(o[b][:, :, tsl].reshape(4, 2, 128, NT)),
                     "gates": np.ascontiguousarray(gates[b][:, :, tsl]),
                     "xT": np.ascontiguousarray(xT_all[b][:, tsl].reshape(8, 128, NT).transpose(1, 0, 2)),
                     "wbr": wbr, "wout": wout, "ga": np.ascontiguousarray(mod[l, b][:, 16:24])})
    return maps


def collect_x(results, key):
    out = np.zeros((2, D, S), np.float32)
    for core in range(8):
        b, q = core // 4, core % 4
        out[b][:, q * NT:(q + 1) * NT] = results[core][key].transpose(1, 0, 2).reshape(D, NT)
    return out


def run_C1(cC1, inp, l, o, gates, xT_all, mod):
    maps = maps_C1(inp, l, o, gates, xT_all, mod)
    res = run_bass_kernel_spmd(cC1.nc, maps, core_ids=list(range(8)))
    return collect_x(res.results, "x1T")


HT = 1024
NFC = 22


def build_C2(n_exp, moe, c=None):
    own = c is None
    if own:
        c = Ctx()
    ph = c.begin_phase()
    nc, s = c.nc, c.s
    x1_d = c.dram("x1T", [128, 8, NT], F32, "ExternalInput")
    mod_d = c.dram("modF", [128, 24], F32, "ExternalInput")
    gn_d = c.dram("gn", [128, 8], F32, "ExternalInput")
    w1_d = c.dram("w1", [n_exp, NFC, 128, 8, 128], F32, "ExternalInput")
    w3_d = c.dram("w3", [n_exp, NFC, 128, 8, 128], F32, "ExternalInput")
    w2_d = c.dram("w2", [n_exp, 8, 128, NFC, 128], F32, "ExternalInput")
    if moe:
        rw_d = c.dram("rw", [128, 8, 8], F32, "ExternalInput")
        oh_d = c.dram("onehot", [8, 8, 128], F32, "ExternalInput")
        id_d = c.dram("ident", [128, 128], F32, "ExternalInput")
    x2_d = c.dram("x2T", [128, 8, NT], F32, "ExternalOutput")

    acc = c.sb([128, 8, HT], F32)
    hT = c.sb([128, 8, HT], BF16)
    act = c.sb([128, NFC, HT], BF16)
    rs = c.sb([128, HT], F32)
    ones = c.sb([128, 128], BF16)
    sqring = [c.sb([128, TT], BF16) for _ in range(2)]
    epsb = c.sb([128, 1], F32)
    mod = c.sb([128, 24], F32)
    gn = c.sb([128, 8], F32)
    Acol = c.sb([128, 8], F32)
    Bcol = c.sb([128, 8], F32)
    ring = [c.sb([128, TT], F32) for _ in range(4)]
    w1s = [c.sb([128, 8, 128], F32) for _ in range(2)]
    w3s = [c.sb([128, 8, 128], F32) for _ in range(2)]
    w1b = [c.sb([128, 8, 128], BF16) for _ in range(3)]
    w3b = [c.sb([128, 8, 128], BF16) for _ in range(3)]
    w2s = [c.sb([128, NFC, 128], F32) for _ in range(2)]
    w2b = [c.sb([128, NFC, 128], BF16) for _ in range(3)]
    sa_r = [c.sb([128, TT], F32) for _ in range(3)]
    u_r = [c.sb([128, TT], F32) for _ in range(3)]
    pa_r = [c.ps([128, TT]) for _ in range(2)]
    pg_r = [c.ps([128, TT]) for _ in range(2)]
    po_r = [c.ps([128, TT]) for _ in range(2)]
    pbank = c.ps([128, TT])
    pmisc = c.ps([128, TT])
    s.op("pool", lambda: nc.gpsimd.memset(ones[:], 1.0), writes=[ones.res])
    s.op("pool", lambda: nc.gpsimd.memset(epsb[:], EPS), writes=[epsb.res])
    s.dma(mod[:], mod_d, writes=[mod.res])
    s.dma(gn[:], gn_d, writes=[gn.res])
    emit_AB(c, gn, mod, 0, 8, Acol, Bcol)
    if moe:
        rw = c.sb([128, 8, 8], F32)
        oh = c.sb([8, 8, 128], F32)
        ident = c.sb([128, 128], F32)
        GT = c.sb([8, HT], F32)
        gb_r = [c.sb([128, HT], F32) for _ in range(2)]
        h32_r = [c.sb([128, TT], F32) for _ in range(2)]
        lg = c.sb([128, 8, 8], F32)
        m8 = c.sb([128, 8], F32)
        nt1 = c.sb([128, 1], F32)
        e2 = c.sb([128, 1], F32)
        ex = c.sb([128, 8], F32)
        selm = c.sb([128, 8], F32)
        Gt = c.sb([128, 8], F32)
        for t, d in ((rw, rw_d), (oh, oh_d), (ident, id_d)):
            s.dma(t[:], d, writes=[t.res])

    for hf in range(NT // HT):
        hsl = slice(hf * HT, (hf + 1) * HT)
        for j in range(8):
            s.dma(acc[:, j, :], x1_d[:, j, hsl], writes=[acc.res])
        if not moe:
            emit_modnorm(c, acc, hT, HT, ones, Acol, Bcol, epsb, ring, pbank, rs, sq_ring=sqring)
        else:
            def after_h(tmp, j, tt, sl):
                h32 = h32_r[j % 2]
                s.op("act", lambda: nc.scalar.activation(out=h32[:], in_=tmp[:], func=AF.Identity, scale=Acol[:, j:j + 1], bias=Bcol[:, j:j + 1]),
                     reads=[tmp.res, Acol.res, Bcol.res], writes=[h32.res])
                s.op("pool", lambda: nc.gpsimd.tensor_copy(out=hT[:, j, sl], in_=h32[:]), reads=[h32.res], writes=[hT.res])
                for tb in range(4):
                    col = (tt * 4 + tb) * 8
                    s.op("pe", (lambda tb=tb, col=col: nc.tensor.matmul(pmisc[:, col:col + 8], lhsT=h32[:, tb * 128:(tb + 1) * 128], rhs=rw[:, j, :],
                                                                        start=(j == 0 and tb == 0 and tt == 0), stop=(j == 7), skip_group_check=True)),
                         reads=[h32.res, rw.res], writes=[pmisc.res])
            emit_modnorm(c, acc, hT, HT, ones, Acol, Bcol, epsb, ring, pbank, rs, after_h=after_h, sq_ring=sqring)
            s.op("act", lambda: nc.scalar.copy(out=lg[:], in_=pmisc[:, 0:64].rearrange("p (a b) -> p a b", b=8)), reads=[pmisc.res], writes=[lg.res])
            for tb in range(8):
                s.op("dve", (lambda tb=tb: nc.vector.max(out=m8[:], in_=lg[:, tb, :])), reads=[lg.res], writes=[m8.res])
                s.op("dve", lambda: nc.vector.tensor_scalar(out=nt1[:], in0=m8[:, 0:1], scalar1=-1.0, scalar2=None, op0=ALU.mult), reads=[m8.res], writes=[nt1.res])
                s.op("act", (lambda tb=tb: nc.scalar.activation(out=ex[:], in_=lg[:, tb, :], func=AF.Exp, bias=nt1[:], scale=1.0)),
                     reads=[lg.res, nt1.res], writes=[ex.res])
                s.op("act", lambda: nc.scalar.activation(out=e2[:], in_=m8[:, 1:2], func=AF.Exp, bias=nt1[:], scale=1.0), reads=[m8.res, nt1.res], writes=[e2.res])
                s.op("dve", lambda: nc.vector.tensor_scalar(out=e2[:], in0=e2[:], scalar1=1.0, scalar2=None, op0=ALU.add), reads=[e2.res], writes=[e2.res])
                s.op("dve", lambda: nc.vector.reciprocal(out=e2[:], in_=e2[:]), reads=[e2.res], writes=[e2.res])
                s.op("dve", (lambda tb=tb: nc.vector.tensor_scalar(out=selm[:], in0=lg[:, tb, :], scalar1=m8[:, 1:2], scalar2=None, op0=ALU.is_ge)),
                     reads=[lg.res, m8.res], writes=[selm.res])
                s.op("dve", lambda: nc.vector.scalar_tensor_tensor(out=Gt[:], in0=ex[:], scalar=e2[:, 0:1], in1=selm[:], op0=ALU.mult, op1=ALU.mult),
                     reads=[ex.res, e2.res, selm.res], writes=[Gt.res])
                s.op("pe", (lambda tb=tb: nc.tensor.transpose(pbank[0:8, (tb % 4) * 128:(tb % 4 + 1) * 128], Gt[:], ident[:])), reads=[Gt.res, ident.res], writes=[pbank.res])
                if tb % 4 == 3:
                    q4 = tb // 4
                    s.op("act", (lambda q4=q4: nc.scalar.copy(out=GT[:, q4 * 512:(q4 + 1) * 512], in_=pbank[0:8, 0:512])), reads=[pbank.res], writes=[GT.res])

        items = []
        kf = kg = 0
        for e in range(n_exp):
            for fc in range(NFC):
                for tt in range(2):
                    items.append(("f", e, fc, tt, kf))
                kf += 1
            for ec in range(8):
                for tt in range(2):
                    items.append(("g", e, ec, tt, kg))
                kg += 1

        def s_load(n):
            kind, e, ci, tt, k = items[n]
            if tt != 0:
                return
            if kind == "f":
                if moe and ci == 0:
                    g_b = gb_r[e % 2]
                    for t2 in range(2):
                        s.op("pe", (lambda t2=t2, e=e: nc.tensor.matmul(pmisc[:], lhsT=oh[:, e, :], rhs=GT[:, t2 * TT:(t2 + 1) * TT], start=True, stop=True)),
                             reads=[oh.res, GT.res], writes=[pmisc.res])
                        s.op("act", (lambda t2=t2, g_b=g_b: nc.scalar.copy(out=g_b[:, t2 * TT:(t2 + 1) * TT], in_=pmisc[:])), reads=[pmisc.res], writes=[g_b.res])
                s.dma(w1s[k % 2][:], w1_d[e, ci], writes=[w1s[k % 2].res], q="act")
                s.dma(w3s[k % 2][:], w3_d[e, ci], writes=[w3s[k % 2].res], q="act")
            else:
                s.dma(w2s[k % 2][:], w2_d[e, ci], writes=[w2s[k % 2].res], q="act")

        def s_cast(n):
            kind, e, ci, tt, k = items[n]
            if tt != 0:
                return
            if kind == "f":
                s.op("pool", lambda: nc.gpsimd.tensor_copy(out=w1b[k % 3][:], in_=w1s[k % 2][:]), reads=[w1s[k % 2].res], writes=[w1b[k % 3].res])
                s.op("dve", lambda: nc.vector.tensor_copy(out=w3b[k % 3][:], in_=w3s[k % 2][:]), reads=[w3s[k % 2].res], writes=[w3b[k % 3].res])
            else:
                half = NFC // 2
                s.op("pool", lambda: nc.gpsimd.tensor_copy(out=w2b[k % 3][:, 0:half, :], in_=w2s[k % 2][:, 0:half, :]), reads=[w2s[k % 2].res], writes=[w2b[k % 3].res])
                s.op("dve", lambda: nc.vector.tensor_copy(out=w2b[k % 3][:, half:NFC, :], in_=w2s[k % 2][:, half:NFC, :]), reads=[w2s[k % 2].res], writes=[w2b[k % 3].res])

        def s_mm(n):
            kind, e, ci, tt, k = items[n]
            sl = slice(tt * TT, (tt + 1) * TT)
            if kind == "f":
                pa, pg = pa_r[n % 2], pg_r[n % 2]
                for j in range(8):
                    s.op("pe", (lambda j=j: nc.tensor.matmul(pa[:], lhsT=w1b[k % 3][:, j, :], rhs=hT[:, j, sl], start=(j == 0), stop=(j == 7))),
                         reads=[w1b[k % 3].res, hT.res], writes=[pa.res])
                for j in range(8):
                    s.op("pe", (lambda j=j: nc.tensor.matmul(pg[:], lhsT=w3b[k % 3][:, j, :], rhs=hT[:, j, sl], start=(j == 0), stop=(j == 7))),
                         reads=[w3b[k % 3].res, hT.res], writes=[pg.res])
            else:
                po = po_r[n % 2]
                for fc in range(NFC):
                    s.op("pe", (lambda fc=fc: nc.tensor.matmul(po[:], lhsT=w2b[k % 3][:, fc, :], rhs=act[:, fc, sl], start=(fc == 0), stop=(fc == NFC - 1))),
                         reads=[w2b[k % 3].res, act.res], writes=[po.res])

        def s_post(n):
            kind, e, ci, tt, k = items[n]
            sl = slice(tt * TT, (tt + 1) * TT)
            if kind == "f":
                pa, pg = pa_r[n % 2], pg_r[n % 2]
                sa = sa_r[n % 3]
                s.op("act", lambda: nc.scalar.activation(out=sa[:], in_=pa[:], func=AF.Silu), reads=[pa.res], writes=[sa.res])
                if not moe:
                    s.op("dve", lambda: nc.vector.tensor_tensor(out=act[:, ci, sl], in0=sa[:], in1=pg[:], op=ALU.mult), reads=[sa.res, pg.res], writes=[act.res])
                else:
                    u = u_r[n % 3]
                    g_b = gb_r[e % 2]
                    s.op("dve", lambda: nc.vector.tensor_tensor(out=u[:], in0=pg[:], in1=g_b[:, sl], op=ALU.mult), reads=[pg.res, g_b.res], writes=[u.res])
                    s.op("pool", lambda: nc.gpsimd.tensor_tensor(out=act[:, ci, sl], in0=sa[:], in1=u[:], op=ALU.mult), reads=[sa.res, u.res], writes=[act.res])
            else:
                po = po_r[n % 2]
                s.op("dve", lambda: nc.vector.scalar_tensor_tensor(out=acc[:, ci, sl], in0=po[:], scalar=mod[:, 16 + ci:17 + ci], in1=acc[:, ci, sl],
                                                                   op0=ALU.mult, op1=ALU.add), reads=[po.res, mod.res, acc.res], writes=[acc.res])
        pipeline(len(items), [s_load, s_cast, s_mm, s_post])
        for j in range(8):
            s.dma(x2_d[:, j, hsl], acc[:, j, :], reads=[acc.res], is_output=True)
    c.end_phase(ph)
    if own:
        c.close()
    return c


def maps_C2(inp, l, x1T_all, mod, moe):
    if moe:
        w1, w3, w2 = inp["moe_w1"][l // 2], inp["moe_w3"][l // 2], inp["moe_w2"][l // 2]
    else:
        w1, w3, w2 = inp["ffn_w1"][l // 2][None], inp["ffn_w3"][l // 2][None], inp["ffn_w2"][l // 2][None]
    E = w1.shape[0]
    lay1 = lambda w: np.ascontiguousarray(w.reshape(E, 8, 128, NFC, 128).transpose(0, 3, 2, 1, 4))
    lay2 = lambda w: np.ascontiguousarray(w.reshape(E, NFC, 128, 8, 128).transpose(0, 3, 2, 1, 4))
    w1l, w3l, w2l = lay1(w1), lay1(w3), lay2(w2)
    gn = np.ascontiguousarray(inp["norm_ffn"][l].reshape(8, 128).T)
    maps = []
    for core in range(8):
        b, q = core // 4, core % 4
        m = {"modF": np.ascontiguousarray(mod[l, b][:, 24:48]), "gn": gn, "w1": w1l, "w3": w3l, "w2": w2l}
        if x1T_all is not None:
            m["x1T"] = np.ascontiguousarray(x1T_all[b][:, q * NT:(q + 1) * NT].reshape(8, 128, NT).transpose(1, 0, 2))
        if moe:
            m["rw"] = np.ascontiguousarray(inp["router_w"][l // 2].reshape(8, 128, 8).transpose(1, 0, 2))
            oh = np.zeros((8, 8, 128), np.float32)
            for e in range(8):
                oh[e, e, :] = 1.0
            m["onehot"] = oh
            m["ident"] = np.eye(128, dtype=np.float32)
        maps.append(m)
    return maps


def run_C2(cC2, inp, l, x1T_all, mod, moe):
    maps = maps_C2(inp, l, x1T_all, mod, moe)
    res = run_bass_kernel_spmd(cC2.nc, maps, core_ids=list(range(8)))
    return collect_x(res.results, "x2T")


def build_CA(moe, with_A):
    c = Ctx()
    c.alias = {}
    c.pre = "c1_"
    c.kind_override = {"c1_x1T": "Internal"}
    build_C1(c)
    c.pre = "c2_"
    c.alias["c2_x1T"] = c.made["c1_x1T"]
    build_C2(8 if moe else 1, moe, c)
    if with_A:
        c.pre = "a_"
        c.alias["a_xT"] = c.made["c2_x2T"]
        build_A(c)
    c.close()
    return c


def run_CA(cCA, inp, l, o, gates, xT_all, mod, moe, with_A):
    m1 = maps_C1(inp, l, o, gates, xT_all, mod)
    m2 = maps_C2(inp, l, None, mod, moe)
    m3 = maps_A(inp, l + 1, None, mod) if with_A else [dict() for _ in range(8)]
    maps = []
    for i in range(8):
        m = {"c1_" + k: v for k, v in m1[i].items()}
        m.update({"c2_" + k: v for k, v in m2[i].items()})
        m.update({"a_" + k: v for k, v in m3[i].items()})
        maps.append(m)
    res = run_bass_kernel_spmd(cCA.nc, maps, core_ids=list(range(8)))
    x2T = collect_x(res.results, "c2_x2T")
    nxt = collect_A(res.results, "a_") if with_A else None
    return x2T, nxt


_PROGS = {}


def _prog(name, fn):
    if name not in _PROGS:
        _PROGS[name] = fn()
    return _PROGS[name]


def kernel(**inp):
    inp = {k: np.asarray(v) for k, v in inp.items()}
    mod = run_M(inp)
    xT_all = np.ascontiguousarray(inp["x"].astype(np.float32).transpose(0, 2, 1))
    cA = _prog("A", build_A)
    proj, gates, misc = run_A(cA, inp, 0, xT_all, mod)
    cB = _prog("B", build_B)
    for l in range(2):
        o = run_B(cB, inp, l, proj, misc)
        moe = (l % 2 == 1)
        with_A = (l == 0)
        cCA = _prog("CA%d" % l, lambda: build_CA(moe, with_A))
        xT_all, nxt = run_CA(cCA, inp, l, o, gates, xT_all, mod, moe, with_A)
        if with_A:
            proj, gates, misc = nxt
    return np.ascontiguousarray(xT_all.transpose(0, 2, 1)).astype(np.float32)
```

```python
import contextlib
import numpy as np
import concourse.bass as bass
import concourse.mybir as mybir
from concourse.bass_utils import run_bass_kernel_spmd

F32 = mybir.dt.float32
BF16 = mybir.dt.bfloat16
I32 = mybir.dt.int32
AF = mybir.ActivationFunctionType
ALU = mybir.AluOpType
AX = mybir.AxisListType


class Res:
    __slots__ = ("w", "r")

    def __init__(self):
        self.w = None
        self.r = {}


class Sched:
    NDMA = 24

    def __init__(self, nc, es):
        self.nc = nc
        self.engs = {"pe": nc.tensor, "act": nc.scalar, "dve": nc.vector,
                     "pool": nc.gpsimd, "sp": nc.sync}
        self.sem = {}
        self.cnt = {}
        for k in self.engs:
            self.sem[k] = es.enter_context(nc.semaphore("s_" + k))
            self.cnt[k] = 0
        for i in range(self.NDMA):
            k = "d%d" % i
            self.sem[k] = es.enter_context(nc.semaphore("s_" + k))
            self.cnt[k] = 0
        self.seen = {k: {} for k in self.engs}
        self.dma_rr = 0
        self.out_events = []

    def _wait(self, e, ev):
        if ev is None:
            return
        key, val = ev
        if key == e and e == "pe":
            return
        if self.seen[e].get(key, 0) >= val:
            return
        self.engs[e].wait_ge(self.sem[key], val)
        self.seen[e][key] = val

    def _deps(self, e, reads, writes):
        for r in reads:
            self._wait(e, r.w)
        for r in writes:
            self._wait(e, r.w)
            for k, v in r.r.items():
                self._wait(e, (k, v))

    def op(self, e, fn, reads=(), writes=()):
        self._deps(e, reads, writes)
        ins = fn()
        self.cnt[e] += 1
        ins.then_inc(self.sem[e], 1)
        ev = (e, self.cnt[e])
        for r in reads:
            r.r[e] = ev[1]
        for r in writes:
            r.w = ev
            r.r = {}
        return ev

    def dma(self, out, in_, reads=(), writes=(), q="sp", is_output=False, **kw):
        k = "d%d" % self.dma_rr
        self.dma_rr = (self.dma_rr + 1) % self.NDMA
        self._wait(q, (k, self.cnt[k]))
        self._deps(q, reads, writes)
        ins = self.engs[q].dma_start(out=out, in_=in_, **kw)
        self.cnt[k] += 16
        ins.then_inc(self.sem[k], 16)
        ev = (k, self.cnt[k])
        for r in reads:
            r.r[k] = ev[1]
        for r in writes:
            r.w = ev
            r.r = {}
        if is_output:
            self.out_events.append(ev)
        return ev

    def finish(self):
        for i in range(self.NDMA):
            k = "d%d" % i
            self._wait("sp", (k, self.cnt[k]))
        for k in ("pe", "act", "dve", "pool"):
            self._wait("sp", (k, self.cnt[k]))


class Tile:
    def __init__(self, t):
        self.t = t
        self.res = Res()

    def __getitem__(self, idx):
        return self.t[idx]


class Ctx:
    def __init__(self, name="k"):
        self.nc = bass.Bass("TRN2", target_bir_lowering=False)
        self.es = contextlib.ExitStack()
        self.s = Sched(self.nc, self.es)
        self.n = 0

    def sb(self, shape, dt, name=None):
        self.n += 1
        return Tile(self.es.enter_context(self.nc.sbuf_tensor(name or ("t%d" % self.n), list(shape), dt)))

    def ps(self, shape, dt=F32, name=None):
        self.n += 1
        return Tile(self.es.enter_context(self.nc.psum_tensor(name or ("p%d" % self.n), list(shape), dt)))

    pre = ""
    alias = None
    kind_override = None

    def dram(self, name, shape, dt, kind):
        full = self.pre + name
        if self.alias and full in self.alias:
            return self.alias[full]
        if self.kind_override and full in self.kind_override:
            kind = self.kind_override[full]
        ap = self.nc.dram_tensor(full, list(shape), dt, kind=kind).ap()
        if self.alias is None:
            self.alias = {}
        self.made = getattr(self, "made", {})
        self.made[full] = ap
        return ap

    def begin_phase(self):
        es = contextlib.ExitStack()
        old, self.es = self.es, es
        return (old, es)

    def end_phase(self, ph):
        barrier(self)
        self.es = ph[0]
        ph[1].close()

    def close(self):
        self.s.finish()
        self.es.close()


D = 1024
S = 8192
NB = 2
NT = 2048
TT = 512
NCH_IN = 57
A_MAXCH = NCH_IN
EPS = 1e-6
TWO_PI = float(2 * np.pi)
C1_2PI = 6.28125
C2_2PI = TWO_PI - C1_2PI


def barrier(c):
    s = c.s
    for e in ("pe", "act", "dve", "pool", "sp"):
        for k in list(s.cnt.keys()):
            if k != e:
                s._wait(e, (k, s.cnt[k]))


def build_M():
    c = Ctx()
    nc, s = c.nc, c.s
    cT_d = c.dram("cT", [128, 8, 2], F32, "ExternalInput")
    w_d = c.dram("w", [12, 128, 8, 128], F32, "ExternalInput")
    b_d = c.dram("b", [128, 12], F32, "ExternalInput")
    o_d = c.dram("modT", [128, 12, 2], F32, "ExternalOutput")
    cT = c.sb([128, 8, 2], F32)
    ca = c.sb([128, 8, 2], F32)
    bt = c.sb([128, 12], F32)
    ot = c.sb([128, 12, 2], F32)
    s.dma(cT[:], cT_d, writes=[cT.res])
    s.dma(bt[:], b_d, writes=[bt.res])
    s.op("act", lambda: nc.scalar.activation(out=ca[:], in_=cT[:], func=AF.Silu), reads=[cT.res], writes=[ca.res])
    wts = [c.sb([128, 8, 128], F32) for _ in range(3)]
    pm = c.ps([128, 12, 2])
    for j in range(12):
        wt = wts[j % 3]
        s.dma(wt[:], w_d[j], writes=[wt.res])
        for k in range(8):
            s.op("pe", (lambda wt=wt, k=k, j=j: nc.tensor.matmul(pm[:, j, :], lhsT=wt[:, k, :], rhs=ca[:, k, :],
                                                                 start=(k == 0), stop=(k == 7))),
                 reads=[wt.res, ca.res], writes=[pm.res])
    for b in range(2):
        s.op("dve", (lambda b=b: nc.vector.tensor_tensor(out=ot[:, :, b], in0=pm[:, :, b], in1=bt[:], op=ALU.add)),
             reads=[pm.res, bt.res], writes=[ot.res])
    s.dma(o_d, ot[:], reads=[ot.res], is_output=True)
    c.close()
    return c


def run_M(inp):
    c = build_M()
    cT = np.ascontiguousarray(inp["c"].T.reshape(8, 128, 2).transpose(1, 0, 2))
    w_all = inp["w_ada"]
    b_all = inp["b_ada"]
    maps = []
    for i in range(8):
        chunks = [(g // 48, g % 48) for g in range(i * 12, i * 12 + 12)]
        w = np.stack([w_all[l][:, n * 128:(n + 1) * 128].reshape(8, 128, 128).transpose(1, 0, 2) for l, n in chunks])
        b = np.stack([b_all[l][n * 128:(n + 1) * 128] for l, n in chunks], axis=1)
        maps.append({"cT": cT, "w": np.ascontiguousarray(w), "b": np.ascontiguousarray(b)})
    res = run_bass_kernel_spmd(c.nc, maps, core_ids=list(range(8)))
    mod = np.zeros((2, 2, 128, 48), np.float32)
    for i in range(8):
        o = res.results[i]["modT"]
        for jj, g in enumerate(range(i * 12, i * 12 + 12)):
            mod[g // 48, :, :, g % 48] = o[:, jj, :].T
    return mod


def emit_modnorm(c, src, hT, ntok, ones, Acol, Bcol, epsb, tmp_ring, pbank, rs, after_h=None, sq_ring=None):
    nc, s = c.nc, c.s
    ntt = ntok // TT
    if sq_ring is None:
        sq_ring = tmp_ring
    for tt in range(ntt):
        sl = slice(tt * TT, (tt + 1) * TT)
        for j in range(8):
            sq = sq_ring[j % len(sq_ring)]
            s.op("act", (lambda sq=sq, j=j: nc.scalar.activation(out=sq[:], in_=src[:, j, sl], func=AF.Square)),
                 reads=[src.res], writes=[sq.res])
            s.op("pe", (lambda sq=sq, j=j: nc.tensor.matmul(pbank[:], lhsT=ones[:], rhs=sq[:], start=(j == 0), stop=(j == 7))),
                 reads=[sq.res, ones.res], writes=[pbank.res])
        sd = tmp_ring[0]
        s.op("act", (lambda sd=sd: nc.scalar.activation(out=sd[:], in_=pbank[:], func=AF.Sqrt, scale=1.0 / D, bias=epsb[:])),
             reads=[pbank.res, epsb.res], writes=[sd.res])
        s.op("dve", (lambda sd=sd: nc.vector.reciprocal(out=rs[:, sl], in_=sd[:])), reads=[sd.res], writes=[rs.res])
        for j in range(8):
            tmp = tmp_ring[1 + (j % (len(tmp_ring) - 1))]
            s.op("dve", (lambda tmp=tmp, j=j: nc.vector.tensor_tensor(out=tmp[:], in0=src[:, j, sl], in1=rs[:, sl], op=ALU.mult)),
                 reads=[src.res, rs.res], writes=[tmp.res])
            if after_h is None:
                s.op("act", (lambda tmp=tmp, j=j: nc.scalar.activation(out=hT[:, j, sl], in_=tmp[:], func=AF.Identity,
                                                                      scale=Acol[:, j:j + 1], bias=Bcol[:, j:j + 1])),
                     reads=[tmp.res, Acol.res, Bcol.res], writes=[hT.res])
            else:
                after_h(tmp, j, tt, sl)


def emit_AB(c, gn, mod, sh_off, sc_off, Acol, Bcol):
    nc, s = c.nc, c.s
    s.op("dve", lambda: nc.vector.scalar_tensor_tensor(out=Acol[:], in0=mod[:, sc_off:sc_off + 8], scalar=1.0, in1=gn[:],
                                                       op0=ALU.add, op1=ALU.mult),
         reads=[mod.res, gn.res], writes=[Acol.res])
    s.op("dve", lambda: nc.vector.tensor_copy(out=Bcol[:], in_=mod[:, sh_off:sh_off + 8]), reads=[mod.res], writes=[Bcol.res])


ROPE_CH = (0, 1, 2, 4, 5, 6, 7)
NORM_CH = (3, 8, 9, 10, 11)
RAW_CH = tuple(range(12, 24))
MISC_CH = 24


def build_A(c=None):
    own = c is None
    if own:
        c = Ctx()
    ph = c.begin_phase()
    nc, s = c.nc, c.s
    xT_d = c.dram("xT", [128, 8, NT], F32, "ExternalInput")
    mod_d = c.dram("modA", [128, 16], F32, "ExternalInput")
    gn_d = c.dram("gn", [128, 8], F32, "ExternalInput")
    pos_d = c.dram("pos", [1, NT], I32, "ExternalInput")
    invf_d = c.dram("invf", [128, 1], F32, "ExternalInput")
    w_d = c.dram("w", [NCH_IN, 128, 8, 128], F32, "ExternalInput")
    gain_d = c.dram("gain", [128, 12], F32, "ExternalInput")
    osc_d = c.dram("osc", [128, 12], F32, "ExternalInput")
    foxb_d = c.dram("foxb", [128, 1], F32, "ExternalInput")
    pm_d = c.dram("pm", [128, 128], F32, "ExternalInput")
    bones_d = c.dram("bones", [128, 128], F32, "ExternalInput")
    proj_d = c.dram("proj", [26, 128, NT], BF16, "ExternalOutput")
    gates_d = c.dram("gates", [32, 128, NT], F32, "ExternalOutput")
    misc_d = c.dram("misc", [64, NT], F32, "ExternalOutput")

    hT = c.sb([128, 8, NT], BF16)
    rs = c.sb([128, NT], F32)
    COS = c.sb([128, NT], F32)
    SIN = c.sb([128, NT], F32)
    ones = c.sb([128, 128], BF16)
    bones_f = c.sb([128, 128], F32)
    pm_f = c.sb([128, 128], F32)
    bones = c.sb([128, 128], BF16)
    pm = c.sb([128, 128], BF16)
    gain = c.sb([128, 12], F32)
    osc = c.sb([128, 12], F32)
    foxb = c.sb([128, 1], F32)
    epsb = c.sb([128, 1], F32)
    negpi = c.sb([128, 1], F32)
    invf = c.sb([128, 1], F32)
    mod = c.sb([128, 16], F32)
    gn = c.sb([128, 8], F32)
    Acol = c.sb([128, 8], F32)
    Bcol = c.sb([128, 8], F32)
    pbank = c.ps([128, TT])

    s.op("pool", lambda: nc.gpsimd.memset(ones[:], 1.0), writes=[ones.res])
    s.op("pool", lambda: nc.gpsimd.memset(epsb[:], EPS), writes=[epsb.res])
    s.op("pool", lambda: nc.gpsimd.memset(negpi[:], -float(np.pi)), writes=[negpi.res])
    for t, d in ((bones_f, bones_d), (pm_f, pm_d), (gain, gain_d), (osc, osc_d), (foxb, foxb_d), (invf, invf_d), (mod, mod_d), (gn, gn_d)):
        s.dma(t[:], d, writes=[t.res])
    s.op("pool", lambda: nc.gpsimd.tensor_copy(out=bones[:], in_=bones_f[:]), reads=[bones_f.res], writes=[bones.res])
    s.op("pool", lambda: nc.gpsimd.tensor_copy(out=pm[:], in_=pm_f[:]), reads=[pm_f.res], writes=[pm.res])
    s.op("dve", lambda: nc.vector.tensor_tensor(out=gain[:], in0=gain[:], in1=osc[:], op=ALU.mult), reads=[gain.res, osc.res], writes=[gain.res])
    emit_AB(c, gn, mod, 0, 8, Acol, Bcol)

    with contextlib.ExitStack() as es1:
        old_es, c.es = c.es, es1
        xT = c.sb([128, 8, NT], F32)
        for j in range(8):
            s.dma(xT[:, j, :], xT_d[:, j, :], writes=[xT.res])
        posi = c.sb([128, NT], I32)
        ang = c.sb([128, NT], F32)
        tq = c.sb([128, NT], F32)
        ki = c.sb([128, NT], I32)
        kf = c.sb([128, NT], F32)
        s.dma(posi[:], pos_d[0, :].partition_broadcast(128), writes=[posi.res])
        s.op("dve", lambda: nc.vector.tensor_copy(out=tq[:], in_=posi[:]), reads=[posi.res], writes=[tq.res])
        s.op("dve", lambda: nc.vector.tensor_scalar(out=ang[:], in0=tq[:], scalar1=invf[:, 0:1], scalar2=None, op0=ALU.mult),
             reads=[tq.res, invf.res], writes=[ang.res])
        for dst, shift in ((SIN, 0.0), (COS, 0.25)):
            s.op("dve", lambda shift=shift: nc.vector.tensor_scalar(out=tq[:], in0=ang[:], scalar1=1.0 / TWO_PI, scalar2=0.5 + shift,
                                                                    op0=ALU.mult, op1=ALU.add), reads=[ang.res], writes=[tq.res])
            s.op("dve", lambda: nc.vector.tensor_copy(out=ki[:], in_=tq[:]), reads=[tq.res], writes=[ki.res])
            s.op("dve", lambda: nc.vector.tensor_copy(out=kf[:], in_=ki[:]), reads=[ki.res], writes=[kf.res])
            s.op("dve", lambda shift=shift: nc.vector.tensor_scalar(out=tq[:], in0=ang[:], scalar1=float(np.pi) + shift * TWO_PI, scalar2=None,
                                                                    op0=ALU.add), reads=[ang.res], writes=[tq.res])
            s.op("dve", lambda: nc.vector.scalar_tensor_tensor(out=tq[:], in0=kf[:], scalar=-C1_2PI, in1=tq[:], op0=ALU.mult, op1=ALU.add),
                 reads=[kf.res, tq.res], writes=[tq.res])
            s.op("dve", lambda: nc.vector.scalar_tensor_tensor(out=tq[:], in0=kf[:], scalar=-C2_2PI, in1=tq[:], op0=ALU.mult, op1=ALU.add),
                 reads=[kf.res, tq.res], writes=[tq.res])
            s.op("dve", lambda: nc.vector.tensor_scalar(out=kf[:], in0=tq[:], scalar1=0.0, scalar2=TWO_PI, op0=ALU.is_lt, op1=ALU.mult),
                 reads=[tq.res], writes=[kf.res])
            s.op("dve", lambda: nc.vector.tensor_tensor(out=tq[:], in0=tq[:], in1=kf[:], op=ALU.add), reads=[tq.res, kf.res], writes=[tq.res])
            s.op("dve", lambda: nc.vector.tensor_scalar(out=tq[:], in0=tq[:], scalar1=0.0, scalar2=TWO_PI, op0=ALU.max, op1=ALU.min),
                 reads=[tq.res], writes=[tq.res])
            s.op("act", lambda dst=dst: nc.scalar.activation(out=dst[:], in_=tq[:], func=AF.Sin, bias=negpi[:], scale=1.0),
                 reads=[tq.res, negpi.res], writes=[dst.res])
        ring = [c.sb([128, TT], F32) for _ in range(4)]
        sqring = [c.sb([128, TT], BF16) for _ in range(4)]
        emit_modnorm(c, xT, hT, NT, ones, Acol, Bcol, epsb, ring, pbank, rs, sq_ring=sqring)
        barrier(c)
        c.es = old_es

    R = 4
    wst = [c.sb([128, 8, 128], F32) for _ in range(3)]
    wbf = [c.sb([128, 8, 128], BF16) for _ in range(3)]
    pp = [c.ps([128, TT]) for _ in range(3)]
    psq = [c.ps([128, TT]) for _ in range(2)]
    pq = [pbank, c.ps([128, TT])]
    sq_r = [c.sb([128, TT], BF16) for _ in range(R)]
    rstd_r = [c.sb([128, TT], F32) for _ in range(R)]
    y_r = [c.sb([128, TT], F32) for _ in range(R)]
    y2_r = [c.sb([128, TT], BF16) for _ in range(R)]
    t1_r = [c.sb([128, TT], F32) for _ in range(R)]
    ob_r = [c.sb([128, TT], BF16) for _ in range(R)]
    ob2_r = [c.sb([128, TT], BF16) for _ in range(R)]
    of_r = [c.sb([128, TT], F32) for _ in range(R)]

    items = [(ch, tt) for ch in range(A_MAXCH) for tt in range(NT // TT)]

    def kind(ch):
        if ch in ROPE_CH:
            return "rope"
        if ch in NORM_CH:
            return "norm"
        if ch in RAW_CH:
            return "raw"
        if ch == MISC_CH:
            return "misc"
        return "sig"

    def stl(n):
        ch, tt = items[n]
        if tt == 0:
            ws = wst[ch % 3]
            s.dma(ws[:], w_d[ch], writes=[ws.res], q="act")

    def stc(n):
        ch, tt = items[n]
        if tt == 0:
            ws, wb = wst[ch % 3], wbf[ch % 3]
            s.op("pool", lambda: nc.gpsimd.tensor_copy(out=wb[:], in_=ws[:]), reads=[ws.res], writes=[wb.res])

    def st0(n):
        ch, tt = items[n]
        wb = wbf[ch % 3]
        p = pp[n % 3]
        for j in range(8):
            s.op("pe", (lambda j=j: nc.tensor.matmul(p[:], lhsT=wb[:, j, :], rhs=hT[:, j, tt * TT:(tt + 1) * TT],
                                                     start=(j == 0), stop=(j == 7))),
                 reads=[wb.res, hT.res], writes=[p.res])

    def st1(n):
        ch, tt = items[n]
        k = kind(ch)
        p = pp[n % 3]
        sl = slice(tt * TT, (tt + 1) * TT)
        if k in ("rope", "norm"):
            sq = sq_r[n % R]
            s.op("act", lambda: nc.scalar.activation(out=sq[:], in_=p[:], func=AF.Square), reads=[p.res], writes=[sq.res])
            s.op("pe", lambda: nc.tensor.matmul(psq[n % 2][:], lhsT=bones[:], rhs=sq[:], start=True, stop=True),
                 reads=[bones.res, sq.res], writes=[psq[n % 2].res])
        elif k == "raw":
            ob = ob_r[n % R]
            sc = 0.125 if ch in (16, 17) else 1.0
            s.op("act", lambda: nc.scalar.activation(out=ob[:], in_=p[:], func=AF.Copy, scale=sc), reads=[p.res], writes=[ob.res])
            s.dma(proj_d[ch, :, sl], ob[:], reads=[ob.res], is_output=True)
        elif k == "sig":
            of = of_r[n % R]
            s.op("act", lambda: nc.scalar.activation(out=of[:], in_=p[:], func=AF.Sigmoid), reads=[p.res], writes=[of.res])
            s.dma(gates_d[ch - 25, :, sl], of[:], reads=[of.res], is_output=True, q=("sp", "act")[n % 2])
        else:
            of = of_r[n % R]
            s.op("act", lambda: nc.scalar.activation(out=of[0:64, :], in_=p[0:64, :], func=AF.Sigmoid, bias=foxb[0:64, :], scale=1.0),
                 reads=[p.res, foxb.res], writes=[of.res])
            s.op("act", lambda: nc.scalar.activation(out=of[32:64, :], in_=of[32:64, :], func=AF.Ln), reads=[of.res], writes=[of.res])
            s.dma(misc_d[:, sl], of[0:64, :], reads=[of.res], is_output=True)

    def st2(n):
        ch, tt = items[n]
        k = kind(ch)
        if k not in ("rope", "norm"):
            return
        p = pp[n % 3]
        sl = slice(tt * TT, (tt + 1) * TT)
        rstd = rstd_r[n % R]
        y = y_r[n % R]
        s.op("act", lambda: nc.scalar.activation(out=rstd[:], in_=psq[n % 2][:], func=AF.Sqrt, scale=1.0 / 64, bias=epsb[:]),
             reads=[psq[n % 2].res, epsb.res], writes=[rstd.res])
        s.op("dve", lambda: nc.vector.reciprocal(out=rstd[:], in_=rstd[:]), reads=[rstd.res], writes=[rstd.res])
        s.op("dve", lambda: nc.vector.tensor_tensor(out=y[:], in0=p[:], in1=rstd[:], op=ALU.mult), reads=[p.res, rstd.res], writes=[y.res])
        if k == "norm":
            ob = ob_r[n % R]
            s.op("act", lambda: nc.scalar.activation(out=ob[:], in_=y[:], func=AF.Copy, scale=gain[:, ch:ch + 1]),
                 reads=[y.res, gain.res], writes=[ob.res])
            s.dma(proj_d[ch, :, sl], ob[:], reads=[ob.res], is_output=True)
        else:
            y2 = y2_r[n % R]
            s.op("act", lambda: nc.scalar.activation(out=y2[:], in_=y[:], func=AF.Copy, scale=gain[:, ch:ch + 1]),
                 reads=[y.res, gain.res], writes=[y2.res])
            s.op("pe", lambda: nc.tensor.matmul(pq[n % 2][:], lhsT=pm[:], rhs=y2[:], start=True, stop=True),
                 reads=[pm.res, y2.res], writes=[pq[n % 2].res])
            if ch in (0, 1):
                s.dma(proj_d[24 + ch, :, sl], y2[:], reads=[y2.res], is_output=True)

    def st3(n):
        ch, tt = items[n]
        if kind(ch) != "rope":
            return
        sl = slice(tt * TT, (tt + 1) * TT)
        y2 = y2_r[n % R]
        t1 = t1_r[n % R]
        y = y_r[n % R]
        ob = ob_r[n % R]
        s.op("pool", lambda: nc.gpsimd.tensor_tensor(out=t1[:], in0=y2[:], in1=COS[:, sl], op=ALU.mult), reads=[y2.res, COS.res], writes=[t1.res])
        s.op("dve", lambda: nc.vector.tensor_tensor(out=y[:], in0=pq[n % 2][:], in1=SIN[:, sl], op=ALU.mult),
             reads=[pq[n % 2].res, SIN.res], writes=[y.res])
        s.op("dve", lambda: nc.vector.tensor_tensor(out=ob[:], in0=t1[:], in1=y[:], op=ALU.add), reads=[t1.res, y.res], writes=[ob.res])
        s.dma(proj_d[ch, :, sl], ob[:], reads=[ob.res], is_output=True)

    pipeline(len(items), [stl, stc, st0, st1, st2, st3])
    c.end_phase(ph)
    if own:
        c.close()
    return c


def in_perm():
    Z = [-1] * 64
    r = lambda a, n: list(range(a, a + n))
    ch = []
    ch.append(r(0, 128)); ch.append(r(128, 128))
    ch.append(r(384, 64) + r(512, 64))
    ch.append(r(256, 64) + Z)
    ch.append(r(652, 128)); ch.append(r(780, 128))
    ch.append(r(908, 128)); ch.append(r(1036, 128))
    ch.append(r(2188, 128)); ch.append(r(2316, 128))
    ch.append(r(2444, 128)); ch.append(r(2572, 128))
    ch.append(r(320, 64) + r(448, 64))
    ch.append(r(576, 64) + Z)
    ch.append(r(1164, 128)); ch.append(r(1292, 128))
    ch.append(r(1420, 128)); ch.append(r(1548, 128))
    ch.append(r(1676, 128)); ch.append(r(1804, 128))
    ch.append(r(1932, 128)); ch.append(r(2060, 128))
    ch.append(r(2700, 128)); ch.append(r(2828, 128))
    ch.append(r(640, 12) + [-1] * 20 + r(2956, 4) + [-1] * 92)
    for g in range(32):
        ch.append(r(2960 + g * 128, 128))
    return np.array(ch, np.int64)


def consts_A():
    inv = (500000.0 ** (-np.arange(0, 16, 2, dtype=np.float32) / 16)).astype(np.float32)
    invf = np.zeros((128, 1), np.float32)
    pm = np.zeros((128, 128), np.float32)
    bones = np.zeros((128, 128), np.float32)
    for hb in (0, 64):
        bones[hb:hb + 64, hb:hb + 64] = 1.0
        for i in range(8):
            invf[hb + i, 0] = inv[i]
            invf[hb + 8 + i, 0] = inv[i]
            pm[hb + i + 8, hb + i] = -1.0
            pm[hb + i, hb + i + 8] = 1.0
    osc = np.ones((128, 12), np.float32)
    for chn in (0, 1, 4, 5, 8, 9):
        osc[:, chn] = 0.125
    return invf, pm, bones, osc


def maps_A(inp, l, xT_all, mod):
    perm = in_perm()
    w = inp["w_in"][l]
    wz = np.concatenate([w, np.zeros((D, 1), np.float32)], axis=1)
    wp = wz[:, perm.reshape(-1)].reshape(D, NCH_IN, 128)
    wp = np.ascontiguousarray(wp.reshape(8, 128, NCH_IN, 128).transpose(2, 1, 0, 3))
    invf, pm, bones, osc = consts_A()
    g = inp["qk_gain"][l]
    t2 = lambda a: np.concatenate([a, a])
    z64 = np.zeros(64, np.float32)
    gain = np.stack([t2(g[0]), t2(g[0]), np.concatenate([g[2], g[3]]), np.concatenate([g[1], z64]),
                     t2(g[4]), t2(g[4]), t2(g[5]), t2(g[5]), t2(g[6]), t2(g[6]), t2(g[7]), t2(g[7])], axis=1).astype(np.float32)
    foxb = np.zeros((128, 1), np.float32)
    foxb[32:36, 0] = inp["fox_bias"][l]
    gn = np.ascontiguousarray(inp["norm_mix"][l].reshape(8, 128).T)
    maps = []
    for i in range(8):
        b, q = i // 4, i % 4
        m = {"modA": np.ascontiguousarray(mod[l, b][:, 0:16]), "gn": gn,
             "pos": np.ascontiguousarray(inp["positions"][b:b + 1, q * NT:(q + 1) * NT]).astype(np.int32),
             "invf": invf, "w": wp, "gain": gain, "osc": osc, "foxb": foxb, "pm": pm, "bones": bones}
        if xT_all is not None:
            xs = xT_all[b][:, q * NT:(q + 1) * NT].reshape(8, 128, NT).transpose(1, 0, 2)
            m["xT"] = np.ascontiguousarray(xs)
        maps.append(m)
    return maps


def collect_A(results, pre=""):
    proj = [np.concatenate([results[b * 4 + q][pre + "proj"] for q in range(4)], axis=2) for b in range(2)]
    gates = [np.concatenate([results[b * 4 + q][pre + "gates"] for q in range(4)], axis=2) for b in range(2)]
    misc = [np.concatenate([results[b * 4 + q][pre + "misc"] for q in range(4)], axis=1) for b in range(2)]
    return proj, gates, misc


def run_A(cA, inp, l, xT_all, mod):
    maps = maps_A(inp, l, xT_all, mod)
    res = run_bass_kernel_spmd(cA.nc, maps, core_ids=list(range(8)))
    return collect_A(res.results)


B_MIXERS = ("dil", "fox", "sb", "nsa")
NKB = S // 128
NQT = S // TT
M_CAUSAL, M_STRICT, M_WIN, M_DIL, M_CMP, M_NEGC = 0, 4, 8, 16, 36, 41
N_MASKS = 45


def consts_B():
    import ml_dtypes
    k = np.arange(128)[:, None]
    cc = np.arange(512)[None, :]
    masks = np.zeros((N_MASKS, 128, 512), np.float32)
    for i in range(4):
        masks[M_CAUSAL + i] = (cc - k >= 128 * i)
        masks[M_STRICT + i] = (cc - k > 128 * i)
    for w in range(8):
        diff = cc - k + 512 - 128 * w
        masks[M_WIN + w] = (diff >= 0) & (diff < 512)
    for w in range(20):
        diff = cc - k + 2048 - 128 * w
        m = np.zeros((128, 512), np.float32)
        for (ww, d) in ((128, 1), (512, 4), (2048, 16)):
            m += ((diff % d == 0) & (diff >= 0) & (diff <= ww))
        masks[M_DIL + w] = m
    for u in range(5):
        masks[M_CMP + u] = (16 * k + 31 <= 512 * u + cc)
    for i in range(4):
        masks[M_NEGC + i] = -30000.0 * (cc - k < 128 * i)
    G = ((np.arange(S)[None, :] // 64) % 64 == np.arange(64)[:, None]).astype(np.float32)
    c0 = np.arange(511) * 16
    s0 = np.arange(128) * 64
    ov = ((c0[:, None] < s0[None, :] + 64) & (c0[:, None] + 32 > s0[None, :])).astype(np.float32)
    ov = np.concatenate([ov, np.zeros((1, 128), np.float32)], 0).reshape(4, 128, 128).transpose(1, 0, 2)
    onesc = np.ones((128, 4, 1), np.float32)
    onesc[127, 3, 0] = 0.0
    Rconst = np.concatenate([ov, onesc], axis=2)
    add = np.zeros((128, 254), np.float32)
    jj = np.arange(254)[None, :] - 126
    cr = (np.arange(128) // 64)[:, None]
    add[(jj == cr) | (jj == cr - 1)] = 1e30
    add[jj > cr] = -1e30
    jn = np.arange(128)[:, None]
    kn = np.arange(128)[None, :]
    nti = -(jn >= kn).astype(np.float32)
    ntc = -(jn < kn).astype(np.float32)
    bf = ml_dtypes.bfloat16
    return dict(masks=masks.astype(bf), G64=G.astype(bf), Rconst=Rconst.astype(bf), add=add,
                nti=nti.astype(bf), ntc=ntc.astype(bf), ident=np.eye(128, dtype=np.float32),
                tri64=(np.arange(64)[:, None] < np.arange(64)[None, :]).astype(np.float32))


def pipeline(n_items, stages):
    K = len(stages)
    for i in range(n_items + K - 1):
        for k, st in enumerate(stages):
            n = i - k
            if 0 <= n < n_items:
                st(n)


def build_B():
    c = Ctx()
    nc, s = c.nc, c.s
    di = lambda name, shape, dt: c.dram(name, shape, dt, "ExternalInput")
    qnr_d = di("qnr", [4, 64, S], BF16)
    qr_d = di("qr", [64, S], BF16)
    kcT_d = di("kcT", [64, S], BF16)
    vcT_d = di("vcT", [64, S], BF16)
    kslT_d = di("kslT", [64, S], BF16)
    kwT_d = di("kwT", [64, S], BF16)
    vsl_d = di("vsl", [128, NKB, 65], BF16)
    vw_d = di("vw", [128, NKB, 65], BF16)
    ag_d = di("ag", [128, NKB, 3], F32)
    dq_d = di("dq", [64, S], BF16)
    dk_d = di("dk", [64, S], BF16)
    dv_d = di("dv", [128, NKB, 65], BF16)
    sq_d = di("sq", [64, S], BF16)
    sk_d = di("sk", [64, S], BF16)
    sv_d = di("sv", [128, NKB, 65], BF16)
    fq_d = di("fq", [64, S], BF16)
    fk_d = di("fk", [64, S], BF16)
    fv_d = di("fv", [128, NKB, 65], BF16)
    lf_d = di("logf", [1, S], F32)
    w1k_d = di("w1k", [64, 32, 128], F32)
    w1v_d = di("w1v", [64, 32, 128], F32)
    w2k_d = di("w2k", [128, 64], F32)
    w2v_d = di("w2v", [128, 64], F32)
    pek_d = di("pek", [64, 32], F32)
    pev_d = di("pev", [64, 32], F32)
    masks_d = di("masks", [N_MASKS, 128, 512], BF16)
    G_d = di("G64", [64, S], BF16)
    Rc_d = di("Rconst", [128, 4, 129], BF16)
    add_d = di("add", [128, 254], F32)
    nti_d = di("nti", [128, 128], BF16)
    ntc_d = di("ntc", [128, 128], BF16)
    ident_d = di("ident", [128, 128], F32)
    tri_d = di("tri64", [64, 64], F32)
    o_d = c.dram("o", [4, 128, NKB, 64], BF16, "ExternalOutput")

    ps_ring = [c.ps([128, 512]) for _ in range(4)]
    po_ring = [c.ps([128, 512]) for _ in range(2)]
    pX = c.ps([128, 512])
    pY = c.ps([128, 512])
    e_ring = [c.sb([128, 512], BF16) for _ in range(4)]
    p_ring = [c.sb([128, 512], BF16) for _ in range(4)]
    pre_ring = [c.sb([128, 512], F32) for _ in range(4)]
    rz_ring = [c.sb([128, 4], F32) for _ in range(2)]
    fac_ring = [c.sb([128, 4], F32) for _ in range(2)]
    ost = c.sb([128, NKB, 64], BF16)

    class Scope:
        def __enter__(self):
            self.es = contextlib.ExitStack()
            self.old = c.es
            c.es = self.es
            return self

        def __exit__(self, *a):
            barrier(c)
            c.es = self.old
            self.es.close()

    def load_masks(lo, n, order=None):
        t = c.sb([128, n, 512], BF16)
        t.parts = [Res() for _ in range(n)]
        for ii, i in enumerate(order if order is not None else range(n)):
            s.dma(t[:, i, :], masks_d[lo + i], writes=[t.parts[i]], q=("sp", "act")[ii % 2])
        return t

    def split_load(QT, KT, V, qT_d, kT_d, v_d):
        qs = ("sp", "act")
        for t in (QT, KT, V):
            if t is not None:
                t.parts = [Res() for _ in range(4)]
        for h in range(4):
            sl = slice(h * 2048, (h + 1) * 2048)
            if QT is not None:
                s.dma(QT[0:64, sl], qT_d[:, sl], reads=[QT.res], writes=[QT.parts[h]], q=qs[h % 2])
            s.dma(KT[0:64, sl], kT_d[:, sl], reads=[KT.res], writes=[KT.parts[h]], q=qs[(h + 1) % 2])
            s.dma(V[:, 16 * h:16 * (h + 1), :], v_d[:, 16 * h:16 * (h + 1), :], reads=[V.res], writes=[V.parts[h]], q=qs[h % 2])

    def rQ(QT, qt):
        return [QT.res, QT.parts[qt // 4]]

    def rK(KT, kb):
        return [KT.res, KT.parts[kb // 16]]

    def attn(items, qk, pv, fin, mask_of, alt=[0], nps=4):
        def st0(n):
            qk(n, ps_ring[n % nps])

        def st1(n):
            it = items[n]
            ps, e = ps_ring[n % nps], e_ring[n % 4]
            if it["mi"] is not None and it["mi"] >= M_NEGC:
                mt, mi = mask_of(it["mi"])
                pre = pre_ring[n % 4]
                s.op("dve", lambda: nc.vector.tensor_tensor(out=pre[:], in0=ps[:], in1=mt[:, mi, :], op=ALU.add),
                     reads=[ps.res, mt.parts[mi]], writes=[pre.res])
                p = p_ring[n % 4]
                s.op("act", lambda: nc.scalar.activation(out=p[:], in_=pre[:], func=AF.Exp), reads=[pre.res], writes=[p.res])
                return
            s.op("act", lambda: nc.scalar.activation(out=e[:], in_=ps[:], func=AF.Exp), reads=[ps.res], writes=[e.res])
            if it["mi"] is not None:
                p = p_ring[n % 4]
                mt, mi = mask_of(it["mi"])
                alt[0] = 1
                if alt[0]:
                    s.op("dve", lambda: nc.vector.tensor_tensor(out=p[:], in0=e[:], in1=mt[:, mi, :], op=ALU.mult),
                         reads=[e.res, mt.parts[mi]], writes=[p.res])
                else:
                    s.op("pool", lambda: nc.gpsimd.tensor_tensor(out=p[:], in0=e[:], in1=mt[:, mi, :], op=ALU.mult),
                         reads=[e.res, mt.parts[mi]], writes=[p.res])

        def st2(n):
            it = items[n]
            pt = p_ring[n % 4] if it["mi"] is not None else e_ring[n % 4]
            pv(n, pt)
            if it["last"]:
                fin(n)
        pipeline(len(items), [st0, st1, (lambda n: None), st2])

    def std_pv(items, V, ncol=65):
        def pv(n, pt):
            it = items[n]
            po = po_ring[it["qt"] % 2]
            for qb in range(4):
                s.op("pe", (lambda qb=qb: nc.tensor.matmul(po[:, qb * 65:qb * 65 + ncol], lhsT=pt[:, qb * 128:(qb + 1) * 128],
                                                           rhs=V[:, it["kb"], 0:ncol], start=(it["first"] and qb == 0), stop=it["last"],
                                                           skip_group_check=True)),
                     reads=[pt.res, V.res, V.parts[it["kb"] // 16]], writes=[po.res])
        return pv

    def rz_of(qt, po, zoff=64, stride=65):
        rz = rz_ring[qt % 2]
        for qb in range(4):
            s.op("dve", (lambda qb=qb: nc.vector.tensor_scalar(out=rz[:, qb:qb + 1], in0=po[:, qb * stride + zoff:qb * stride + zoff + 1],
                                                               scalar1=1e-30, scalar2=None, op0=ALU.max)),
                 reads=[po.res], writes=[rz.res])
        s.op("dve", lambda: nc.vector.reciprocal(out=rz[:], in_=rz[:]), reads=[rz.res], writes=[rz.res])
        return rz

    def std_items(blocks_of):
        items = []
        for qt in range(NQT):
            bl = blocks_of(qt)
            for ii, (kb, mi) in enumerate(bl):
                items.append(dict(qt=qt, kb=kb, mi=mi, first=(ii == 0), last=(ii == len(bl) - 1)))
        return items

    def simple_mixer(m, qT_d, kT_d, v_d, blocks_of, mask_lo, mask_n, kdim=64, prep=None, mask_order=None):
        with Scope():
            QT = c.sb([128, S], BF16)
            KT = c.sb([128, S], BF16)
            V = c.sb([128, NKB, 65], BF16)
            if prep is not None:
                prep(QT, KT)
            split_load(QT, KT, V, qT_d, kT_d, v_d)
            mt = load_masks(mask_lo, mask_n, order=mask_order)
            items = std_items(blocks_of)

            def qk(n, ps):
                it = items[n]
                s.op("pe", lambda: nc.tensor.matmul(ps[:], lhsT=KT[0:kdim, it["kb"] * 128:(it["kb"] + 1) * 128],
                                                    rhs=QT[0:kdim, it["qt"] * 512:(it["qt"] + 1) * 512], start=True, stop=True),
                     reads=rK(KT, it["kb"]) + rQ(QT, it["qt"]), writes=[ps.res])

            def fin(n):
                qt = items[n]["qt"]
                po = po_ring[qt % 2]
                rz = rz_of(qt, po)
                for qb in range(4):
                    s.op("dve", (lambda qb=qb: nc.vector.tensor_scalar(out=ost[:, 4 * qt + qb, :], in0=po[:, qb * 65:qb * 65 + 64],
                                                                       scalar1=rz[:, qb:qb + 1], scalar2=None, op0=ALU.mult)),
                         reads=[po.res, rz.res], writes=[ost.res])
            attn(items, qk, std_pv(items, V), fin, lambda mi: (mt, mi - mask_lo))
            s.dma(o_d[m], ost[:], reads=[ost.res], is_output=True)

    def dil_blocks(qt):
        return [(4 * qt - 16 + w, M_DIL + w) for w in range(20) if 4 * qt - 16 + w >= 0]
    if "dil" in B_MIXERS:
        simple_mixer(1, dq_d, dk_d, dv_d, dil_blocks, M_DIL, 20, mask_order=[16, 17, 18, 19, 12, 13, 14, 15, 8, 9, 10, 11, 4, 5, 6, 7, 0, 1, 2, 3])

    fs_d = c.dram("fsplit", [6, S], BF16, "Internal")
    fs_res = Res()

    def fox_prep(QT, KT):
        lf = c.sb([64, 128], F32)
        F = c.sb([64, 128], F32)
        zr = c.sb([64, 128], F32)
        r1 = c.sb([64, 128], F32)
        off = c.sb([64, 1], F32)
        U = c.sb([64, 64], F32)
        sp3 = [c.sb([64, 128], BF16) for _ in range(3)]
        ng3 = [c.sb([64, 128], BF16) for _ in range(3)]
        s.dma(lf[:], lf_d.rearrange("o (p j) -> (o p) j", j=128), writes=[lf.res])
        s.dma(U[:], tri_d, writes=[U.res])
        s.op("pool", lambda: nc.gpsimd.memset(zr[:], 0.0), writes=[zr.res])
        s.op("pool", lambda: nc.gpsimd.memset(QT[:], 0.0), writes=[QT.res])
        s.op("pool", lambda: nc.gpsimd.memset(KT[:], 0.0), writes=[KT.res])
        s.op("pool", lambda: nc.gpsimd.memset(QT[96:99, :], 1.0), writes=[QT.res])
        s.op("pool", lambda: nc.gpsimd.memset(KT[64:67, :], 1.0), writes=[KT.res])
        s.op("dve", lambda: nc.vector.tensor_tensor_scan(out=F[:], data0=lf[:], data1=zr[:], initial=0.0, op0=ALU.add, op1=ALU.add),
             reads=[lf.res, zr.res], writes=[F.res])
        s.op("pe", lambda: nc.tensor.matmul(pY[0:64, 0:1], lhsT=U[:], rhs=F[:, 127:128], start=True, stop=True),
             reads=[U.res, F.res], writes=[pY.res])
        s.op("act", lambda: nc.scalar.copy(out=off[:], in_=pY[0:64, 0:1]), reads=[pY.res], writes=[off.res])
        s.op("dve", lambda: nc.vector.tensor_scalar(out=F[:], in0=F[:], scalar1=off[:, 0:1], scalar2=None, op0=ALU.add),
             reads=[F.res, off.res], writes=[F.res])
        cur = F
        for i in range(3):
            s.op("dve", (lambda i=i, cur=cur: nc.vector.tensor_copy(out=sp3[i][:], in_=cur[:])), reads=[cur.res], writes=[sp3[i].res])
            s.op("dve", (lambda i=i: nc.vector.tensor_scalar(out=ng3[i][:], in0=sp3[i][:], scalar1=-1.0, scalar2=None, op0=ALU.mult)),
                 reads=[sp3[i].res], writes=[ng3[i].res])
            if i < 2:
                s.op("dve", (lambda i=i, cur=cur: nc.vector.tensor_tensor(out=r1[:], in0=cur[:], in1=sp3[i][:], op=ALU.subtract)),
                     reads=[cur.res, sp3[i].res], writes=[r1.res])
                cur = r1
            s.dma(fs_d[i, :].rearrange("(p j) -> p j", j=128), sp3[i][:], reads=[sp3[i].res], writes=[fs_res])
            s.dma(fs_d[3 + i, :].rearrange("(p j) -> p j", j=128), ng3[i][:], reads=[ng3[i].res], writes=[fs_res])
        s.dma(QT[64:67, :], fs_d[0:3, :], reads=[fs_res], writes=[QT.res])
        s.dma(KT[96:99, :], fs_d[3:6, :], reads=[fs_res], writes=[KT.res])

    def causal_blocks(qt):
        return [(kb, None) for kb in range(4 * qt)] + [(4 * qt + i, M_CAUSAL + i) for i in range(4)]

    def negc_blocks(qt):
        return [(kb, None) for kb in range(4 * qt)] + [(4 * qt + i, M_NEGC + i) for i in range(4)]
    if "fox" in B_MIXERS:
        simple_mixer(3, fq_d, fk_d, fv_d, negc_blocks, M_NEGC, 4, kdim=99, prep=fox_prep)

    with (Scope() if "sb" in B_MIXERS else contextlib.nullcontext()):
      if "sb" in B_MIXERS:
        QT = c.sb([64, S], BF16)
        KT = c.sb([64, S], BF16)
        V = c.sb([128, NKB, 65], BF16)
        nti = c.sb([128, 128], BF16)
        ntc = c.sb([128, 128], BF16)
        split_load(QT, KT, V, sq_d, sk_d, sv_d)
        s.dma(nti[:], nti_d, writes=[nti.res])
        s.dma(ntc[:], ntc_d, writes=[ntc.res])
        mt = load_masks(M_STRICT, 4)
        E_r = [c.sb([128, 512], F32) for _ in range(4)]
        L_r = [c.sb([128, 512], BF16) for _ in range(4)]
        X_r = [c.sb([128, 512], F32) for _ in range(4)]
        A_r = [c.sb([128, 512], BF16) for _ in range(4)]
        def sb_blocks(qt):
            bl = [(4 * qt + i, M_STRICT + i) for i in (3, 2, 1, 0)] + [(kb, None) for kb in range(4 * qt - 1, -1, -1)]
            return [dict(qt=qt, kb=kb, mi=mi, first=(ii == 0), last=(ii == len(bl) - 1)) for ii, (kb, mi) in enumerate(bl)]
        items = []
        for pr in range(NQT // 2):
            la, lb = sb_blocks(2 * pr), sb_blocks(2 * pr + 1)
            for ii in range(len(lb)):
                if ii < len(la):
                    items.append(la[ii])
                items.append(lb[ii])
        pXs = (pX, pY)

        def sb0(n):
            it = items[n]
            ps = ps_ring[n % 4]
            s.op("pe", lambda: nc.tensor.matmul(ps[:], lhsT=KT[:, it["kb"] * 128:(it["kb"] + 1) * 128],
                                                rhs=QT[:, it["qt"] * 512:(it["qt"] + 1) * 512], start=True, stop=True),
                 reads=rK(KT, it["kb"]) + rQ(QT, it["qt"]), writes=[ps.res])

        def sb1(n):
            it = items[n]
            ps, E, L = ps_ring[n % 4], E_r[n % 4], L_r[n % 4]
            s.op("act", lambda: nc.scalar.activation(out=E[:], in_=ps[:], func=AF.Exp), reads=[ps.res], writes=[E.res])
            s.op("act", lambda: nc.scalar.activation(out=L[:], in_=E[:], func=AF.Ln, bias=1.0, scale=1.0), reads=[E.res], writes=[L.res])
            if it["mi"] is not None:
                mi = it["mi"] - M_STRICT
                s.op("pool", lambda: nc.gpsimd.tensor_tensor(out=L[:], in0=L[:], in1=mt[:, mi, :], op=ALU.mult),
                     reads=[L.res, mt.parts[mi]], writes=[L.res])
                s.op("dve", lambda: nc.vector.tensor_tensor(out=E[:], in0=E[:], in1=mt[:, mi, :], op=ALU.mult),
                     reads=[E.res, mt.parts[mi]], writes=[E.res])

        def sb2(n):
            it = items[n]
            L, X = L_r[n % 4], X_r[n % 4]
            pXq = pXs[it["qt"] % 2]
            s.op("pe", lambda: nc.tensor.matmul(pXq[:], lhsT=nti[:], rhs=L[:], start=it["first"], stop=False, skip_group_check=True),
                 reads=[nti.res, L.res], writes=[pXq.res])
            s.op("act", lambda: nc.scalar.activation(out=X[:], in_=pXq[:], func=AF.Exp), reads=[pXq.res], writes=[X.res])

        def sb3(n):
            it = items[n]
            L, X, E, A = L_r[n % 4], X_r[n % 4], E_r[n % 4], A_r[n % 4]
            pXq = pXs[it["qt"] % 2]
            s.op("pe", lambda: nc.tensor.matmul(pXq[:], lhsT=ntc[:], rhs=L[:], start=False, stop=it["last"], skip_group_check=True),
                 reads=[ntc.res, L.res], writes=[pXq.res])
            s.op("dve", lambda: nc.vector.tensor_tensor(out=A[:], in0=E[:], in1=X[:], op=ALU.mult), reads=[E.res, X.res], writes=[A.res])

        def sb4(n):
            it = items[n]
            A = A_r[n % 4]
            qt = it["qt"]
            po = po_ring[qt % 2]
            for qb in range(4):
                s.op("pe", (lambda qb=qb: nc.tensor.matmul(po[:, qb * 65:qb * 65 + 64], lhsT=A[:, qb * 128:(qb + 1) * 128],
                                                           rhs=V[:, it["kb"], 0:64], start=(it["first"] and qb == 0), stop=it["last"],
                                                           skip_group_check=True)),
                     reads=[A.res, V.res, V.parts[it["kb"] // 16]], writes=[po.res])
            if it["last"]:
                for qb in range(4):
                    s.op("act", (lambda qb=qb: nc.scalar.copy(out=ost[:, 4 * qt + qb, :], in_=po[:, qb * 65:qb * 65 + 64])),
                         reads=[po.res], writes=[ost.res])
        K_ = len(items)
        for i in range(K_ + 4):
            if i < K_:
                sb0(i)
            if 0 <= i - 1 < K_:
                sb1(i - 1)
            if 0 <= i - 3 < K_:
                sb3(i - 3)
            if 0 <= i - 2 < K_:
                sb2(i - 2)
            if 0 <= i - 4 < K_:
                sb4(i - 4)
        s.dma(o_d[2], ost[:], reads=[ost.res], is_output=True)

    hsel_d = di("hsel", [128, 4], F32)
    with (Scope() if "nsa" in B_MIXERS else contextlib.nullcontext()):
      if "nsa" in B_MIXERS:
        QA = c.sb([128, S], BF16)
        QB = c.sb([128, S], BF16)
        oa = c.sb([128, NKB, 64], F32)
        ag = c.sb([128, NKB, 3], F32)
        ident = c.sb([128, 128], F32)
        addt = c.sb([128, 254], F32)
        hsel = c.sb([128, 4], F32)
        for h in range(4):
            sl = slice(h * 2048, (h + 1) * 2048)
            s.dma(QA[0:64, sl], qr_d[:, sl], writes=[QA.res], q="act")
            s.dma(QB[0:64, sl], qr_d[:, sl], writes=[QB.res], q="act")
        for t, d in ((ag, ag_d), (ident, ident_d), (addt, add_d), (hsel, hsel_d)):
            s.dma(t[:], d, writes=[t.res])
        with Scope():
            kcT = c.sb([64, S], BF16)
            vcT = c.sb([64, S], BF16)
            for h in range(4):
                sl = slice(h * 2048, (h + 1) * 2048)
                s.dma(kcT[:, sl], kcT_d[:, sl], writes=[kcT.res])
                s.dma(vcT[:, sl], vcT_d[:, sl], writes=[vcT.res])
            kccT = c.sb([64, 512], BF16)
            Rt = c.sb([128, 4, 193], BF16)
            s.dma(Rt[:, :, 0:129], Rc_d, writes=[Rt.res])
            w1f = c.sb([64, 32, 128], F32)
            w1b = c.sb([64, 32, 128], BF16)
            w2f = c.sb([128, 64], F32)
            w2b = c.sb([128, 64], BF16)
            pef = c.sb([64, 32], F32)
            peb = c.sb([128, 1], F32)
            xg = c.sb([128, 512], F32)
            x2 = c.sb([128, 512], F32)
            gT = c.sb([128, 512], BF16)
            for which in ("k", "v"):
                src = kcT if which == "k" else vcT
                s.dma(w1f[:], w1k_d if which == "k" else w1v_d, writes=[w1f.res])
                s.dma(w2f[:], w2k_d if which == "k" else w2v_d, writes=[w2f.res])
                s.dma(pef[:], pek_d if which == "k" else pev_d, writes=[pef.res])
                s.op("pool", lambda: nc.gpsimd.tensor_copy(out=w1b[:], in_=w1f[:]), reads=[w1f.res], writes=[w1b.res])
                s.op("pool", lambda: nc.gpsimd.tensor_copy(out=w2b[:], in_=w2f[:]), reads=[w2f.res], writes=[w2b.res])
                for l in range(32):
                    s.op("pe", (lambda l=l: nc.tensor.matmul(pY[:, 0:1], lhsT=w1f[:, l, :], rhs=pef[:, l:l + 1], start=(l == 0), stop=(l == 31))),
                         reads=[w1f.res, pef.res], writes=[pY.res])
                s.op("act", lambda: nc.scalar.copy(out=peb[:], in_=pY[:, 0:1]), reads=[pY.res], writes=[peb.res])
                srcv = src[:].rearrange("p (c s) -> p c s", s=16)
                for l in range(32):
                    rhs = srcv[:, 0:511, l] if l < 16 else srcv[:, 1:512, l - 16]
                    s.op("pe", (lambda l=l, rhs=rhs: nc.tensor.matmul(pX[:, 0:511], lhsT=w1b[:, l, :], rhs=rhs, start=(l == 0), stop=(l == 31))),
                         reads=[w1b.res, src.res], writes=[pX.res])
                s.op("act", lambda: nc.scalar.activation(out=xg[:, 0:511], in_=pX[:, 0:511], func=AF.Identity, bias=peb[:], scale=1.0),
                     reads=[pX.res, peb.res], writes=[xg.res])
                s.op("dve", lambda: nc.vector.tensor_tensor(out=x2[:, 0:511], in0=xg[:, 0:511], in1=xg[:, 0:511], op=ALU.mult), reads=[xg.res], writes=[x2.res])
                s.op("dve", lambda: nc.vector.tensor_scalar(out=x2[:, 0:511], in0=x2[:, 0:511], scalar1=0.044715, scalar2=1.0, op0=ALU.mult, op1=ALU.add),
                     reads=[x2.res], writes=[x2.res])
                s.op("dve", lambda: nc.vector.tensor_tensor(out=x2[:, 0:511], in0=x2[:, 0:511], in1=xg[:, 0:511], op=ALU.mult), reads=[x2.res, xg.res], writes=[x2.res])
                s.op("act", lambda: nc.scalar.activation(out=x2[:, 0:511], in_=x2[:, 0:511], func=AF.Sigmoid, scale=1.5957691216057308),
                     reads=[x2.res], writes=[x2.res])
                s.op("pool", lambda: nc.gpsimd.memset(gT[:], 0.0), writes=[gT.res])
                s.op("dve", lambda: nc.vector.tensor_tensor(out=gT[:, 0:511], in0=xg[:, 0:511], in1=x2[:, 0:511], op=ALU.mult), reads=[xg.res, x2.res, gT.res], writes=[gT.res])
                if which == "k":
                    s.op("pe", lambda: nc.tensor.matmul(pY[0:64, :], lhsT=w2b[:], rhs=gT[:], start=True, stop=True), reads=[w2b.res, gT.res], writes=[pY.res])
                    s.op("act", lambda: nc.scalar.copy(out=kccT[:], in_=pY[0:64, :]), reads=[pY.res], writes=[kccT.res])
                else:
                    for cc in range(4):
                        s.op("pe", (lambda cc=cc: nc.tensor.matmul(pY[:, cc * 64:(cc + 1) * 64], lhsT=gT[:, cc * 128:(cc + 1) * 128], rhs=w2b[:],
                                                                   start=True, stop=True)),
                             reads=[w2b.res, gT.res], writes=[pY.res])
                    s.op("act", lambda: nc.scalar.copy(out=Rt[:, :, 129:193], in_=pY[:, 0:256].rearrange("p (a b) -> p a b", b=64)),
                         reads=[pY.res], writes=[Rt.res])
            mt = load_masks(M_CMP, 5)
            qring = [c.sb([64, 512], BF16) for _ in range(4)]
            imp = [c.sb([128, 4, 128], F32) for _ in range(2)]
            sc_r = [c.sb([128, 128], F32) for _ in range(4)]
            sc2_r = [c.sb([128, 128], F32) for _ in range(4)]
            sb_r = [c.sb([128, 128], F32) for _ in range(4)]
            sbr_r = [c.sb([128, 128], F32) for _ in range(4)]
            for t in sbr_r:
                s.op("pool", (lambda t=t: nc.gpsimd.memset(t[:], 0.0)), writes=[t.res])
            m8_r = [c.sb([128, 16], F32) for _ in range(4)]
            pT = ps_ring[3]
            usets = [(po_ring[0], po_ring[1]), (pX, pY)]
            items = []
            for qt in range(NQT):
                for h in range(4):
                    ccs = [cc for cc in range(4) if qt - 4 * cc >= 0]
                    for ii, cc in enumerate(ccs):
                        u = qt - 4 * cc
                        items.append(dict(qt=qt, h=h, kb=cc, mi=(M_CMP + u if u <= 4 else None), first=(ii == 0), last=(ii == len(ccs) - 1)))

            def qk_c(n, ps):
                it = items[n]
                qtile = qring[(it["qt"] * 4 + it["h"]) % 4]
                if it["first"]:
                    s.dma(qtile[:], qnr_d[it["h"], :, it["qt"] * 512:(it["qt"] + 1) * 512], writes=[qtile.res])
                s.op("pe", lambda: nc.tensor.matmul(ps[:], lhsT=kccT[:, it["kb"] * 128:(it["kb"] + 1) * 128], rhs=qtile[:], start=True, stop=True),
                     reads=[kccT.res, qtile.res], writes=[ps.res])

            def pv_c(n, pt):
                it = items[n]
                us = usets[(it["qt"] * 4 + it["h"]) % 2]
                for qb in range(4):
                    tl = us[qb // 2]
                    off = (qb % 2) * 193
                    s.op("pe", (lambda qb=qb, tl=tl, off=off: nc.tensor.matmul(tl[:, off:off + 193], lhsT=pt[:, qb * 128:(qb + 1) * 128], rhs=Rt[:, it["kb"], :],
                                                                             start=(it["first"] and qb % 2 == 0), stop=it["last"], skip_group_check=True)),
                         reads=[pt.res, Rt.res], writes=[tl.res])

            def fin_c(n):
                it = items[n]
                qt, h = it["qt"], it["h"]
                us = usets[(qt * 4 + h) % 2]
                rz = rz_ring[h % 2]
                fac = fac_ring[h % 2]
                imp_t = imp[qt % 2]
                for qb in range(4):
                    tl, off = us[qb // 2], (qb % 2) * 193
                    s.op("dve", (lambda qb=qb, tl=tl, off=off: nc.vector.tensor_scalar(out=rz[:, qb:qb + 1], in0=tl[:, off + 128:off + 129], scalar1=1e-30,
                                                                                     scalar2=None, op0=ALU.max)), reads=[tl.res], writes=[rz.res])
                s.op("dve", lambda: nc.vector.reciprocal(out=rz[:], in_=rz[:]), reads=[rz.res], writes=[rz.res])
                s.op("dve", lambda: nc.vector.tensor_tensor(out=fac[:], in0=rz[:], in1=ag[:, 4 * qt:4 * qt + 4, 0], op=ALU.mult), reads=[rz.res, ag.res], writes=[fac.res])
                s.op("dve", lambda: nc.vector.tensor_scalar(out=fac[:], in0=fac[:], scalar1=hsel[:, h:h + 1], scalar2=None, op0=ALU.mult),
                     reads=[fac.res, hsel.res], writes=[fac.res])
                for qb in range(4):
                    tl, off = us[qb // 2], (qb % 2) * 193
                    tb = 4 * qt + qb
                    if h == 0:
                        s.op("dve", (lambda qb=qb, tl=tl, off=off: nc.vector.tensor_scalar(out=imp_t[:, qb, :], in0=tl[:, off:off + 128], scalar1=rz[:, qb:qb + 1],
                                                                                         scalar2=None, op0=ALU.mult)), reads=[tl.res, rz.res], writes=[imp_t.res])
                        s.op("dve", (lambda qb=qb, tl=tl, off=off, tb=tb: nc.vector.tensor_scalar(out=oa[:, tb, :], in0=tl[:, off + 129:off + 193], scalar1=fac[:, qb:qb + 1],
                                                                                                scalar2=None, op0=ALU.mult)), reads=[tl.res, fac.res], writes=[oa.res])
                    else:
                        s.op("dve", (lambda qb=qb, tl=tl, off=off: nc.vector.scalar_tensor_tensor(out=imp_t[:, qb, :], in0=tl[:, off:off + 128], scalar=rz[:, qb:qb + 1],
                                                                                                in1=imp_t[:, qb, :], op0=ALU.mult, op1=ALU.add)),
                             reads=[tl.res, rz.res, imp_t.res], writes=[imp_t.res])
                        s.op("dve", (lambda qb=qb, tl=tl, off=off, tb=tb: nc.vector.scalar_tensor_tensor(out=oa[:, tb, :], in0=tl[:, off + 129:off + 193], scalar=fac[:, qb:qb + 1],
                                                                                                       in1=oa[:, tb, :], op0=ALU.mult, op1=ALU.add)),
                             reads=[tl.res, fac.res, oa.res], writes=[oa.res])
                if h != 3:
                    return
                for qb in range(4):
                    tb = 4 * qt + qb
                    sc, sc2, sbt, m8 = sc_r[qb], sc2_r[qb], sb_r[qb], m8_r[qb]
                    s.op("dve", (lambda qb=qb, sc=sc, tb=tb: nc.vector.tensor_tensor(out=sc[:], in0=imp_t[:, qb, :], in1=addt[:, 126 - 2 * tb:254 - 2 * tb], op=ALU.add)),
                         reads=[imp_t.res, addt.res], writes=[sc.res])
                    s.op("dve", (lambda sc=sc: nc.vector.memset(sc[:, 0:1], 1e30)), reads=[sc.res], writes=[sc.res])
                    s.op("dve", (lambda sc=sc, m8=m8: nc.vector.max(out=m8[:, 0:8], in_=sc[:])), reads=[sc.res], writes=[m8.res])
                    s.op("dve", (lambda sc=sc, sc2=sc2, m8=m8: nc.vector.match_replace(out=sc2[:], in_to_replace=m8[:, 0:8], in_values=sc[:], imm_value=-3e38)),
                         reads=[sc.res, m8.res], writes=[sc2.res])
                    s.op("dve", (lambda sc2=sc2, m8=m8: nc.vector.max(out=m8[:, 8:16], in_=sc2[:])), reads=[sc2.res, m8.res], writes=[m8.res])
                    s.op("dve", (lambda sc=sc, sbt=sbt, m8=m8: nc.vector.tensor_scalar(out=sbt[:], in0=sc[:], scalar1=m8[:, 15:16], scalar2=-30000.0,
                                                                                   op0=ALU.is_lt, op1=ALU.mult)), reads=[sc.res, m8.res], writes=[sbt.res])
                    sbr = sbr_r[qb]
                    s.op("dve", (lambda sc=sc, sbr=sbr, m8=m8: nc.vector.tensor_scalar(out=sbr[:, 64:128], in0=sc[:, 0:64], scalar1=m8[:, 15:16], scalar2=-30000.0,
                                                                                   op0=ALU.is_lt, op1=ALU.mult)), reads=[sc.res, m8.res], writes=[sbr.res])
                for qb in range(4):
                    s.op("pe", (lambda qb=qb: nc.tensor.transpose(pT[:, qb * 128:(qb + 1) * 128], sb_r[qb][:], ident[:])),
                         reads=[sb_r[qb].res, ident.res], writes=[pT.res])
                s.op("act", lambda: nc.scalar.copy(out=QB[64:128, qt * 512:(qt + 1) * 512], in_=pT[64:128, :]), reads=[pT.res], writes=[QB.res])
                for qb in range(4):
                    s.op("pe", (lambda qb=qb: nc.tensor.transpose(pT[:, qb * 128:(qb + 1) * 128], sbr_r[qb][:], ident[:])),
                         reads=[sbr_r[qb].res, ident.res], writes=[pT.res])
                s.op("act", lambda: nc.scalar.copy(out=QA[64:128, qt * 512:(qt + 1) * 512], in_=pT[64:128, :]), reads=[pT.res], writes=[QA.res])
            attn(items, qk_c, pv_c, fin_c, lambda mi: (mt, mi - M_CMP), nps=3)

        with Scope():
            KS = c.sb([128, S], BF16)
            KW = c.sb([64, S], BF16)
            VS = c.sb([128, NKB, 65], BF16)
            VW = c.sb([128, NKB, 65], BF16)
            mtc = load_masks(M_CAUSAL, 4)
            for h in range(4):
                sl = slice(h * 2048, (h + 1) * 2048)
                s.dma(KS[64:128, sl], G_d[:, sl], writes=[KS.res], q="act")
            split_load(None, KS, VS, None, kslT_d, vsl_d)
            split_load(None, KW, VW, None, kwT_d, vw_d)
            mtw = load_masks(M_WIN, 8)

            def branch(KT, V, blocks_of, mt, mask_lo, br, with_sel):
                items = std_items(blocks_of)

                def qk(n, ps):
                    it = items[n]
                    kd = 128 if with_sel else 64
                    Qx = (QA if it["kb"] < 32 else QB) if with_sel else QA
                    s.op("pe", lambda: nc.tensor.matmul(ps[:], lhsT=KT[0:kd, it["kb"] * 128:(it["kb"] + 1) * 128],
                                                        rhs=Qx[0:kd, it["qt"] * 512:(it["qt"] + 1) * 512], start=True, stop=True),
                         reads=rK(KT, it["kb"]) + [Qx.res], writes=[ps.res])

                def fin(n):
                    qt = items[n]["qt"]
                    po = po_ring[qt % 2]
                    rz = rz_of(qt, po)
                    fac = fac_ring[qt % 2]
                    s.op("dve", lambda: nc.vector.tensor_tensor(out=fac[:], in0=rz[:], in1=ag[:, 4 * qt:4 * qt + 4, br], op=ALU.mult),
                         reads=[rz.res, ag.res], writes=[fac.res])
                    for qb in range(4):
                        tb = 4 * qt + qb
                        s.op("dve", (lambda qb=qb, tb=tb: nc.vector.scalar_tensor_tensor(out=oa[:, tb, :], in0=po[:, qb * 65:qb * 65 + 64], scalar=fac[:, qb:qb + 1],
                                                                                         in1=oa[:, tb, :], op0=ALU.mult, op1=ALU.add)),
                             reads=[po.res, fac.res, oa.res], writes=[oa.res])
                attn(items, qk, std_pv(items, V), fin, lambda mi: (mt, mi - mask_lo))
            if "nosel" not in B_MIXERS:
                branch(KS, VS, causal_blocks, mtc, M_CAUSAL, 1, True)
            if "nowin" not in B_MIXERS:
                branch(KW, VW, lambda qt: [(4 * qt - 4 + w, M_WIN + w) for w in range(8) if 4 * qt - 4 + w >= 0], mtw, M_WIN, 2, False)
        s.op("act", lambda: nc.scalar.copy(out=ost[:], in_=oa[:]), reads=[oa.res], writes=[ost.res])
        s.dma(o_d[0], ost[:], reads=[ost.res], is_output=True)
    c.close()
    return c


def run_B(cB, inp, l, proj, misc):
    import ml_dtypes
    bf = ml_dtypes.bfloat16
    cst = consts_B()

    def head_rows(P, ch0, i):
        return np.ascontiguousarray(P[ch0 + i // 2][(i % 2) * 64:(i % 2) * 64 + 64])

    def vaug(vT):
        v = vT.T.reshape(NKB, 128, 64).transpose(1, 0, 2)
        return np.ascontiguousarray(np.concatenate([v, np.ones((128, NKB, 1), bf)], axis=2))
    cl = 32 * 64
    w1k = np.ascontiguousarray(inp["nsa_ck_w1"][l].reshape(32, 64, 128).transpose(1, 0, 2))
    w1v = np.ascontiguousarray(inp["nsa_cv_w1"][l].reshape(32, 64, 128).transpose(1, 0, 2))
    pek = np.ascontiguousarray(inp["nsa_pe_k"][l].T)
    pev = np.ascontiguousarray(inp["nsa_pe_v"][l].T)
    maps = []
    for core in range(8):
        b, i = core // 4, core % 4
        P = proj[b]
        m = dict(cst)
        m["qnr"] = np.ascontiguousarray(np.stack([head_rows(P, 24, h) for h in range(4)]))
        m["qr"] = head_rows(P, 0, i)
        m["kcT"] = np.ascontiguousarray(P[3][0:64]); m["vcT"] = np.ascontiguousarray(P[12][0:64])
        m["kslT"] = np.ascontiguousarray(P[2][0:64]); m["kwT"] = np.ascontiguousarray(P[2][64:128])
        m["vsl"] = vaug(P[12][64:128]); m["vw"] = vaug(P[13][0:64])
        ag = misc[b][3 * i:3 * i + 3]
        m["ag"] = np.ascontiguousarray(ag.T.reshape(NKB, 128, 3).transpose(1, 0, 2))
        m["dq"] = head_rows(P, 4, i); m["dk"] = head_rows(P, 6, i); m["dv"] = vaug(head_rows(P, 14, i))
        m["sq"] = head_rows(P, 16, i); m["sk"] = head_rows(P, 18, i); m["sv"] = vaug(head_rows(P, 20, i))
        m["fq"] = head_rows(P, 8, i); m["fk"] = head_rows(P, 10, i); m["fv"] = vaug(head_rows(P, 22, i))
        m["logf"] = np.ascontiguousarray(misc[b][32 + i:33 + i])
        m["w1k"] = w1k; m["w1v"] = w1v; m["w2k"] = inp["nsa_ck_w2"][l]; m["w2v"] = inp["nsa_cv_w2"][l]
        m["pek"] = pek; m["pev"] = pev
        hs = np.zeros((128, 4), np.float32); hs[:, i] = 1.0
        m["hsel"] = hs
        maps.append(m)
    res = run_bass_kernel_spmd(cB.nc, maps, core_ids=list(range(8)))
    outs = []
    for b in range(2):
        ob = np.zeros((4, 256, S), bf)
        for i in range(4):
            o = res.results[b * 4 + i]["o"]
            for mm in range(4):
                tok = o[mm].transpose(1, 0, 2).reshape(S, 64)
                ob[mm, i * 64:(i + 1) * 64, :] = tok.T
        outs.append(ob)
    return outs


def build_C1(c=None):
    own = c is None
    if own:
        c = Ctx()
    ph = c.begin_phase()
    nc, s = c.nc, c.s
    oT_d = c.dram("oT", [4, 2, 128, NT], BF16, "ExternalInput")
    gates_d = c.dram("gates", [32, 128, NT], F32, "ExternalInput")
    xT_d = c.dram("xT", [128, 8, NT], F32, "ExternalInput")
    wbr_d = c.dram("wbr", [128, 8, 1024], F32, "ExternalInput")
    wout_d = c.dram("wout", [128, 8, 1024], F32, "ExternalInput")
    ga_d = c.dram("ga", [128, 8], F32, "ExternalInput")
    x1_d = c.dram("x1T", [128, 8, NT], F32, "ExternalOutput")

    xT = c.sb([128, 8, NT], F32)
    wbr = c.sb([128, 8, 1024], BF16)
    wout = c.sb([128, 8, 1024], BF16)
    ga = c.sb([128, 8], F32)
    stg = [c.sb([128, 2, 1024], F32) for _ in range(2)]
    s.dma(ga[:], ga_d, writes=[ga.res])
    for j in range(8):
        s.dma(xT[:, j, :], xT_d[:, j, :], writes=[xT.res])
    k = 0
    for (dst, src) in ((wbr, wbr_d), (wout, wout_d)):
        for q in range(4):
            st = stg[k % 2]
            k += 1
            s.dma(st[:], src[:, 2 * q:2 * q + 2, :], writes=[st.res])
            s.op("pool", (lambda st=st, dst=dst, q=q: nc.gpsimd.tensor_copy(out=dst[:, 2 * q:2 * q + 2, :], in_=st[:])), reads=[st.res], writes=[dst.res])
    ot_r = [c.sb([128, 8, TT], BF16) for _ in range(2)]
    gt_r = [[c.sb([128, TT], F32) for _ in range(4)] for _ in range(3)]
    zT_r = [c.sb([128, 8, TT], BF16) for _ in range(2)]
    pm_r = [c.ps([128, TT]) for _ in range(4)]
    pmix = [c.ps([128, TT]) for _ in range(2)]
    t_r = [c.sb([128, TT], F32) for _ in range(4)]
    items = [(tt, dc) for tt in range(NT // TT) for dc in range(8)]

    def c_load(n):
        tt, dc = items[n]
        sl = slice(tt * TT, (tt + 1) * TT)
        if dc == 0:
            ot = ot_r[tt % 2]
            for m in range(4):
                for kc in range(2):
                    s.dma(ot[:, m * 2 + kc, :], oT_d[m, kc, :, sl], writes=[ot.res], q="act")
        gts = gt_r[n % 3]
        for m in range(4):
            s.dma(gts[m][:], gates_d[m * 8 + dc, :, sl], writes=[gts[m].res], q=("sp", "act")[m % 2])

    def c_comp(n):
        tt, dc = items[n]
        sl = slice(tt * TT, (tt + 1) * TT)
        ot = ot_r[tt % 2]
        gts = gt_r[n % 3]
        zT = zT_r[tt % 2]
        for m in range(4):
            for kc in range(2):
                s.op("pe", (lambda m=m, kc=kc: nc.tensor.matmul(pm_r[m][:], lhsT=wbr[:, m * 2 + kc, dc * 128:(dc + 1) * 128], rhs=ot[:, m * 2 + kc, :],
                                                                start=(kc == 0), stop=(kc == 1))),
                     reads=[wbr.res, ot.res], writes=[pm_r[m].res])
        for m in range(4):
            s.op("dve", (lambda m=m: nc.vector.tensor_tensor(out=t_r[m][:], in0=pm_r[m][:], in1=gts[m][:], op=ALU.mult)),
                 reads=[pm_r[m].res, gts[m].res], writes=[t_r[m].res])
        s.op("pool", lambda: nc.gpsimd.tensor_tensor(out=t_r[0][:], in0=t_r[0][:], in1=t_r[1][:], op=ALU.add), reads=[t_r[0].res, t_r[1].res], writes=[t_r[0].res])
        s.op("pool", lambda: nc.gpsimd.tensor_tensor(out=t_r[2][:], in0=t_r[2][:], in1=t_r[3][:], op=ALU.add), reads=[t_r[2].res, t_r[3].res], writes=[t_r[2].res])
        s.op("pool", lambda: nc.gpsimd.tensor_tensor(out=zT[:, dc, :], in0=t_r[0][:], in1=t_r[2][:], op=ALU.add),
             reads=[t_r[0].res, t_r[2].res], writes=[zT.res])
        if dc != 7:
            return
        for ec in range(8):
            pmx = pmix[ec % 2]
            for j in range(8):
                s.op("pe", (lambda j=j, ec=ec, pmx=pmx: nc.tensor.matmul(pmx[:], lhsT=wout[:, j, ec * 128:(ec + 1) * 128], rhs=zT[:, j, :],
                                                                         start=(j == 0), stop=(j == 7))),
                     reads=[wout.res, zT.res], writes=[pmx.res])
            s.op("dve", (lambda ec=ec, pmx=pmx: nc.vector.scalar_tensor_tensor(out=xT[:, ec, sl], in0=pmx[:], scalar=ga[:, ec:ec + 1], in1=xT[:, ec, sl],
                                                                              op0=ALU.mult, op1=ALU.add)),
                 reads=[pmx.res, ga.res, xT.res], writes=[xT.res])
    pipeline(len(items), [c_load, c_comp])
    for j in range(8):
        s.dma(x1_d[:, j, :], xT[:, j, :], reads=[xT.res], is_output=True)
    c.end_phase(ph)
    if own:
        c.close()
    return c


def maps_C1(inp, l, o, gates, xT_all, mod):
    wbr = np.ascontiguousarray(inp["w_branch"][l].reshape(4, 2, 128, D).transpose(2, 0, 1, 3).reshape(128, 8, D))
    wout = np.ascontiguousarray(inp["w_out"][l].reshape(8, 128, D).transpose(1, 0, 2))
    maps = []
    for core in range(8):
        b, q = core // 4, core % 4
        tsl = slice(q * NT, (q + 1) * NT)
        maps.append({"oT": np.ascontiguousarray(o[b][:, :, tsl].reshape(4, 2, 128, NT)),
                     "gates": np.ascontiguousarray(gates[b][:, :, tsl]),
                     "xT": np.ascontiguousarray(xT_all[b][:, tsl].reshape(8, 128, NT).transpose(1, 0, 2)),
                     "wbr": wbr, "wout": wout, "ga": np.ascontiguousarray(mod[l, b][:, 16:24])})
    return maps


def collect_x(results, key):
    out = np.zeros((2, D, S), np.float32)
    for core in range(8):
        b, q = core // 4, core % 4
        out[b][:, q * NT:(q + 1) * NT] = results[core][key].transpose(1, 0, 2).reshape(D, NT)
    return out


def run_C1(cC1, inp, l, o, gates, xT_all, mod):
    maps = maps_C1(inp, l, o, gates, xT_all, mod)
    res = run_bass_kernel_spmd(cC1.nc, maps, core_ids=list(range(8)))
    return collect_x(res.results, "x1T")


HT = 1024
NFC = 22


def build_C2(n_exp, moe, c=None):
    own = c is None
    if own:
        c = Ctx()
    ph = c.begin_phase()
    nc, s = c.nc, c.s
    x1_d = c.dram("x1T", [128, 8, NT], F32, "ExternalInput")
    mod_d = c.dram("modF", [128, 24], F32, "ExternalInput")
    gn_d = c.dram("gn", [128, 8], F32, "ExternalInput")
    w1_d = c.dram("w1", [n_exp, NFC, 128, 8, 128], F32, "ExternalInput")
    w3_d = c.dram("w3", [n_exp, NFC, 128, 8, 128], F32, "ExternalInput")
    w2_d = c.dram("w2", [n_exp, 8, 128, NFC, 128], F32, "ExternalInput")
    if moe:
        rw_d = c.dram("rw", [128, 8, 8], F32, "ExternalInput")
        oh_d = c.dram("onehot", [8, 8, 128], F32, "ExternalInput")
        id_d = c.dram("ident", [128, 128], F32, "ExternalInput")
    x2_d = c.dram("x2T", [128, 8, NT], F32, "ExternalOutput")

    acc = c.sb([128, 8, HT], F32)
    hT = c.sb([128, 8, HT], BF16)
    act = c.sb([128, NFC, HT], BF16)
    rs = c.sb([128, HT], F32)
    ones = c.sb([128, 128], BF16)
    sqring = [c.sb([128, TT], BF16) for _ in range(2)]
    epsb = c.sb([128, 1], F32)
    mod = c.sb([128, 24], F32)
    gn = c.sb([128, 8], F32)
    Acol = c.sb([128, 8], F32)
    Bcol = c.sb([128, 8], F32)
    ring = [c.sb([128, TT], F32) for _ in range(4)]
    w1s = [c.sb([128, 8, 128], F32) for _ in range(2)]
    w3s = [c.sb([128, 8, 128], F32) for _ in range(2)]
    w1b = [c.sb([128, 8, 128], BF16) for _ in range(3)]
    w3b = [c.sb([128, 8, 128], BF16) for _ in range(3)]
    w2s = [c.sb([128, NFC, 128], F32) for _ in range(2)]
    w2b = [c.sb([128, NFC, 128], BF16) for _ in range(3)]
    sa_r = [c.sb([128, TT], F32) for _ in range(3)]
    u_r = [c.sb([128, TT], F32) for _ in range(3)]
    pa_r = [c.ps([128, TT]) for _ in range(2)]
    pg_r = [c.ps([128, TT]) for _ in range(2)]
    po_r = [c.ps([128, TT]) for _ in range(2)]
    pbank = c.ps([128, TT])
    pmisc = c.ps([128, TT])
    s.op("pool", lambda: nc.gpsimd.memset(ones[:], 1.0), writes=[ones.res])
    s.op("pool", lambda: nc.gpsimd.memset(epsb[:], EPS), writes=[epsb.res])
    s.dma(mod[:], mod_d, writes=[mod.res])
    s.dma(gn[:], gn_d, writes=[gn.res])
    emit_AB(c, gn, mod, 0, 8, Acol, Bcol)
    if moe:
        rw = c.sb([128, 8, 8], F32)
        oh = c.sb([8, 8, 128], F32)
        ident = c.sb([128, 128], F32)
        GT = c.sb([8, HT], F32)
        gb_r = [c.sb([128, HT], F32) for _ in range(2)]
        h32_r = [c.sb([128, TT], F32) for _ in range(2)]
        lg = c.sb([128, 8, 8], F32)
        m8 = c.sb([128, 8], F32)
        nt1 = c.sb([128, 1], F32)
        e2 = c.sb([128, 1], F32)
        ex = c.sb([128, 8], F32)
        selm = c.sb([128, 8], F32)
        Gt = c.sb([128, 8], F32)
        for t, d in ((rw, rw_d), (oh, oh_d), (ident, id_d)):
            s.dma(t[:], d, writes=[t.res])

    for hf in range(NT // HT):
        hsl = slice(hf * HT, (hf + 1) * HT)
        for j in range(8):
            s.dma(acc[:, j, :], x1_d[:, j, hsl], writes=[acc.res])
        if not moe:
            emit_modnorm(c, acc, hT, HT, ones, Acol, Bcol, epsb, ring, pbank, rs, sq_ring=sqring)
        else:
            def after_h(tmp, j, tt, sl):
                h32 = h32_r[j % 2]
                s.op("act", lambda: nc.scalar.activation(out=h32[:], in_=tmp[:], func=AF.Identity, scale=Acol[:, j:j + 1], bias=Bcol[:, j:j + 1]),
                     reads=[tmp.res, Acol.res, Bcol.res], writes=[h32.res])
                s.op("pool", lambda: nc.gpsimd.tensor_copy(out=hT[:, j, sl], in_=h32[:]), reads=[h32.res], writes=[hT.res])
                for tb in range(4):
                    col = (tt * 4 + tb) * 8
                    s.op("pe", (lambda tb=tb, col=col: nc.tensor.matmul(pmisc[:, col:col + 8], lhsT=h32[:, tb * 128:(tb + 1) * 128], rhs=rw[:, j, :],
                                                                        start=(j == 0 and tb == 0 and tt == 0), stop=(j == 7), skip_group_check=True)),
                         reads=[h32.res, rw.res], writes=[pmisc.res])
            emit_modnorm(c, acc, hT, HT, ones, Acol, Bcol, epsb, ring, pbank, rs, after_h=after_h, sq_ring=sqring)
            s.op("act", lambda: nc.scalar.copy(out=lg[:], in_=pmisc[:, 0:64].rearrange("p (a b) -> p a b", b=8)), reads=[pmisc.res], writes=[lg.res])
            for tb in range(8):
                s.op("dve", (lambda tb=tb: nc.vector.max(out=m8[:], in_=lg[:, tb, :])), reads=[lg.res], writes=[m8.res])
                s.op("dve", lambda: nc.vector.tensor_scalar(out=nt1[:], in0=m8[:, 0:1], scalar1=-1.0, scalar2=None, op0=ALU.mult), reads=[m8.res], writes=[nt1.res])
                s.op("act", (lambda tb=tb: nc.scalar.activation(out=ex[:], in_=lg[:, tb, :], func=AF.Exp, bias=nt1[:], scale=1.0)),
                     reads=[lg.res, nt1.res], writes=[ex.res])
                s.op("act", lambda: nc.scalar.activation(out=e2[:], in_=m8[:, 1:2], func=AF.Exp, bias=nt1[:], scale=1.0), reads=[m8.res, nt1.res], writes=[e2.res])
                s.op("dve", lambda: nc.vector.tensor_scalar(out=e2[:], in0=e2[:], scalar1=1.0, scalar2=None, op0=ALU.add), reads=[e2.res], writes=[e2.res])
                s.op("dve", lambda: nc.vector.reciprocal(out=e2[:], in_=e2[:]), reads=[e2.res], writes=[e2.res])
                s.op("dve", (lambda tb=tb: nc.vector.tensor_scalar(out=selm[:], in0=lg[:, tb, :], scalar1=m8[:, 1:2], scalar2=None, op0=ALU.is_ge)),
                     reads=[lg.res, m8.res], writes=[selm.res])
                s.op("dve", lambda: nc.vector.scalar_tensor_tensor(out=Gt[:], in0=ex[:], scalar=e2[:, 0:1], in1=selm[:], op0=ALU.mult, op1=ALU.mult),
                     reads=[ex.res, e2.res, selm.res], writes=[Gt.res])
                s.op("pe", (lambda tb=tb: nc.tensor.transpose(pbank[0:8, (tb % 4) * 128:(tb % 4 + 1) * 128], Gt[:], ident[:])), reads=[Gt.res, ident.res], writes=[pbank.res])
                if tb % 4 == 3:
                    q4 = tb // 4
                    s.op("act", (lambda q4=q4: nc.scalar.copy(out=GT[:, q4 * 512:(q4 + 1) * 512], in_=pbank[0:8, 0:512])), reads=[pbank.res], writes=[GT.res])

        items = []
        kf = kg = 0
        for e in range(n_exp):
            for fc in range(NFC):
                for tt in range(2):
                    items.append(("f", e, fc, tt, kf))
                kf += 1
            for ec in range(8):
                for tt in range(2):
                    items.append(("g", e, ec, tt, kg))
                kg += 1

        def s_load(n):
            kind, e, ci, tt, k = items[n]
            if tt != 0:
                return
            if kind == "f":
                if moe and ci == 0:
                    g_b = gb_r[e % 2]
                    for t2 in range(2):
                        s.op("pe", (lambda t2=t2, e=e: nc.tensor.matmul(pmisc[:], lhsT=oh[:, e, :], rhs=GT[:, t2 * TT:(t2 + 1) * TT], start=True, stop=True)),
                             reads=[oh.res, GT.res], writes=[pmisc.res])
                        s.op("act", (lambda t2=t2, g_b=g_b: nc.scalar.copy(out=g_b[:, t2 * TT:(t2 + 1) * TT], in_=pmisc[:])), reads=[pmisc.res], writes=[g_b.res])
                s.dma(w1s[k % 2][:], w1_d[e, ci], writes=[w1s[k % 2].res], q="act")
                s.dma(w3s[k % 2][:], w3_d[e, ci], writes=[w3s[k % 2].res], q="act")
            else:
                s.dma(w2s[k % 2][:], w2_d[e, ci], writes=[w2s[k % 2].res], q="act")

        def s_cast(n):
            kind, e, ci, tt, k = items[n]
            if tt != 0:
                return
            if kind == "f":
                s.op("pool", lambda: nc.gpsimd.tensor_copy(out=w1b[k % 3][:], in_=w1s[k % 2][:]), reads=[w1s[k % 2].res], writes=[w1b[k % 3].res])
                s.op("dve", lambda: nc.vector.tensor_copy(out=w3b[k % 3][:], in_=w3s[k % 2][:]), reads=[w3s[k % 2].res], writes=[w3b[k % 3].res])
            else:
                half = NFC // 2
                s.op("pool", lambda: nc.gpsimd.tensor_copy(out=w2b[k % 3][:, 0:half, :], in_=w2s[k % 2][:, 0:half, :]), reads=[w2s[k % 2].res], writes=[w2b[k % 3].res])
                s.op("dve", lambda: nc.vector.tensor_copy(out=w2b[k % 3][:, half:NFC, :], in_=w2s[k % 2][:, half:NFC, :]), reads=[w2s[k % 2].res], writes=[w2b[k % 3].res])

        def s_mm(n):
            kind, e, ci, tt, k = items[n]
            sl = slice(tt * TT, (tt + 1) * TT)
            if kind == "f":
                pa, pg = pa_r[n % 2], pg_r[n % 2]
                for j in range(8):
                    s.op("pe", (lambda j=j: nc.tensor.matmul(pa[:], lhsT=w1b[k % 3][:, j, :], rhs=hT[:, j, sl], start=(j == 0), stop=(j == 7))),
                         reads=[w1b[k % 3].res, hT.res], writes=[pa.res])
                for j in range(8):
                    s.op("pe", (lambda j=j: nc.tensor.matmul(pg[:], lhsT=w3b[k % 3][:, j, :], rhs=hT[:, j, sl], start=(j == 0), stop=(j == 7))),
                         reads=[w3b[k % 3].res, hT.res], writes=[pg.res])
            else:
                po = po_r[n % 2]
                for fc in range(NFC):
                    s.op("pe", (lambda fc=fc: nc.tensor.matmul(po[:], lhsT=w2b[k % 3][:, fc, :], rhs=act[:, fc, sl], start=(fc == 0), stop=(fc == NFC - 1))),
                         reads=[w2b[k % 3].res, act.res], writes=[po.res])

        def s_post(n):
            kind, e, ci, tt, k = items[n]
            sl = slice(tt * TT, (tt + 1) * TT)
            if kind == "f":
                pa, pg = pa_r[n % 2], pg_r[n % 2]
                sa = sa_r[n % 3]
                s.op("act", lambda: nc.scalar.activation(out=sa[:], in_=pa[:], func=AF.Silu), reads=[pa.res], writes=[sa.res])
                if not moe:
                    s.op("dve", lambda: nc.vector.tensor_tensor(out=act[:, ci, sl], in0=sa[:], in1=pg[:], op=ALU.mult), reads=[sa.res, pg.res], writes=[act.res])
                else:
                    u = u_r[n % 3]
                    g_b = gb_r[e % 2]
                    s.op("dve", lambda: nc.vector.tensor_tensor(out=u[:], in0=pg[:], in1=g_b[:, sl], op=ALU.mult), reads=[pg.res, g_b.res], writes=[u.res])
                    s.op("pool", lambda: nc.gpsimd.tensor_tensor(out=act[:, ci, sl], in0=sa[:], in1=u[:], op=ALU.mult), reads=[sa.res, u.res], writes=[act.res])
            else:
                po = po_r[n % 2]
                s.op("dve", lambda: nc.vector.scalar_tensor_tensor(out=acc[:, ci, sl], in0=po[:], scalar=mod[:, 16 + ci:17 + ci], in1=acc[:, ci, sl],
                                                                   op0=ALU.mult, op1=ALU.add), reads=[po.res, mod.res, acc.res], writes=[acc.res])
        pipeline(len(items), [s_load, s_cast, s_mm, s_post])
        for j in range(8):
            s.dma(x2_d[:, j, hsl], acc[:, j, :], reads=[acc.res], is_output=True)
    c.end_phase(ph)
    if own:
        c.close()
    return c


def maps_C2(inp, l, x1T_all, mod, moe):
    if moe:
        w1, w3, w2 = inp["moe_w1"][l // 2], inp["moe_w3"][l // 2], inp["moe_w2"][l // 2]
    else:
        w1, w3, w2 = inp["ffn_w1"][l // 2][None], inp["ffn_w3"][l // 2][None], inp["ffn_w2"][l // 2][None]
    E = w1.shape[0]
    lay1 = lambda w: np.ascontiguousarray(w.reshape(E, 8, 128, NFC, 128).transpose(0, 3, 2, 1, 4))
    lay2 = lambda w: np.ascontiguousarray(w.reshape(E, NFC, 128, 8, 128).transpose(0, 3, 2, 1, 4))
    w1l, w3l, w2l = lay1(w1), lay1(w3), lay2(w2)
    gn = np.ascontiguousarray(inp["norm_ffn"][l].reshape(8, 128).T)
    maps = []
    for core in range(8):
        b, q = core // 4, core % 4
        m = {"modF": np.ascontiguousarray(mod[l, b][:, 24:48]), "gn": gn, "w1": w1l, "w3": w3l, "w2": w2l}
        if x1T_all is not None:
            m["x1T"] = np.ascontiguousarray(x1T_all[b][:, q * NT:(q + 1) * NT].reshape(8, 128, NT).transpose(1, 0, 2))
        if moe:
            m["rw"] = np.ascontiguousarray(inp["router_w"][l // 2].reshape(8, 128, 8).transpose(1, 0, 2))
            oh = np.zeros((8, 8, 128), np.float32)
            for e in range(8):
                oh[e, e, :] = 1.0
            m["onehot"] = oh
            m["ident"] = np.eye(128, dtype=np.float32)
        maps.append(m)
    return maps


def run_C2(cC2, inp, l, x1T_all, mod, moe):
    maps = maps_C2(inp, l, x1T_all, mod, moe)
    res = run_bass_kernel_spmd(cC2.nc, maps, core_ids=list(range(8)))
    return collect_x(res.results, "x2T")


def build_CA(moe, with_A):
    c = Ctx()
    c.alias = {}
    c.pre = "c1_"
    c.kind_override = {"c1_x1T": "Internal"}
    build_C1(c)
    c.pre = "c2_"
    c.alias["c2_x1T"] = c.made["c1_x1T"]
    build_C2(8 if moe else 1, moe, c)
    if with_A:
        c.pre = "a_"
        c.alias["a_xT"] = c.made["c2_x2T"]
        build_A(c)
    c.close()
    return c


def run_CA(cCA, inp, l, o, gates, xT_all, mod, moe, with_A):
    m1 = maps_C1(inp, l, o, gates, xT_all, mod)
    m2 = maps_C2(inp, l, None, mod, moe)
    m3 = maps_A(inp, l + 1, None, mod) if with_A else [dict() for _ in range(8)]
    maps = []
    for i in range(8):
        m = {"c1_" + k: v for k, v in m1[i].items()}
        m.update({"c2_" + k: v for k, v in m2[i].items()})
        m.update({"a_" + k: v for k, v in m3[i].items()})
        maps.append(m)
    res = run_bass_kernel_spmd(cCA.nc, maps, core_ids=list(range(8)))
    x2T = collect_x(res.results, "c2_x2T")
    nxt = collect_A(res.results, "a_") if with_A else None
    return x2T, nxt


_PROGS = {}


def _prog(name, fn):
    if name not in _PROGS:
        _PROGS[name] = fn()
    return _PROGS[name]


def kernel(**inp):
    inp = {k: np.asarray(v) for k, v in inp.items()}
    mod = run_M(inp)
    xT_all = np.ascontiguousarray(inp["x"].astype(np.float32).transpose(0, 2, 1))
    cA = _prog("A", build_A)
    proj, gates, misc = run_A(cA, inp, 0, xT_all, mod)
    cB = _prog("B", build_B)
    for l in range(2):
        o = run_B(cB, inp, l, proj, misc)
        moe = (l % 2 == 1)
        with_A = (l == 0)
        cCA = _prog("CA%d" % l, lambda: build_CA(moe, with_A))
        xT_all, nxt = run_CA(cCA, inp, l, o, gates, xT_all, mod, moe, with_A)
        if with_A:
            proj, gates, misc = nxt
    return np.ascontiguousarray(xT_all.transpose(0, 2, 1)).astype(np.float32)
```

```python
import contextlib
import numpy as np
import concourse.bass as bass
import concourse.mybir as mybir
from concourse.bass_utils import run_bass_kernel_spmd

F32 = mybir.dt.float32
BF16 = mybir.dt.bfloat16
I32 = mybir.dt.int32
AF = mybir.ActivationFunctionType
ALU = mybir.AluOpType
AX = mybir.AxisListType


class Res:
    __slots__ = ("w", "r")

    def __init__(self):
        self.w = None
        self.r = {}


class Sched:
    NDMA = 24

    def __init__(self, nc, es):
        self.nc = nc
        self.engs = {"pe": nc.tensor, "act": nc.scalar, "dve": nc.vector,
                     "pool": nc.gpsimd, "sp": nc.sync}
        self.sem = {}
        self.cnt = {}
        for k in self.engs:
            self.sem[k] = es.enter_context(nc.semaphore("s_" + k))
            self.cnt[k] = 0
        for i in range(self.NDMA):
            k = "d%d" % i
            self.sem[k] = es.enter_context(nc.semaphore("s_" + k))
            self.cnt[k] = 0
        self.seen = {k: {} for k in self.engs}
        self.dma_rr = 0
        self.out_events = []

    def _wait(self, e, ev):
        if ev is None:
            return
        key, val = ev
        if key == e and e == "pe":
            return
        if self.seen[e].get(key, 0) >= val:
            return
        self.engs[e].wait_ge(self.sem[key], val)
        self.seen[e][key] = val

    def _deps(self, e, reads, writes):
        for r in reads:
            self._wait(e, r.w)
        for r in writes:
            self._wait(e, r.w)
            for k, v in r.r.items():
                self._wait(e, (k, v))

    def op(self, e, fn, reads=(), writes=()):
        self._deps(e, reads, writes)
        ins = fn()
        self.cnt[e] += 1
        ins.then_inc(self.sem[e], 1)
        ev = (e, self.cnt[e])
        for r in reads:
            r.r[e] = ev[1]
        for r in writes:
            r.w = ev
            r.r = {}
        return ev

    def dma(self, out, in_, reads=(), writes=(), q="sp", is_output=False, **kw):
        k = "d%d" % self.dma_rr
        self.dma_rr = (self.dma_rr + 1) % self.NDMA
        self._wait(q, (k, self.cnt[k]))
        self._deps(q, reads, writes)
        ins = self.engs[q].dma_start(out=out, in_=in_, **kw)
        self.cnt[k] += 16
        ins.then_inc(self.sem[k], 16)
        ev = (k, self.cnt[k])
        for r in reads:
            r.r[k] = ev[1]
        for r in writes:
            r.w = ev
            r.r = {}
        if is_output:
            self.out_events.append(ev)
        return ev

    def finish(self):
        for i in range(self.NDMA):
            k = "d%d" % i
            self._wait("sp", (k, self.cnt[k]))
        for k in ("pe", "act", "dve", "pool"):
            self._wait("sp", (k, self.cnt[k]))


class Tile:
    def __init__(self, t):
        self.t = t
        self.res = Res()

    def __getitem__(self, idx):
        return self.t[idx]


class Ctx:
    def __init__(self, name="k"):
        self.nc = bass.Bass("TRN2", target_bir_lowering=False)
        self.es = contextlib.ExitStack()
        self.s = Sched(self.nc, self.es)
        self.n = 0

    def sb(self, shape, dt, name=None):
        self.n += 1
        return Tile(self.es.enter_context(self.nc.sbuf_tensor(name or ("t%d" % self.n), list(shape), dt)))

    def ps(self, shape, dt=F32, name=None):
        self.n += 1
        return Tile(self.es.enter_context(self.nc.psum_tensor(name or ("p%d" % self.n), list(shape), dt)))

    pre = ""
    alias = None
    kind_override = None

    def dram(self, name, shape, dt, kind):
        full = self.pre + name
        if self.alias and full in self.alias:
            return self.alias[full]
        if self.kind_override and full in self.kind_override:
            kind = self.kind_override[full]
        ap = self.nc.dram_tensor(full, list(shape), dt, kind=kind).ap()
        if self.alias is None:
            self.alias = {}
        self.made = getattr(self, "made", {})
        self.made[full] = ap
        return ap

    def begin_phase(self):
        es = contextlib.ExitStack()
        old, self.es = self.es, es
        return (old, es)

    def end_phase(self, ph):
        barrier(self)
        self.es = ph[0]
        ph[1].close()

    def close(self):
        self.s.finish()
        self.es.close()


D = 1024
S = 8192
NB = 2
NT = 2048
TT = 512
NCH_IN = 57
A_MAXCH = NCH_IN
EPS = 1e-6
TWO_PI = float(2 * np.pi)
C1_2PI = 6.28125
C2_2PI = TWO_PI - C1_2PI


def barrier(c):
    s = c.s
    for e in ("pe", "act", "dve", "pool", "sp"):
        for k in list(s.cnt.keys()):
            if k != e:
                s._wait(e, (k, s.cnt[k]))


def build_M():
    c = Ctx()
    nc, s = c.nc, c.s
    cT_d = c.dram("cT", [128, 8, 2], F32, "ExternalInput")
    w_d = c.dram("w", [12, 128, 8, 128], F32, "ExternalInput")
    b_d = c.dram("b", [128, 12], F32, "ExternalInput")
    o_d = c.dram("modT", [128, 12, 2], F32, "ExternalOutput")
    cT = c.sb([128, 8, 2], F32)
    ca = c.sb([128, 8, 2], F32)
    bt = c.sb([128, 12], F32)
    ot = c.sb([128, 12, 2], F32)
    s.dma(cT[:], cT_d, writes=[cT.res])
    s.dma(bt[:], b_d, writes=[bt.res])
    s.op("act", lambda: nc.scalar.activation(out=ca[:], in_=cT[:], func=AF.Silu), reads=[cT.res], writes=[ca.res])
    wts = [c.sb([128, 8, 128], F32) for _ in range(3)]
    pm = c.ps([128, 12, 2])
    for j in range(12):
        wt = wts[j % 3]
        s.dma(wt[:], w_d[j], writes=[wt.res])
        for k in range(8):
            s.op("pe", (lambda wt=wt, k=k, j=j: nc.tensor.matmul(pm[:, j, :], lhsT=wt[:, k, :], rhs=ca[:, k, :],
                                                                 start=(k == 0), stop=(k == 7))),
                 reads=[wt.res, ca.res], writes=[pm.res])
    for b in range(2):
        s.op("dve", (lambda b=b: nc.vector.tensor_tensor(out=ot[:, :, b], in0=pm[:, :, b], in1=bt[:], op=ALU.add)),
             reads=[pm.res, bt.res], writes=[ot.res])
    s.dma(o_d, ot[:], reads=[ot.res], is_output=True)
    c.close()
    return c


def run_M(inp):
    c = build_M()
    cT = np.ascontiguousarray(inp["c"].T.reshape(8, 128, 2).transpose(1, 0, 2))
    w_all = inp["w_ada"]
    b_all = inp["b_ada"]
    maps = []
    for i in range(8):
        chunks = [(g // 48, g % 48) for g in range(i * 12, i * 12 + 12)]
        w = np.stack([w_all[l][:, n * 128:(n + 1) * 128].reshape(8, 128, 128).transpose(1, 0, 2) for l, n in chunks])
        b = np.stack([b_all[l][n * 128:(n + 1) * 128] for l, n in chunks], axis=1)
        maps.append({"cT": cT, "w": np.ascontiguousarray(w), "b": np.ascontiguousarray(b)})
    res = run_bass_kernel_spmd(c.nc, maps, core_ids=list(range(8)))
    mod = np.zeros((2, 2, 128, 48), np.float32)
    for i in range(8):
        o = res.results[i]["modT"]
        for jj, g in enumerate(range(i * 12, i * 12 + 12)):
            mod[g // 48, :, :, g % 48] = o[:, jj, :].T
    return mod


def emit_modnorm(c, src, hT, ntok, ones, Acol, Bcol, epsb, tmp_ring, pbank, rs, after_h=None, sq_ring=None):
    nc, s = c.nc, c.s
    ntt = ntok // TT
    if sq_ring is None:
        sq_ring = tmp_ring
    for tt in range(ntt):
        sl = slice(tt * TT, (tt + 1) * TT)
        for j in range(8):
            sq = sq_ring[j % len(sq_ring)]
            s.op("act", (lambda sq=sq, j=j: nc.scalar.activation(out=sq[:], in_=src[:, j, sl], func=AF.Square)),
                 reads=[src.res], writes=[sq.res])
            s.op("pe", (lambda sq=sq, j=j: nc.tensor.matmul(pbank[:], lhsT=ones[:], rhs=sq[:], start=(j == 0), stop=(j == 7))),
                 reads=[sq.res, ones.res], writes=[pbank.res])
        sd = tmp_ring[0]
        s.op("act", (lambda sd=sd: nc.scalar.activation(out=sd[:], in_=pbank[:], func=AF.Sqrt, scale=1.0 / D, bias=epsb[:])),
             reads=[pbank.res, epsb.res], writes=[sd.res])
        s.op("dve", (lambda sd=sd: nc.vector.reciprocal(out=rs[:, sl], in_=sd[:])), reads=[sd.res], writes=[rs.res])
        for j in range(8):
            tmp = tmp_ring[1 + (j % (len(tmp_ring) - 1))]
            s.op("dve", (lambda tmp=tmp, j=j: nc.vector.tensor_tensor(out=tmp[:], in0=src[:, j, sl], in1=rs[:, sl], op=ALU.mult)),
                 reads=[src.res, rs.res], writes=[tmp.res])
            if after_h is None:
                s.op("act", (lambda tmp=tmp, j=j: nc.scalar.activation(out=hT[:, j, sl], in_=tmp[:], func=AF.Identity,
                                                                      scale=Acol[:, j:j + 1], bias=Bcol[:, j:j + 1])),
                     reads=[tmp.res, Acol.res, Bcol.res], writes=[hT.res])
            else:
                after_h(tmp, j, tt, sl)


def emit_AB(c, gn, mod, sh_off, sc_off, Acol, Bcol):
    nc, s = c.nc, c.s
    s.op("dve", lambda: nc.vector.scalar_tensor_tensor(out=Acol[:], in0=mod[:, sc_off:sc_off + 8], scalar=1.0, in1=gn[:],
                                                       op0=ALU.add, op1=ALU.mult),
         reads=[mod.res, gn.res], writes=[Acol.res])
    s.op("dve", lambda: nc.vector.tensor_copy(out=Bcol[:], in_=mod[:, sh_off:sh_off + 8]), reads=[mod.res], writes=[Bcol.res])


ROPE_CH = (0, 1, 2, 4, 5, 6, 7)
NORM_CH = (3, 8, 9, 10, 11)
RAW_CH = tuple(range(12, 24))
MISC_CH = 24


def build_A(c=None):
    own = c is None
    if own:
        c = Ctx()
    ph = c.begin_phase()
    nc, s = c.nc, c.s
    xT_d = c.dram("xT", [128, 8, NT], F32, "ExternalInput")
    mod_d = c.dram("modA", [128, 16], F32, "ExternalInput")
    gn_d = c.dram("gn", [128, 8], F32, "ExternalInput")
    pos_d = c.dram("pos", [1, NT], I32, "ExternalInput")
    invf_d = c.dram("invf", [128, 1], F32, "ExternalInput")
    w_d = c.dram("w", [NCH_IN, 128, 8, 128], F32, "ExternalInput")
    gain_d = c.dram("gain", [128, 12], F32, "ExternalInput")
    osc_d = c.dram("osc", [128, 12], F32, "ExternalInput")
    foxb_d = c.dram("foxb", [128, 1], F32, "ExternalInput")
    pm_d = c.dram("pm", [128, 128], F32, "ExternalInput")
    bones_d = c.dram("bones", [128, 128], F32, "ExternalInput")
    proj_d = c.dram("proj", [26, 128, NT], BF16, "ExternalOutput")
    gates_d = c.dram("gates", [32, 128, NT], F32, "ExternalOutput")
    misc_d = c.dram("misc", [64, NT], F32, "ExternalOutput")

    hT = c.sb([128, 8, NT], BF16)
    rs = c.sb([128, NT], F32)
    COS = c.sb([128, NT], F32)
    SIN = c.sb([128, NT], F32)
    ones = c.sb([128, 128], BF16)
    bones_f = c.sb([128, 128], F32)
    pm_f = c.sb([128, 128], F32)
    bones = c.sb([128, 128], BF16)
    pm = c.sb([128, 128], BF16)
    gain = c.sb([128, 12], F32)
    osc = c.sb([128, 12], F32)
    foxb = c.sb([128, 1], F32)
    epsb = c.sb([128, 1], F32)
    negpi = c.sb([128, 1], F32)
    invf = c.sb([128, 1], F32)
    mod = c.sb([128, 16], F32)
    gn = c.sb([128, 8], F32)
    Acol = c.sb([128, 8], F32)
    Bcol = c.sb([128, 8], F32)
    pbank = c.ps([128, TT])

    s.op("pool", lambda: nc.gpsimd.memset(ones[:], 1.0), writes=[ones.res])
    s.op("pool", lambda: nc.gpsimd.memset(epsb[:], EPS), writes=[epsb.res])
    s.op("pool", lambda: nc.gpsimd.memset(negpi[:], -float(np.pi)), writes=[negpi.res])
    for t, d in ((bones_f, bones_d), (pm_f, pm_d), (gain, gain_d), (osc, osc_d), (foxb, foxb_d), (invf, invf_d), (mod, mod_d), (gn, gn_d)):
        s.dma(t[:], d, writes=[t.res])
    s.op("pool", lambda: nc.gpsimd.tensor_copy(out=bones[:], in_=bones_f[:]), reads=[bones_f.res], writes=[bones.res])
    s.op("pool", lambda: nc.gpsimd.tensor_copy(out=pm[:], in_=pm_f[:]), reads=[pm_f.res], writes=[pm.res])
    s.op("dve", lambda: nc.vector.tensor_tensor(out=gain[:], in0=gain[:], in1=osc[:], op=ALU.mult), reads=[gain.res, osc.res], writes=[gain.res])
    emit_AB(c, gn, mod, 0, 8, Acol, Bcol)

    with contextlib.ExitStack() as es1:
        old_es, c.es = c.es, es1
        xT = c.sb([128, 8, NT], F32)
        for j in range(8):
            s.dma(xT[:, j, :], xT_d[:, j, :], writes=[xT.res])
        posi = c.sb([128, NT], I32)
        ang = c.sb([128, NT], F32)
        tq = c.sb([128, NT], F32)
        ki = c.sb([128, NT], I32)
        kf = c.sb([128, NT], F32)
        s.dma(posi[:], pos_d[0, :].partition_broadcast(128), writes=[posi.res])
        s.op("dve", lambda: nc.vector.tensor_copy(out=tq[:], in_=posi[:]), reads=[posi.res], writes=[tq.res])
        s.op("dve", lambda: nc.vector.tensor_scalar(out=ang[:], in0=tq[:], scalar1=invf[:, 0:1], scalar2=None, op0=ALU.mult),
             reads=[tq.res, invf.res], writes=[ang.res])
        for dst, shift in ((SIN, 0.0), (COS, 0.25)):
            s.op("dve", lambda shift=shift: nc.vector.tensor_scalar(out=tq[:], in0=ang[:], scalar1=1.0 / TWO_PI, scalar2=0.5 + shift,
                                                                    op0=ALU.mult, op1=ALU.add), reads=[ang.res], writes=[tq.res])
            s.op("dve", lambda: nc.vector.tensor_copy(out=ki[:], in_=tq[:]), reads=[tq.res], writes=[ki.res])
            s.op("dve", lambda: nc.vector.tensor_copy(out=kf[:], in_=ki[:]), reads=[ki.res], writes=[kf.res])
            s.op("dve", lambda shift=shift: nc.vector.tensor_scalar(out=tq[:], in0=ang[:], scalar1=float(np.pi) + shift * TWO_PI, scalar2=None,
                                                                    op0=ALU.add), reads=[ang.res], writes=[tq.res])
            s.op("dve", lambda: nc.vector.scalar_tensor_tensor(out=tq[:], in0=kf[:], scalar=-C1_2PI, in1=tq[:], op0=ALU.mult, op1=ALU.add),
                 reads=[kf.res, tq.res], writes=[tq.res])
            s.op("dve", lambda: nc.vector.scalar_tensor_tensor(out=tq[:], in0=kf[:], scalar=-C2_2PI, in1=tq[:], op0=ALU.mult, op1=ALU.add),
                 reads=[kf.res, tq.res], writes=[tq.res])
            s.op("dve", lambda: nc.vector.tensor_scalar(out=kf[:], in0=tq[:], scalar1=0.0, scalar2=TWO_PI, op0=ALU.is_lt, op1=ALU.mult),
                 reads=[tq.res], writes=[kf.res])
            s.op("dve", lambda: nc.vector.tensor_tensor(out=tq[:], in0=tq[:], in1=kf[:], op=ALU.add), reads=[tq.res, kf.res], writes=[tq.res])
            s.op("dve", lambda: nc.vector.tensor_scalar(out=tq[:], in0=tq[:], scalar1=0.0, scalar2=TWO_PI, op0=ALU.max, op1=ALU.min),
                 reads=[tq.res], writes=[tq.res])
            s.op("act", lambda dst=dst: nc.scalar.activation(out=dst[:], in_=tq[:], func=AF.Sin, bias=negpi[:], scale=1.0),
                 reads=[tq.res, negpi.res], writes=[dst.res])
        ring = [c.sb([128, TT], F32) for _ in range(4)]
        sqring = [c.sb([128, TT], BF16) for _ in range(4)]
        emit_modnorm(c, xT, hT, NT, ones, Acol, Bcol, epsb, ring, pbank, rs, sq_ring=sqring)
        barrier(c)
        c.es = old_es

    R = 4
    wst = [c.sb([128, 8, 128], F32) for _ in range(3)]
    wbf = [c.sb([128, 8, 128], BF16) for _ in range(3)]
    pp = [c.ps([128, TT]) for _ in range(3)]
    psq = [c.ps([128, TT]) for _ in range(2)]
    pq = [pbank, c.ps([128, TT])]
    sq_r = [c.sb([128, TT], BF16) for _ in range(R)]
    rstd_r = [c.sb([128, TT], F32) for _ in range(R)]
    y_r = [c.sb([128, TT], F32) for _ in range(R)]
    y2_r = [c.sb([128, TT], BF16) for _ in range(R)]
    t1_r = [c.sb([128, TT], F32) for _ in range(R)]
    ob_r = [c.sb([128, TT], BF16) for _ in range(R)]
    ob2_r = [c.sb([128, TT], BF16) for _ in range(R)]
    of_r = [c.sb([128, TT], F32) for _ in range(R)]

    items = [(ch, tt) for ch in range(A_MAXCH) for tt in range(NT // TT)]

    def kind(ch):
        if ch in ROPE_CH:
            return "rope"
        if ch in NORM_CH:
            return "norm"
        if ch in RAW_CH:
            return "raw"
        if ch == MISC_CH:
            return "misc"
        return "sig"

    def stl(n):
        ch, tt = items[n]
        if tt == 0:
            ws = wst[ch % 3]
            s.dma(ws[:], w_d[ch], writes=[ws.res], q="act")

    def stc(n):
        ch, tt = items[n]
        if tt == 0:
            ws, wb = wst[ch % 3], wbf[ch % 3]
            s.op("pool", lambda: nc.gpsimd.tensor_copy(out=wb[:], in_=ws[:]), reads=[ws.res], writes=[wb.res])

    def st0(n):
        ch, tt = items[n]
        wb = wbf[ch % 3]
        p = pp[n % 3]
        for j in range(8):
            s.op("pe", (lambda j=j: nc.tensor.matmul(p[:], lhsT=wb[:, j, :], rhs=hT[:, j, tt * TT:(tt + 1) * TT],
                                                     start=(j == 0), stop=(j == 7))),
                 reads=[wb.res, hT.res], writes=[p.res])

    def st1(n):
        ch, tt = items[n]
        k = kind(ch)
        p = pp[n % 3]
        sl = slice(tt * TT, (tt + 1) * TT)
        if k in ("rope", "norm"):
            sq = sq_r[n % R]
            s.op("act", lambda: nc.scalar.activation(out=sq[:], in_=p[:], func=AF.Square), reads=[p.res], writes=[sq.res])
            s.op("pe", lambda: nc.tensor.matmul(psq[n % 2][:], lhsT=bones[:], rhs=sq[:], start=True, stop=True),
                 reads=[bones.res, sq.res], writes=[psq[n % 2].res])
        elif k == "raw":
            ob = ob_r[n % R]
            sc = 0.125 if ch in (16, 17) else 1.0
            s.op("act", lambda: nc.scalar.activation(out=ob[:], in_=p[:], func=AF.Copy, scale=sc), reads=[p.res], writes=[ob.res])
            s.dma(proj_d[ch, :, sl], ob[:], reads=[ob.res], is_output=True)
        elif k == "sig":
            of = of_r[n % R]
            s.op("act", lambda: nc.scalar.activation(out=of[:], in_=p[:], func=AF.Sigmoid), reads=[p.res], writes=[of.res])
            s.dma(gates_d[ch - 25, :, sl], of[:], reads=[of.res], is_output=True, q=("sp", "act")[n % 2])
        else:
            of = of_r[n % R]
            s.op("act", lambda: nc.scalar.activation(out=of[0:64, :], in_=p[0:64, :], func=AF.Sigmoid, bias=foxb[0:64, :], scale=1.0),
                 reads=[p.res, foxb.res], writes=[of.res])
            s.op("act", lambda: nc.scalar.activation(out=of[32:64, :], in_=of[32:64, :], func=AF.Ln), reads=[of.res], writes=[of.res])
            s.dma(misc_d[:, sl], of[0:64, :], reads=[of.res], is_output=True)

    def st2(n):
        ch, tt = items[n]
        k = kind(ch)
        if k not in ("rope", "norm"):
            return
        p = pp[n % 3]
        sl = slice(tt * TT, (tt + 1) * TT)
        rstd = rstd_r[n % R]
        y = y_r[n % R]
        s.op("act", lambda: nc.scalar.activation(out=rstd[:], in_=psq[n % 2][:], func=AF.Sqrt, scale=1.0 / 64, bias=epsb[:]),
             reads=[psq[n % 2].res, epsb.res], writes=[rstd.res])
        s.op("dve", lambda: nc.vector.reciprocal(out=rstd[:], in_=rstd[:]), reads=[rstd.res], writes=[rstd.res])
        s.op("dve", lambda: nc.vector.tensor_tensor(out=y[:], in0=p[:], in1=rstd[:], op=ALU.mult), reads=[p.res, rstd.res], writes=[y.res])
        if k == "norm":
            ob = ob_r[n % R]
            s.op("act", lambda: nc.scalar.activation(out=ob[:], in_=y[:], func=AF.Copy, scale=gain[:, ch:ch + 1]),
                 reads=[y.res, gain.res], writes=[ob.res])
            s.dma(proj_d[ch, :, sl], ob[:], reads=[ob.res], is_output=True)
        else:
            y2 = y2_r[n % R]
            s.op("act", lambda: nc.scalar.activation(out=y2[:], in_=y[:], func=AF.Copy, scale=gain[:, ch:ch + 1]),
                 reads=[y.res, gain.res], writes=[y2.res])
            s.op("pe", lambda: nc.tensor.matmul(pq[n % 2][:], lhsT=pm[:], rhs=y2[:], start=True, stop=True),
                 reads=[pm.res, y2.res], writes=[pq[n % 2].res])
            if ch in (0, 1):
                s.dma(proj_d[24 + ch, :, sl], y2[:], reads=[y2.res], is_output=True)

    def st3(n):
        ch, tt = items[n]
        if kind(ch) != "rope":
            return
        sl = slice(tt * TT, (tt + 1) * TT)
        y2 = y2_r[n % R]
        t1 = t1_r[n % R]
        y = y_r[n % R]
        ob = ob_r[n % R]
        s.op("pool", lambda: nc.gpsimd.tensor_tensor(out=t1[:], in0=y2[:], in1=COS[:, sl], op=ALU.mult), reads=[y2.res, COS.res], writes=[t1.res])
        s.op("dve", lambda: nc.vector.tensor_tensor(out=y[:], in0=pq[n % 2][:], in1=SIN[:, sl], op=ALU.mult),
             reads=[pq[n % 2].res, SIN.res], writes=[y.res])
        s.op("dve", lambda: nc.vector.tensor_tensor(out=ob[:], in0=t1[:], in1=y[:], op=ALU.add), reads=[t1.res, y.res], writes=[ob.res])
        s.dma(proj_d[ch, :, sl], ob[:], reads=[ob.res], is_output=True)

    pipeline(len(items), [stl, stc, st0, st1, st2, st3])
    c.end_phase(ph)
    if own:
        c.close()
    return c


def in_perm():
    Z = [-1] * 64
    r = lambda a, n: list(range(a, a + n))
    ch = []
    ch.append(r(0, 128)); ch.append(r(128, 128))
    ch.append(r(384, 64) + r(512, 64))
    ch.append(r(256, 64) + Z)
    ch.append(r(652, 128)); ch.append(r(780, 128))
    ch.append(r(908, 128)); ch.append(r(1036, 128))
    ch.append(r(2188, 128)); ch.append(r(2316, 128))
    ch.append(r(2444, 128)); ch.append(r(2572, 128))
    ch.append(r(320, 64) + r(448, 64))
    ch.append(r(576, 64) + Z)
    ch.append(r(1164, 128)); ch.append(r(1292, 128))
    ch.append(r(1420, 128)); ch.append(r(1548, 128))
    ch.append(r(1676, 128)); ch.append(r(1804, 128))
    ch.append(r(1932, 128)); ch.append(r(2060, 128))
    ch.append(r(2700, 128)); ch.append(r(2828, 128))
    ch.append(r(640, 12) + [-1] * 20 + r(2956, 4) + [-1] * 92)
    for g in range(32):
        ch.append(r(2960 + g * 128, 128))
    return np.array(ch, np.int64)


def consts_A():
    inv = (500000.0 ** (-np.arange(0, 16, 2, dtype=np.float32) / 16)).astype(np.float32)
    invf = np.zeros((128, 1), np.float32)
    pm = np.zeros((128, 128), np.float32)
    bones = np.zeros((128, 128), np.float32)
    for hb in (0, 64):
        bones[hb:hb + 64, hb:hb + 64] = 1.0
        for i in range(8):
            invf[hb + i, 0] = inv[i]
            invf[hb + 8 + i, 0] = inv[i]
            pm[hb + i + 8, hb + i] = -1.0
            pm[hb + i, hb + i + 8] = 1.0
    osc = np.ones((128, 12), np.float32)
    for chn in (0, 1, 4, 5, 8, 9):
        osc[:, chn] = 0.125
    return invf, pm, bones, osc


def maps_A(inp, l, xT_all, mod):
    perm = in_perm()
    w = inp["w_in"][l]
    wz = np.concatenate([w, np.zeros((D, 1), np.float32)], axis=1)
    wp = wz[:, perm.reshape(-1)].reshape(D, NCH_IN, 128)
    wp = np.ascontiguousarray(wp.reshape(8, 128, NCH_IN, 128).transpose(2, 1, 0, 3))
    invf, pm, bones, osc = consts_A()
    g = inp["qk_gain"][l]
    t2 = lambda a: np.concatenate([a, a])
    z64 = np.zeros(64, np.float32)
    gain = np.stack([t2(g[0]), t2(g[0]), np.concatenate([g[2], g[3]]), np.concatenate([g[1], z64]),
                     t2(g[4]), t2(g[4]), t2(g[5]), t2(g[5]), t2(g[6]), t2(g[6]), t2(g[7]), t2(g[7])], axis=1).astype(np.float32)
    foxb = np.zeros((128, 1), np.float32)
    foxb[32:36, 0] = inp["fox_bias"][l]
    gn = np.ascontiguousarray(inp["norm_mix"][l].reshape(8, 128).T)
    maps = []
    for i in range(8):
        b, q = i // 4, i % 4
        m = {"modA": np.ascontiguousarray(mod[l, b][:, 0:16]), "gn": gn,
             "pos": np.ascontiguousarray(inp["positions"][b:b + 1, q * NT:(q + 1) * NT]).astype(np.int32),
             "invf": invf, "w": wp, "gain": gain, "osc": osc, "foxb": foxb, "pm": pm, "bones": bones}
        if xT_all is not None:
            xs = xT_all[b][:, q * NT:(q + 1) * NT].reshape(8, 128, NT).transpose(1, 0, 2)
            m["xT"] = np.ascontiguousarray(xs)
        maps.append(m)
    return maps


def collect_A(results, pre=""):
    proj = [np.concatenate([results[b * 4 + q][pre + "proj"] for q in range(4)], axis=2) for b in range(2)]
    gates = [np.concatenate([results[b * 4 + q][pre + "gates"] for q in range(4)], axis=2) for b in range(2)]
    misc = [np.concatenate([results[b * 4 + q][pre + "misc"] for q in range(4)], axis=1) for b in range(2)]
    return proj, gates, misc


def run_A(cA, inp, l, xT_all, mod):
    maps = maps_A(inp, l, xT_all, mod)
    res = run_bass_kernel_spmd(cA.nc, maps, core_ids=list(range(8)))
    return collect_A(res.results)


B_MIXERS = ("dil", "fox", "sb", "nsa")
NKB = S // 128
NQT = S // TT
M_CAUSAL, M_STRICT, M_WIN, M_DIL, M_CMP, M_NEGC = 0, 4, 8, 16, 36, 41
N_MASKS = 45


def consts_B():
    import ml_dtypes
    k = np.arange(128)[:, None]
    cc = np.arange(512)[None, :]
    masks = np.zeros((N_MASKS, 128, 512), np.float32)
    for i in range(4):
        masks[M_CAUSAL + i] = (cc - k >= 128 * i)
        masks[M_STRICT + i] = (cc - k > 128 * i)
    for w in range(8):
        diff = cc - k + 512 - 128 * w
        masks[M_WIN + w] = (diff >= 0) & (diff < 512)
    for w in range(20):
        diff = cc - k + 2048 - 128 * w
        m = np.zeros((128, 512), np.float32)
        for (ww, d) in ((128, 1), (512, 4), (2048, 16)):
            m += ((diff % d == 0) & (diff >= 0) & (diff <= ww))
        masks[M_DIL + w] = m
    for u in range(5):
        masks[M_CMP + u] = (16 * k + 31 <= 512 * u + cc)
    for i in range(4):
        masks[M_NEGC + i] = -30000.0 * (cc - k < 128 * i)
    G = ((np.arange(S)[None, :] // 64) % 64 == np.arange(64)[:, None]).astype(np.float32)
    c0 = np.arange(511) * 16
    s0 = np.arange(128) * 64
    ov = ((c0[:, None] < s0[None, :] + 64) & (c0[:, None] + 32 > s0[None, :])).astype(np.float32)
    ov = np.concatenate([ov, np.zeros((1, 128), np.float32)], 0).reshape(4, 128, 128).transpose(1, 0, 2)
    onesc = np.ones((128, 4, 1), np.float32)
    onesc[127, 3, 0] = 0.0
    Rconst = np.concatenate([ov, onesc], axis=2)
    add = np.zeros((128, 254), np.float32)
    jj = np.arange(254)[None, :] - 126
    cr = (np.arange(128) // 64)[:, None]
    add[(jj == cr) | (jj == cr - 1)] = 1e30
    add[jj > cr] = -1e30
    jn = np.arange(128)[:, None]
    kn = np.arange(128)[None, :]
    nti = -(jn >= kn).astype(np.float32)
    ntc = -(jn < kn).astype(np.float32)
    bf = ml_dtypes.bfloat16
    return dict(masks=masks.astype(bf), G64=G.astype(bf), Rconst=Rconst.astype(bf), add=add,
                nti=nti.astype(bf), ntc=ntc.astype(bf), ident=np.eye(128, dtype=np.float32),
                tri64=(np.arange(64)[:, None] < np.arange(64)[None, :]).astype(np.float32))


def pipeline(n_items, stages):
    K = len(stages)
    for i in range(n_items + K - 1):
        for k, st in enumerate(stages):
            n = i - k
            if 0 <= n < n_items:
                st(n)


def build_B():
    c = Ctx()
    nc, s = c.nc, c.s
    di = lambda name, shape, dt: c.dram(name, shape, dt, "ExternalInput")
    qnr_d = di("qnr", [4, 64, S], BF16)
    qr_d = di("qr", [64, S], BF16)
    kcT_d = di("kcT", [64, S], BF16)
    vcT_d = di("vcT", [64, S], BF16)
    kslT_d = di("kslT", [64, S], BF16)
    kwT_d = di("kwT", [64, S], BF16)
    vsl_d = di("vsl", [128, NKB, 65], BF16)
    vw_d = di("vw", [128, NKB, 65], BF16)
    ag_d = di("ag", [128, NKB, 3], F32)
    dq_d = di("dq", [64, S], BF16)
    dk_d = di("dk", [64, S], BF16)
    dv_d = di("dv", [128, NKB, 65], BF16)
    sq_d = di("sq", [64, S], BF16)
    sk_d = di("sk", [64, S], BF16)
    sv_d = di("sv", [128, NKB, 65], BF16)
    fq_d = di("fq", [64, S], BF16)
    fk_d = di("fk", [64, S], BF16)
    fv_d = di("fv", [128, NKB, 65], BF16)
    lf_d = di("logf", [1, S], F32)
    w1k_d = di("w1k", [64, 32, 128], F32)
    w1v_d = di("w1v", [64, 32, 128], F32)
    w2k_d = di("w2k", [128, 64], F32)
    w2v_d = di("w2v", [128, 64], F32)
    pek_d = di("pek", [64, 32], F32)
    pev_d = di("pev", [64, 32], F32)
    masks_d = di("masks", [N_MASKS, 128, 512], BF16)
    G_d = di("G64", [64, S], BF16)
    Rc_d = di("Rconst", [128, 4, 129], BF16)
    add_d = di("add", [128, 254], F32)
    nti_d = di("nti", [128, 128], BF16)
    ntc_d = di("ntc", [128, 128], BF16)
    ident_d = di("ident", [128, 128], F32)
    tri_d = di("tri64", [64, 64], F32)
    o_d = c.dram("o", [4, 128, NKB, 64], BF16, "ExternalOutput")

    ps_ring = [c.ps([128, 512]) for _ in range(4)]
    po_ring = [c.ps([128, 512]) for _ in range(2)]
    pX = c.ps([128, 512])
    pY = c.ps([128, 512])
    e_ring = [c.sb([128, 512], BF16) for _ in range(4)]
    p_ring = [c.sb([128, 512], BF16) for _ in range(4)]
    pre_ring = [c.sb([128, 512], F32) for _ in range(4)]
    rz_ring = [c.sb([128, 4], F32) for _ in range(2)]
    fac_ring = [c.sb([128, 4], F32) for _ in range(2)]
    ost = c.sb([128, NKB, 64], BF16)

    class Scope:
        def __enter__(self):
            self.es = contextlib.ExitStack()
            self.old = c.es
            c.es = self.es
            return self

        def __exit__(self, *a):
            barrier(c)
            c.es = self.old
            self.es.close()

    def load_masks(lo, n, order=None):
        t = c.sb([128, n, 512], BF16)
        t.parts = [Res() for _ in range(n)]
        for ii, i in enumerate(order if order is not None else range(n)):
            s.dma(t[:, i, :], masks_d[lo + i], writes=[t.parts[i]], q=("sp", "act")[ii % 2])
        return t

    def split_load(QT, KT, V, qT_d, kT_d, v_d):
        qs = ("sp", "act")
        for t in (QT, KT, V):
            if t is not None:
                t.parts = [Res() for _ in range(4)]
        for h in range(4):
            sl = slice(h * 2048, (h + 1) * 2048)
            if QT is not None:
                s.dma(QT[0:64, sl], qT_d[:, sl], reads=[QT.res], writes=[QT.parts[h]], q=qs[h % 2])
            s.dma(KT[0:64, sl], kT_d[:, sl], reads=[KT.res], writes=[KT.parts[h]], q=qs[(h + 1) % 2])
            s.dma(V[:, 16 * h:16 * (h + 1), :], v_d[:, 16 * h:16 * (h + 1), :], reads=[V.res], writes=[V.parts[h]], q=qs[h % 2])

    def rQ(QT, qt):
        return [QT.res, QT.parts[qt // 4]]

    def rK(KT, kb):
        return [KT.res, KT.parts[kb // 16]]

    def attn(items, qk, pv, fin, mask_of, alt=[0], nps=4):
        def st0(n):
            qk(n, ps_ring[n % nps])

        def st1(n):
            it = items[n]
            ps, e = ps_ring[n % nps], e_ring[n % 4]
            if it["mi"] is not None and it["mi"] >= M_NEGC:
                mt, mi = mask_of(it["mi"])
                pre = pre_ring[n % 4]
                s.op("dve", lambda: nc.vector.tensor_tensor(out=pre[:], in0=ps[:], in1=mt[:, mi, :], op=ALU.add),
                     reads=[ps.res, mt.parts[mi]], writes=[pre.res])
                p = p_ring[n % 4]
                s.op("act", lambda: nc.scalar.activation(out=p[:], in_=pre[:], func=AF.Exp), reads=[pre.res], writes=[p.res])
                return
            s.op("act", lambda: nc.scalar.activation(out=e[:], in_=ps[:], func=AF.Exp), reads=[ps.res], writes=[e.res])
            if it["mi"] is not None:
                p = p_ring[n % 4]
                mt, mi = mask_of(it["mi"])
                alt[0] = 1
                if alt[0]:
                    s.op("dve", lambda: nc.vector.tensor_tensor(out=p[:], in0=e[:], in1=mt[:, mi, :], op=ALU.mult),
                         reads=[e.res, mt.parts[mi]], writes=[p.res])
                else:
                    s.op("pool", lambda: nc.gpsimd.tensor_tensor(out=p[:], in0=e[:], in1=mt[:, mi, :], op=ALU.mult),
                         reads=[e.res, mt.parts[mi]], writes=[p.res])

        def st2(n):
            it = items[n]
            pt = p_ring[n % 4] if it["mi"] is not None else e_ring[n % 4]
            pv(n, pt)
            if it["last"]:
                fin(n)
        pipeline(len(items), [st0, st1, (lambda n: None), st2])

    def std_pv(items, V, ncol=65):
        def pv(n, pt):
            it = items[n]
            po = po_ring[it["qt"] % 2]
            for qb in range(4):
                s.op("pe", (lambda qb=qb: nc.tensor.matmul(po[:, qb * 65:qb * 65 + ncol], lhsT=pt[:, qb * 128:(qb + 1) * 128],
                                                           rhs=V[:, it["kb"], 0:ncol], start=(it["first"] and qb == 0), stop=it["last"],
                                                           skip_group_check=True)),
                     reads=[pt.res, V.res, V.parts[it["kb"] // 16]], writes=[po.res])
        return pv

    def rz_of(qt, po, zoff=64, stride=65):
        rz = rz_ring[qt % 2]
        s.op("dve", lambda: nc.vector.tensor_scalar(out=rz[:], in0=po[:, zoff:zoff + 3 * stride + 1:stride], scalar1=1e-30, scalar2=None, op0=ALU.max),
             reads=[po.res], writes=[rz.res])
        s.op("dve", lambda: nc.vector.reciprocal(out=rz[:], in_=rz[:]), reads=[rz.res], writes=[rz.res])
        return rz

    def std_items(blocks_of):
        items = []
        for qt in range(NQT):
            bl = blocks_of(qt)
            for ii, (kb, mi) in enumerate(bl):
                items.append(dict(qt=qt, kb=kb, mi=mi, first=(ii == 0), last=(ii == len(bl) - 1)))
        return items

    def simple_mixer(m, qT_d, kT_d, v_d, blocks_of, mask_lo, mask_n, kdim=64, prep=None, mask_order=None):
        with Scope():
            QT = c.sb([128, S], BF16)
            KT = c.sb([128, S], BF16)
            V = c.sb([128, NKB, 65], BF16)
            if prep is not None:
                prep(QT, KT)
            split_load(QT, KT, V, qT_d, kT_d, v_d)
            mt = load_masks(mask_lo, mask_n, order=mask_order)
            items = std_items(blocks_of)

            def qk(n, ps):
                it = items[n]
                s.op("pe", lambda: nc.tensor.matmul(ps[:], lhsT=KT[0:kdim, it["kb"] * 128:(it["kb"] + 1) * 128],
                                                    rhs=QT[0:kdim, it["qt"] * 512:(it["qt"] + 1) * 512], start=True, stop=True),
                     reads=rK(KT, it["kb"]) + rQ(QT, it["qt"]), writes=[ps.res])

            def fin(n):
                qt = items[n]["qt"]
                po = po_ring[qt % 2]
                rz = rz_of(qt, po)
                for qb in range(4):
                    s.op("dve", (lambda qb=qb: nc.vector.tensor_scalar(out=ost[:, 4 * qt + qb, :], in0=po[:, qb * 65:qb * 65 + 64],
                                                                       scalar1=rz[:, qb:qb + 1], scalar2=None, op0=ALU.mult)),
                         reads=[po.res, rz.res], writes=[ost.res])
            attn(items, qk, std_pv(items, V), fin, lambda mi: (mt, mi - mask_lo))
            s.dma(o_d[m], ost[:], reads=[ost.res], is_output=True)

    def dil_blocks(qt):
        return [(4 * qt - 16 + w, M_DIL + w) for w in range(20) if 4 * qt - 16 + w >= 0]
    if "dil" in B_MIXERS:
        simple_mixer(1, dq_d, dk_d, dv_d, dil_blocks, M_DIL, 20, mask_order=[16, 17, 18, 19, 12, 13, 14, 15, 8, 9, 10, 11, 4, 5, 6, 7, 0, 1, 2, 3])

    fs_d = c.dram("fsplit", [6, S], BF16, "Internal")
    fs_res = Res()

    def fox_prep(QT, KT):
        lf = c.sb([64, 128], F32)
        F = c.sb([64, 128], F32)
        zr = c.sb([64, 128], F32)
        r1 = c.sb([64, 128], F32)
        off = c.sb([64, 1], F32)
        U = c.sb([64, 64], F32)
        sp3 = [c.sb([64, 128], BF16) for _ in range(3)]
        ng3 = [c.sb([64, 128], BF16) for _ in range(3)]
        s.dma(lf[:], lf_d.rearrange("o (p j) -> (o p) j", j=128), writes=[lf.res])
        s.dma(U[:], tri_d, writes=[U.res])
        s.op("pool", lambda: nc.gpsimd.memset(zr[:], 0.0), writes=[zr.res])
        s.op("pool", lambda: nc.gpsimd.memset(QT[:], 0.0), writes=[QT.res])
        s.op("pool", lambda: nc.gpsimd.memset(KT[:], 0.0), writes=[KT.res])
        s.op("pool", lambda: nc.gpsimd.memset(QT[96:99, :], 1.0), writes=[QT.res])
        s.op("pool", lambda: nc.gpsimd.memset(KT[64:67, :], 1.0), writes=[KT.res])
        s.op("dve", lambda: nc.vector.tensor_tensor_scan(out=F[:], data0=lf[:], data1=zr[:], initial=0.0, op0=ALU.add, op1=ALU.add),
             reads=[lf.res, zr.res], writes=[F.res])
        s.op("pe", lambda: nc.tensor.matmul(pY[0:64, 0:1], lhsT=U[:], rhs=F[:, 127:128], start=True, stop=True),
             reads=[U.res, F.res], writes=[pY.res])
        s.op("act", lambda: nc.scalar.copy(out=off[:], in_=pY[0:64, 0:1]), reads=[pY.res], writes=[off.res])
        s.op("dve", lambda: nc.vector.tensor_scalar(out=F[:], in0=F[:], scalar1=off[:, 0:1], scalar2=None, op0=ALU.add),
             reads=[F.res, off.res], writes=[F.res])
        cur = F
        for i in range(3):
            s.op("dve", (lambda i=i, cur=cur: nc.vector.tensor_copy(out=sp3[i][:], in_=cur[:])), reads=[cur.res], writes=[sp3[i].res])
            s.op("dve", (lambda i=i: nc.vector.tensor_scalar(out=ng3[i][:], in0=sp3[i][:], scalar1=-1.0, scalar2=None, op0=ALU.mult)),
                 reads=[sp3[i].res], writes=[ng3[i].res])
            if i < 2:
                s.op("dve", (lambda i=i, cur=cur: nc.vector.tensor_tensor(out=r1[:], in0=cur[:], in1=sp3[i][:], op=ALU.subtract)),
                     reads=[cur.res, sp3[i].res], writes=[r1.res])
                cur = r1
            s.dma(fs_d[i, :].rearrange("(p j) -> p j", j=128), sp3[i][:], reads=[sp3[i].res], writes=[fs_res])
            s.dma(fs_d[3 + i, :].rearrange("(p j) -> p j", j=128), ng3[i][:], reads=[ng3[i].res], writes=[fs_res])
        s.dma(QT[64:67, :], fs_d[0:3, :], reads=[fs_res], writes=[QT.res])
        s.dma(KT[96:99, :], fs_d[3:6, :], reads=[fs_res], writes=[KT.res])

    def causal_blocks(qt):
        return [(kb, None) for kb in range(4 * qt)] + [(4 * qt + i, M_CAUSAL + i) for i in range(4)]

    def negc_blocks(qt):
        return [(kb, None) for kb in range(4 * qt)] + [(4 * qt + i, M_NEGC + i) for i in range(4)]
    if "fox" in B_MIXERS:
        simple_mixer(3, fq_d, fk_d, fv_d, negc_blocks, M_NEGC, 4, kdim=99, prep=fox_prep)

    with (Scope() if "sb" in B_MIXERS else contextlib.nullcontext()):
      if "sb" in B_MIXERS:
        QT = c.sb([64, S], BF16)
        KT = c.sb([64, S], BF16)
        V = c.sb([128, NKB, 65], BF16)
        nti = c.sb([128, 128], BF16)
        ntc = c.sb([128, 128], BF16)
        split_load(QT, KT, V, sq_d, sk_d, sv_d)
        s.dma(nti[:], nti_d, writes=[nti.res])
        s.dma(ntc[:], ntc_d, writes=[ntc.res])
        mt = load_masks(M_STRICT, 4)
        E_r = [c.sb([128, 512], F32) for _ in range(4)]
        L_r = [c.sb([128, 512], BF16) for _ in range(4)]
        X_r = [c.sb([128, 512], F32) for _ in range(4)]
        A_r = [c.sb([128, 512], BF16) for _ in range(4)]
        def sb_blocks(qt):
            bl = [(4 * qt + i, M_STRICT + i) for i in (3, 2, 1, 0)] + [(kb, None) for kb in range(4 * qt - 1, -1, -1)]
            return [dict(qt=qt, kb=kb, mi=mi, first=(ii == 0), last=(ii == len(bl) - 1)) for ii, (kb, mi) in enumerate(bl)]
        items = []
        for pr in range(NQT // 2):
            la, lb = sb_blocks(2 * pr), sb_blocks(2 * pr + 1)
            for ii in range(len(lb)):
                if ii < len(la):
                    items.append(la[ii])
                items.append(lb[ii])
        pXs = (pX, pY)

        def sb0(n):
            it = items[n]
            ps = ps_ring[n % 4]
            s.op("pe", lambda: nc.tensor.matmul(ps[:], lhsT=KT[:, it["kb"] * 128:(it["kb"] + 1) * 128],
                                                rhs=QT[:, it["qt"] * 512:(it["qt"] + 1) * 512], start=True, stop=True),
                 reads=rK(KT, it["kb"]) + rQ(QT, it["qt"]), writes=[ps.res])

        def sb1(n):
            it = items[n]
            ps, E, L = ps_ring[n % 4], E_r[n % 4], L_r[n % 4]
            s.op("act", lambda: nc.scalar.activation(out=E[:], in_=ps[:], func=AF.Exp), reads=[ps.res], writes=[E.res])
            s.op("act", lambda: nc.scalar.activation(out=L[:], in_=E[:], func=AF.Ln, bias=1.0, scale=1.0), reads=[E.res], writes=[L.res])
            if it["mi"] is not None:
                mi = it["mi"] - M_STRICT
                s.op("pool", lambda: nc.gpsimd.tensor_tensor(out=L[:], in0=L[:], in1=mt[:, mi, :], op=ALU.mult),
                     reads=[L.res, mt.parts[mi]], writes=[L.res])
                s.op("dve", lambda: nc.vector.tensor_tensor(out=E[:], in0=E[:], in1=mt[:, mi, :], op=ALU.mult),
                     reads=[E.res, mt.parts[mi]], writes=[E.res])

        def sb2(n):
            it = items[n]
            L, X = L_r[n % 4], X_r[n % 4]
            pXq = pXs[it["qt"] % 2]
            s.op("pe", lambda: nc.tensor.matmul(pXq[:], lhsT=nti[:], rhs=L[:], start=it["first"], stop=False, skip_group_check=True),
                 reads=[nti.res, L.res], writes=[pXq.res])
            s.op("act", lambda: nc.scalar.activation(out=X[:], in_=pXq[:], func=AF.Exp), reads=[pXq.res], writes=[X.res])

        def sb3(n):
            it = items[n]
            L, X, E, A = L_r[n % 4], X_r[n % 4], E_r[n % 4], A_r[n % 4]
            pXq = pXs[it["qt"] % 2]
            s.op("pe", lambda: nc.tensor.matmul(pXq[:], lhsT=ntc[:], rhs=L[:], start=False, stop=it["last"], skip_group_check=True),
                 reads=[ntc.res, L.res], writes=[pXq.res])
            s.op("dve", lambda: nc.vector.tensor_tensor(out=A[:], in0=E[:], in1=X[:], op=ALU.mult), reads=[E.res, X.res], writes=[A.res])

        def sb4(n):
            it = items[n]
            A = A_r[n % 4]
            qt = it["qt"]
            po = po_ring[qt % 2]
            for qb in range(4):
                s.op("pe", (lambda qb=qb: nc.tensor.matmul(po[:, qb * 65:qb * 65 + 64], lhsT=A[:, qb * 128:(qb + 1) * 128],
                                                           rhs=V[:, it["kb"], 0:64], start=(it["first"] and qb == 0), stop=it["last"],
                                                           skip_group_check=True)),
                     reads=[A.res, V.res, V.parts[it["kb"] // 16]], writes=[po.res])
            if it["last"]:
                for qb in range(4):
                    s.op("act", (lambda qb=qb: nc.scalar.copy(out=ost[:, 4 * qt + qb, :], in_=po[:, qb * 65:qb * 65 + 64])),
                         reads=[po.res], writes=[ost.res])
        K_ = len(items)
        for i in range(K_ + 4):
            if i < K_:
                sb0(i)
            if 0 <= i - 1 < K_:
                sb1(i - 1)
            if 0 <= i - 3 < K_:
                sb3(i - 3)
            if 0 <= i - 2 < K_:
                sb2(i - 2)
            if 0 <= i - 4 < K_:
                sb4(i - 4)
        s.dma(o_d[2], ost[:], reads=[ost.res], is_output=True)

    hsel_d = di("hsel", [128, 4], F32)
    with (Scope() if "nsa" in B_MIXERS else contextlib.nullcontext()):
      if "nsa" in B_MIXERS:
        QA = c.sb([128, S], BF16)
        QB = c.sb([128, S], BF16)
        oa = c.sb([128, NKB, 64], F32)
        oa.parts = [Res() for _ in range(NKB)]
        ag = c.sb([128, NKB, 3], F32)
        ident = c.sb([128, 128], F32)
        addt = c.sb([128, 254], F32)
        hsel = c.sb([128, 4], F32)
        for h in range(4):
            sl = slice(h * 2048, (h + 1) * 2048)
            s.dma(QA[0:64, sl], qr_d[:, sl], writes=[QA.res], q="act")
            s.dma(QB[0:64, sl], qr_d[:, sl], writes=[QB.res], q="act")
        for t, d in ((ag, ag_d), (ident, ident_d), (addt, add_d), (hsel, hsel_d)):
            s.dma(t[:], d, writes=[t.res])
        with Scope():
            kcT = c.sb([64, S], BF16)
            vcT = c.sb([64, S], BF16)
            for h in range(4):
                sl = slice(h * 2048, (h + 1) * 2048)
                s.dma(kcT[:, sl], kcT_d[:, sl], writes=[kcT.res])
                s.dma(vcT[:, sl], vcT_d[:, sl], writes=[vcT.res])
            kccT = c.sb([64, 512], BF16)
            Rt = c.sb([128, 4, 193], BF16)
            s.dma(Rt[:, :, 0:129], Rc_d, writes=[Rt.res])
            w1f = c.sb([64, 32, 128], F32)
            w1b = c.sb([64, 32, 128], BF16)
            w2f = c.sb([128, 64], F32)
            w2b = c.sb([128, 64], BF16)
            pef = c.sb([64, 32], F32)
            peb = c.sb([128, 1], F32)
            xg = c.sb([128, 512], F32)
            x2 = c.sb([128, 512], F32)
            gT = c.sb([128, 512], BF16)
            for which in ("k", "v"):
                src = kcT if which == "k" else vcT
                s.dma(w1f[:], w1k_d if which == "k" else w1v_d, writes=[w1f.res])
                s.dma(w2f[:], w2k_d if which == "k" else w2v_d, writes=[w2f.res])
                s.dma(pef[:], pek_d if which == "k" else pev_d, writes=[pef.res])
                s.op("pool", lambda: nc.gpsimd.tensor_copy(out=w1b[:], in_=w1f[:]), reads=[w1f.res], writes=[w1b.res])
                s.op("pool", lambda: nc.gpsimd.tensor_copy(out=w2b[:], in_=w2f[:]), reads=[w2f.res], writes=[w2b.res])
                for l in range(32):
                    s.op("pe", (lambda l=l: nc.tensor.matmul(pY[:, 0:1], lhsT=w1f[:, l, :], rhs=pef[:, l:l + 1], start=(l == 0), stop=(l == 31))),
                         reads=[w1f.res, pef.res], writes=[pY.res])
                s.op("act", lambda: nc.scalar.copy(out=peb[:], in_=pY[:, 0:1]), reads=[pY.res], writes=[peb.res])
                srcv = src[:].rearrange("p (c s) -> p c s", s=16)
                for l in range(32):
                    rhs = srcv[:, 0:511, l] if l < 16 else srcv[:, 1:512, l - 16]
                    s.op("pe", (lambda l=l, rhs=rhs: nc.tensor.matmul(pX[:, 0:511], lhsT=w1b[:, l, :], rhs=rhs, start=(l == 0), stop=(l == 31))),
                         reads=[w1b.res, src.res], writes=[pX.res])
                s.op("act", lambda: nc.scalar.activation(out=xg[:, 0:511], in_=pX[:, 0:511], func=AF.Identity, bias=peb[:], scale=1.0),
                     reads=[pX.res, peb.res], writes=[xg.res])
                s.op("dve", lambda: nc.vector.tensor_tensor(out=x2[:, 0:511], in0=xg[:, 0:511], in1=xg[:, 0:511], op=ALU.mult), reads=[xg.res], writes=[x2.res])
                s.op("dve", lambda: nc.vector.tensor_scalar(out=x2[:, 0:511], in0=x2[:, 0:511], scalar1=0.044715, scalar2=1.0, op0=ALU.mult, op1=ALU.add),
                     reads=[x2.res], writes=[x2.res])
                s.op("dve", lambda: nc.vector.tensor_tensor(out=x2[:, 0:511], in0=x2[:, 0:511], in1=xg[:, 0:511], op=ALU.mult), reads=[x2.res, xg.res], writes=[x2.res])
                s.op("act", lambda: nc.scalar.activation(out=x2[:, 0:511], in_=x2[:, 0:511], func=AF.Sigmoid, scale=1.5957691216057308),
                     reads=[x2.res], writes=[x2.res])
                s.op("pool", lambda: nc.gpsimd.memset(gT[:], 0.0), writes=[gT.res])
                s.op("dve", lambda: nc.vector.tensor_tensor(out=gT[:, 0:511], in0=xg[:, 0:511], in1=x2[:, 0:511], op=ALU.mult), reads=[xg.res, x2.res, gT.res], writes=[gT.res])
                if which == "k":
                    s.op("pe", lambda: nc.tensor.matmul(pY[0:64, :], lhsT=w2b[:], rhs=gT[:], start=True, stop=True), reads=[w2b.res, gT.res], writes=[pY.res])
                    s.op("act", lambda: nc.scalar.copy(out=kccT[:], in_=pY[0:64, :]), reads=[pY.res], writes=[kccT.res])
                else:
                    for cc in range(4):
                        s.op("pe", (lambda cc=cc: nc.tensor.matmul(pY[:, cc * 64:(cc + 1) * 64], lhsT=gT[:, cc * 128:(cc + 1) * 128], rhs=w2b[:],
                                                                   start=True, stop=True)),
                             reads=[w2b.res, gT.res], writes=[pY.res])
                    s.op("act", lambda: nc.scalar.copy(out=Rt[:, :, 129:193], in_=pY[:, 0:256].rearrange("p (a b) -> p a b", b=64)),
                         reads=[pY.res], writes=[Rt.res])
            mt = load_masks(M_CMP, 5)
            aghs = c.sb([128, 4, NKB], F32)
            for h in range(4):
                s.op("dve", (lambda h=h: nc.vector.tensor_scalar(out=aghs[:, h, :], in0=ag[:, :, 0], scalar1=hsel[:, h:h + 1], scalar2=None, op0=ALU.mult)),
                     reads=[ag.res, hsel.res], writes=[aghs.res])
            qring = [c.sb([64, 512], BF16) for _ in range(4)]
            imp = [c.sb([128, 4, 128], F32) for _ in range(2)]
            for t in imp:
                t.parts = [Res() for _ in range(4)]
            sc_r = [c.sb([128, 128], F32) for _ in range(4)]
            sc2_r = [c.sb([128, 128], F32) for _ in range(4)]
            sb_r = [c.sb([128, 128], F32) for _ in range(4)]
            sbr_r = [c.sb([128, 128], F32) for _ in range(4)]
            for t in sbr_r:
                s.op("pool", (lambda t=t: nc.gpsimd.memset(t[:], 0.0)), writes=[t.res])
            m8_r = [c.sb([128, 16], F32) for _ in range(4)]
            pT = ps_ring[3]
            usets = [(po_ring[0], po_ring[1]), (pX, pY)]
            items = []
            for qt in range(NQT):
                for h in range(4):
                    ccs = [cc for cc in range(4) if qt - 4 * cc >= 0]
                    for ii, cc in enumerate(ccs):
                        u = qt - 4 * cc
                        items.append(dict(qt=qt, h=h, kb=cc, mi=(M_CMP + u if u <= 4 else None), first=(ii == 0), last=(ii == len(ccs) - 1)))

            def qk_c(n, ps):
                it = items[n]
                qtile = qring[(it["qt"] * 4 + it["h"]) % 4]
                if it["first"]:
                    s.dma(qtile[:], qnr_d[it["h"], :, it["qt"] * 512:(it["qt"] + 1) * 512], writes=[qtile.res])
                s.op("pe", lambda: nc.tensor.matmul(ps[:], lhsT=kccT[:, it["kb"] * 128:(it["kb"] + 1) * 128], rhs=qtile[:], start=True, stop=True),
                     reads=[kccT.res, qtile.res], writes=[ps.res])

            def pv_c(n, pt):
                it = items[n]
                us = usets[(it["qt"] * 4 + it["h"]) % 2]
                for qb in range(4):
                    tl = us[qb // 2]
                    off = (qb % 2) * 193
                    s.op("pe", (lambda qb=qb, tl=tl, off=off: nc.tensor.matmul(tl[:, off:off + 193], lhsT=pt[:, qb * 128:(qb + 1) * 128], rhs=Rt[:, it["kb"], :],
                                                                             start=(it["first"] and qb % 2 == 0), stop=it["last"], skip_group_check=True)),
                         reads=[pt.res, Rt.res], writes=[tl.res])

            def fin_c(n):
                it = items[n]
                qt, h = it["qt"], it["h"]
                us = usets[(qt * 4 + h) % 2]
                rz = rz_ring[h % 2]
                fac = fac_ring[h % 2]
                imp_t = imp[qt % 2]
                for half in range(2):
                    tl = us[half]
                    s.op("dve", (lambda half=half, tl=tl: nc.vector.tensor_scalar(out=rz[:, 2 * half:2 * half + 2], in0=tl[:, 128:322:193], scalar1=1e-30,
                                                                                scalar2=None, op0=ALU.max)), reads=[tl.res], writes=[rz.res])
                s.op("dve", lambda: nc.vector.reciprocal(out=rz[:], in_=rz[:]), reads=[rz.res], writes=[rz.res])
                s.op("dve", lambda: nc.vector.tensor_tensor(out=fac[:], in0=rz[:], in1=aghs[:, h, 4 * qt:4 * qt + 4], op=ALU.mult), reads=[rz.res, aghs.res], writes=[fac.res])
                for qb in range(4):
                    tl, off = us[qb // 2], (qb % 2) * 193
                    tb = 4 * qt + qb
                    if h == 0:
                        s.op("dve", (lambda qb=qb, tl=tl, off=off: nc.vector.tensor_scalar(out=imp_t[:, qb, :], in0=tl[:, off:off + 128], scalar1=rz[:, qb:qb + 1],
                                                                                         scalar2=None, op0=ALU.mult)), reads=[tl.res, rz.res], writes=[imp_t.parts[qb]])
                        s.op("dve", (lambda qb=qb, tl=tl, off=off, tb=tb: nc.vector.tensor_scalar(out=oa[:, tb, :], in0=tl[:, off + 129:off + 193], scalar1=fac[:, qb:qb + 1],
                                                                                                scalar2=None, op0=ALU.mult)), reads=[tl.res, fac.res], writes=[oa.parts[tb]])
                    else:
                        s.op("dve", (lambda qb=qb, tl=tl, off=off: nc.vector.scalar_tensor_tensor(out=imp_t[:, qb, :], in0=tl[:, off:off + 128], scalar=rz[:, qb:qb + 1],
                                                                                                in1=imp_t[:, qb, :], op0=ALU.mult, op1=ALU.add)),
                             reads=[tl.res, rz.res, imp_t.parts[qb]], writes=[imp_t.parts[qb]])
                        s.op("dve", (lambda qb=qb, tl=tl, off=off, tb=tb: nc.vector.scalar_tensor_tensor(out=oa[:, tb, :], in0=tl[:, off + 129:off + 193], scalar=fac[:, qb:qb + 1],
                                                                                                       in1=oa[:, tb, :], op0=ALU.mult, op1=ALU.add)),
                             reads=[tl.res, fac.res, oa.parts[tb]], writes=[oa.parts[tb]])
                if h != 3:
                    return
                for qb in range(4):
                    tb = 4 * qt + qb
                    sc, sc2, sbt, m8 = sc_r[qb], sc2_r[qb], sb_r[qb], m8_r[qb]
                    s.op("dve", (lambda qb=qb, sc=sc, tb=tb: nc.vector.tensor_tensor(out=sc[:], in0=imp_t[:, qb, :], in1=addt[:, 126 - 2 * tb:254 - 2 * tb], op=ALU.add)),
                         reads=[imp_t.parts[qb], addt.res], writes=[sc.res])
                    s.op("dve", (lambda sc=sc: nc.vector.memset(sc[:, 0:1], 1e30)), reads=[sc.res], writes=[sc.res])
                    s.op("dve", (lambda sc=sc, m8=m8: nc.vector.max(out=m8[:, 0:8], in_=sc[:])), reads=[sc.res], writes=[m8.res])
                    s.op("dve", (lambda sc=sc, sc2=sc2, m8=m8: nc.vector.match_replace(out=sc2[:], in_to_replace=m8[:, 0:8], in_values=sc[:], imm_value=-3e38)),
                         reads=[sc.res, m8.res], writes=[sc2.res])
                    s.op("dve", (lambda sc2=sc2, m8=m8: nc.vector.max(out=m8[:, 8:16], in_=sc2[:])), reads=[sc2.res, m8.res], writes=[m8.res])
                    s.op("dve", (lambda sc=sc, sbt=sbt, m8=m8: nc.vector.tensor_scalar(out=sbt[:], in0=sc[:], scalar1=m8[:, 15:16], scalar2=-30000.0,
                                                                                   op0=ALU.is_lt, op1=ALU.mult)), reads=[sc.res, m8.res], writes=[sbt.res])
                    sbr = sbr_r[qb]
                    s.op("dve", (lambda sc=sc, sbr=sbr, m8=m8: nc.vector.tensor_scalar(out=sbr[:, 64:128], in0=sc[:, 0:64], scalar1=m8[:, 15:16], scalar2=-30000.0,
                                                                                   op0=ALU.is_lt, op1=ALU.mult)), reads=[sc.res, m8.res], writes=[sbr.res])
                for qb in range(4):
                    s.op("pe", (lambda qb=qb: nc.tensor.transpose(pT[:, qb * 128:(qb + 1) * 128], sb_r[qb][:], ident[:])),
                         reads=[sb_r[qb].res, ident.res], writes=[pT.res])
                s.op("act", lambda: nc.scalar.copy(out=QB[64:128, qt * 512:(qt + 1) * 512], in_=pT[64:128, :]), reads=[pT.res], writes=[QB.res])
                for qb in range(4):
                    s.op("pe", (lambda qb=qb: nc.tensor.transpose(pT[:, qb * 128:(qb + 1) * 128], sbr_r[qb][:], ident[:])),
                         reads=[sbr_r[qb].res, ident.res], writes=[pT.res])
                s.op("act", lambda: nc.scalar.copy(out=QA[64:128, qt * 512:(qt + 1) * 512], in_=pT[64:128, :]), reads=[pT.res], writes=[QA.res])
            attn(items, qk_c, pv_c, fin_c, lambda mi: (mt, mi - M_CMP), nps=3)

        with Scope():
            KS = c.sb([128, S], BF16)
            KW = c.sb([64, S], BF16)
            VS = c.sb([128, NKB, 65], BF16)
            VW = c.sb([128, NKB, 65], BF16)
            mtc = load_masks(M_CAUSAL, 4)
            for h in range(4):
                sl = slice(h * 2048, (h + 1) * 2048)
                s.dma(KS[64:128, sl], G_d[:, sl], writes=[KS.res], q="act")
            split_load(None, KS, VS, None, kslT_d, vsl_d)
            split_load(None, KW, VW, None, kwT_d, vw_d)
            mtw = load_masks(M_WIN, 8)

            def branch(KT, V, blocks_of, mt, mask_lo, br, with_sel):
                items = std_items(blocks_of)

                def qk(n, ps):
                    it = items[n]
                    kd = 128 if with_sel else 64
                    Qx = (QA if it["kb"] < 32 else QB) if with_sel else QA
                    s.op("pe", lambda: nc.tensor.matmul(ps[:], lhsT=KT[0:kd, it["kb"] * 128:(it["kb"] + 1) * 128],
                                                        rhs=Qx[0:kd, it["qt"] * 512:(it["qt"] + 1) * 512], start=True, stop=True),
                         reads=rK(KT, it["kb"]) + [Qx.res], writes=[ps.res])

                def fin(n):
                    qt = items[n]["qt"]
                    po = po_ring[qt % 2]
                    rz = rz_of(qt, po)
                    fac = fac_ring[qt % 2]
                    s.op("dve", lambda: nc.vector.tensor_tensor(out=fac[:], in0=rz[:], in1=ag[:, 4 * qt:4 * qt + 4, br], op=ALU.mult),
                         reads=[rz.res, ag.res], writes=[fac.res])
                    for qb in range(4):
                        tb = 4 * qt + qb
                        s.op("dve", (lambda qb=qb, tb=tb: nc.vector.scalar_tensor_tensor(out=oa[:, tb, :], in0=po[:, qb * 65:qb * 65 + 64], scalar=fac[:, qb:qb + 1],
                                                                                         in1=oa[:, tb, :], op0=ALU.mult, op1=ALU.add)),
                             reads=[po.res, fac.res, oa.parts[tb]], writes=[oa.parts[tb]])
                attn(items, qk, std_pv(items, V), fin, lambda mi: (mt, mi - mask_lo))
            if "nosel" not in B_MIXERS:
                branch(KS, VS, causal_blocks, mtc, M_CAUSAL, 1, True)
            if "nowin" not in B_MIXERS:
                branch(KW, VW, lambda qt: [(4 * qt - 4 + w, M_WIN + w) for w in range(8) if 4 * qt - 4 + w >= 0], mtw, M_WIN, 2, False)
        s.op("act", lambda: nc.scalar.copy(out=ost[:], in_=oa[:]), reads=[oa.res] + oa.parts, writes=[ost.res])
        s.dma(o_d[0], ost[:], reads=[ost.res], is_output=True)
    c.close()
    return c


def run_B(cB, inp, l, proj, misc):
    import ml_dtypes
    bf = ml_dtypes.bfloat16
    cst = consts_B()

    def head_rows(P, ch0, i):
        return np.ascontiguousarray(P[ch0 + i // 2][(i % 2) * 64:(i % 2) * 64 + 64])

    def vaug(vT):
        v = vT.T.reshape(NKB, 128, 64).transpose(1, 0, 2)
        return np.ascontiguousarray(np.concatenate([v, np.ones((128, NKB, 1), bf)], axis=2))
    cl = 32 * 64
    w1k = np.ascontiguousarray(inp["nsa_ck_w1"][l].reshape(32, 64, 128).transpose(1, 0, 2))
    w1v = np.ascontiguousarray(inp["nsa_cv_w1"][l].reshape(32, 64, 128).transpose(1, 0, 2))
    pek = np.ascontiguousarray(inp["nsa_pe_k"][l].T)
    pev = np.ascontiguousarray(inp["nsa_pe_v"][l].T)
    maps = []
    for core in range(8):
        b, i = core // 4, core % 4
        P = proj[b]
        m = dict(cst)
        m["qnr"] = np.ascontiguousarray(np.stack([head_rows(P, 24, h) for h in range(4)]))
        m["qr"] = head_rows(P, 0, i)
        m["kcT"] = np.ascontiguousarray(P[3][0:64]); m["vcT"] = np.ascontiguousarray(P[12][0:64])
        m["kslT"] = np.ascontiguousarray(P[2][0:64]); m["kwT"] = np.ascontiguousarray(P[2][64:128])
        m["vsl"] = vaug(P[12][64:128]); m["vw"] = vaug(P[13][0:64])
        ag = misc[b][3 * i:3 * i + 3]
        m["ag"] = np.ascontiguousarray(ag.T.reshape(NKB, 128, 3).transpose(1, 0, 2))
        m["dq"] = head_rows(P, 4, i); m["dk"] = head_rows(P, 6, i); m["dv"] = vaug(head_rows(P, 14, i))
        m["sq"] = head_rows(P, 16, i); m["sk"] = head_rows(P, 18, i); m["sv"] = vaug(head_rows(P, 20, i))
        m["fq"] = head_rows(P, 8, i); m["fk"] = head_rows(P, 10, i); m["fv"] = vaug(head_rows(P, 22, i))
        m["logf"] = np.ascontiguousarray(misc[b][32 + i:33 + i])
        m["w1k"] = w1k; m["w1v"] = w1v; m["w2k"] = inp["nsa_ck_w2"][l]; m["w2v"] = inp["nsa_cv_w2"][l]
        m["pek"] = pek; m["pev"] = pev
        hs = np.zeros((128, 4), np.float32); hs[:, i] = 1.0
        m["hsel"] = hs
        maps.append(m)
    res = run_bass_kernel_spmd(cB.nc, maps, core_ids=list(range(8)))
    outs = []
    for b in range(2):
        ob = np.zeros((4, 256, S), bf)
        for i in range(4):
            o = res.results[b * 4 + i]["o"]
            for mm in range(4):
                tok = o[mm].transpose(1, 0, 2).reshape(S, 64)
                ob[mm, i * 64:(i + 1) * 64, :] = tok.T
        outs.append(ob)
    return outs


def build_C1(c=None):
    own = c is None
    if own:
        c = Ctx()
    ph = c.begin_phase()
    nc, s = c.nc, c.s
    oT_d = c.dram("oT", [4, 2, 128, NT], BF16, "ExternalInput")
    gates_d = c.dram("gates", [32, 128, NT], F32, "ExternalInput")
    xT_d = c.dram("xT", [128, 8, NT], F32, "ExternalInput")
    wbr_d = c.dram("wbr", [128, 8, 1024], F32, "ExternalInput")
    wout_d = c.dram("wout", [128, 8, 1024], F32, "ExternalInput")
    ga_d = c.dram("ga", [128, 8], F32, "ExternalInput")
    x1_d = c.dram("x1T", [128, 8, NT], F32, "ExternalOutput")

    xT = c.sb([128, 8, NT], F32)
    wbr = c.sb([128, 8, 1024], BF16)
    wout = c.sb([128, 8, 1024], BF16)
    ga = c.sb([128, 8], F32)
    stg = [c.sb([128, 2, 1024], F32) for _ in range(2)]
    s.dma(ga[:], ga_d, writes=[ga.res])
    for j in range(8):
        s.dma(xT[:, j, :], xT_d[:, j, :], writes=[xT.res])
    k = 0
    for (dst, src) in ((wbr, wbr_d), (wout, wout_d)):
        for q in range(4):
            st = stg[k % 2]
            k += 1
            s.dma(st[:], src[:, 2 * q:2 * q + 2, :], writes=[st.res])
            s.op("pool", (lambda st=st, dst=dst, q=q: nc.gpsimd.tensor_copy(out=dst[:, 2 * q:2 * q + 2, :], in_=st[:])), reads=[st.res], writes=[dst.res])
    ot_r = [c.sb([128, 8, TT], BF16) for _ in range(2)]
    gt_r = [[c.sb([128, TT], F32) for _ in range(4)] for _ in range(3)]
    zT_r = [c.sb([128, 8, TT], BF16) for _ in range(2)]
    pm_r = [c.ps([128, TT]) for _ in range(4)]
    pmix = [c.ps([128, TT]) for _ in range(2)]
    t_r = [c.sb([128, TT], F32) for _ in range(4)]
    items = [(tt, dc) for tt in range(NT // TT) for dc in range(8)]

    def c_load(n):
        tt, dc = items[n]
        sl = slice(tt * TT, (tt + 1) * TT)
        if dc == 0:
            ot = ot_r[tt % 2]
            for m in range(4):
                for kc in range(2):
                    s.dma(ot[:, m * 2 + kc, :], oT_d[m, kc, :, sl], writes=[ot.res], q="act")
        gts = gt_r[n % 3]
        for m in range(4):
            s.dma(gts[m][:], gates_d[m * 8 + dc, :, sl], writes=[gts[m].res], q=("sp", "act")[m % 2])

    def c_comp(n):
        tt, dc = items[n]
        sl = slice(tt * TT, (tt + 1) * TT)
        ot = ot_r[tt % 2]
        gts = gt_r[n % 3]
        zT = zT_r[tt % 2]
        for m in range(4):
            for kc in range(2):
                s.op("pe", (lambda m=m, kc=kc: nc.tensor.matmul(pm_r[m][:], lhsT=wbr[:, m * 2 + kc, dc * 128:(dc + 1) * 128], rhs=ot[:, m * 2 + kc, :],
                                                                start=(kc == 0), stop=(kc == 1))),
                     reads=[wbr.res, ot.res], writes=[pm_r[m].res])
        for m in range(4):
            s.op("dve", (lambda m=m: nc.vector.tensor_tensor(out=t_r[m][:], in0=pm_r[m][:], in1=gts[m][:], op=ALU.mult)),
                 reads=[pm_r[m].res, gts[m].res], writes=[t_r[m].res])
        s.op("pool", lambda: nc.gpsimd.tensor_tensor(out=t_r[0][:], in0=t_r[0][:], in1=t_r[1][:], op=ALU.add), reads=[t_r[0].res, t_r[1].res], writes=[t_r[0].res])
        s.op("pool", lambda: nc.gpsimd.tensor_tensor(out=t_r[2][:], in0=t_r[2][:], in1=t_r[3][:], op=ALU.add), reads=[t_r[2].res, t_r[3].res], writes=[t_r[2].res])
        s.op("pool", lambda: nc.gpsimd.tensor_tensor(out=zT[:, dc, :], in0=t_r[0][:], in1=t_r[2][:], op=ALU.add),
             reads=[t_r[0].res, t_r[2].res], writes=[zT.res])
        if dc != 7:
            return
        for ec in range(8):
            pmx = pmix[ec % 2]
            for j in range(8):
                s.op("pe", (lambda j=j, ec=ec, pmx=pmx: nc.tensor.matmul(pmx[:], lhsT=wout[:, j, ec * 128:(ec + 1) * 128], rhs=zT[:, j, :],
                                                                         start=(j == 0), stop=(j == 7))),
                     reads=[wout.res, zT.res], writes=[pmx.res])
            s.op("dve", (lambda ec=ec, pmx=pmx: nc.vector.scalar_tensor_tensor(out=xT[:, ec, sl], in0=pmx[:], scalar=ga[:, ec:ec + 1], in1=xT[:, ec, sl],
                                                                              op0=ALU.mult, op1=ALU.add)),
                 reads=[pmx.res, ga.res, xT.res], writes=[xT.res])
    pipeline(len(items), [c_load, c_comp])
    for j in range(8):
        s.dma(x1_d[:, j, :], xT[:, j, :], reads=[xT.res], is_output=True)
    c.end_phase(ph)
    if own:
        c.close()
    return c


def maps_C1(inp, l, o, gates, xT_all, mod):
    wbr = np.ascontiguousarray(inp["w_branch"][l].reshape(4, 2, 128, D).transpose(2, 0, 1, 3).reshape(128, 8, D))
    wout = np.ascontiguousarray(inp["w_out"][l].reshape(8, 128, D).transpose(1, 0, 2))
    maps = []
    for core in range(8):
        b, q = core // 4, core % 4
        tsl = slice(q * NT, (q + 1) * NT)
        maps.append({"oT": np.ascontiguousarray(o[b][:, :, tsl].reshape(4, 2, 128, NT)),
                     "gates": np.ascontiguousarray(gates[b][:, :, tsl]),
                     "xT": np.ascontiguousarray(xT_all[b][:, tsl].reshape(8, 128, NT).transpose(1, 0, 2)),
                     "wbr": wbr, "wout": wout, "ga": np.ascontiguousarray(mod[l, b][:, 16:24])})
    return maps


def collect_x(results, key):
    out = np.zeros((2, D, S), np.float32)
    for core in range(8):
        b, q = core // 4, core % 4
        out[b][:, q * NT:(q + 1) * NT] = results[core][key].transpose(1, 0, 2).reshape(D, NT)
    return out


def run_C1(cC1, inp, l, o, gates, xT_all, mod):
    maps = maps_C1(inp, l, o, gates, xT_all, mod)
    res = run_bass_kernel_spmd(cC1.nc, maps, core_ids=list(range(8)))
    return collect_x(res.results, "x1T")


HT = 1024
NFC = 22


def build_C2(n_exp, moe, c=None):
    own = c is None
    if own:
        c = Ctx()
    ph = c.begin_phase()
    nc, s = c.nc, c.s
    x1_d = c.dram("x1T", [128, 8, NT], F32, "ExternalInput")
    mod_d = c.dram("modF", [128, 24], F32, "ExternalInput")
    gn_d = c.dram("gn", [128, 8], F32, "ExternalInput")
    w1_d = c.dram("w1", [n_exp, NFC, 128, 8, 128], F32, "ExternalInput")
    w3_d = c.dram("w3", [n_exp, NFC, 128, 8, 128], F32, "ExternalInput")
    w2_d = c.dram("w2", [n_exp, 8, 128, NFC, 128], F32, "ExternalInput")
    if moe:
        rw_d = c.dram("rw", [128, 8, 8], F32, "ExternalInput")
        oh_d = c.dram("onehot", [8, 8, 128], F32, "ExternalInput")
        id_d = c.dram("ident", [128, 128], F32, "ExternalInput")
    x2_d = c.dram("x2T", [128, 8, NT], F32, "ExternalOutput")

    acc = c.sb([128, 8, HT], F32)
    hT = c.sb([128, 8, HT], BF16)
    act = c.sb([128, NFC, HT], BF16)
    rs = c.sb([128, HT], F32)
    ones = c.sb([128, 128], BF16)
    sqring = [c.sb([128, TT], BF16) for _ in range(2)]
    epsb = c.sb([128, 1], F32)
    mod = c.sb([128, 24], F32)
    gn = c.sb([128, 8], F32)
    Acol = c.sb([128, 8], F32)
    Bcol = c.sb([128, 8], F32)
    ring = [c.sb([128, TT], F32) for _ in range(4)]
    w1s = [c.sb([128, 8, 128], F32) for _ in range(2)]
    w3s = [c.sb([128, 8, 128], F32) for _ in range(2)]
    w1b = [c.sb([128, 8, 128], BF16) for _ in range(3)]
    w3b = [c.sb([128, 8, 128], BF16) for _ in range(3)]
    w2s = [c.sb([128, NFC, 128], F32) for _ in range(2)]
    w2b = [c.sb([128, NFC, 128], BF16) for _ in range(3)]
    sa_r = [c.sb([128, TT], F32) for _ in range(3)]
    u_r = [c.sb([128, TT], F32) for _ in range(3)]
    pa_r = [c.ps([128, TT]) for _ in range(2)]
    pg_r = [c.ps([128, TT]) for _ in range(2)]
    po_r = [c.ps([128, TT]) for _ in range(2)]
    pbank = c.ps([128, TT])
    pmisc = c.ps([128, TT])
    s.op("pool", lambda: nc.gpsimd.memset(ones[:], 1.0), writes=[ones.res])
    s.op("pool", lambda: nc.gpsimd.memset(epsb[:], EPS), writes=[epsb.res])
    s.dma(mod[:], mod_d, writes=[mod.res])
    s.dma(gn[:], gn_d, writes=[gn.res])
    emit_AB(c, gn, mod, 0, 8, Acol, Bcol)
    if moe:
        rw = c.sb([128, 8, 8], F32)
        oh = c.sb([8, 8, 128], F32)
        ident = c.sb([128, 128], F32)
        GT = c.sb([8, HT], F32)
        gb_r = [c.sb([128, HT], F32) for _ in range(2)]
        h32_r = [c.sb([128, TT], F32) for _ in range(2)]
        lg = c.sb([128, 8, 8], F32)
        m8 = c.sb([128, 8], F32)
        nt1 = c.sb([128, 1], F32)
        e2 = c.sb([128, 1], F32)
        ex = c.sb([128, 8], F32)
        selm = c.sb([128, 8], F32)
        Gt = c.sb([128, 8], F32)
        for t, d in ((rw, rw_d), (oh, oh_d), (ident, id_d)):
            s.dma(t[:], d, writes=[t.res])

    for hf in range(NT // HT):
        hsl = slice(hf * HT, (hf + 1) * HT)
        for j in range(8):
            s.dma(acc[:, j, :], x1_d[:, j, hsl], writes=[acc.res])
        if not moe:
            emit_modnorm(c, acc, hT, HT, ones, Acol, Bcol, epsb, ring, pbank, rs, sq_ring=sqring)
        else:
            def after_h(tmp, j, tt, sl):
                h32 = h32_r[j % 2]
                s.op("act", lambda: nc.scalar.activation(out=h32[:], in_=tmp[:], func=AF.Identity, scale=Acol[:, j:j + 1], bias=Bcol[:, j:j + 1]),
                     reads=[tmp.res, Acol.res, Bcol.res], writes=[h32.res])
                s.op("pool", lambda: nc.gpsimd.tensor_copy(out=hT[:, j, sl], in_=h32[:]), reads=[h32.res], writes=[hT.res])
                for tb in range(4):
                    col = (tt * 4 + tb) * 8
                    s.op("pe", (lambda tb=tb, col=col: nc.tensor.matmul(pmisc[:, col:col + 8], lhsT=h32[:, tb * 128:(tb + 1) * 128], rhs=rw[:, j, :],
                                                                        start=(j == 0 and tb == 0 and tt == 0), stop=(j == 7), skip_group_check=True)),
                         reads=[h32.res, rw.res], writes=[pmisc.res])
            emit_modnorm(c, acc, hT, HT, ones, Acol, Bcol, epsb, ring, pbank, rs, after_h=after_h, sq_ring=sqring)
            s.op("act", lambda: nc.scalar.copy(out=lg[:], in_=pmisc[:, 0:64].rearrange("p (a b) -> p a b", b=8)), reads=[pmisc.res], writes=[lg.res])
            for tb in range(8):
                s.op("dve", (lambda tb=tb: nc.vector.max(out=m8[:], in_=lg[:, tb, :])), reads=[lg.res], writes=[m8.res])
                s.op("dve", lambda: nc.vector.tensor_scalar(out=nt1[:], in0=m8[:, 0:1], scalar1=-1.0, scalar2=None, op0=ALU.mult), reads=[m8.res], writes=[nt1.res])
                s.op("act", (lambda tb=tb: nc.scalar.activation(out=ex[:], in_=lg[:, tb, :], func=AF.Exp, bias=nt1[:], scale=1.0)),
                     reads=[lg.res, nt1.res], writes=[ex.res])
                s.op("act", lambda: nc.scalar.activation(out=e2[:], in_=m8[:, 1:2], func=AF.Exp, bias=nt1[:], scale=1.0), reads=[m8.res, nt1.res], writes=[e2.res])
                s.op("dve", lambda: nc.vector.tensor_scalar(out=e2[:], in0=e2[:], scalar1=1.0, scalar2=None, op0=ALU.add), reads=[e2.res], writes=[e2.res])
                s.op("dve", lambda: nc.vector.reciprocal(out=e2[:], in_=e2[:]), reads=[e2.res], writes=[e2.res])
                s.op("dve", (lambda tb=tb: nc.vector.tensor_scalar(out=selm[:], in0=lg[:, tb, :], scalar1=m8[:, 1:2], scalar2=None, op0=ALU.is_ge)),
                     reads=[lg.res, m8.res], writes=[selm.res])
                s.op("dve", lambda: nc.vector.scalar_tensor_tensor(out=Gt[:], in0=ex[:], scalar=e2[:, 0:1], in1=selm[:], op0=ALU.mult, op1=ALU.mult),
                     reads=[ex.res, e2.res, selm.res], writes=[Gt.res])
                s.op("pe", (lambda tb=tb: nc.tensor.transpose(pbank[0:8, (tb % 4) * 128:(tb % 4 + 1) * 128], Gt[:], ident[:])), reads=[Gt.res, ident.res], writes=[pbank.res])
                if tb % 4 == 3:
                    q4 = tb // 4
                    s.op("act", (lambda q4=q4: nc.scalar.copy(out=GT[:, q4 * 512:(q4 + 1) * 512], in_=pbank[0:8, 0:512])), reads=[pbank.res], writes=[GT.res])

        items = []
        kf = kg = 0
        for e in range(n_exp):
            for fc in range(NFC):
                for tt in range(2):
                    items.append(("f", e, fc, tt, kf))
                kf += 1
            for ec in range(8):
                for tt in range(2):
                    items.append(("g", e, ec, tt, kg))
                kg += 1

        def s_load(n):
            kind, e, ci, tt, k = items[n]
            if tt != 0:
                return
            if kind == "f":
                if moe and ci == 0:
                    g_b = gb_r[e % 2]
                    for t2 in range(2):
                        s.op("pe", (lambda t2=t2, e=e: nc.tensor.matmul(pmisc[:], lhsT=oh[:, e, :], rhs=GT[:, t2 * TT:(t2 + 1) * TT], start=True, stop=True)),
                             reads=[oh.res, GT.res], writes=[pmisc.res])
                        s.op("act", (lambda t2=t2, g_b=g_b: nc.scalar.copy(out=g_b[:, t2 * TT:(t2 + 1) * TT], in_=pmisc[:])), reads=[pmisc.res], writes=[g_b.res])
                s.dma(w1s[k % 2][:], w1_d[e, ci], writes=[w1s[k % 2].res], q="act")
                s.dma(w3s[k % 2][:], w3_d[e, ci], writes=[w3s[k % 2].res], q="act")
            else:
                s.dma(w2s[k % 2][:], w2_d[e, ci], writes=[w2s[k % 2].res], q="act")

        def s_cast(n):
            kind, e, ci, tt, k = items[n]
            if tt != 0:
                return
            if kind == "f":
                s.op("pool", lambda: nc.gpsimd.tensor_copy(out=w1b[k % 3][:], in_=w1s[k % 2][:]), reads=[w1s[k % 2].res], writes=[w1b[k % 3].res])
                s.op("dve", lambda: nc.vector.tensor_copy(out=w3b[k % 3][:], in_=w3s[k % 2][:]), reads=[w3s[k % 2].res], writes=[w3b[k % 3].res])
            else:
                half = NFC // 2
                s.op("pool", lambda: nc.gpsimd.tensor_copy(out=w2b[k % 3][:, 0:half, :], in_=w2s[k % 2][:, 0:half, :]), reads=[w2s[k % 2].res], writes=[w2b[k % 3].res])
                s.op("dve", lambda: nc.vector.tensor_copy(out=w2b[k % 3][:, half:NFC, :], in_=w2s[k % 2][:, half:NFC, :]), reads=[w2s[k % 2].res], writes=[w2b[k % 3].res])

        def s_mm(n):
            kind, e, ci, tt, k = items[n]
            sl = slice(tt * TT, (tt + 1) * TT)
            if kind == "f":
                pa, pg = pa_r[n % 2], pg_r[n % 2]
                for j in range(8):
                    s.op("pe", (lambda j=j: nc.tensor.matmul(pa[:], lhsT=w1b[k % 3][:, j, :], rhs=hT[:, j, sl], start=(j == 0), stop=(j == 7))),
                         reads=[w1b[k % 3].res, hT.res], writes=[pa.res])
                for j in range(8):
                    s.op("pe", (lambda j=j: nc.tensor.matmul(pg[:], lhsT=w3b[k % 3][:, j, :], rhs=hT[:, j, sl], start=(j == 0), stop=(j == 7))),
                         reads=[w3b[k % 3].res, hT.res], writes=[pg.res])
            else:
                po = po_r[n % 2]
                for fc in range(NFC):
                    s.op("pe", (lambda fc=fc: nc.tensor.matmul(po[:], lhsT=w2b[k % 3][:, fc, :], rhs=act[:, fc, sl], start=(fc == 0), stop=(fc == NFC - 1))),
                         reads=[w2b[k % 3].res, act.res], writes=[po.res])

        def s_post(n):
            kind, e, ci, tt, k = items[n]
            sl = slice(tt * TT, (tt + 1) * TT)
            if kind == "f":
                pa, pg = pa_r[n % 2], pg_r[n % 2]
                sa = sa_r[n % 3]
                s.op("act", lambda: nc.scalar.activation(out=sa[:], in_=pa[:], func=AF.Silu), reads=[pa.res], writes=[sa.res])
                if not moe:
                    s.op("dve", lambda: nc.vector.tensor_tensor(out=act[:, ci, sl], in0=sa[:], in1=pg[:], op=ALU.mult), reads=[sa.res, pg.res], writes=[act.res])
                else:
                    u = u_r[n % 3]
                    g_b = gb_r[e % 2]
                    s.op("dve", lambda: nc.vector.tensor_tensor(out=u[:], in0=pg[:], in1=g_b[:, sl], op=ALU.mult), reads=[pg.res, g_b.res], writes=[u.res])
                    s.op("pool", lambda: nc.gpsimd.tensor_tensor(out=act[:, ci, sl], in0=sa[:], in1=u[:], op=ALU.mult), reads=[sa.res, u.res], writes=[act.res])
            else:
                po = po_r[n % 2]
                s.op("dve", lambda: nc.vector.scalar_tensor_tensor(out=acc[:, ci, sl], in0=po[:], scalar=mod[:, 16 + ci:17 + ci], in1=acc[:, ci, sl],
                                                                   op0=ALU.mult, op1=ALU.add), reads=[po.res, mod.res, acc.res], writes=[acc.res])
        pipeline(len(items), [s_load, s_cast, s_mm, s_post])
        for j in range(8):
            s.dma(x2_d[:, j, hsl], acc[:, j, :], reads=[acc.res], is_output=True)
    c.end_phase(ph)
    if own:
        c.close()
    return c


def maps_C2(inp, l, x1T_all, mod, moe):
    if moe:
        w1, w3, w2 = inp["moe_w1"][l // 2], inp["moe_w3"][l // 2], inp["moe_w2"][l // 2]
    else:
        w1, w3, w2 = inp["ffn_w1"][l // 2][None], inp["ffn_w3"][l // 2][None], inp["ffn_w2"][l // 2][None]
    E = w1.shape[0]
    lay1 = lambda w: np.ascontiguousarray(w.reshape(E, 8, 128, NFC, 128).transpose(0, 3, 2, 1, 4))
    lay2 = lambda w: np.ascontiguousarray(w.reshape(E, NFC, 128, 8, 128).transpose(0, 3, 2, 1, 4))
    w1l, w3l, w2l = lay1(w1), lay1(w3), lay2(w2)
    gn = np.ascontiguousarray(inp["norm_ffn"][l].reshape(8, 128).T)
    maps = []
    for core in range(8):
        b, q = core // 4, core % 4
        m = {"modF": np.ascontiguousarray(mod[l, b][:, 24:48]), "gn": gn, "w1": w1l, "w3": w3l, "w2": w2l}
        if x1T_all is not None:
            m["x1T"] = np.ascontiguousarray(x1T_all[b][:, q * NT:(q + 1) * NT].reshape(8, 128, NT).transpose(1, 0, 2))
        if moe:
            m["rw"] = np.ascontiguousarray(inp["router_w"][l // 2].reshape(8, 128, 8).transpose(1, 0, 2))
            oh = np.zeros((8, 8, 128), np.float32)
            for e in range(8):
                oh[e, e, :] = 1.0
            m["onehot"] = oh
            m["ident"] = np.eye(128, dtype=np.float32)
        maps.append(m)
    return maps


def run_C2(cC2, inp, l, x1T_all, mod, moe):
    maps = maps_C2(inp, l, x1T_all, mod, moe)
    res = run_bass_kernel_spmd(cC2.nc, maps, core_ids=list(range(8)))
    return collect_x(res.results, "x2T")


def build_CA(moe, with_A):
    c = Ctx()
    c.alias = {}
    c.pre = "c1_"
    c.kind_override = {"c1_x1T": "Internal"}
    build_C1(c)
    c.pre = "c2_"
    c.alias["c2_x1T"] = c.made["c1_x1T"]
    build_C2(8 if moe else 1, moe, c)
    if with_A:
        c.pre = "a_"
        c.alias["a_xT"] = c.made["c2_x2T"]
        build_A(c)
    c.close()
    return c


def run_CA(cCA, inp, l, o, gates, xT_all, mod, moe, with_A):
    m1 = maps_C1(inp, l, o, gates, xT_all, mod)
    m2 = maps_C2(inp, l, None, mod, moe)
    m3 = maps_A(inp, l + 1, None, mod) if with_A else [dict() for _ in range(8)]
    maps = []
    for i in range(8):
        m = {"c1_" + k: v for k, v in m1[i].items()}
        m.update({"c2_" + k: v for k, v in m2[i].items()})
        m.update({"a_" + k: v for k, v in m3[i].items()})
        maps.append(m)
    res = run_bass_kernel_spmd(cCA.nc, maps, core_ids=list(range(8)))
    x2T = collect_x(res.results, "c2_x2T")
    nxt = collect_A(res.results, "a_") if with_A else None
    return x2T, nxt


_PROGS = {}


def _prog(name, fn):
    if name not in _PROGS:
        _PROGS[name] = fn()
    return _PROGS[name]


def kernel(**inp):
    inp = {k: np.asarray(v) for k, v in inp.items()}
    mod = run_M(inp)
    xT_all = np.ascontiguousarray(inp["x"].astype(np.float32).transpose(0, 2, 1))
    cA = _prog("A", build_A)
    proj, gates, misc = run_A(cA, inp, 0, xT_all, mod)
    cB = _prog("B", build_B)
    for l in range(2):
        o = run_B(cB, inp, l, proj, misc)
        moe = (l % 2 == 1)
        with_A = (l == 0)
        cCA = _prog("CA%d" % l, lambda: build_CA(moe, with_A))
        xT_all, nxt = run_CA(cCA, inp, l, o, gates, xT_all, mod, moe, with_A)
        if with_A:
            proj, gates, misc = nxt
    return np.ascontiguousarray(xT_all.transpose(0, 2, 1)).astype(np.float32)
```

```python
import contextlib
import numpy as np
import concourse.bass as bass
import concourse.mybir as mybir
from concourse.bass_utils import run_bass_kernel_spmd

F32 = mybir.dt.float32
BF16 = mybir.dt.bfloat16
I32 = mybir.dt.int32
AF = mybir.ActivationFunctionType
ALU = mybir.AluOpType
AX = mybir.AxisListType


class Res:
    __slots__ = ("w", "r")

    def __init__(self):
        self.w = None
        self.r = {}


class Sched:
    NDMA = 24

    def __init__(self, nc, es):
        self.nc = nc
        self.engs = {"pe": nc.tensor, "act": nc.scalar, "dve": nc.vector,
                     "pool": nc.gpsimd, "sp": nc.sync}
        self.sem = {}
        self.cnt = {}
        for k in self.engs:
            self.sem[k] = es.enter_context(nc.semaphore("s_" + k))
            self.cnt[k] = 0
        for i in range(self.NDMA):
            k = "d%d" % i
            self.sem[k] = es.enter_context(nc.semaphore("s_" + k))
            self.cnt[k] = 0
        self.seen = {k: {} for k in self.engs}
        self.dma_rr = 0
        self.out_events = []

    def _wait(self, e, ev):
        if ev is None:
            return
        key, val = ev
        if key == e and e == "pe":
            return
        if self.seen[e].get(key, 0) >= val:
            return
        self.engs[e].wait_ge(self.sem[key], val)
        self.seen[e][key] = val

    def _deps(self, e, reads, writes):
        for r in reads:
            self._wait(e, r.w)
        for r in writes:
            self._wait(e, r.w)
            for k, v in r.r.items():
                self._wait(e, (k, v))

    def op(self, e, fn, reads=(), writes=()):
        self._deps(e, reads, writes)
        ins = fn()
        self.cnt[e] += 1
        ins.then_inc(self.sem[e], 1)
        ev = (e, self.cnt[e])
        for r in reads:
            r.r[e] = ev[1]
        for r in writes:
            r.w = ev
            r.r = {}
        return ev

    def dma(self, out, in_, reads=(), writes=(), q="sp", is_output=False, **kw):
        k = "d%d" % self.dma_rr
        self.dma_rr = (self.dma_rr + 1) % self.NDMA
        self._wait(q, (k, self.cnt[k]))
        self._deps(q, reads, writes)
        ins = self.engs[q].dma_start(out=out, in_=in_, **kw)
        self.cnt[k] += 16
        ins.then_inc(self.sem[k], 16)
        ev = (k, self.cnt[k])
        for r in reads:
            r.r[k] = ev[1]
        for r in writes:
            r.w = ev
            r.r = {}
        if is_output:
            self.out_events.append(ev)
        return ev

    def finish(self):
        for i in range(self.NDMA):
            k = "d%d" % i
            self._wait("sp", (k, self.cnt[k]))
        for k in ("pe", "act", "dve", "pool"):
            self._wait("sp", (k, self.cnt[k]))


class Tile:
    def __init__(self, t):
        self.t = t
        self.res = Res()

    def __getitem__(self, idx):
        return self.t[idx]


class Ctx:
    def __init__(self, name="k"):
        self.nc = bass.Bass("TRN2", target_bir_lowering=False)
        self.es = contextlib.ExitStack()
        self.s = Sched(self.nc, self.es)
        self.n = 0

    def sb(self, shape, dt, name=None):
        self.n += 1
        return Tile(self.es.enter_context(self.nc.sbuf_tensor(name or ("t%d" % self.n), list(shape), dt)))

    def ps(self, shape, dt=F32, name=None):
        self.n += 1
        return Tile(self.es.enter_context(self.nc.psum_tensor(name or ("p%d" % self.n), list(shape), dt)))

    pre = ""
    alias = None
    kind_override = None

    def dram(self, name, shape, dt, kind):
        full = self.pre + name
        if self.alias and full in self.alias:
            return self.alias[full]
        if self.kind_override and full in self.kind_override:
            kind = self.kind_override[full]
        ap = self.nc.dram_tensor(full, list(shape), dt, kind=kind).ap()
        if self.alias is None:
            self.alias = {}
        self.made = getattr(self, "made", {})
        self.made[full] = ap
        return ap

    def begin_phase(self):
        es = contextlib.ExitStack()
        old, self.es = self.es, es
        return (old, es)

    def end_phase(self, ph):
        barrier(self)
        self.es = ph[0]
        ph[1].close()

    def close(self):
        self.s.finish()
        self.es.close()


D = 1024
S = 8192
NB = 2
NT = 2048
TT = 512
NCH_IN = 57
A_MAXCH = NCH_IN
EPS = 1e-6
TWO_PI = float(2 * np.pi)
C1_2PI = 6.28125
C2_2PI = TWO_PI - C1_2PI


def barrier(c):
    s = c.s
    for e in ("pe", "act", "dve", "pool", "sp"):
        for k in list(s.cnt.keys()):
            if k != e:
                s._wait(e, (k, s.cnt[k]))


def build_M():
    c = Ctx()
    nc, s = c.nc, c.s
    cT_d = c.dram("cT", [128, 8, 2], F32, "ExternalInput")
    w_d = c.dram("w", [12, 128, 8, 128], F32, "ExternalInput")
    b_d = c.dram("b", [128, 12], F32, "ExternalInput")
    o_d = c.dram("modT", [128, 12, 2], F32, "ExternalOutput")
    cT = c.sb([128, 8, 2], F32)
    ca = c.sb([128, 8, 2], F32)
    bt = c.sb([128, 12], F32)
    ot = c.sb([128, 12, 2], F32)
    s.dma(cT[:], cT_d, writes=[cT.res])
    s.dma(bt[:], b_d, writes=[bt.res])
    s.op("act", lambda: nc.scalar.activation(out=ca[:], in_=cT[:], func=AF.Silu), reads=[cT.res], writes=[ca.res])
    wts = [c.sb([128, 8, 128], F32) for _ in range(3)]
    pm = c.ps([128, 12, 2])
    for j in range(12):
        wt = wts[j % 3]
        s.dma(wt[:], w_d[j], writes=[wt.res])
        for k in range(8):
            s.op("pe", (lambda wt=wt, k=k, j=j: nc.tensor.matmul(pm[:, j, :], lhsT=wt[:, k, :], rhs=ca[:, k, :],
                                                                 start=(k == 0), stop=(k == 7))),
                 reads=[wt.res, ca.res], writes=[pm.res])
    for b in range(2):
        s.op("dve", (lambda b=b: nc.vector.tensor_tensor(out=ot[:, :, b], in0=pm[:, :, b], in1=bt[:], op=ALU.add)),
             reads=[pm.res, bt.res], writes=[ot.res])
    s.dma(o_d, ot[:], reads=[ot.res], is_output=True)
    c.close()
    return c


def run_M(inp):
    c = build_M()
    cT = np.ascontiguousarray(inp["c"].T.reshape(8, 128, 2).transpose(1, 0, 2))
    w_all = inp["w_ada"]
    b_all = inp["b_ada"]
    maps = []
    for i in range(8):
        chunks = [(g // 48, g % 48) for g in range(i * 12, i * 12 + 12)]
        w = np.stack([w_all[l][:, n * 128:(n + 1) * 128].reshape(8, 128, 128).transpose(1, 0, 2) for l, n in chunks])
        b = np.stack([b_all[l][n * 128:(n + 1) * 128] for l, n in chunks], axis=1)
        maps.append({"cT": cT, "w": np.ascontiguousarray(w), "b": np.ascontiguousarray(b)})
    res = run_bass_kernel_spmd(c.nc, maps, core_ids=list(range(8)))
    mod = np.zeros((2, 2, 128, 48), np.float32)
    for i in range(8):
        o = res.results[i]["modT"]
        for jj, g in enumerate(range(i * 12, i * 12 + 12)):
            mod[g // 48, :, :, g % 48] = o[:, jj, :].T
    return mod


def emit_modnorm(c, src, hT, ntok, ones, Acol, Bcol, epsb, tmp_ring, pbank, rs, after_h=None, sq_ring=None):
    nc, s = c.nc, c.s
    ntt = ntok // TT
    if sq_ring is None:
        sq_ring = tmp_ring
    for tt in range(ntt):
        sl = slice(tt * TT, (tt + 1) * TT)
        for j in range(8):
            sq = sq_ring[j % len(sq_ring)]
            s.op("act", (lambda sq=sq, j=j: nc.scalar.activation(out=sq[:], in_=src[:, j, sl], func=AF.Square)),
                 reads=[src.res], writes=[sq.res])
            s.op("pe", (lambda sq=sq, j=j: nc.tensor.matmul(pbank[:], lhsT=ones[:], rhs=sq[:], start=(j == 0), stop=(j == 7))),
                 reads=[sq.res, ones.res], writes=[pbank.res])
        sd = tmp_ring[0]
        s.op("act", (lambda sd=sd: nc.scalar.activation(out=sd[:], in_=pbank[:], func=AF.Sqrt, scale=1.0 / D, bias=epsb[:])),
             reads=[pbank.res, epsb.res], writes=[sd.res])
        s.op("dve", (lambda sd=sd: nc.vector.reciprocal(out=rs[:, sl], in_=sd[:])), reads=[sd.res], writes=[rs.res])
        for j in range(8):
            tmp = tmp_ring[1 + (j % (len(tmp_ring) - 1))]
            s.op("dve", (lambda tmp=tmp, j=j: nc.vector.tensor_tensor(out=tmp[:], in0=src[:, j, sl], in1=rs[:, sl], op=ALU.mult)),
                 reads=[src.res, rs.res], writes=[tmp.res])
            if after_h is None:
                s.op("act", (lambda tmp=tmp, j=j: nc.scalar.activation(out=hT[:, j, sl], in_=tmp[:], func=AF.Identity,
                                                                      scale=Acol[:, j:j + 1], bias=Bcol[:, j:j + 1])),
                     reads=[tmp.res, Acol.res, Bcol.res], writes=[hT.res])
            else:
                after_h(tmp, j, tt, sl)


def emit_AB(c, gn, mod, sh_off, sc_off, Acol, Bcol):
    nc, s = c.nc, c.s
    s.op("dve", lambda: nc.vector.scalar_tensor_tensor(out=Acol[:], in0=mod[:, sc_off:sc_off + 8], scalar=1.0, in1=gn[:],
                                                       op0=ALU.add, op1=ALU.mult),
         reads=[mod.res, gn.res], writes=[Acol.res])
    s.op("dve", lambda: nc.vector.tensor_copy(out=Bcol[:], in_=mod[:, sh_off:sh_off + 8]), reads=[mod.res], writes=[Bcol.res])


ROPE_CH = (0, 1, 2, 4, 5, 6, 7)
NORM_CH = (3, 8, 9, 10, 11)
RAW_CH = tuple(range(12, 24))
MISC_CH = 24


def build_A(c=None):
    own = c is None
    if own:
        c = Ctx()
    ph = c.begin_phase()
    nc, s = c.nc, c.s
    xT_d = c.dram("xT", [128, 8, NT], F32, "ExternalInput")
    mod_d = c.dram("modA", [128, 16], F32, "ExternalInput")
    gn_d = c.dram("gn", [128, 8], F32, "ExternalInput")
    pos_d = c.dram("pos", [1, NT], I32, "ExternalInput")
    invf_d = c.dram("invf", [128, 1], F32, "ExternalInput")
    w_d = c.dram("w", [NCH_IN, 128, 8, 128], F32, "ExternalInput")
    gain_d = c.dram("gain", [128, 12], F32, "ExternalInput")
    osc_d = c.dram("osc", [128, 12], F32, "ExternalInput")
    foxb_d = c.dram("foxb", [128, 1], F32, "ExternalInput")
    pm_d = c.dram("pm", [128, 128], F32, "ExternalInput")
    bones_d = c.dram("bones", [128, 128], F32, "ExternalInput")
    proj_d = c.dram("proj", [26, 128, NT], BF16, "ExternalOutput")
    gates_d = c.dram("gates", [32, 128, NT], F32, "ExternalOutput")
    misc_d = c.dram("misc", [64, NT], F32, "ExternalOutput")

    hT = c.sb([128, 8, NT], BF16)
    rs = c.sb([128, NT], F32)
    COS = c.sb([128, NT], F32)
    SIN = c.sb([128, NT], F32)
    ones = c.sb([128, 128], BF16)
    bones_f = c.sb([128, 128], F32)
    pm_f = c.sb([128, 128], F32)
    bones = c.sb([128, 128], BF16)
    pm = c.sb([128, 128], BF16)
    gain = c.sb([128, 12], F32)
    osc = c.sb([128, 12], F32)
    foxb = c.sb([128, 1], F32)
    epsb = c.sb([128, 1], F32)
    negpi = c.sb([128, 1], F32)
    invf = c.sb([128, 1], F32)
    mod = c.sb([128, 16], F32)
    gn = c.sb([128, 8], F32)
    Acol = c.sb([128, 8], F32)
    Bcol = c.sb([128, 8], F32)
    pbank = c.ps([128, TT])

    s.op("pool", lambda: nc.gpsimd.memset(ones[:], 1.0), writes=[ones.res])
    s.op("pool", lambda: nc.gpsimd.memset(epsb[:], EPS), writes=[epsb.res])
    s.op("pool", lambda: nc.gpsimd.memset(negpi[:], -float(np.pi)), writes=[negpi.res])
    for t, d in ((bones_f, bones_d), (pm_f, pm_d), (gain, gain_d), (osc, osc_d), (foxb, foxb_d), (invf, invf_d), (mod, mod_d), (gn, gn_d)):
        s.dma(t[:], d, writes=[t.res])
    s.op("pool", lambda: nc.gpsimd.tensor_copy(out=bones[:], in_=bones_f[:]), reads=[bones_f.res], writes=[bones.res])
    s.op("pool", lambda: nc.gpsimd.tensor_copy(out=pm[:], in_=pm_f[:]), reads=[pm_f.res], writes=[pm.res])
    s.op("dve", lambda: nc.vector.tensor_tensor(out=gain[:], in0=gain[:], in1=osc[:], op=ALU.mult), reads=[gain.res, osc.res], writes=[gain.res])
    emit_AB(c, gn, mod, 0, 8, Acol, Bcol)

    with contextlib.ExitStack() as es1:
        old_es, c.es = c.es, es1
        xT = c.sb([128, 8, NT], F32)
        for j in range(8):
            s.dma(xT[:, j, :], xT_d[:, j, :], writes=[xT.res], q=("sp", "act")[j % 2])
        posi = c.sb([128, NT], I32)
        ang = c.sb([128, NT], F32)
        tq = c.sb([128, NT], F32)
        ki = c.sb([128, NT], I32)
        kf = c.sb([128, NT], F32)
        s.dma(posi[:], pos_d[0, :].partition_broadcast(128), writes=[posi.res])
        s.op("dve", lambda: nc.vector.tensor_copy(out=tq[:], in_=posi[:]), reads=[posi.res], writes=[tq.res])
        s.op("dve", lambda: nc.vector.tensor_scalar(out=ang[:], in0=tq[:], scalar1=invf[:, 0:1], scalar2=None, op0=ALU.mult),
             reads=[tq.res, invf.res], writes=[ang.res])
        for dst, shift in ((SIN, 0.0), (COS, 0.25)):
            s.op("dve", lambda shift=shift: nc.vector.tensor_scalar(out=tq[:], in0=ang[:], scalar1=1.0 / TWO_PI, scalar2=0.5 + shift,
                                                                    op0=ALU.mult, op1=ALU.add), reads=[ang.res], writes=[tq.res])
            s.op("dve", lambda: nc.vector.tensor_copy(out=ki[:], in_=tq[:]), reads=[tq.res], writes=[ki.res])
            s.op("dve", lambda: nc.vector.tensor_copy(out=kf[:], in_=ki[:]), reads=[ki.res], writes=[kf.res])
            s.op("dve", lambda shift=shift: nc.vector.tensor_scalar(out=tq[:], in0=ang[:], scalar1=float(np.pi) + shift * TWO_PI, scalar2=None,
                                                                    op0=ALU.add), reads=[ang.res], writes=[tq.res])
            s.op("dve", lambda: nc.vector.scalar_tensor_tensor(out=tq[:], in0=kf[:], scalar=-C1_2PI, in1=tq[:], op0=ALU.mult, op1=ALU.add),
                 reads=[kf.res, tq.res], writes=[tq.res])
            s.op("dve", lambda: nc.vector.scalar_tensor_tensor(out=tq[:], in0=kf[:], scalar=-C2_2PI, in1=tq[:], op0=ALU.mult, op1=ALU.add),
                 reads=[kf.res, tq.res], writes=[tq.res])
            s.op("dve", lambda: nc.vector.tensor_scalar(out=kf[:], in0=tq[:], scalar1=0.0, scalar2=TWO_PI, op0=ALU.is_lt, op1=ALU.mult),
                 reads=[tq.res], writes=[kf.res])
            s.op("dve", lambda: nc.vector.tensor_tensor(out=tq[:], in0=tq[:], in1=kf[:], op=ALU.add), reads=[tq.res, kf.res], writes=[tq.res])
            s.op("dve", lambda: nc.vector.tensor_scalar(out=tq[:], in0=tq[:], scalar1=0.0, scalar2=TWO_PI, op0=ALU.max, op1=ALU.min),
                 reads=[tq.res], writes=[tq.res])
            s.op("act", lambda dst=dst: nc.scalar.activation(out=dst[:], in_=tq[:], func=AF.Sin, bias=negpi[:], scale=1.0),
                 reads=[tq.res, negpi.res], writes=[dst.res])
        ring = [c.sb([128, TT], F32) for _ in range(4)]
        sqring = [c.sb([128, TT], BF16) for _ in range(4)]
        emit_modnorm(c, xT, hT, NT, ones, Acol, Bcol, epsb, ring, pbank, rs, sq_ring=sqring)
        barrier(c)
        c.es = old_es

    R = 4
    wst = [c.sb([128, 8, 128], F32) for _ in range(3)]
    wbf = [c.sb([128, 8, 128], BF16) for _ in range(3)]
    pp = [c.ps([128, TT]) for _ in range(3)]
    psq = [c.ps([128, TT]) for _ in range(2)]
    pq = [pbank, c.ps([128, TT])]
    sq_r = [c.sb([128, TT], BF16) for _ in range(R)]
    rstd_r = [c.sb([128, TT], F32) for _ in range(R)]
    y_r = [c.sb([128, TT], F32) for _ in range(R)]
    y2_r = [c.sb([128, TT], BF16) for _ in range(R)]
    t1_r = [c.sb([128, TT], F32) for _ in range(R)]
    ob_r = [c.sb([128, TT], BF16) for _ in range(R)]
    ob2_r = [c.sb([128, TT], BF16) for _ in range(R)]
    of_r = [c.sb([128, TT], F32) for _ in range(R)]

    items = [(ch, tt) for ch in range(A_MAXCH) for tt in range(NT // TT)]

    def kind(ch):
        if ch in ROPE_CH:
            return "rope"
        if ch in NORM_CH:
            return "norm"
        if ch in RAW_CH:
            return "raw"
        if ch == MISC_CH:
            return "misc"
        return "sig"

    def stl(n):
        ch, tt = items[n]
        if tt == 0:
            ws = wst[ch % 3]
            s.dma(ws[:], w_d[ch], writes=[ws.res], q="act")

    def stc(n):
        ch, tt = items[n]
        if tt == 0:
            ws, wb = wst[ch % 3], wbf[ch % 3]
            s.op("pool", lambda: nc.gpsimd.tensor_copy(out=wb[:], in_=ws[:]), reads=[ws.res], writes=[wb.res])

    def st0(n):
        ch, tt = items[n]
        wb = wbf[ch % 3]
        p = pp[n % 3]
        for j in range(8):
            s.op("pe", (lambda j=j: nc.tensor.matmul(p[:], lhsT=wb[:, j, :], rhs=hT[:, j, tt * TT:(tt + 1) * TT],
                                                     start=(j == 0), stop=(j == 7))),
                 reads=[wb.res, hT.res], writes=[p.res])

    def st1(n):
        ch, tt = items[n]
        k = kind(ch)
        p = pp[n % 3]
        sl = slice(tt * TT, (tt + 1) * TT)
        if k in ("rope", "norm"):
            sq = sq_r[n % R]
            s.op("act", lambda: nc.scalar.activation(out=sq[:], in_=p[:], func=AF.Square), reads=[p.res], writes=[sq.res])
            s.op("pe", lambda: nc.tensor.matmul(psq[n % 2][:], lhsT=bones[:], rhs=sq[:], start=True, stop=True),
                 reads=[bones.res, sq.res], writes=[psq[n % 2].res])
        elif k == "raw":
            ob = ob_r[n % R]
            sc = 0.125 if ch in (16, 17) else 1.0
            s.op("act", lambda: nc.scalar.activation(out=ob[:], in_=p[:], func=AF.Copy, scale=sc), reads=[p.res], writes=[ob.res])
            s.dma(proj_d[ch, :, sl], ob[:], reads=[ob.res], is_output=True)
        elif k == "sig":
            of = of_r[n % R]
            s.op("act", lambda: nc.scalar.activation(out=of[:], in_=p[:], func=AF.Sigmoid), reads=[p.res], writes=[of.res])
            s.dma(gates_d[ch - 25, :, sl], of[:], reads=[of.res], is_output=True, q=("sp", "act")[n % 2])
        else:
            of = of_r[n % R]
            s.op("act", lambda: nc.scalar.activation(out=of[0:64, :], in_=p[0:64, :], func=AF.Sigmoid, bias=foxb[0:64, :], scale=1.0),
                 reads=[p.res, foxb.res], writes=[of.res])
            s.op("act", lambda: nc.scalar.activation(out=of[32:64, :], in_=of[32:64, :], func=AF.Ln), reads=[of.res], writes=[of.res])
            s.dma(misc_d[:, sl], of[0:64, :], reads=[of.res], is_output=True)

    def st2(n):
        ch, tt = items[n]
        k = kind(ch)
        if k not in ("rope", "norm"):
            return
        p = pp[n % 3]
        sl = slice(tt * TT, (tt + 1) * TT)
        rstd = rstd_r[n % R]
        y = y_r[n % R]
        s.op("act", lambda: nc.scalar.activation(out=rstd[:], in_=psq[n % 2][:], func=AF.Sqrt, scale=1.0 / 64, bias=epsb[:]),
             reads=[psq[n % 2].res, epsb.res], writes=[rstd.res])
        s.op("dve", lambda: nc.vector.reciprocal(out=rstd[:], in_=rstd[:]), reads=[rstd.res], writes=[rstd.res])
        s.op("dve", lambda: nc.vector.tensor_tensor(out=y[:], in0=p[:], in1=rstd[:], op=ALU.mult), reads=[p.res, rstd.res], writes=[y.res])
        if k == "norm":
            ob = ob_r[n % R]
            s.op("act", lambda: nc.scalar.activation(out=ob[:], in_=y[:], func=AF.Copy, scale=gain[:, ch:ch + 1]),
                 reads=[y.res, gain.res], writes=[ob.res])
            s.dma(proj_d[ch, :, sl], ob[:], reads=[ob.res], is_output=True)
        else:
            y2 = y2_r[n % R]
            s.op("act", lambda: nc.scalar.activation(out=y2[:], in_=y[:], func=AF.Copy, scale=gain[:, ch:ch + 1]),
                 reads=[y.res, gain.res], writes=[y2.res])
            s.op("pe", lambda: nc.tensor.matmul(pq[n % 2][:], lhsT=pm[:], rhs=y2[:], start=True, stop=True),
                 reads=[pm.res, y2.res], writes=[pq[n % 2].res])
            if ch in (0, 1):
                s.dma(proj_d[24 + ch, :, sl], y2[:], reads=[y2.res], is_output=True)

    def st3(n):
        ch, tt = items[n]
        if kind(ch) != "rope":
            return
        sl = slice(tt * TT, (tt + 1) * TT)
        y2 = y2_r[n % R]
        t1 = t1_r[n % R]
        y = y_r[n % R]
        ob = ob_r[n % R]
        s.op("pool", lambda: nc.gpsimd.tensor_tensor(out=t1[:], in0=y2[:], in1=COS[:, sl], op=ALU.mult), reads=[y2.res, COS.res], writes=[t1.res])
        s.op("dve", lambda: nc.vector.tensor_tensor(out=y[:], in0=pq[n % 2][:], in1=SIN[:, sl], op=ALU.mult),
             reads=[pq[n % 2].res, SIN.res], writes=[y.res])
        s.op("dve", lambda: nc.vector.tensor_tensor(out=ob[:], in0=t1[:], in1=y[:], op=ALU.add), reads=[t1.res, y.res], writes=[ob.res])
        s.dma(proj_d[ch, :, sl], ob[:], reads=[ob.res], is_output=True)

    pipeline(len(items), [stl, stc, st0, st1, st2, st3])
    c.end_phase(ph)
    if own:
        c.close()
    return c


def in_perm():
    Z = [-1] * 64
    r = lambda a, n: list(range(a, a + n))
    ch = []
    ch.append(r(0, 128)); ch.append(r(128, 128))
    ch.append(r(384, 64) + r(512, 64))
    ch.append(r(256, 64) + Z)
    ch.append(r(652, 128)); ch.append(r(780, 128))
    ch.append(r(908, 128)); ch.append(r(1036, 128))
    ch.append(r(2188, 128)); ch.append(r(2316, 128))
    ch.append(r(2444, 128)); ch.append(r(2572, 128))
    ch.append(r(320, 64) + r(448, 64))
    ch.append(r(576, 64) + Z)
    ch.append(r(1164, 128)); ch.append(r(1292, 128))
    ch.append(r(1420, 128)); ch.append(r(1548, 128))
    ch.append(r(1676, 128)); ch.append(r(1804, 128))
    ch.append(r(1932, 128)); ch.append(r(2060, 128))
    ch.append(r(2700, 128)); ch.append(r(2828, 128))
    ch.append(r(640, 12) + [-1] * 20 + r(2956, 4) + [-1] * 92)
    for g in range(32):
        ch.append(r(2960 + g * 128, 128))
    return np.array(ch, np.int64)


def consts_A():
    inv = (500000.0 ** (-np.arange(0, 16, 2, dtype=np.float32) / 16)).astype(np.float32)
    invf = np.zeros((128, 1), np.float32)
    pm = np.zeros((128, 128), np.float32)
    bones = np.zeros((128, 128), np.float32)
    for hb in (0, 64):
        bones[hb:hb + 64, hb:hb + 64] = 1.0
        for i in range(8):
            invf[hb + i, 0] = inv[i]
            invf[hb + 8 + i, 0] = inv[i]
            pm[hb + i + 8, hb + i] = -1.0
            pm[hb + i, hb + i + 8] = 1.0
    osc = np.ones((128, 12), np.float32)
    for chn in (0, 1, 4, 5, 8, 9):
        osc[:, chn] = 0.125
    return invf, pm, bones, osc


def maps_A(inp, l, xT_all, mod):
    perm = in_perm()
    w = inp["w_in"][l]
    wz = np.concatenate([w, np.zeros((D, 1), np.float32)], axis=1)
    wp = wz[:, perm.reshape(-1)].reshape(D, NCH_IN, 128)
    wp = np.ascontiguousarray(wp.reshape(8, 128, NCH_IN, 128).transpose(2, 1, 0, 3))
    invf, pm, bones, osc = consts_A()
    g = inp["qk_gain"][l]
    t2 = lambda a: np.concatenate([a, a])
    z64 = np.zeros(64, np.float32)
    gain = np.stack([t2(g[0]), t2(g[0]), np.concatenate([g[2], g[3]]), np.concatenate([g[1], z64]),
                     t2(g[4]), t2(g[4]), t2(g[5]), t2(g[5]), t2(g[6]), t2(g[6]), t2(g[7]), t2(g[7])], axis=1).astype(np.float32)
    foxb = np.zeros((128, 1), np.float32)
    foxb[32:36, 0] = inp["fox_bias"][l]
    gn = np.ascontiguousarray(inp["norm_mix"][l].reshape(8, 128).T)
    maps = []
    for i in range(8):
        b, q = i // 4, i % 4
        m = {"modA": np.ascontiguousarray(mod[l, b][:, 0:16]), "gn": gn,
             "pos": np.ascontiguousarray(inp["positions"][b:b + 1, q * NT:(q + 1) * NT]).astype(np.int32),
             "invf": invf, "w": wp, "gain": gain, "osc": osc, "foxb": foxb, "pm": pm, "bones": bones}
        if xT_all is not None:
            xs = xT_all[b][:, q * NT:(q + 1) * NT].reshape(8, 128, NT).transpose(1, 0, 2)
            m["xT"] = np.ascontiguousarray(xs)
        maps.append(m)
    return maps


def collect_A(results, pre=""):
    proj = [np.concatenate([results[b * 4 + q][pre + "proj"] for q in range(4)], axis=2) for b in range(2)]
    gates = [np.concatenate([results[b * 4 + q][pre + "gates"] for q in range(4)], axis=2) for b in range(2)]
    misc = [np.concatenate([results[b * 4 + q][pre + "misc"] for q in range(4)], axis=1) for b in range(2)]
    return proj, gates, misc


def run_A(cA, inp, l, xT_all, mod):
    maps = maps_A(inp, l, xT_all, mod)
    res = run_bass_kernel_spmd(cA.nc, maps, core_ids=list(range(8)))
    return collect_A(res.results)


B_MIXERS = ("dil", "fox", "sb", "nsa")
NKB = S // 128
NQT = S // TT
M_CAUSAL, M_STRICT, M_WIN, M_DIL, M_CMP, M_NEGC = 0, 4, 8, 16, 36, 41
N_MASKS = 45


def consts_B():
    import ml_dtypes
    k = np.arange(128)[:, None]
    cc = np.arange(512)[None, :]
    masks = np.zeros((N_MASKS, 128, 512), np.float32)
    for i in range(4):
        masks[M_CAUSAL + i] = (cc - k >= 128 * i)
        masks[M_STRICT + i] = (cc - k > 128 * i)
    for w in range(8):
        diff = cc - k + 512 - 128 * w
        masks[M_WIN + w] = (diff >= 0) & (diff < 512)
    for w in range(20):
        diff = cc - k + 2048 - 128 * w
        m = np.zeros((128, 512), np.float32)
        for (ww, d) in ((128, 1), (512, 4), (2048, 16)):
            m += ((diff % d == 0) & (diff >= 0) & (diff <= ww))
        masks[M_DIL + w] = m
    for u in range(5):
        masks[M_CMP + u] = (16 * k + 31 <= 512 * u + cc)
    for i in range(4):
        masks[M_NEGC + i] = -30000.0 * (cc - k < 128 * i)
    G = ((np.arange(S)[None, :] // 64) % 64 == np.arange(64)[:, None]).astype(np.float32)
    c0 = np.arange(511) * 16
    s0 = np.arange(128) * 64
    ov = ((c0[:, None] < s0[None, :] + 64) & (c0[:, None] + 32 > s0[None, :])).astype(np.float32)
    ov = np.concatenate([ov, np.zeros((1, 128), np.float32)], 0).reshape(4, 128, 128).transpose(1, 0, 2)
    onesc = np.ones((128, 4, 1), np.float32)
    onesc[127, 3, 0] = 0.0
    Rconst = np.concatenate([ov, onesc], axis=2)
    add = np.zeros((128, 254), np.float32)
    jj = np.arange(254)[None, :] - 126
    cr = (np.arange(128) // 64)[:, None]
    add[(jj == cr) | (jj == cr - 1)] = 1e30
    add[jj > cr] = -1e30
    jn = np.arange(128)[:, None]
    kn = np.arange(128)[None, :]
    nti = -(jn >= kn).astype(np.float32)
    ntc = -(jn < kn).astype(np.float32)
    bf = ml_dtypes.bfloat16
    return dict(masks=masks.astype(bf), G64=G.astype(bf), Rconst=Rconst.astype(bf), add=add,
                nti=nti.astype(bf), ntc=ntc.astype(bf), ident=np.eye(128, dtype=np.float32),
                tri64=(np.arange(64)[:, None] < np.arange(64)[None, :]).astype(np.float32))


def pipeline(n_items, stages):
    K = len(stages)
    for i in range(n_items + K - 1):
        for k, st in enumerate(stages):
            n = i - k
            if 0 <= n < n_items:
                st(n)


def build_B():
    c = Ctx()
    nc, s = c.nc, c.s
    di = lambda name, shape, dt: c.dram(name, shape, dt, "ExternalInput")
    qnr_d = di("qnr", [4, 64, S], BF16)
    qr_d = di("qr", [64, S], BF16)
    kcT_d = di("kcT", [64, S], BF16)
    vcT_d = di("vcT", [64, S], BF16)
    kslT_d = di("kslT", [64, S], BF16)
    kwT_d = di("kwT", [64, S], BF16)
    vsl_d = di("vsl", [128, NKB, 65], BF16)
    vw_d = di("vw", [128, NKB, 65], BF16)
    ag_d = di("ag", [128, NKB, 3], F32)
    dq_d = di("dq", [64, S], BF16)
    dk_d = di("dk", [64, S], BF16)
    dv_d = di("dv", [128, NKB, 65], BF16)
    sq_d = di("sq", [64, S], BF16)
    sk_d = di("sk", [64, S], BF16)
    sv_d = di("sv", [128, NKB, 65], BF16)
    fq_d = di("fq", [64, S], BF16)
    fk_d = di("fk", [64, S], BF16)
    fv_d = di("fv", [128, NKB, 65], BF16)
    lf_d = di("logf", [1, S], F32)
    w1k_d = di("w1k", [64, 32, 128], F32)
    w1v_d = di("w1v", [64, 32, 128], F32)
    w2k_d = di("w2k", [128, 64], F32)
    w2v_d = di("w2v", [128, 64], F32)
    pek_d = di("pek", [64, 32], F32)
    pev_d = di("pev", [64, 32], F32)
    masks_d = di("masks", [N_MASKS, 128, 512], BF16)
    G_d = di("G64", [64, S], BF16)
    Rc_d = di("Rconst", [128, 4, 129], BF16)
    add_d = di("add", [128, 254], F32)
    nti_d = di("nti", [128, 128], BF16)
    ntc_d = di("ntc", [128, 128], BF16)
    ident_d = di("ident", [128, 128], F32)
    tri_d = di("tri64", [64, 64], F32)
    o_d = c.dram("o", [4, 128, NKB, 64], BF16, "ExternalOutput")

    ps_ring = [c.ps([128, 512]) for _ in range(4)]
    po_ring = [c.ps([128, 512]) for _ in range(2)]
    pX = c.ps([128, 512])
    pY = c.ps([128, 512])
    e_ring = [c.sb([128, 512], BF16) for _ in range(4)]
    p_ring = [c.sb([128, 512], BF16) for _ in range(4)]
    pre_ring = [c.sb([128, 512], F32) for _ in range(4)]
    rz_ring = [c.sb([128, 4], F32) for _ in range(2)]
    fac_ring = [c.sb([128, 4], F32) for _ in range(2)]
    ost = c.sb([128, NKB, 64], BF16)

    class Scope:
        def __enter__(self):
            self.es = contextlib.ExitStack()
            self.old = c.es
            c.es = self.es
            return self

        def __exit__(self, *a):
            barrier(c)
            c.es = self.old
            self.es.close()

    def load_masks(lo, n, order=None):
        t = c.sb([128, n, 512], BF16)
        t.parts = [Res() for _ in range(n)]
        for ii, i in enumerate(order if order is not None else range(n)):
            s.dma(t[:, i, :], masks_d[lo + i], writes=[t.parts[i]], q=("sp", "act")[ii % 2])
        return t

    def split_load(QT, KT, V, qT_d, kT_d, v_d):
        qs = ("sp", "act")
        for t in (QT, KT, V):
            if t is not None:
                t.parts = [Res() for _ in range(4)]
        for h in range(4):
            sl = slice(h * 2048, (h + 1) * 2048)
            if QT is not None:
                s.dma(QT[0:64, sl], qT_d[:, sl], reads=[QT.res], writes=[QT.parts[h]], q=qs[h % 2])
            s.dma(KT[0:64, sl], kT_d[:, sl], reads=[KT.res], writes=[KT.parts[h]], q=qs[(h + 1) % 2])
            s.dma(V[:, 16 * h:16 * (h + 1), :], v_d[:, 16 * h:16 * (h + 1), :], reads=[V.res], writes=[V.parts[h]], q=qs[h % 2])

    def rQ(QT, qt):
        return [QT.res, QT.parts[qt // 4]]

    def rK(KT, kb):
        return [KT.res, KT.parts[kb // 16]]

    def attn(items, qk, pv, fin, mask_of, alt=[0], nps=4):
        def st0(n):
            qk(n, ps_ring[n % nps])

        def st1(n):
            it = items[n]
            ps, e = ps_ring[n % nps], e_ring[n % 4]
            if it["mi"] is not None and it["mi"] >= M_NEGC:
                mt, mi = mask_of(it["mi"])
                pre = pre_ring[n % 4]
                s.op("dve", lambda: nc.vector.tensor_tensor(out=pre[:], in0=ps[:], in1=mt[:, mi, :], op=ALU.add),
                     reads=[ps.res, mt.parts[mi]], writes=[pre.res])
                p = p_ring[n % 4]
                s.op("act", lambda: nc.scalar.activation(out=p[:], in_=pre[:], func=AF.Exp), reads=[pre.res], writes=[p.res])
                return
            s.op("act", lambda: nc.scalar.activation(out=e[:], in_=ps[:], func=AF.Exp), reads=[ps.res], writes=[e.res])
            if it["mi"] is not None:
                p = p_ring[n % 4]
                mt, mi = mask_of(it["mi"])
                alt[0] = 1
                if alt[0]:
                    s.op("dve", lambda: nc.vector.tensor_tensor(out=p[:], in0=e[:], in1=mt[:, mi, :], op=ALU.mult),
                         reads=[e.res, mt.parts[mi]], writes=[p.res])
                else:
                    s.op("pool", lambda: nc.gpsimd.tensor_tensor(out=p[:], in0=e[:], in1=mt[:, mi, :], op=ALU.mult),
                         reads=[e.res, mt.parts[mi]], writes=[p.res])

        def st2(n):
            it = items[n]
            pt = p_ring[n % 4] if it["mi"] is not None else e_ring[n % 4]
            pv(n, pt)
            if it["last"]:
                fin(n)
        pipeline(len(items), [st0, st1, (lambda n: None), st2])

    def std_pv(items, V, ncol=65):
        def pv(n, pt):
            it = items[n]
            po = po_ring[it["qt"] % 2]
            for qb in range(4):
                s.op("pe", (lambda qb=qb: nc.tensor.matmul(po[:, qb * 65:qb * 65 + ncol], lhsT=pt[:, qb * 128:(qb + 1) * 128],
                                                           rhs=V[:, it["kb"], 0:ncol], start=(it["first"] and qb == 0), stop=it["last"],
                                                           skip_group_check=True)),
                     reads=[pt.res, V.res, V.parts[it["kb"] // 16]], writes=[po.res])
        return pv

    def rz_of(qt, po, zoff=64, stride=65):
        rz = rz_ring[qt % 2]
        s.op("dve", lambda: nc.vector.tensor_scalar(out=rz[:], in0=po[:, zoff:zoff + 3 * stride + 1:stride], scalar1=1e-30, scalar2=None, op0=ALU.max),
             reads=[po.res], writes=[rz.res])
        s.op("dve", lambda: nc.vector.reciprocal(out=rz[:], in_=rz[:]), reads=[rz.res], writes=[rz.res])
        return rz

    def std_items(blocks_of):
        items = []
        for qt in range(NQT):
            bl = blocks_of(qt)
            for ii, (kb, mi) in enumerate(bl):
                items.append(dict(qt=qt, kb=kb, mi=mi, first=(ii == 0), last=(ii == len(bl) - 1)))
        return items

    def simple_mixer(m, qT_d, kT_d, v_d, blocks_of, mask_lo, mask_n, kdim=64, prep=None, mask_order=None):
        with Scope():
            QT = c.sb([128, S], BF16)
            KT = c.sb([128, S], BF16)
            V = c.sb([128, NKB, 65], BF16)
            if prep is not None:
                prep(QT, KT)
            split_load(QT, KT, V, qT_d, kT_d, v_d)
            mt = load_masks(mask_lo, mask_n, order=mask_order)
            items = std_items(blocks_of)

            def qk(n, ps):
                it = items[n]
                s.op("pe", lambda: nc.tensor.matmul(ps[:], lhsT=KT[0:kdim, it["kb"] * 128:(it["kb"] + 1) * 128],
                                                    rhs=QT[0:kdim, it["qt"] * 512:(it["qt"] + 1) * 512], start=True, stop=True),
                     reads=rK(KT, it["kb"]) + rQ(QT, it["qt"]), writes=[ps.res])

            def fin(n):
                qt = items[n]["qt"]
                po = po_ring[qt % 2]
                rz = rz_of(qt, po)
                for qb in range(4):
                    s.op("dve", (lambda qb=qb: nc.vector.tensor_scalar(out=ost[:, 4 * qt + qb, :], in0=po[:, qb * 65:qb * 65 + 64],
                                                                       scalar1=rz[:, qb:qb + 1], scalar2=None, op0=ALU.mult)),
                         reads=[po.res, rz.res], writes=[ost.res])
            attn(items, qk, std_pv(items, V), fin, lambda mi: (mt, mi - mask_lo))
            s.dma(o_d[m], ost[:], reads=[ost.res], is_output=True)

    def dil_blocks(qt):
        return [(4 * qt - 16 + w, M_DIL + w) for w in range(20) if 4 * qt - 16 + w >= 0]
    if "dil" in B_MIXERS:
        simple_mixer(1, dq_d, dk_d, dv_d, dil_blocks, M_DIL, 20, mask_order=[16, 17, 18, 19, 12, 13, 14, 15, 8, 9, 10, 11, 4, 5, 6, 7, 0, 1, 2, 3])

    fs_d = c.dram("fsplit", [6, S], BF16, "Internal")
    fs_res = Res()

    def fox_prep(QT, KT):
        lf = c.sb([64, 128], F32)
        F = c.sb([64, 128], F32)
        zr = c.sb([64, 128], F32)
        r1 = c.sb([64, 128], F32)
        off = c.sb([64, 1], F32)
        U = c.sb([64, 64], F32)
        sp3 = [c.sb([64, 128], BF16) for _ in range(3)]
        ng3 = [c.sb([64, 128], BF16) for _ in range(3)]
        s.dma(lf[:], lf_d.rearrange("o (p j) -> (o p) j", j=128), writes=[lf.res])
        s.dma(U[:], tri_d, writes=[U.res])
        s.op("pool", lambda: nc.gpsimd.memset(zr[:], 0.0), writes=[zr.res])
        s.op("pool", lambda: nc.gpsimd.memset(QT[:], 0.0), writes=[QT.res])
        s.op("pool", lambda: nc.gpsimd.memset(KT[:], 0.0), writes=[KT.res])
        s.op("pool", lambda: nc.gpsimd.memset(QT[96:99, :], 1.0), writes=[QT.res])
        s.op("pool", lambda: nc.gpsimd.memset(KT[64:67, :], 1.0), writes=[KT.res])
        s.op("dve", lambda: nc.vector.tensor_tensor_scan(out=F[:], data0=lf[:], data1=zr[:], initial=0.0, op0=ALU.add, op1=ALU.add),
             reads=[lf.res, zr.res], writes=[F.res])
        s.op("pe", lambda: nc.tensor.matmul(pY[0:64, 0:1], lhsT=U[:], rhs=F[:, 127:128], start=True, stop=True),
             reads=[U.res, F.res], writes=[pY.res])
        s.op("act", lambda: nc.scalar.copy(out=off[:], in_=pY[0:64, 0:1]), reads=[pY.res], writes=[off.res])
        s.op("dve", lambda: nc.vector.tensor_scalar(out=F[:], in0=F[:], scalar1=off[:, 0:1], scalar2=None, op0=ALU.add),
             reads=[F.res, off.res], writes=[F.res])
        cur = F
        for i in range(3):
            s.op("dve", (lambda i=i, cur=cur: nc.vector.tensor_copy(out=sp3[i][:], in_=cur[:])), reads=[cur.res], writes=[sp3[i].res])
            s.op("dve", (lambda i=i: nc.vector.tensor_scalar(out=ng3[i][:], in0=sp3[i][:], scalar1=-1.0, scalar2=None, op0=ALU.mult)),
                 reads=[sp3[i].res], writes=[ng3[i].res])
            if i < 2:
                s.op("dve", (lambda i=i, cur=cur: nc.vector.tensor_tensor(out=r1[:], in0=cur[:], in1=sp3[i][:], op=ALU.subtract)),
                     reads=[cur.res, sp3[i].res], writes=[r1.res])
                cur = r1
            s.dma(fs_d[i, :].rearrange("(p j) -> p j", j=128), sp3[i][:], reads=[sp3[i].res], writes=[fs_res])
            s.dma(fs_d[3 + i, :].rearrange("(p j) -> p j", j=128), ng3[i][:], reads=[ng3[i].res], writes=[fs_res])
        s.dma(QT[64:67, :], fs_d[0:3, :], reads=[fs_res], writes=[QT.res])
        s.dma(KT[96:99, :], fs_d[3:6, :], reads=[fs_res], writes=[KT.res])

    def causal_blocks(qt):
        return [(kb, None) for kb in range(4 * qt)] + [(4 * qt + i, M_CAUSAL + i) for i in range(4)]

    def negc_blocks(qt):
        return [(kb, None) for kb in range(4 * qt)] + [(4 * qt + i, M_NEGC + i) for i in range(4)]
    if "fox" in B_MIXERS:
        simple_mixer(3, fq_d, fk_d, fv_d, negc_blocks, M_NEGC, 4, kdim=99, prep=fox_prep)

    with (Scope() if "sb" in B_MIXERS else contextlib.nullcontext()):
      if "sb" in B_MIXERS:
        QT = c.sb([64, S], BF16)
        KT = c.sb([64, S], BF16)
        V = c.sb([128, NKB, 65], BF16)
        nti = c.sb([128, 128], BF16)
        ntc = c.sb([128, 128], BF16)
        split_load(QT, KT, V, sq_d, sk_d, sv_d)
        s.dma(nti[:], nti_d, writes=[nti.res])
        s.dma(ntc[:], ntc_d, writes=[ntc.res])
        mt = load_masks(M_STRICT, 4)
        E_r = [c.sb([128, 512], F32) for _ in range(4)]
        L_r = [c.sb([128, 512], BF16) for _ in range(4)]
        X_r = [c.sb([128, 512], F32) for _ in range(4)]
        A_r = [c.sb([128, 512], BF16) for _ in range(4)]
        def sb_blocks(qt):
            bl = [(4 * qt + i, M_STRICT + i) for i in (3, 2, 1, 0)] + [(kb, None) for kb in range(4 * qt - 1, -1, -1)]
            return [dict(qt=qt, kb=kb, mi=mi, first=(ii == 0), last=(ii == len(bl) - 1)) for ii, (kb, mi) in enumerate(bl)]
        items = []
        for pr in range(NQT // 2):
            la, lb = sb_blocks(2 * pr), sb_blocks(2 * pr + 1)
            for ii in range(len(lb)):
                if ii < len(la):
                    items.append(la[ii])
                items.append(lb[ii])
        pXs = (pX, pY)

        def sb0(n):
            it = items[n]
            ps = ps_ring[n % 4]
            s.op("pe", lambda: nc.tensor.matmul(ps[:], lhsT=KT[:, it["kb"] * 128:(it["kb"] + 1) * 128],
                                                rhs=QT[:, it["qt"] * 512:(it["qt"] + 1) * 512], start=True, stop=True),
                 reads=rK(KT, it["kb"]) + rQ(QT, it["qt"]), writes=[ps.res])

        def sb1(n):
            it = items[n]
            ps, E, L = ps_ring[n % 4], E_r[n % 4], L_r[n % 4]
            s.op("act", lambda: nc.scalar.activation(out=E[:], in_=ps[:], func=AF.Exp), reads=[ps.res], writes=[E.res])
            s.op("act", lambda: nc.scalar.activation(out=L[:], in_=E[:], func=AF.Ln, bias=1.0, scale=1.0), reads=[E.res], writes=[L.res])
            if it["mi"] is not None:
                mi = it["mi"] - M_STRICT
                s.op("pool", lambda: nc.gpsimd.tensor_tensor(out=L[:], in0=L[:], in1=mt[:, mi, :], op=ALU.mult),
                     reads=[L.res, mt.parts[mi]], writes=[L.res])
                s.op("dve", lambda: nc.vector.tensor_tensor(out=E[:], in0=E[:], in1=mt[:, mi, :], op=ALU.mult),
                     reads=[E.res, mt.parts[mi]], writes=[E.res])

        def sb2(n):
            it = items[n]
            L, X = L_r[n % 4], X_r[n % 4]
            pXq = pXs[it["qt"] % 2]
            s.op("pe", lambda: nc.tensor.matmul(pXq[:], lhsT=nti[:], rhs=L[:], start=it["first"], stop=False, skip_group_check=True),
                 reads=[nti.res, L.res], writes=[pXq.res])
            s.op("act", lambda: nc.scalar.activation(out=X[:], in_=pXq[:], func=AF.Exp), reads=[pXq.res], writes=[X.res])

        def sb3(n):
            it = items[n]
            L, X, E, A = L_r[n % 4], X_r[n % 4], E_r[n % 4], A_r[n % 4]
            pXq = pXs[it["qt"] % 2]
            s.op("pe", lambda: nc.tensor.matmul(pXq[:], lhsT=ntc[:], rhs=L[:], start=False, stop=it["last"], skip_group_check=True),
                 reads=[ntc.res, L.res], writes=[pXq.res])
            s.op("dve", lambda: nc.vector.tensor_tensor(out=A[:], in0=E[:], in1=X[:], op=ALU.mult), reads=[E.res, X.res], writes=[A.res])

        def sb4(n):
            it = items[n]
            A = A_r[n % 4]
            qt = it["qt"]
            po = po_ring[qt % 2]
            for qb in range(4):
                s.op("pe", (lambda qb=qb: nc.tensor.matmul(po[:, qb * 65:qb * 65 + 64], lhsT=A[:, qb * 128:(qb + 1) * 128],
                                                           rhs=V[:, it["kb"], 0:64], start=(it["first"] and qb == 0), stop=it["last"],
                                                           skip_group_check=True)),
                     reads=[A.res, V.res, V.parts[it["kb"] // 16]], writes=[po.res])
            if it["last"]:
                for qb in range(4):
                    s.op("act", (lambda qb=qb: nc.scalar.copy(out=ost[:, 4 * qt + qb, :], in_=po[:, qb * 65:qb * 65 + 64])),
                         reads=[po.res], writes=[ost.res])
        K_ = len(items)
        for i in range(K_ + 4):
            if i < K_:
                sb0(i)
            if 0 <= i - 1 < K_:
                sb1(i - 1)
            if 0 <= i - 3 < K_:
                sb3(i - 3)
            if 0 <= i - 2 < K_:
                sb2(i - 2)
            if 0 <= i - 4 < K_:
                sb4(i - 4)
        s.dma(o_d[2], ost[:], reads=[ost.res], is_output=True)

    hsel_d = di("hsel", [128, 4], F32)
    with (Scope() if "nsa" in B_MIXERS else contextlib.nullcontext()):
      if "nsa" in B_MIXERS:
        QA = c.sb([128, S], BF16)
        QB = c.sb([128, S], BF16)
        oa = c.sb([128, NKB, 64], F32)
        oa.parts = [Res() for _ in range(NKB)]
        ag = c.sb([128, NKB, 3], F32)
        ident = c.sb([128, 128], F32)
        addt = c.sb([128, 254], F32)
        hsel = c.sb([128, 4], F32)
        for h in range(4):
            sl = slice(h * 2048, (h + 1) * 2048)
            s.dma(QA[0:64, sl], qr_d[:, sl], writes=[QA.res], q="act")
            s.dma(QB[0:64, sl], qr_d[:, sl], writes=[QB.res], q="act")
        for t, d in ((ag, ag_d), (ident, ident_d), (addt, add_d), (hsel, hsel_d)):
            s.dma(t[:], d, writes=[t.res])
        with Scope():
            kcT = c.sb([64, S], BF16)
            vcT = c.sb([64, S], BF16)
            for h in range(4):
                sl = slice(h * 2048, (h + 1) * 2048)
                s.dma(kcT[:, sl], kcT_d[:, sl], writes=[kcT.res])
                s.dma(vcT[:, sl], vcT_d[:, sl], writes=[vcT.res])
            kccT = c.sb([64, 512], BF16)
            Rt = c.sb([128, 4, 193], BF16)
            s.dma(Rt[:, :, 0:129], Rc_d, writes=[Rt.res])
            w1f = c.sb([64, 32, 128], F32)
            w1b = c.sb([64, 32, 128], BF16)
            w2f = c.sb([128, 64], F32)
            w2b = c.sb([128, 64], BF16)
            pef = c.sb([64, 32], F32)
            peb = c.sb([128, 1], F32)
            xg = c.sb([128, 512], F32)
            x2 = c.sb([128, 512], F32)
            gT = c.sb([128, 512], BF16)
            for which in ("k", "v"):
                src = kcT if which == "k" else vcT
                s.dma(w1f[:], w1k_d if which == "k" else w1v_d, writes=[w1f.res])
                s.dma(w2f[:], w2k_d if which == "k" else w2v_d, writes=[w2f.res])
                s.dma(pef[:], pek_d if which == "k" else pev_d, writes=[pef.res])
                s.op("pool", lambda: nc.gpsimd.tensor_copy(out=w1b[:], in_=w1f[:]), reads=[w1f.res], writes=[w1b.res])
                s.op("pool", lambda: nc.gpsimd.tensor_copy(out=w2b[:], in_=w2f[:]), reads=[w2f.res], writes=[w2b.res])
                for l in range(32):
                    s.op("pe", (lambda l=l: nc.tensor.matmul(pY[:, 0:1], lhsT=w1f[:, l, :], rhs=pef[:, l:l + 1], start=(l == 0), stop=(l == 31))),
                         reads=[w1f.res, pef.res], writes=[pY.res])
                s.op("act", lambda: nc.scalar.copy(out=peb[:], in_=pY[:, 0:1]), reads=[pY.res], writes=[peb.res])
                srcv = src[:].rearrange("p (c s) -> p c s", s=16)
                for l in range(32):
                    rhs = srcv[:, 0:511, l] if l < 16 else srcv[:, 1:512, l - 16]
                    s.op("pe", (lambda l=l, rhs=rhs: nc.tensor.matmul(pX[:, 0:511], lhsT=w1b[:, l, :], rhs=rhs, start=(l == 0), stop=(l == 31))),
                         reads=[w1b.res, src.res], writes=[pX.res])
                s.op("act", lambda: nc.scalar.activation(out=xg[:, 0:511], in_=pX[:, 0:511], func=AF.Identity, bias=peb[:], scale=1.0),
                     reads=[pX.res, peb.res], writes=[xg.res])
                s.op("dve", lambda: nc.vector.tensor_tensor(out=x2[:, 0:511], in0=xg[:, 0:511], in1=xg[:, 0:511], op=ALU.mult), reads=[xg.res], writes=[x2.res])
                s.op("dve", lambda: nc.vector.tensor_scalar(out=x2[:, 0:511], in0=x2[:, 0:511], scalar1=0.044715, scalar2=1.0, op0=ALU.mult, op1=ALU.add),
                     reads=[x2.res], writes=[x2.res])
                s.op("dve", lambda: nc.vector.tensor_tensor(out=x2[:, 0:511], in0=x2[:, 0:511], in1=xg[:, 0:511], op=ALU.mult), reads=[x2.res, xg.res], writes=[x2.res])
                s.op("act", lambda: nc.scalar.activation(out=x2[:, 0:511], in_=x2[:, 0:511], func=AF.Sigmoid, scale=1.5957691216057308),
                     reads=[x2.res], writes=[x2.res])
                s.op("pool", lambda: nc.gpsimd.memset(gT[:], 0.0), writes=[gT.res])
                s.op("dve", lambda: nc.vector.tensor_tensor(out=gT[:, 0:511], in0=xg[:, 0:511], in1=x2[:, 0:511], op=ALU.mult), reads=[xg.res, x2.res, gT.res], writes=[gT.res])
                if which == "k":
                    s.op("pe", lambda: nc.tensor.matmul(pY[0:64, :], lhsT=w2b[:], rhs=gT[:], start=True, stop=True), reads=[w2b.res, gT.res], writes=[pY.res])
                    s.op("act", lambda: nc.scalar.copy(out=kccT[:], in_=pY[0:64, :]), reads=[pY.res], writes=[kccT.res])
                else:
                    for cc in range(4):
                        s.op("pe", (lambda cc=cc: nc.tensor.matmul(pY[:, cc * 64:(cc + 1) * 64], lhsT=gT[:, cc * 128:(cc + 1) * 128], rhs=w2b[:],
                                                                   start=True, stop=True)),
                             reads=[w2b.res, gT.res], writes=[pY.res])
                    s.op("act", lambda: nc.scalar.copy(out=Rt[:, :, 129:193], in_=pY[:, 0:256].rearrange("p (a b) -> p a b", b=64)),
                         reads=[pY.res], writes=[Rt.res])
            mt = load_masks(M_CMP, 5)
            aghs = c.sb([128, 4, NKB], F32)
            for h in range(4):
                s.op("dve", (lambda h=h: nc.vector.tensor_scalar(out=aghs[:, h, :], in0=ag[:, :, 0], scalar1=hsel[:, h:h + 1], scalar2=None, op0=ALU.mult)),
                     reads=[ag.res, hsel.res], writes=[aghs.res])
            qring = [c.sb([64, 512], BF16) for _ in range(4)]
            imp = [c.sb([128, 4, 128], F32) for _ in range(2)]
            for t in imp:
                t.parts = [Res() for _ in range(4)]
            sc_r = [c.sb([128, 128], F32) for _ in range(4)]
            sc2_r = [c.sb([128, 128], F32) for _ in range(4)]
            sb_r = [c.sb([128, 128], F32) for _ in range(4)]
            sbr_r = [c.sb([128, 128], F32) for _ in range(4)]
            for t in sbr_r:
                s.op("pool", (lambda t=t: nc.gpsimd.memset(t[:], 0.0)), writes=[t.res])
            m8_r = [c.sb([128, 16], F32) for _ in range(4)]
            pT = ps_ring[3]
            usets = [(po_ring[0], po_ring[1]), (pX, pY)]
            items = []
            for qt in range(NQT):
                for h in range(4):
                    ccs = [cc for cc in range(4) if qt - 4 * cc >= 0]
                    for ii, cc in enumerate(ccs):
                        u = qt - 4 * cc
                        items.append(dict(qt=qt, h=h, kb=cc, mi=(M_CMP + u if u <= 4 else None), first=(ii == 0), last=(ii == len(ccs) - 1)))

            def qk_c(n, ps):
                it = items[n]
                qtile = qring[(it["qt"] * 4 + it["h"]) % 4]
                if it["first"]:
                    s.dma(qtile[:], qnr_d[it["h"], :, it["qt"] * 512:(it["qt"] + 1) * 512], writes=[qtile.res])
                s.op("pe", lambda: nc.tensor.matmul(ps[:], lhsT=kccT[:, it["kb"] * 128:(it["kb"] + 1) * 128], rhs=qtile[:], start=True, stop=True),
                     reads=[kccT.res, qtile.res], writes=[ps.res])

            def pv_c(n, pt):
                it = items[n]
                us = usets[(it["qt"] * 4 + it["h"]) % 2]
                for qb in range(4):
                    tl = us[qb // 2]
                    off = (qb % 2) * 193
                    s.op("pe", (lambda qb=qb, tl=tl, off=off: nc.tensor.matmul(tl[:, off:off + 193], lhsT=pt[:, qb * 128:(qb + 1) * 128], rhs=Rt[:, it["kb"], :],
                                                                             start=(it["first"] and qb % 2 == 0), stop=it["last"], skip_group_check=True)),
                         reads=[pt.res, Rt.res], writes=[tl.res])

            def fin_c(n):
                it = items[n]
                qt, h = it["qt"], it["h"]
                us = usets[(qt * 4 + h) % 2]
                rz = rz_ring[h % 2]
                fac = fac_ring[h % 2]
                imp_t = imp[qt % 2]
                for half in range(2):
                    tl = us[half]
                    s.op("dve", (lambda half=half, tl=tl: nc.vector.tensor_scalar(out=rz[:, 2 * half:2 * half + 2], in0=tl[:, 128:322:193], scalar1=1e-30,
                                                                                scalar2=None, op0=ALU.max)), reads=[tl.res], writes=[rz.res])
                s.op("dve", lambda: nc.vector.reciprocal(out=rz[:], in_=rz[:]), reads=[rz.res], writes=[rz.res])
                s.op("dve", lambda: nc.vector.tensor_tensor(out=fac[:], in0=rz[:], in1=aghs[:, h, 4 * qt:4 * qt + 4], op=ALU.mult), reads=[rz.res, aghs.res], writes=[fac.res])
                for qb in range(4):
                    tl, off = us[qb // 2], (qb % 2) * 193
                    tb = 4 * qt + qb
                    if h == 0:
                        s.op("dve", (lambda qb=qb, tl=tl, off=off: nc.vector.tensor_scalar(out=imp_t[:, qb, :], in0=tl[:, off:off + 128], scalar1=rz[:, qb:qb + 1],
                                                                                         scalar2=None, op0=ALU.mult)), reads=[tl.res, rz.res], writes=[imp_t.parts[qb]])
                        s.op("dve", (lambda qb=qb, tl=tl, off=off, tb=tb: nc.vector.tensor_scalar(out=oa[:, tb, :], in0=tl[:, off + 129:off + 193], scalar1=fac[:, qb:qb + 1],
                                                                                                scalar2=None, op0=ALU.mult)), reads=[tl.res, fac.res], writes=[oa.parts[tb]])
                    else:
                        s.op("dve", (lambda qb=qb, tl=tl, off=off: nc.vector.scalar_tensor_tensor(out=imp_t[:, qb, :], in0=tl[:, off:off + 128], scalar=rz[:, qb:qb + 1],
                                                                                                in1=imp_t[:, qb, :], op0=ALU.mult, op1=ALU.add)),
                             reads=[tl.res, rz.res, imp_t.parts[qb]], writes=[imp_t.parts[qb]])
                        s.op("dve", (lambda qb=qb, tl=tl, off=off, tb=tb: nc.vector.scalar_tensor_tensor(out=oa[:, tb, :], in0=tl[:, off + 129:off + 193], scalar=fac[:, qb:qb + 1],
                                                                                                       in1=oa[:, tb, :], op0=ALU.mult, op1=ALU.add)),
                             reads=[tl.res, fac.res, oa.parts[tb]], writes=[oa.parts[tb]])
                if h != 3:
                    return
                for qb in range(4):
                    tb = 4 * qt + qb
                    sc, sc2, sbt, m8 = sc_r[qb], sc2_r[qb], sb_r[qb], m8_r[qb]
                    s.op("dve", (lambda qb=qb, sc=sc, tb=tb: nc.vector.tensor_tensor(out=sc[:], in0=imp_t[:, qb, :], in1=addt[:, 126 - 2 * tb:254 - 2 * tb], op=ALU.add)),
                         reads=[imp_t.parts[qb], addt.res], writes=[sc.res])
                    s.op("dve", (lambda sc=sc: nc.vector.memset(sc[:, 0:1], 1e30)), reads=[sc.res], writes=[sc.res])
                    s.op("dve", (lambda sc=sc, m8=m8: nc.vector.max(out=m8[:, 0:8], in_=sc[:])), reads=[sc.res], writes=[m8.res])
                    s.op("dve", (lambda sc=sc, sc2=sc2, m8=m8: nc.vector.match_replace(out=sc2[:], in_to_replace=m8[:, 0:8], in_values=sc[:], imm_value=-3e38)),
                         reads=[sc.res, m8.res], writes=[sc2.res])
                    s.op("dve", (lambda sc2=sc2, m8=m8: nc.vector.max(out=m8[:, 8:16], in_=sc2[:])), reads=[sc2.res, m8.res], writes=[m8.res])
                    s.op("dve", (lambda sc=sc, sbt=sbt, m8=m8: nc.vector.tensor_scalar(out=sbt[:], in0=sc[:], scalar1=m8[:, 15:16], scalar2=-30000.0,
                                                                                   op0=ALU.is_lt, op1=ALU.mult)), reads=[sc.res, m8.res], writes=[sbt.res])
                    sbr = sbr_r[qb]
                    s.op("dve", (lambda sc=sc, sbr=sbr, m8=m8: nc.vector.tensor_scalar(out=sbr[:, 64:128], in0=sc[:, 0:64], scalar1=m8[:, 15:16], scalar2=-30000.0,
                                                                                   op0=ALU.is_lt, op1=ALU.mult)), reads=[sc.res, m8.res], writes=[sbr.res])
                for qb in range(4):
                    s.op("pe", (lambda qb=qb: nc.tensor.transpose(pT[:, qb * 128:(qb + 1) * 128], sb_r[qb][:], ident[:])),
                         reads=[sb_r[qb].res, ident.res], writes=[pT.res])
                s.op("act", lambda: nc.scalar.copy(out=QB[64:128, qt * 512:(qt + 1) * 512], in_=pT[64:128, :]), reads=[pT.res], writes=[QB.res])
                for qb in range(4):
                    s.op("pe", (lambda qb=qb: nc.tensor.transpose(pT[:, qb * 128:(qb + 1) * 128], sbr_r[qb][:], ident[:])),
                         reads=[sbr_r[qb].res, ident.res], writes=[pT.res])
                s.op("act", lambda: nc.scalar.copy(out=QA[64:128, qt * 512:(qt + 1) * 512], in_=pT[64:128, :]), reads=[pT.res], writes=[QA.res])
            attn(items, qk_c, pv_c, fin_c, lambda mi: (mt, mi - M_CMP), nps=3)

        with Scope():
            KS = c.sb([128, S], BF16)
            KW = c.sb([64, S], BF16)
            VS = c.sb([128, NKB, 65], BF16)
            VW = c.sb([128, NKB, 65], BF16)
            mtc = load_masks(M_CAUSAL, 4)
            for h in range(4):
                sl = slice(h * 2048, (h + 1) * 2048)
                s.dma(KS[64:128, sl], G_d[:, sl], writes=[KS.res], q="act")
            split_load(None, KS, VS, None, kslT_d, vsl_d)
            split_load(None, KW, VW, None, kwT_d, vw_d)
            mtw = load_masks(M_WIN, 8)

            def branch(KT, V, blocks_of, mt, mask_lo, br, with_sel):
                items = std_items(blocks_of)

                def qk(n, ps):
                    it = items[n]
                    kd = 128 if with_sel else 64
                    Qx = (QA if it["kb"] < 32 else QB) if with_sel else QA
                    s.op("pe", lambda: nc.tensor.matmul(ps[:], lhsT=KT[0:kd, it["kb"] * 128:(it["kb"] + 1) * 128],
                                                        rhs=Qx[0:kd, it["qt"] * 512:(it["qt"] + 1) * 512], start=True, stop=True),
                         reads=rK(KT, it["kb"]) + [Qx.res], writes=[ps.res])

                def fin(n):
                    qt = items[n]["qt"]
                    po = po_ring[qt % 2]
                    rz = rz_of(qt, po)
                    fac = fac_ring[qt % 2]
                    s.op("dve", lambda: nc.vector.tensor_tensor(out=fac[:], in0=rz[:], in1=ag[:, 4 * qt:4 * qt + 4, br], op=ALU.mult),
                         reads=[rz.res, ag.res], writes=[fac.res])
                    for qb in range(4):
                        tb = 4 * qt + qb
                        s.op("dve", (lambda qb=qb, tb=tb: nc.vector.scalar_tensor_tensor(out=oa[:, tb, :], in0=po[:, qb * 65:qb * 65 + 64], scalar=fac[:, qb:qb + 1],
                                                                                         in1=oa[:, tb, :], op0=ALU.mult, op1=ALU.add)),
                             reads=[po.res, fac.res, oa.parts[tb]], writes=[oa.parts[tb]])
                attn(items, qk, std_pv(items, V), fin, lambda mi: (mt, mi - mask_lo))
            if "nosel" not in B_MIXERS:
                branch(KS, VS, causal_blocks, mtc, M_CAUSAL, 1, True)
            if "nowin" not in B_MIXERS:
                branch(KW, VW, lambda qt: [(4 * qt - 4 + w, M_WIN + w) for w in range(8) if 4 * qt - 4 + w >= 0], mtw, M_WIN, 2, False)
        s.op("act", lambda: nc.scalar.copy(out=ost[:], in_=oa[:]), reads=[oa.res] + oa.parts, writes=[ost.res])
        s.dma(o_d[0], ost[:], reads=[ost.res], is_output=True)
    c.close()
    return c


def run_B(cB, inp, l, proj, misc):
    import ml_dtypes
    bf = ml_dtypes.bfloat16
    cst = consts_B()

    def head_rows(P, ch0, i):
        return np.ascontiguousarray(P[ch0 + i // 2][(i % 2) * 64:(i % 2) * 64 + 64])

    def vaug(vT):
        v = vT.T.reshape(NKB, 128, 64).transpose(1, 0, 2)
        return np.ascontiguousarray(np.concatenate([v, np.ones((128, NKB, 1), bf)], axis=2))
    cl = 32 * 64
    w1k = np.ascontiguousarray(inp["nsa_ck_w1"][l].reshape(32, 64, 128).transpose(1, 0, 2))
    w1v = np.ascontiguousarray(inp["nsa_cv_w1"][l].reshape(32, 64, 128).transpose(1, 0, 2))
    pek = np.ascontiguousarray(inp["nsa_pe_k"][l].T)
    pev = np.ascontiguousarray(inp["nsa_pe_v"][l].T)
    maps = []
    for core in range(8):
        b, i = core // 4, core % 4
        P = proj[b]
        m = dict(cst)
        m["qnr"] = np.ascontiguousarray(np.stack([head_rows(P, 24, h) for h in range(4)]))
        m["qr"] = head_rows(P, 0, i)
        m["kcT"] = np.ascontiguousarray(P[3][0:64]); m["vcT"] = np.ascontiguousarray(P[12][0:64])
        m["kslT"] = np.ascontiguousarray(P[2][0:64]); m["kwT"] = np.ascontiguousarray(P[2][64:128])
        m["vsl"] = vaug(P[12][64:128]); m["vw"] = vaug(P[13][0:64])
        ag = misc[b][3 * i:3 * i + 3]
        m["ag"] = np.ascontiguousarray(ag.T.reshape(NKB, 128, 3).transpose(1, 0, 2))
        m["dq"] = head_rows(P, 4, i); m["dk"] = head_rows(P, 6, i); m["dv"] = vaug(head_rows(P, 14, i))
        m["sq"] = head_rows(P, 16, i); m["sk"] = head_rows(P, 18, i); m["sv"] = vaug(head_rows(P, 20, i))
        m["fq"] = head_rows(P, 8, i); m["fk"] = head_rows(P, 10, i); m["fv"] = vaug(head_rows(P, 22, i))
        m["logf"] = np.ascontiguousarray(misc[b][32 + i:33 + i])
        m["w1k"] = w1k; m["w1v"] = w1v; m["w2k"] = inp["nsa_ck_w2"][l]; m["w2v"] = inp["nsa_cv_w2"][l]
        m["pek"] = pek; m["pev"] = pev
        hs = np.zeros((128, 4), np.float32); hs[:, i] = 1.0
        m["hsel"] = hs
        maps.append(m)
    res = run_bass_kernel_spmd(cB.nc, maps, core_ids=list(range(8)))
    outs = []
    for b in range(2):
        ob = np.zeros((4, 256, S), bf)
        for i in range(4):
            o = res.results[b * 4 + i]["o"]
            for mm in range(4):
                tok = o[mm].transpose(1, 0, 2).reshape(S, 64)
                ob[mm, i * 64:(i + 1) * 64, :] = tok.T
        outs.append(ob)
    return outs


def build_C1(c=None):
    own = c is None
    if own:
        c = Ctx()
    ph = c.begin_phase()
    nc, s = c.nc, c.s
    oT_d = c.dram("oT", [4, 2, 128, NT], BF16, "ExternalInput")
    gates_d = c.dram("gates", [32, 128, NT], F32, "ExternalInput")
    xT_d = c.dram("xT", [128, 8, NT], F32, "ExternalInput")
    wbr_d = c.dram("wbr", [128, 8, 1024], F32, "ExternalInput")
    wout_d = c.dram("wout", [128, 8, 1024], F32, "ExternalInput")
    ga_d = c.dram("ga", [128, 8], F32, "ExternalInput")
    x1_d = c.dram("x1T", [128, 8, NT], F32, "ExternalOutput")

    xT = c.sb([128, 8, NT], F32)
    wbr = c.sb([128, 8, 1024], BF16)
    wout = c.sb([128, 8, 1024], BF16)
    ga = c.sb([128, 8], F32)
    stg = [c.sb([128, 2, 1024], F32) for _ in range(2)]
    s.dma(ga[:], ga_d, writes=[ga.res])
    for j in range(8):
        s.dma(xT[:, j, :], xT_d[:, j, :], writes=[xT.res], q=("sp", "act")[j % 2])
    k = 0
    for (dst, src) in ((wbr, wbr_d), (wout, wout_d)):
        for q in range(4):
            st = stg[k % 2]
            k += 1
            s.dma(st[:], src[:, 2 * q:2 * q + 2, :], writes=[st.res])
            s.op("pool", (lambda st=st, dst=dst, q=q: nc.gpsimd.tensor_copy(out=dst[:, 2 * q:2 * q + 2, :], in_=st[:])), reads=[st.res], writes=[dst.res])
    ot_r = [c.sb([128, 8, TT], BF16) for _ in range(2)]
    gt_r = [[c.sb([128, TT], F32) for _ in range(4)] for _ in range(3)]
    zT_r = [c.sb([128, 8, TT], BF16) for _ in range(2)]
    pm_r = [c.ps([128, TT]) for _ in range(4)]
    pmix = [c.ps([128, TT]) for _ in range(2)]
    t_r = [c.sb([128, TT], F32) for _ in range(4)]
    items = [(tt, dc) for tt in range(NT // TT) for dc in range(8)]

    def c_load(n):
        tt, dc = items[n]
        sl = slice(tt * TT, (tt + 1) * TT)
        if dc == 0:
            ot = ot_r[tt % 2]
            for m in range(4):
                for kc in range(2):
                    s.dma(ot[:, m * 2 + kc, :], oT_d[m, kc, :, sl], writes=[ot.res], q="act")
        gts = gt_r[n % 3]
        for m in range(4):
            s.dma(gts[m][:], gates_d[m * 8 + dc, :, sl], writes=[gts[m].res], q=("sp", "act")[m % 2])

    def c_comp(n):
        tt, dc = items[n]
        sl = slice(tt * TT, (tt + 1) * TT)
        ot = ot_r[tt % 2]
        gts = gt_r[n % 3]
        zT = zT_r[tt % 2]
        for m in range(4):
            for kc in range(2):
                s.op("pe", (lambda m=m, kc=kc: nc.tensor.matmul(pm_r[m][:], lhsT=wbr[:, m * 2 + kc, dc * 128:(dc + 1) * 128], rhs=ot[:, m * 2 + kc, :],
                                                                start=(kc == 0), stop=(kc == 1))),
                     reads=[wbr.res, ot.res], writes=[pm_r[m].res])
        for m in range(4):
            s.op("dve", (lambda m=m: nc.vector.tensor_tensor(out=t_r[m][:], in0=pm_r[m][:], in1=gts[m][:], op=ALU.mult)),
                 reads=[pm_r[m].res, gts[m].res], writes=[t_r[m].res])
        s.op("pool", lambda: nc.gpsimd.tensor_tensor(out=t_r[0][:], in0=t_r[0][:], in1=t_r[1][:], op=ALU.add), reads=[t_r[0].res, t_r[1].res], writes=[t_r[0].res])
        s.op("pool", lambda: nc.gpsimd.tensor_tensor(out=t_r[2][:], in0=t_r[2][:], in1=t_r[3][:], op=ALU.add), reads=[t_r[2].res, t_r[3].res], writes=[t_r[2].res])
        s.op("pool", lambda: nc.gpsimd.tensor_tensor(out=zT[:, dc, :], in0=t_r[0][:], in1=t_r[2][:], op=ALU.add),
             reads=[t_r[0].res, t_r[2].res], writes=[zT.res])
        if dc != 7:
            return
        for ec in range(8):
            pmx = pmix[ec % 2]
            for j in range(8):
                s.op("pe", (lambda j=j, ec=ec, pmx=pmx: nc.tensor.matmul(pmx[:], lhsT=wout[:, j, ec * 128:(ec + 1) * 128], rhs=zT[:, j, :],
                                                                         start=(j == 0), stop=(j == 7))),
                     reads=[wout.res, zT.res], writes=[pmx.res])
            s.op("dve", (lambda ec=ec, pmx=pmx: nc.vector.scalar_tensor_tensor(out=xT[:, ec, sl], in0=pmx[:], scalar=ga[:, ec:ec + 1], in1=xT[:, ec, sl],
                                                                              op0=ALU.mult, op1=ALU.add)),
                 reads=[pmx.res, ga.res, xT.res], writes=[xT.res])
    pipeline(len(items), [c_load, c_comp])
    for j in range(8):
        s.dma(x1_d[:, j, :], xT[:, j, :], reads=[xT.res], is_output=True)
    c.end_phase(ph)
    if own:
        c.close()
    return c


def maps_C1(inp, l, o, gates, xT_all, mod):
    wbr = np.ascontiguousarray(inp["w_branch"][l].reshape(4, 2, 128, D).transpose(2, 0, 1, 3).reshape(128, 8, D))
    wout = np.ascontiguousarray(inp["w_out"][l].reshape(8, 128, D).transpose(1, 0, 2))
    maps = []
    for core in range(8):
        b, q = core // 4, core % 4
        tsl = slice(q * NT, (q + 1) * NT)
        maps.append({"oT": np.ascontiguousarray(o[b][:, :, tsl].reshape(4, 2, 128, NT)),
                     "gates": np.ascontiguousarray(gates[b][:, :, tsl]),
                     "xT": np.ascontiguousarray(xT_all[b][:, tsl].reshape(8, 128, NT).transpose(1, 0, 2)),
                     "wbr": wbr, "wout": wout, "ga": np.ascontiguousarray(mod[l, b][:, 16:24])})
    return maps


def collect_x(results, key):
    out = np.zeros((2, D, S), np.float32)
    for core in range(8):
        b, q = core // 4, core % 4
        out[b][:, q * NT:(q + 1) * NT] = results[core][key].transpose(1, 0, 2).reshape(D, NT)
    return out


def run_C1(cC1, inp, l, o, gates, xT_all, mod):
    maps = maps_C1(inp, l, o, gates, xT_all, mod)
    res = run_bass_kernel_spmd(cC1.nc, maps, core_ids=list(range(8)))
    return collect_x(res.results, "x1T")


HT = 1024
NFC = 22


def build_C2(n_exp, moe, c=None):
    own = c is None
    if own:
        c = Ctx()
    ph = c.begin_phase()
    nc, s = c.nc, c.s
    x1_d = c.dram("x1T", [128, 8, NT], F32, "ExternalInput")
    mod_d = c.dram("modF", [128, 24], F32, "ExternalInput")
    gn_d = c.dram("gn", [128, 8], F32, "ExternalInput")
    w1_d = c.dram("w1", [n_exp, NFC, 128, 8, 128], F32, "ExternalInput")
    w3_d = c.dram("w3", [n_exp, NFC, 128, 8, 128], F32, "ExternalInput")
    w2_d = c.dram("w2", [n_exp, 8, 128, NFC, 128], F32, "ExternalInput")
    if moe:
        rw_d = c.dram("rw", [128, 8, 8], F32, "ExternalInput")
        oh_d = c.dram("onehot", [8, 8, 128], F32, "ExternalInput")
        id_d = c.dram("ident", [128, 128], F32, "ExternalInput")
    x2_d = c.dram("x2T", [128, 8, NT], F32, "ExternalOutput")

    acc = c.sb([128, 8, HT], F32)
    hT = c.sb([128, 8, HT], BF16)
    act = c.sb([128, NFC, HT], BF16)
    rs = c.sb([128, HT], F32)
    ones = c.sb([128, 128], BF16)
    sqring = [c.sb([128, TT], BF16) for _ in range(2)]
    epsb = c.sb([128, 1], F32)
    mod = c.sb([128, 24], F32)
    gn = c.sb([128, 8], F32)
    Acol = c.sb([128, 8], F32)
    Bcol = c.sb([128, 8], F32)
    ring = [c.sb([128, TT], F32) for _ in range(4)]
    w1s = [c.sb([128, 8, 128], F32) for _ in range(2)]
    w3s = [c.sb([128, 8, 128], F32) for _ in range(2)]
    w1b = [c.sb([128, 8, 128], BF16) for _ in range(3)]
    w3b = [c.sb([128, 8, 128], BF16) for _ in range(3)]
    w2s = [c.sb([128, NFC, 128], F32) for _ in range(2)]
    w2b = [c.sb([128, NFC, 128], BF16) for _ in range(3)]
    sa_r = [c.sb([128, TT], F32) for _ in range(3)]
    u_r = [c.sb([128, TT], F32) for _ in range(3)]
    pa_r = [c.ps([128, TT]) for _ in range(2)]
    pg_r = [c.ps([128, TT]) for _ in range(2)]
    po_r = [c.ps([128, TT]) for _ in range(2)]
    pbank = c.ps([128, TT])
    pmisc = c.ps([128, TT])
    s.op("pool", lambda: nc.gpsimd.memset(ones[:], 1.0), writes=[ones.res])
    s.op("pool", lambda: nc.gpsimd.memset(epsb[:], EPS), writes=[epsb.res])
    s.dma(mod[:], mod_d, writes=[mod.res])
    s.dma(gn[:], gn_d, writes=[gn.res])
    emit_AB(c, gn, mod, 0, 8, Acol, Bcol)
    if moe:
        rw = c.sb([128, 8, 8], F32)
        oh = c.sb([8, 8, 128], F32)
        ident = c.sb([128, 128], F32)
        GT = c.sb([8, HT], F32)
        gb_r = [c.sb([128, HT], F32) for _ in range(2)]
        h32_r = [c.sb([128, TT], F32) for _ in range(2)]
        lg = c.sb([128, 8, 8], F32)
        m8 = c.sb([128, 8], F32)
        nt1 = c.sb([128, 1], F32)
        e2 = c.sb([128, 1], F32)
        ex = c.sb([128, 8], F32)
        selm = c.sb([128, 8], F32)
        Gt = c.sb([128, 8], F32)
        for t, d in ((rw, rw_d), (oh, oh_d), (ident, id_d)):
            s.dma(t[:], d, writes=[t.res])

    for hf in range(NT // HT):
        hsl = slice(hf * HT, (hf + 1) * HT)
        for j in range(8):
            s.dma(acc[:, j, :], x1_d[:, j, hsl], writes=[acc.res], q=("sp", "act")[j % 2])
        if not moe:
            emit_modnorm(c, acc, hT, HT, ones, Acol, Bcol, epsb, ring, pbank, rs, sq_ring=sqring)
        else:
            def after_h(tmp, j, tt, sl):
                h32 = h32_r[j % 2]
                s.op("act", lambda: nc.scalar.activation(out=h32[:], in_=tmp[:], func=AF.Identity, scale=Acol[:, j:j + 1], bias=Bcol[:, j:j + 1]),
                     reads=[tmp.res, Acol.res, Bcol.res], writes=[h32.res])
                s.op("pool", lambda: nc.gpsimd.tensor_copy(out=hT[:, j, sl], in_=h32[:]), reads=[h32.res], writes=[hT.res])
                for tb in range(4):
                    col = (tt * 4 + tb) * 8
                    s.op("pe", (lambda tb=tb, col=col: nc.tensor.matmul(pmisc[:, col:col + 8], lhsT=h32[:, tb * 128:(tb + 1) * 128], rhs=rw[:, j, :],
                                                                        start=(j == 0 and tb == 0 and tt == 0), stop=(j == 7), skip_group_check=True)),
                         reads=[h32.res, rw.res], writes=[pmisc.res])
            emit_modnorm(c, acc, hT, HT, ones, Acol, Bcol, epsb, ring, pbank, rs, after_h=after_h, sq_ring=sqring)
            s.op("act", lambda: nc.scalar.copy(out=lg[:], in_=pmisc[:, 0:64].rearrange("p (a b) -> p a b", b=8)), reads=[pmisc.res], writes=[lg.res])
            for tb in range(8):
                s.op("dve", (lambda tb=tb: nc.vector.max(out=m8[:], in_=lg[:, tb, :])), reads=[lg.res], writes=[m8.res])
                s.op("dve", lambda: nc.vector.tensor_scalar(out=nt1[:], in0=m8[:, 0:1], scalar1=-1.0, scalar2=None, op0=ALU.mult), reads=[m8.res], writes=[nt1.res])
                s.op("act", (lambda tb=tb: nc.scalar.activation(out=ex[:], in_=lg[:, tb, :], func=AF.Exp, bias=nt1[:], scale=1.0)),
                     reads=[lg.res, nt1.res], writes=[ex.res])
                s.op("act", lambda: nc.scalar.activation(out=e2[:], in_=m8[:, 1:2], func=AF.Exp, bias=nt1[:], scale=1.0), reads=[m8.res, nt1.res], writes=[e2.res])
                s.op("dve", lambda: nc.vector.tensor_scalar(out=e2[:], in0=e2[:], scalar1=1.0, scalar2=None, op0=ALU.add), reads=[e2.res], writes=[e2.res])
                s.op("dve", lambda: nc.vector.reciprocal(out=e2[:], in_=e2[:]), reads=[e2.res], writes=[e2.res])
                s.op("dve", (lambda tb=tb: nc.vector.tensor_scalar(out=selm[:], in0=lg[:, tb, :], scalar1=m8[:, 1:2], scalar2=None, op0=ALU.is_ge)),
                     reads=[lg.res, m8.res], writes=[selm.res])
                s.op("dve", lambda: nc.vector.scalar_tensor_tensor(out=Gt[:], in0=ex[:], scalar=e2[:, 0:1], in1=selm[:], op0=ALU.mult, op1=ALU.mult),
                     reads=[ex.res, e2.res, selm.res], writes=[Gt.res])
                s.op("pe", (lambda tb=tb: nc.tensor.transpose(pbank[0:8, (tb % 4) * 128:(tb % 4 + 1) * 128], Gt[:], ident[:])), reads=[Gt.res, ident.res], writes=[pbank.res])
                if tb % 4 == 3:
                    q4 = tb // 4
                    s.op("act", (lambda q4=q4: nc.scalar.copy(out=GT[:, q4 * 512:(q4 + 1) * 512], in_=pbank[0:8, 0:512])), reads=[pbank.res], writes=[GT.res])

        items = []
        kf = kg = 0
        for e in range(n_exp):
            for fc in range(NFC):
                for tt in range(2):
                    items.append(("f", e, fc, tt, kf))
                kf += 1
            for ec in range(8):
                for tt in range(2):
                    items.append(("g", e, ec, tt, kg))
                kg += 1

        def s_load(n):
            kind, e, ci, tt, k = items[n]
            if tt != 0:
                return
            if kind == "f":
                if moe and ci == 0:
                    g_b = gb_r[e % 2]
                    for t2 in range(2):
                        s.op("pe", (lambda t2=t2, e=e: nc.tensor.matmul(pmisc[:], lhsT=oh[:, e, :], rhs=GT[:, t2 * TT:(t2 + 1) * TT], start=True, stop=True)),
                             reads=[oh.res, GT.res], writes=[pmisc.res])
                        s.op("act", (lambda t2=t2, g_b=g_b: nc.scalar.copy(out=g_b[:, t2 * TT:(t2 + 1) * TT], in_=pmisc[:])), reads=[pmisc.res], writes=[g_b.res])
                s.dma(w1s[k % 2][:], w1_d[e, ci], writes=[w1s[k % 2].res], q="act")
                s.dma(w3s[k % 2][:], w3_d[e, ci], writes=[w3s[k % 2].res], q="act")
            else:
                s.dma(w2s[k % 2][:], w2_d[e, ci], writes=[w2s[k % 2].res], q="act")

        def s_cast(n):
            kind, e, ci, tt, k = items[n]
            if tt != 0:
                return
            if kind == "f":
                s.op("pool", lambda: nc.gpsimd.tensor_copy(out=w1b[k % 3][:], in_=w1s[k % 2][:]), reads=[w1s[k % 2].res], writes=[w1b[k % 3].res])
                s.op("dve", lambda: nc.vector.tensor_copy(out=w3b[k % 3][:], in_=w3s[k % 2][:]), reads=[w3s[k % 2].res], writes=[w3b[k % 3].res])
            else:
                half = NFC // 2
                s.op("pool", lambda: nc.gpsimd.tensor_copy(out=w2b[k % 3][:, 0:half, :], in_=w2s[k % 2][:, 0:half, :]), reads=[w2s[k % 2].res], writes=[w2b[k % 3].res])
                s.op("dve", lambda: nc.vector.tensor_copy(out=w2b[k % 3][:, half:NFC, :], in_=w2s[k % 2][:, half:NFC, :]), reads=[w2s[k % 2].res], writes=[w2b[k % 3].res])

        def s_mm(n):
            kind, e, ci, tt, k = items[n]
            sl = slice(tt * TT, (tt + 1) * TT)
            if kind == "f":
                pa, pg = pa_r[n % 2], pg_r[n % 2]
                for j in range(8):
                    s.op("pe", (lambda j=j: nc.tensor.matmul(pa[:], lhsT=w1b[k % 3][:, j, :], rhs=hT[:, j, sl], start=(j == 0), stop=(j == 7))),
                         reads=[w1b[k % 3].res, hT.res], writes=[pa.res])
                for j in range(8):
                    s.op("pe", (lambda j=j: nc.tensor.matmul(pg[:], lhsT=w3b[k % 3][:, j, :], rhs=hT[:, j, sl], start=(j == 0), stop=(j == 7))),
                         reads=[w3b[k % 3].res, hT.res], writes=[pg.res])
            else:
                po = po_r[n % 2]
                for fc in range(NFC):
                    s.op("pe", (lambda fc=fc: nc.tensor.matmul(po[:], lhsT=w2b[k % 3][:, fc, :], rhs=act[:, fc, sl], start=(fc == 0), stop=(fc == NFC - 1))),
                         reads=[w2b[k % 3].res, act.res], writes=[po.res])

        def s_post(n):
            kind, e, ci, tt, k = items[n]
            sl = slice(tt * TT, (tt + 1) * TT)
            if kind == "f":
                pa, pg = pa_r[n % 2], pg_r[n % 2]
                sa = sa_r[n % 3]
                s.op("act", lambda: nc.scalar.activation(out=sa[:], in_=pa[:], func=AF.Silu), reads=[pa.res], writes=[sa.res])
                if not moe:
                    s.op("dve", lambda: nc.vector.tensor_tensor(out=act[:, ci, sl], in0=sa[:], in1=pg[:], op=ALU.mult), reads=[sa.res, pg.res], writes=[act.res])
                else:
                    u = u_r[n % 3]
                    g_b = gb_r[e % 2]
                    s.op("dve", lambda: nc.vector.tensor_tensor(out=u[:], in0=pg[:], in1=g_b[:, sl], op=ALU.mult), reads=[pg.res, g_b.res], writes=[u.res])
                    s.op("pool", lambda: nc.gpsimd.tensor_tensor(out=act[:, ci, sl], in0=sa[:], in1=u[:], op=ALU.mult), reads=[sa.res, u.res], writes=[act.res])
            else:
                po = po_r[n % 2]
                s.op("dve", lambda: nc.vector.scalar_tensor_tensor(out=acc[:, ci, sl], in0=po[:], scalar=mod[:, 16 + ci:17 + ci], in1=acc[:, ci, sl],
                                                                   op0=ALU.mult, op1=ALU.add), reads=[po.res, mod.res, acc.res], writes=[acc.res])
        pipeline(len(items), [s_load, s_cast, s_mm, s_post])
        for j in range(8):
            s.dma(x2_d[:, j, hsl], acc[:, j, :], reads=[acc.res], is_output=True)
    c.end_phase(ph)
    if own:
        c.close()
    return c


def maps_C2(inp, l, x1T_all, mod, moe):
    if moe:
        w1, w3, w2 = inp["moe_w1"][l // 2], inp["moe_w3"][l // 2], inp["moe_w2"][l // 2]
    else:
        w1, w3, w2 = inp["ffn_w1"][l // 2][None], inp["ffn_w3"][l // 2][None], inp["ffn_w2"][l // 2][None]
    E = w1.shape[0]
    lay1 = lambda w: np.ascontiguousarray(w.reshape(E, 8, 128, NFC, 128).transpose(0, 3, 2, 1, 4))
    lay2 = lambda w: np.ascontiguousarray(w.reshape(E, NFC, 128, 8, 128).transpose(0, 3, 2, 1, 4))
    w1l, w3l, w2l = lay1(w1), lay1(w3), lay2(w2)
    gn = np.ascontiguousarray(inp["norm_ffn"][l].reshape(8, 128).T)
    maps = []
    for core in range(8):
        b, q = core // 4, core % 4
        m = {"modF": np.ascontiguousarray(mod[l, b][:, 24:48]), "gn": gn, "w1": w1l, "w3": w3l, "w2": w2l}
        if x1T_all is not None:
            m["x1T"] = np.ascontiguousarray(x1T_all[b][:, q * NT:(q + 1) * NT].reshape(8, 128, NT).transpose(1, 0, 2))
        if moe:
            m["rw"] = np.ascontiguousarray(inp["router_w"][l // 2].reshape(8, 128, 8).transpose(1, 0, 2))
            oh = np.zeros((8, 8, 128), np.float32)
            for e in range(8):
                oh[e, e, :] = 1.0
            m["onehot"] = oh
            m["ident"] = np.eye(128, dtype=np.float32)
        maps.append(m)
    return maps


def run_C2(cC2, inp, l, x1T_all, mod, moe):
    maps = maps_C2(inp, l, x1T_all, mod, moe)
    res = run_bass_kernel_spmd(cC2.nc, maps, core_ids=list(range(8)))
    return collect_x(res.results, "x2T")


def build_CA(moe, with_A):
    c = Ctx()
    c.alias = {}
    c.pre = "c1_"
    c.kind_override = {"c1_x1T": "Internal"}
    build_C1(c)
    c.pre = "c2_"
    c.alias["c2_x1T"] = c.made["c1_x1T"]
    build_C2(8 if moe else 1, moe, c)
    if with_A:
        c.pre = "a_"
        c.alias["a_xT"] = c.made["c2_x2T"]
        build_A(c)
    c.close()
    return c


def run_CA(cCA, inp, l, o, gates, xT_all, mod, moe, with_A):
    m1 = maps_C1(inp, l, o, gates, xT_all, mod)
    m2 = maps_C2(inp, l, None, mod, moe)
    m3 = maps_A(inp, l + 1, None, mod) if with_A else [dict() for _ in range(8)]
    maps = []
    for i in range(8):
        m = {"c1_" + k: v for k, v in m1[i].items()}
        m.update({"c2_" + k: v for k, v in m2[i].items()})
        m.update({"a_" + k: v for k, v in m3[i].items()})
        maps.append(m)
    res = run_bass_kernel_spmd(cCA.nc, maps, core_ids=list(range(8)))
    x2T = collect_x(res.results, "c2_x2T")
    nxt = collect_A(res.results, "a_") if with_A else None
    return x2T, nxt


_PROGS = {}


def _prog(name, fn):
    if name not in _PROGS:
        _PROGS[name] = fn()
    return _PROGS[name]


def kernel(**inp):
    inp = {k: np.asarray(v) for k, v in inp.items()}
    mod = run_M(inp)
    xT_all = np.ascontiguousarray(inp["x"].astype(np.float32).transpose(0, 2, 1))
    cA = _prog("A", build_A)
    proj, gates, misc = run_A(cA, inp, 0, xT_all, mod)
    cB = _prog("B", build_B)
    for l in range(2):
        o = run_B(cB, inp, l, proj, misc)
        moe = (l % 2 == 1)
        with_A = (l == 0)
        cCA = _prog("CA%d" % l, lambda: build_CA(moe, with_A))
        xT_all, nxt = run_CA(cCA, inp, l, o, gates, xT_all, mod, moe, with_A)
        if with_A:
            proj, gates, misc = nxt
    return np.ascontiguousarray(xT_all.transpose(0, 2, 1)).astype(np.float32)
```

```python
import contextlib
import numpy as np
import concourse.bass as bass
import concourse.mybir as mybir
from concourse.bass_utils import run_bass_kernel_spmd

F32 = mybir.dt.float32
BF16 = mybir.dt.bfloat16
I32 = mybir.dt.int32
AF = mybir.ActivationFunctionType
ALU = mybir.AluOpType
AX = mybir.AxisListType


class Res:
    __slots__ = ("w", "r")

    def __init__(self):
        self.w = None
        self.r = {}


class Sched:
    NDMA = 24

    def __init__(self, nc, es):
        self.nc = nc
        self.engs = {"pe": nc.tensor, "act": nc.scalar, "dve": nc.vector,
                     "pool": nc.gpsimd, "sp": nc.sync}
        self.sem = {}
        self.cnt = {}
        for k in self.engs:
            self.sem[k] = es.enter_context(nc.semaphore("s_" + k))
            self.cnt[k] = 0
        for i in range(self.NDMA):
            k = "d%d" % i
            self.sem[k] = es.enter_context(nc.semaphore("s_" + k))
            self.cnt[k] = 0
        self.seen = {k: {} for k in self.engs}
        self.dma_rr = 0
        self.out_events = []

    def _wait(self, e, ev):
        if ev is None:
            return
        key, val = ev
        if key == e and e == "pe":
            return
        if self.seen[e].get(key, 0) >= val:
            return
        self.engs[e].wait_ge(self.sem[key], val)
        self.seen[e][key] = val

    def _deps(self, e, reads, writes):
        for r in reads:
            self._wait(e, r.w)
        for r in writes:
            self._wait(e, r.w)
            for k, v in r.r.items():
                self._wait(e, (k, v))

    def op(self, e, fn, reads=(), writes=()):
        self._deps(e, reads, writes)
        ins = fn()
        self.cnt[e] += 1
        ins.then_inc(self.sem[e], 1)
        ev = (e, self.cnt[e])
        for r in reads:
            r.r[e] = ev[1]
        for r in writes:
            r.w = ev
            r.r = {}
        return ev

    def dma(self, out, in_, reads=(), writes=(), q="sp", is_output=False, **kw):
        k = "d%d" % self.dma_rr
        self.dma_rr = (self.dma_rr + 1) % self.NDMA
        self._wait(q, (k, self.cnt[k]))
        self._deps(q, reads, writes)
        ins = self.engs[q].dma_start(out=out, in_=in_, **kw)
        self.cnt[k] += 16
        ins.then_inc(self.sem[k], 16)
        ev = (k, self.cnt[k])
        for r in reads:
            r.r[k] = ev[1]
        for r in writes:
            r.w = ev
            r.r = {}
        if is_output:
            self.out_events.append(ev)
        return ev

    def finish(self):
        for i in range(self.NDMA):
            k = "d%d" % i
            self._wait("sp", (k, self.cnt[k]))
        for k in ("pe", "act", "dve", "pool"):
            self._wait("sp", (k, self.cnt[k]))


class Tile:
    def __init__(self, t):
        self.t = t
        self.res = Res()

    def __getitem__(self, idx):
        return self.t[idx]


class Ctx:
    def __init__(self, name="k"):
        self.nc = bass.Bass("TRN2", target_bir_lowering=False)
        self.es = contextlib.ExitStack()
        self.s = Sched(self.nc, self.es)
        self.n = 0

    def sb(self, shape, dt, name=None):
        self.n += 1
        return Tile(self.es.enter_context(self.nc.sbuf_tensor(name or ("t%d" % self.n), list(shape), dt)))

    def ps(self, shape, dt=F32, name=None):
        self.n += 1
        return Tile(self.es.enter_context(self.nc.psum_tensor(name or ("p%d" % self.n), list(shape), dt)))

    pre = ""
    alias = None
    kind_override = None

    def dram(self, name, shape, dt, kind):
        full = self.pre + name
        if self.alias and full in self.alias:
            return self.alias[full]
        if self.kind_override and full in self.kind_override:
            kind = self.kind_override[full]
        ap = self.nc.dram_tensor(full, list(shape), dt, kind=kind).ap()
        if self.alias is None:
            self.alias = {}
        self.made = getattr(self, "made", {})
        self.made[full] = ap
        return ap

    def begin_phase(self):
        es = contextlib.ExitStack()
        old, self.es = self.es, es
        return (old, es)

    def end_phase(self, ph):
        barrier(self)
        self.es = ph[0]
        ph[1].close()

    def close(self):
        self.s.finish()
        self.es.close()


D = 1024
S = 8192
NB = 2
NT = 2048
TT = 512
NCH_IN = 57
A_MAXCH = NCH_IN
EPS = 1e-6
TWO_PI = float(2 * np.pi)
C1_2PI = 6.28125
C2_2PI = TWO_PI - C1_2PI


def barrier(c):
    s = c.s
    for e in ("pe", "act", "dve", "pool", "sp"):
        for k in list(s.cnt.keys()):
            if k != e:
                s._wait(e, (k, s.cnt[k]))


def build_M():
    c = Ctx()
    nc, s = c.nc, c.s
    cT_d = c.dram("cT", [128, 8, 2], F32, "ExternalInput")
    w_d = c.dram("w", [12, 128, 8, 128], F32, "ExternalInput")
    b_d = c.dram("b", [128, 12], F32, "ExternalInput")
    o_d = c.dram("modT", [128, 12, 2], F32, "ExternalOutput")
    cT = c.sb([128, 8, 2], F32)
    ca = c.sb([128, 8, 2], F32)
    bt = c.sb([128, 12], F32)
    ot = c.sb([128, 12, 2], F32)
    s.dma(cT[:], cT_d, writes=[cT.res])
    s.dma(bt[:], b_d, writes=[bt.res])
    s.op("act", lambda: nc.scalar.activation(out=ca[:], in_=cT[:], func=AF.Silu), reads=[cT.res], writes=[ca.res])
    wts = [c.sb([128, 8, 128], F32) for _ in range(3)]
    pm = c.ps([128, 12, 2])
    for j in range(12):
        wt = wts[j % 3]
        s.dma(wt[:], w_d[j], writes=[wt.res])
        for k in range(8):
            s.op("pe", (lambda wt=wt, k=k, j=j: nc.tensor.matmul(pm[:, j, :], lhsT=wt[:, k, :], rhs=ca[:, k, :],
                                                                 start=(k == 0), stop=(k == 7))),
                 reads=[wt.res, ca.res], writes=[pm.res])
    for b in range(2):
        s.op("dve", (lambda b=b: nc.vector.tensor_tensor(out=ot[:, :, b], in0=pm[:, :, b], in1=bt[:], op=ALU.add)),
             reads=[pm.res, bt.res], writes=[ot.res])
    s.dma(o_d, ot[:], reads=[ot.res], is_output=True)
    c.close()
    return c


def run_M(inp):
    c = build_M()
    cT = np.ascontiguousarray(inp["c"].T.reshape(8, 128, 2).transpose(1, 0, 2))
    w_all = inp["w_ada"]
    b_all = inp["b_ada"]
    maps = []
    for i in range(8):
        chunks = [(g // 48, g % 48) for g in range(i * 12, i * 12 + 12)]
        w = np.stack([w_all[l][:, n * 128:(n + 1) * 128].reshape(8, 128, 128).transpose(1, 0, 2) for l, n in chunks])
        b = np.stack([b_all[l][n * 128:(n + 1) * 128] for l, n in chunks], axis=1)
        maps.append({"cT": cT, "w": np.ascontiguousarray(w), "b": np.ascontiguousarray(b)})
    res = run_bass_kernel_spmd(c.nc, maps, core_ids=list(range(8)))
    mod = np.zeros((2, 2, 128, 48), np.float32)
    for i in range(8):
        o = res.results[i]["modT"]
        for jj, g in enumerate(range(i * 12, i * 12 + 12)):
            mod[g // 48, :, :, g % 48] = o[:, jj, :].T
    return mod


def emit_modnorm(c, src, hT, ntok, ones, Acol, Bcol, epsb, tmp_ring, pbank, rs, after_h=None, sq_ring=None):
    nc, s = c.nc, c.s
    ntt = ntok // TT
    if sq_ring is None:
        sq_ring = tmp_ring
    for tt in range(ntt):
        sl = slice(tt * TT, (tt + 1) * TT)
        for j in range(8):
            sq = sq_ring[j % len(sq_ring)]
            s.op("act", (lambda sq=sq, j=j: nc.scalar.activation(out=sq[:], in_=src[:, j, sl], func=AF.Square)),
                 reads=[src.res], writes=[sq.res])
            s.op("pe", (lambda sq=sq, j=j: nc.tensor.matmul(pbank[:], lhsT=ones[:], rhs=sq[:], start=(j == 0), stop=(j == 7))),
                 reads=[sq.res, ones.res], writes=[pbank.res])
        sd = tmp_ring[0]
        s.op("act", (lambda sd=sd: nc.scalar.activation(out=sd[:], in_=pbank[:], func=AF.Sqrt, scale=1.0 / D, bias=epsb[:])),
             reads=[pbank.res, epsb.res], writes=[sd.res])
        s.op("dve", (lambda sd=sd: nc.vector.reciprocal(out=rs[:, sl], in_=sd[:])), reads=[sd.res], writes=[rs.res])
        for j in range(8):
            tmp = tmp_ring[1 + (j % (len(tmp_ring) - 1))]
            s.op("dve", (lambda tmp=tmp, j=j: nc.vector.tensor_tensor(out=tmp[:], in0=src[:, j, sl], in1=rs[:, sl], op=ALU.mult)),
                 reads=[src.res, rs.res], writes=[tmp.res])
            if after_h is None:
                s.op("act", (lambda tmp=tmp, j=j: nc.scalar.activation(out=hT[:, j, sl], in_=tmp[:], func=AF.Identity,
                                                                      scale=Acol[:, j:j + 1], bias=Bcol[:, j:j + 1])),
                     reads=[tmp.res, Acol.res, Bcol.res], writes=[hT.res])
            else:
                after_h(tmp, j, tt, sl)


def emit_AB(c, gn, mod, sh_off, sc_off, Acol, Bcol):
    nc, s = c.nc, c.s
    s.op("dve", lambda: nc.vector.scalar_tensor_tensor(out=Acol[:], in0=mod[:, sc_off:sc_off + 8], scalar=1.0, in1=gn[:],
                                                       op0=ALU.add, op1=ALU.mult),
         reads=[mod.res, gn.res], writes=[Acol.res])
    s.op("dve", lambda: nc.vector.tensor_copy(out=Bcol[:], in_=mod[:, sh_off:sh_off + 8]), reads=[mod.res], writes=[Bcol.res])


ROPE_CH = (0, 1, 2, 4, 5, 6, 7)
NORM_CH = (3, 8, 9, 10, 11)
RAW_CH = tuple(range(12, 24))
MISC_CH = 24


def build_A(c=None):
    own = c is None
    if own:
        c = Ctx()
    ph = c.begin_phase()
    nc, s = c.nc, c.s
    xT_d = c.dram("xT", [128, 8, NT], F32, "ExternalInput")
    mod_d = c.dram("modA", [128, 16], F32, "ExternalInput")
    gn_d = c.dram("gn", [128, 8], F32, "ExternalInput")
    pos_d = c.dram("pos", [1, NT], I32, "ExternalInput")
    invf_d = c.dram("invf", [128, 1], F32, "ExternalInput")
    w_d = c.dram("w", [NCH_IN, 128, 8, 128], F32, "ExternalInput")
    gain_d = c.dram("gain", [128, 12], F32, "ExternalInput")
    osc_d = c.dram("osc", [128, 12], F32, "ExternalInput")
    foxb_d = c.dram("foxb", [128, 1], F32, "ExternalInput")
    pm_d = c.dram("pm", [128, 128], F32, "ExternalInput")
    bones_d = c.dram("bones", [128, 128], F32, "ExternalInput")
    proj_d = c.dram("proj", [26, 128, NT], BF16, "ExternalOutput")
    gates_d = c.dram("gates", [32, 128, NT], F32, "ExternalOutput")
    misc_d = c.dram("misc", [64, NT], F32, "ExternalOutput")

    hT = c.sb([128, 8, NT], BF16)
    rs = c.sb([128, NT], F32)
    COS = c.sb([128, NT], F32)
    SIN = c.sb([128, NT], F32)
    ones = c.sb([128, 128], BF16)
    bones_f = c.sb([128, 128], F32)
    pm_f = c.sb([128, 128], F32)
    bones = c.sb([128, 128], BF16)
    pm = c.sb([128, 128], BF16)
    gain = c.sb([128, 12], F32)
    osc = c.sb([128, 12], F32)
    foxb = c.sb([128, 1], F32)
    epsb = c.sb([128, 1], F32)
    negpi = c.sb([128, 1], F32)
    invf = c.sb([128, 1], F32)
    mod = c.sb([128, 16], F32)
    gn = c.sb([128, 8], F32)
    Acol = c.sb([128, 8], F32)
    Bcol = c.sb([128, 8], F32)
    pbank = c.ps([128, TT])

    s.op("pool", lambda: nc.gpsimd.memset(ones[:], 1.0), writes=[ones.res])
    s.op("pool", lambda: nc.gpsimd.memset(epsb[:], EPS), writes=[epsb.res])
    s.op("pool", lambda: nc.gpsimd.memset(negpi[:], -float(np.pi)), writes=[negpi.res])
    for t, d in ((bones_f, bones_d), (pm_f, pm_d), (gain, gain_d), (osc, osc_d), (foxb, foxb_d), (invf, invf_d), (mod, mod_d), (gn, gn_d)):
        s.dma(t[:], d, writes=[t.res])
    s.op("pool", lambda: nc.gpsimd.tensor_copy(out=bones[:], in_=bones_f[:]), reads=[bones_f.res], writes=[bones.res])
    s.op("pool", lambda: nc.gpsimd.tensor_copy(out=pm[:], in_=pm_f[:]), reads=[pm_f.res], writes=[pm.res])
    s.op("dve", lambda: nc.vector.tensor_tensor(out=gain[:], in0=gain[:], in1=osc[:], op=ALU.mult), reads=[gain.res, osc.res], writes=[gain.res])
    emit_AB(c, gn, mod, 0, 8, Acol, Bcol)

    with contextlib.ExitStack() as es1:
        old_es, c.es = c.es, es1
        xT = c.sb([128, 8, NT], F32)
        for j in range(8):
            s.dma(xT[:, j, :], xT_d[:, j, :], writes=[xT.res], q=("sp", "act")[j % 2])
        posi = c.sb([128, NT], I32)
        ang = c.sb([128, NT], F32)
        tq = c.sb([128, NT], F32)
        ki = c.sb([128, NT], I32)
        kf = c.sb([128, NT], F32)
        s.dma(posi[:], pos_d[0, :].partition_broadcast(128), writes=[posi.res])
        s.op("dve", lambda: nc.vector.tensor_copy(out=tq[:], in_=posi[:]), reads=[posi.res], writes=[tq.res])
        s.op("dve", lambda: nc.vector.tensor_scalar(out=ang[:], in0=tq[:], scalar1=invf[:, 0:1], scalar2=None, op0=ALU.mult),
             reads=[tq.res, invf.res], writes=[ang.res])
        for dst, shift in ((SIN, 0.0), (COS, 0.25)):
            s.op("dve", lambda shift=shift: nc.vector.tensor_scalar(out=tq[:], in0=ang[:], scalar1=1.0 / TWO_PI, scalar2=0.5 + shift,
                                                                    op0=ALU.mult, op1=ALU.add), reads=[ang.res], writes=[tq.res])
            s.op("dve", lambda: nc.vector.tensor_copy(out=ki[:], in_=tq[:]), reads=[tq.res], writes=[ki.res])
            s.op("dve", lambda: nc.vector.tensor_copy(out=kf[:], in_=ki[:]), reads=[ki.res], writes=[kf.res])
            s.op("dve", lambda shift=shift: nc.vector.tensor_scalar(out=tq[:], in0=ang[:], scalar1=float(np.pi) + shift * TWO_PI, scalar2=None,
                                                                    op0=ALU.add), reads=[ang.res], writes=[tq.res])
            s.op("dve", lambda: nc.vector.scalar_tensor_tensor(out=tq[:], in0=kf[:], scalar=-C1_2PI, in1=tq[:], op0=ALU.mult, op1=ALU.add),
                 reads=[kf.res, tq.res], writes=[tq.res])
            s.op("dve", lambda: nc.vector.scalar_tensor_tensor(out=tq[:], in0=kf[:], scalar=-C2_2PI, in1=tq[:], op0=ALU.mult, op1=ALU.add),
                 reads=[kf.res, tq.res], writes=[tq.res])
            s.op("dve", lambda: nc.vector.tensor_scalar(out=kf[:], in0=tq[:], scalar1=0.0, scalar2=TWO_PI, op0=ALU.is_lt, op1=ALU.mult),
                 reads=[tq.res], writes=[kf.res])
            s.op("dve", lambda: nc.vector.tensor_tensor(out=tq[:], in0=tq[:], in1=kf[:], op=ALU.add), reads=[tq.res, kf.res], writes=[tq.res])
            s.op("dve", lambda: nc.vector.tensor_scalar(out=tq[:], in0=tq[:], scalar1=0.0, scalar2=TWO_PI, op0=ALU.max, op1=ALU.min),
                 reads=[tq.res], writes=[tq.res])
            s.op("act", lambda dst=dst: nc.scalar.activation(out=dst[:], in_=tq[:], func=AF.Sin, bias=negpi[:], scale=1.0),
                 reads=[tq.res, negpi.res], writes=[dst.res])
        ring = [c.sb([128, TT], F32) for _ in range(4)]
        sqring = [c.sb([128, TT], BF16) for _ in range(4)]
        emit_modnorm(c, xT, hT, NT, ones, Acol, Bcol, epsb, ring, pbank, rs, sq_ring=sqring)
        barrier(c)
        c.es = old_es

    R = 4
    wst = [c.sb([128, 8, 128], F32) for _ in range(3)]
    wbf = [c.sb([128, 8, 128], BF16) for _ in range(3)]
    pp = [c.ps([128, TT]) for _ in range(3)]
    psq = [c.ps([128, TT]) for _ in range(2)]
    pq = [pbank, c.ps([128, TT])]
    sq_r = [c.sb([128, TT], BF16) for _ in range(R)]
    rstd_r = [c.sb([128, TT], F32) for _ in range(R)]
    y_r = [c.sb([128, TT], F32) for _ in range(R)]
    y2_r = [c.sb([128, TT], BF16) for _ in range(R)]
    t1_r = [c.sb([128, TT], F32) for _ in range(R)]
    ob_r = [c.sb([128, TT], BF16) for _ in range(R)]
    ob2_r = [c.sb([128, TT], BF16) for _ in range(R)]
    of_r = [c.sb([128, TT], F32) for _ in range(R)]

    items = [(ch, tt) for ch in range(A_MAXCH) for tt in range(NT // TT)]

    def kind(ch):
        if ch in ROPE_CH:
            return "rope"
        if ch in NORM_CH:
            return "norm"
        if ch in RAW_CH:
            return "raw"
        if ch == MISC_CH:
            return "misc"
        return "sig"

    def stl(n):
        ch, tt = items[n]
        if tt == 0:
            ws = wst[ch % 3]
            s.dma(ws[:], w_d[ch], writes=[ws.res], q="act")

    def stc(n):
        ch, tt = items[n]
        if tt == 0:
            ws, wb = wst[ch % 3], wbf[ch % 3]
            s.op("pool", lambda: nc.gpsimd.tensor_copy(out=wb[:], in_=ws[:]), reads=[ws.res], writes=[wb.res])

    def st0(n):
        ch, tt = items[n]
        wb = wbf[ch % 3]
        p = pp[n % 3]
        for j in range(8):
            s.op("pe", (lambda j=j: nc.tensor.matmul(p[:], lhsT=wb[:, j, :], rhs=hT[:, j, tt * TT:(tt + 1) * TT],
                                                     start=(j == 0), stop=(j == 7))),
                 reads=[wb.res, hT.res], writes=[p.res])

    def st1(n):
        ch, tt = items[n]
        k = kind(ch)
        p = pp[n % 3]
        sl = slice(tt * TT, (tt + 1) * TT)
        if k in ("rope", "norm"):
            sq = sq_r[n % R]
            s.op("act", lambda: nc.scalar.activation(out=sq[:], in_=p[:], func=AF.Square), reads=[p.res], writes=[sq.res])
            s.op("pe", lambda: nc.tensor.matmul(psq[n % 2][:], lhsT=bones[:], rhs=sq[:], start=True, stop=True),
                 reads=[bones.res, sq.res], writes=[psq[n % 2].res])
        elif k == "raw":
            ob = ob_r[n % R]
            sc = 0.125 if ch in (16, 17) else 1.0
            s.op("act", lambda: nc.scalar.activation(out=ob[:], in_=p[:], func=AF.Copy, scale=sc), reads=[p.res], writes=[ob.res])
            s.dma(proj_d[ch, :, sl], ob[:], reads=[ob.res], is_output=True)
        elif k == "sig":
            of = of_r[n % R]
            s.op("act", lambda: nc.scalar.activation(out=of[:], in_=p[:], func=AF.Sigmoid), reads=[p.res], writes=[of.res])
            s.dma(gates_d[ch - 25, :, sl], of[:], reads=[of.res], is_output=True, q=("sp", "act")[n % 2])
        else:
            of = of_r[n % R]
            s.op("act", lambda: nc.scalar.activation(out=of[0:64, :], in_=p[0:64, :], func=AF.Sigmoid, bias=foxb[0:64, :], scale=1.0),
                 reads=[p.res, foxb.res], writes=[of.res])
            s.op("act", lambda: nc.scalar.activation(out=of[32:64, :], in_=of[32:64, :], func=AF.Ln), reads=[of.res], writes=[of.res])
            s.dma(misc_d[:, sl], of[0:64, :], reads=[of.res], is_output=True)

    def st2(n):
        ch, tt = items[n]
        k = kind(ch)
        if k not in ("rope", "norm"):
            return
        p = pp[n % 3]
        sl = slice(tt * TT, (tt + 1) * TT)
        rstd = rstd_r[n % R]
        y = y_r[n % R]
        s.op("act", lambda: nc.scalar.activation(out=rstd[:], in_=psq[n % 2][:], func=AF.Sqrt, scale=1.0 / 64, bias=epsb[:]),
             reads=[psq[n % 2].res, epsb.res], writes=[rstd.res])
        s.op("dve", lambda: nc.vector.reciprocal(out=rstd[:], in_=rstd[:]), reads=[rstd.res], writes=[rstd.res])
        s.op("dve", lambda: nc.vector.tensor_tensor(out=y[:], in0=p[:], in1=rstd[:], op=ALU.mult), reads=[p.res, rstd.res], writes=[y.res])
        if k == "norm":
            ob = ob_r[n % R]
            s.op("act", lambda: nc.scalar.activation(out=ob[:], in_=y[:], func=AF.Copy, scale=gain[:, ch:ch + 1]),
                 reads=[y.res, gain.res], writes=[ob.res])
            s.dma(proj_d[ch, :, sl], ob[:], reads=[ob.res], is_output=True)
        else:
            y2 = y2_r[n % R]
            s.op("act", lambda: nc.scalar.activation(out=y2[:], in_=y[:], func=AF.Copy, scale=gain[:, ch:ch + 1]),
                 reads=[y.res, gain.res], writes=[y2.res])
            s.op("pe", lambda: nc.tensor.matmul(pq[n % 2][:], lhsT=pm[:], rhs=y2[:], start=True, stop=True),
                 reads=[pm.res, y2.res], writes=[pq[n % 2].res])
            if ch in (0, 1):
                s.dma(proj_d[24 + ch, :, sl], y2[:], reads=[y2.res], is_output=True)

    def st3(n):
        ch, tt = items[n]
        if kind(ch) != "rope":
            return
        sl = slice(tt * TT, (tt + 1) * TT)
        y2 = y2_r[n % R]
        t1 = t1_r[n % R]
        y = y_r[n % R]
        ob = ob_r[n % R]
        s.op("pool", lambda: nc.gpsimd.tensor_tensor(out=t1[:], in0=y2[:], in1=COS[:, sl], op=ALU.mult), reads=[y2.res, COS.res], writes=[t1.res])
        s.op("dve", lambda: nc.vector.tensor_tensor(out=y[:], in0=pq[n % 2][:], in1=SIN[:, sl], op=ALU.mult),
             reads=[pq[n % 2].res, SIN.res], writes=[y.res])
        s.op("dve", lambda: nc.vector.tensor_tensor(out=ob[:], in0=t1[:], in1=y[:], op=ALU.add), reads=[t1.res, y.res], writes=[ob.res])
        s.dma(proj_d[ch, :, sl], ob[:], reads=[ob.res], is_output=True)

    pipeline(len(items), [stl, stc, st0, st1, st2, st3])
    c.end_phase(ph)
    if own:
        c.close()
    return c


def in_perm():
    Z = [-1] * 64
    r = lambda a, n: list(range(a, a + n))
    ch = []
    ch.append(r(0, 128)); ch.append(r(128, 128))
    ch.append(r(384, 64) + r(512, 64))
    ch.append(r(256, 64) + Z)
    ch.append(r(652, 128)); ch.append(r(780, 128))
    ch.append(r(908, 128)); ch.append(r(1036, 128))
    ch.append(r(2188, 128)); ch.append(r(2316, 128))
    ch.append(r(2444, 128)); ch.append(r(2572, 128))
    ch.append(r(320, 64) + r(448, 64))
    ch.append(r(576, 64) + Z)
    ch.append(r(1164, 128)); ch.append(r(1292, 128))
    ch.append(r(1420, 128)); ch.append(r(1548, 128))
    ch.append(r(1676, 128)); ch.append(r(1804, 128))
    ch.append(r(1932, 128)); ch.append(r(2060, 128))
    ch.append(r(2700, 128)); ch.append(r(2828, 128))
    ch.append(r(640, 12) + [-1] * 20 + r(2956, 4) + [-1] * 92)
    for g in range(32):
        ch.append(r(2960 + g * 128, 128))
    return np.array(ch, np.int64)


def consts_A():
    inv = (500000.0 ** (-np.arange(0, 16, 2, dtype=np.float32) / 16)).astype(np.float32)
    invf = np.zeros((128, 1), np.float32)
    pm = np.zeros((128, 128), np.float32)
    bones = np.zeros((128, 128), np.float32)
    for hb in (0, 64):
        bones[hb:hb + 64, hb:hb + 64] = 1.0
        for i in range(8):
            invf[hb + i, 0] = inv[i]
            invf[hb + 8 + i, 0] = inv[i]
            pm[hb + i + 8, hb + i] = -1.0
            pm[hb + i, hb + i + 8] = 1.0
    osc = np.ones((128, 12), np.float32)
    for chn in (0, 1, 4, 5, 8, 9):
        osc[:, chn] = 0.125
    return invf, pm, bones, osc


def maps_A(inp, l, xT_all, mod):
    perm = in_perm()
    w = inp["w_in"][l]
    wz = np.concatenate([w, np.zeros((D, 1), np.float32)], axis=1)
    wp = wz[:, perm.reshape(-1)].reshape(D, NCH_IN, 128)
    wp = np.ascontiguousarray(wp.reshape(8, 128, NCH_IN, 128).transpose(2, 1, 0, 3))
    invf, pm, bones, osc = consts_A()
    g = inp["qk_gain"][l]
    t2 = lambda a: np.concatenate([a, a])
    z64 = np.zeros(64, np.float32)
    gain = np.stack([t2(g[0]), t2(g[0]), np.concatenate([g[2], g[3]]), np.concatenate([g[1], z64]),
                     t2(g[4]), t2(g[4]), t2(g[5]), t2(g[5]), t2(g[6]), t2(g[6]), t2(g[7]), t2(g[7])], axis=1).astype(np.float32)
    foxb = np.zeros((128, 1), np.float32)
    foxb[32:36, 0] = inp["fox_bias"][l]
    gn = np.ascontiguousarray(inp["norm_mix"][l].reshape(8, 128).T)
    maps = []
    for i in range(8):
        b, q = i // 4, i % 4
        m = {"modA": np.ascontiguousarray(mod[l, b][:, 0:16]), "gn": gn,
             "pos": np.ascontiguousarray(inp["positions"][b:b + 1, q * NT:(q + 1) * NT]).astype(np.int32),
             "invf": invf, "w": wp, "gain": gain, "osc": osc, "foxb": foxb, "pm": pm, "bones": bones}
        if xT_all is not None:
            xs = xT_all[b][:, q * NT:(q + 1) * NT].reshape(8, 128, NT).transpose(1, 0, 2)
            m["xT"] = np.ascontiguousarray(xs)
        maps.append(m)
    return maps


def collect_A(results, pre=""):
    proj = [np.concatenate([results[b * 4 + q][pre + "proj"] for q in range(4)], axis=2) for b in range(2)]
    gates = [np.concatenate([results[b * 4 + q][pre + "gates"] for q in range(4)], axis=2) for b in range(2)]
    misc = [np.concatenate([results[b * 4 + q][pre + "misc"] for q in range(4)], axis=1) for b in range(2)]
    return proj, gates, misc


def run_A(cA, inp, l, xT_all, mod):
    maps = maps_A(inp, l, xT_all, mod)
    res = run_bass_kernel_spmd(cA.nc, maps, core_ids=list(range(8)))
    return collect_A(res.results)


B_MIXERS = ("dil", "fox", "sb", "nsa")
NKB = S // 128
NQT = S // TT
M_CAUSAL, M_STRICT, M_WIN, M_DIL, M_CMP, M_NEGC = 0, 4, 8, 16, 36, 41
N_MASKS = 45


def consts_B():
    import ml_dtypes
    k = np.arange(128)[:, None]
    cc = np.arange(512)[None, :]
    masks = np.zeros((N_MASKS, 128, 512), np.float32)
    for i in range(4):
        masks[M_CAUSAL + i] = (cc - k >= 128 * i)
        masks[M_STRICT + i] = (cc - k > 128 * i)
    for w in range(8):
        diff = cc - k + 512 - 128 * w
        masks[M_WIN + w] = (diff >= 0) & (diff < 512)
    for w in range(20):
        diff = cc - k + 2048 - 128 * w
        m = np.zeros((128, 512), np.float32)
        for (ww, d) in ((128, 1), (512, 4), (2048, 16)):
            m += ((diff % d == 0) & (diff >= 0) & (diff <= ww))
        masks[M_DIL + w] = m
    for u in range(5):
        masks[M_CMP + u] = (16 * k + 31 <= 512 * u + cc)
    for i in range(4):
        masks[M_NEGC + i] = -30000.0 * (cc - k < 128 * i)
    G = ((np.arange(S)[None, :] // 64) % 64 == np.arange(64)[:, None]).astype(np.float32)
    c0 = np.arange(511) * 16
    s0 = np.arange(128) * 64
    ov = ((c0[:, None] < s0[None, :] + 64) & (c0[:, None] + 32 > s0[None, :])).astype(np.float32)
    ov = np.concatenate([ov, np.zeros((1, 128), np.float32)], 0).reshape(4, 128, 128).transpose(1, 0, 2)
    onesc = np.ones((128, 4, 1), np.float32)
    onesc[127, 3, 0] = 0.0
    Rconst = np.concatenate([ov, onesc], axis=2)
    add = np.zeros((128, 254), np.float32)
    jj = np.arange(254)[None, :] - 126
    cr = (np.arange(128) // 64)[:, None]
    add[(jj == cr) | (jj == cr - 1)] = 1e30
    add[jj > cr] = -1e30
    jn = np.arange(128)[:, None]
    kn = np.arange(128)[None, :]
    nti = -(jn >= kn).astype(np.float32)
    ntc = -(jn < kn).astype(np.float32)
    bf = ml_dtypes.bfloat16
    return dict(masks=masks.astype(bf), G64=G.astype(bf), Rconst=Rconst.astype(bf), add=add,
                nti=nti.astype(bf), ntc=ntc.astype(bf), ident=np.eye(128, dtype=np.float32),
                tri64=(np.arange(64)[:, None] < np.arange(64)[None, :]).astype(np.float32))


def pipeline(n_items, stages):
    K = len(stages)
    for i in range(n_items + K - 1):
        for k, st in enumerate(stages):
            n = i - k
            if 0 <= n < n_items:
                st(n)


def build_B():
    c = Ctx()
    nc, s = c.nc, c.s
    di = lambda name, shape, dt: c.dram(name, shape, dt, "ExternalInput")
    qnr_d = di("qnr", [4, 64, S], BF16)
    qr_d = di("qr", [64, S], BF16)
    kcT_d = di("kcT", [64, S], BF16)
    vcT_d = di("vcT", [64, S], BF16)
    kslT_d = di("kslT", [64, S], BF16)
    kwT_d = di("kwT", [64, S], BF16)
    vsl_d = di("vsl", [128, NKB, 65], BF16)
    vw_d = di("vw", [128, NKB, 65], BF16)
    ag_d = di("ag", [128, NKB, 3], F32)
    dq_d = di("dq", [64, S], BF16)
    dk_d = di("dk", [64, S], BF16)
    dv_d = di("dv", [128, NKB, 65], BF16)
    sq_d = di("sq", [64, S], BF16)
    sk_d = di("sk", [64, S], BF16)
    sv_d = di("sv", [128, NKB, 65], BF16)
    fq_d = di("fq", [64, S], BF16)
    fk_d = di("fk", [64, S], BF16)
    fv_d = di("fv", [128, NKB, 65], BF16)
    lf_d = di("logf", [1, S], F32)
    w1k_d = di("w1k", [64, 32, 128], F32)
    w1v_d = di("w1v", [64, 32, 128], F32)
    w2k_d = di("w2k", [128, 64], F32)
    w2v_d = di("w2v", [128, 64], F32)
    pek_d = di("pek", [64, 32], F32)
    pev_d = di("pev", [64, 32], F32)
    masks_d = di("masks", [N_MASKS, 128, 512], BF16)
    G_d = di("G64", [64, S], BF16)
    Rc_d = di("Rconst", [128, 4, 129], BF16)
    add_d = di("add", [128, 254], F32)
    nti_d = di("nti", [128, 128], BF16)
    ntc_d = di("ntc", [128, 128], BF16)
    ident_d = di("ident", [128, 128], F32)
    tri_d = di("tri64", [64, 64], F32)
    o_d = c.dram("o", [4, 128, NKB, 64], BF16, "ExternalOutput")

    ps_ring = [c.ps([128, 512]) for _ in range(4)]
    po_ring = [c.ps([128, 512]) for _ in range(2)]
    pX = c.ps([128, 512])
    pY = c.ps([128, 512])
    e_ring = [c.sb([128, 512], BF16) for _ in range(4)]
    p_ring = [c.sb([128, 512], BF16) for _ in range(4)]
    pre_ring = [c.sb([128, 512], F32) for _ in range(4)]
    rz_ring = [c.sb([128, 4], F32) for _ in range(2)]
    fac_ring = [c.sb([128, 4], F32) for _ in range(2)]
    ost = c.sb([128, NKB, 64], BF16)

    class Scope:
        def __enter__(self):
            self.es = contextlib.ExitStack()
            self.old = c.es
            c.es = self.es
            return self

        def __exit__(self, *a):
            barrier(c)
            c.es = self.old
            self.es.close()

    def load_masks(lo, n, order=None):
        t = c.sb([128, n, 512], BF16)
        t.parts = [Res() for _ in range(n)]
        for ii, i in enumerate(order if order is not None else range(n)):
            s.dma(t[:, i, :], masks_d[lo + i], writes=[t.parts[i]], q=("sp", "act")[ii % 2])
        return t

    def split_load(QT, KT, V, qT_d, kT_d, v_d):
        qs = ("sp", "act")
        for t in (QT, KT, V):
            if t is not None:
                t.parts = [Res() for _ in range(4)]
        for h in range(4):
            sl = slice(h * 2048, (h + 1) * 2048)
            if QT is not None:
                s.dma(QT[0:64, sl], qT_d[:, sl], reads=[QT.res], writes=[QT.parts[h]], q=qs[h % 2])
            s.dma(KT[0:64, sl], kT_d[:, sl], reads=[KT.res], writes=[KT.parts[h]], q=qs[(h + 1) % 2])
            s.dma(V[:, 16 * h:16 * (h + 1), :], v_d[:, 16 * h:16 * (h + 1), :], reads=[V.res], writes=[V.parts[h]], q=qs[h % 2])

    def rQ(QT, qt):
        return [QT.res, QT.parts[qt // 4]]

    def rK(KT, kb):
        return [KT.res, KT.parts[kb // 16]]

    def attn(items, qk, pv, fin, mask_of, alt=[0], nps=4):
        def st0(n):
            qk(n, ps_ring[n % nps])

        def st1(n):
            it = items[n]
            ps, e = ps_ring[n % nps], e_ring[n % 4]
            if it["mi"] is not None and it["mi"] >= M_NEGC:
                mt, mi = mask_of(it["mi"])
                pre = pre_ring[n % 4]
                s.op("dve", lambda: nc.vector.tensor_tensor(out=pre[:], in0=ps[:], in1=mt[:, mi, :], op=ALU.add),
                     reads=[ps.res, mt.parts[mi]], writes=[pre.res])
                p = p_ring[n % 4]
                s.op("act", lambda: nc.scalar.activation(out=p[:], in_=pre[:], func=AF.Exp), reads=[pre.res], writes=[p.res])
                return
            s.op("act", lambda: nc.scalar.activation(out=e[:], in_=ps[:], func=AF.Exp), reads=[ps.res], writes=[e.res])
            if it["mi"] is not None:
                p = p_ring[n % 4]
                mt, mi = mask_of(it["mi"])
                alt[0] = 1
                if alt[0]:
                    s.op("dve", lambda: nc.vector.tensor_tensor(out=p[:], in0=e[:], in1=mt[:, mi, :], op=ALU.mult),
                         reads=[e.res, mt.parts[mi]], writes=[p.res])
                else:
                    s.op("pool", lambda: nc.gpsimd.tensor_tensor(out=p[:], in0=e[:], in1=mt[:, mi, :], op=ALU.mult),
                         reads=[e.res, mt.parts[mi]], writes=[p.res])

        def st2(n):
            it = items[n]
            pt = p_ring[n % 4] if it["mi"] is not None else e_ring[n % 4]
            pv(n, pt)
            if it["last"]:
                fin(n)
        pipeline(len(items), [st0, st1, (lambda n: None), st2])

    def std_pv(items, V, ncol=65):
        def pv(n, pt):
            it = items[n]
            po = po_ring[it["qt"] % 2]
            for qb in range(4):
                s.op("pe", (lambda qb=qb: nc.tensor.matmul(po[:, qb * 65:qb * 65 + ncol], lhsT=pt[:, qb * 128:(qb + 1) * 128],
                                                           rhs=V[:, it["kb"], 0:ncol], start=(it["first"] and qb == 0), stop=it["last"],
                                                           skip_group_check=True)),
                     reads=[pt.res, V.res, V.parts[it["kb"] // 16]], writes=[po.res])
        return pv

    def rz_of(qt, po, zoff=64, stride=65):
        rz = rz_ring[qt % 2]
        s.op("dve", lambda: nc.vector.tensor_scalar(out=rz[:], in0=po[:, zoff:zoff + 3 * stride + 1:stride], scalar1=1e-30, scalar2=None, op0=ALU.max),
             reads=[po.res], writes=[rz.res])
        s.op("dve", lambda: nc.vector.reciprocal(out=rz[:], in_=rz[:]), reads=[rz.res], writes=[rz.res])
        return rz

    def std_items(blocks_of):
        items = []
        for qt in range(NQT):
            bl = blocks_of(qt)
            for ii, (kb, mi) in enumerate(bl):
                items.append(dict(qt=qt, kb=kb, mi=mi, first=(ii == 0), last=(ii == len(bl) - 1)))
        return items

    def simple_mixer(m, qT_d, kT_d, v_d, blocks_of, mask_lo, mask_n, kdim=64, prep=None, mask_order=None):
        with Scope():
            QT = c.sb([128, S], BF16)
            KT = c.sb([128, S], BF16)
            V = c.sb([128, NKB, 65], BF16)
            if prep is not None:
                prep(QT, KT)
            split_load(QT, KT, V, qT_d, kT_d, v_d)
            mt = load_masks(mask_lo, mask_n, order=mask_order)
            items = std_items(blocks_of)

            def qk(n, ps):
                it = items[n]
                s.op("pe", lambda: nc.tensor.matmul(ps[:], lhsT=KT[0:kdim, it["kb"] * 128:(it["kb"] + 1) * 128],
                                                    rhs=QT[0:kdim, it["qt"] * 512:(it["qt"] + 1) * 512], start=True, stop=True),
                     reads=rK(KT, it["kb"]) + rQ(QT, it["qt"]), writes=[ps.res])

            def fin(n):
                qt = items[n]["qt"]
                po = po_ring[qt % 2]
                rz = rz_of(qt, po)
                for qb in range(4):
                    s.op("dve", (lambda qb=qb: nc.vector.tensor_scalar(out=ost[:, 4 * qt + qb, :], in0=po[:, qb * 65:qb * 65 + 64],
                                                                       scalar1=rz[:, qb:qb + 1], scalar2=None, op0=ALU.mult)),
                         reads=[po.res, rz.res], writes=[ost.res])
            attn(items, qk, std_pv(items, V), fin, lambda mi: (mt, mi - mask_lo))
            s.dma(o_d[m], ost[:], reads=[ost.res], is_output=True)

    def dil_blocks(qt):
        return [(4 * qt - 16 + w, M_DIL + w) for w in range(20) if 4 * qt - 16 + w >= 0]
    if "dil" in B_MIXERS:
        simple_mixer(1, dq_d, dk_d, dv_d, dil_blocks, M_DIL, 20, mask_order=[16, 17, 18, 19, 12, 13, 14, 15, 8, 9, 10, 11, 4, 5, 6, 7, 0, 1, 2, 3])

    fs_d = c.dram("fsplit", [6, S], BF16, "Internal")
    fs_res = Res()

    def fox_prep(QT, KT):
        lf = c.sb([64, 128], F32)
        F = c.sb([64, 128], F32)
        zr = c.sb([64, 128], F32)
        r1 = c.sb([64, 128], F32)
        off = c.sb([64, 1], F32)
        U = c.sb([64, 64], F32)
        sp3 = [c.sb([64, 128], BF16) for _ in range(3)]
        ng3 = [c.sb([64, 128], BF16) for _ in range(3)]
        s.dma(lf[:], lf_d.rearrange("o (p j) -> (o p) j", j=128), writes=[lf.res])
        s.dma(U[:], tri_d, writes=[U.res])
        s.op("pool", lambda: nc.gpsimd.memset(zr[:], 0.0), writes=[zr.res])
        s.op("pool", lambda: nc.gpsimd.memset(QT[:], 0.0), writes=[QT.res])
        s.op("pool", lambda: nc.gpsimd.memset(KT[:], 0.0), writes=[KT.res])
        s.op("pool", lambda: nc.gpsimd.memset(QT[96:99, :], 1.0), writes=[QT.res])
        s.op("pool", lambda: nc.gpsimd.memset(KT[64:67, :], 1.0), writes=[KT.res])
        s.op("dve", lambda: nc.vector.tensor_tensor_scan(out=F[:], data0=lf[:], data1=zr[:], initial=0.0, op0=ALU.add, op1=ALU.add),
             reads=[lf.res, zr.res], writes=[F.res])
        s.op("pe", lambda: nc.tensor.matmul(pY[0:64, 0:1], lhsT=U[:], rhs=F[:, 127:128], start=True, stop=True),
             reads=[U.res, F.res], writes=[pY.res])
        s.op("act", lambda: nc.scalar.copy(out=off[:], in_=pY[0:64, 0:1]), reads=[pY.res], writes=[off.res])
        s.op("dve", lambda: nc.vector.tensor_scalar(out=F[:], in0=F[:], scalar1=off[:, 0:1], scalar2=None, op0=ALU.add),
             reads=[F.res, off.res], writes=[F.res])
        cur = F
        for i in range(3):
            s.op("dve", (lambda i=i, cur=cur: nc.vector.tensor_copy(out=sp3[i][:], in_=cur[:])), reads=[cur.res], writes=[sp3[i].res])
            s.op("dve", (lambda i=i: nc.vector.tensor_scalar(out=ng3[i][:], in0=sp3[i][:], scalar1=-1.0, scalar2=None, op0=ALU.mult)),
                 reads=[sp3[i].res], writes=[ng3[i].res])
            if i < 2:
                s.op("dve", (lambda i=i, cur=cur: nc.vector.tensor_tensor(out=r1[:], in0=cur[:], in1=sp3[i][:], op=ALU.subtract)),
                     reads=[cur.res, sp3[i].res], writes=[r1.res])
                cur = r1
            s.dma(fs_d[i, :].rearrange("(p j) -> p j", j=128), sp3[i][:], reads=[sp3[i].res], writes=[fs_res])
            s.dma(fs_d[3 + i, :].rearrange("(p j) -> p j", j=128), ng3[i][:], reads=[ng3[i].res], writes=[fs_res])
        s.dma(QT[64:67, :], fs_d[0:3, :], reads=[fs_res], writes=[QT.res])
        s.dma(KT[96:99, :], fs_d[3:6, :], reads=[fs_res], writes=[KT.res])

    def causal_blocks(qt):
        return [(kb, None) for kb in range(4 * qt)] + [(4 * qt + i, M_CAUSAL + i) for i in range(4)]

    def negc_blocks(qt):
        return [(kb, None) for kb in range(4 * qt)] + [(4 * qt + i, M_NEGC + i) for i in range(4)]
    if "fox" in B_MIXERS:
        simple_mixer(3, fq_d, fk_d, fv_d, negc_blocks, M_NEGC, 4, kdim=99, prep=fox_prep)

    with (Scope() if "sb" in B_MIXERS else contextlib.nullcontext()):
      if "sb" in B_MIXERS:
        QT = c.sb([64, S], BF16)
        KT = c.sb([64, S], BF16)
        V = c.sb([128, NKB, 65], BF16)
        nti = c.sb([128, 128], BF16)
        ntc = c.sb([128, 128], BF16)
        split_load(QT, KT, V, sq_d, sk_d, sv_d)
        s.dma(nti[:], nti_d, writes=[nti.res])
        s.dma(ntc[:], ntc_d, writes=[ntc.res])
        mt = load_masks(M_STRICT, 4)
        E_r = [c.sb([128, 512], F32) for _ in range(4)]
        L_r = [c.sb([128, 512], BF16) for _ in range(4)]
        X_r = [c.sb([128, 512], F32) for _ in range(4)]
        A_r = [c.sb([128, 512], BF16) for _ in range(4)]
        def sb_blocks(qt):
            bl = [(4 * qt + i, M_STRICT + i) for i in (3, 2, 1, 0)] + [(kb, None) for kb in range(4 * qt - 1, -1, -1)]
            return [dict(qt=qt, kb=kb, mi=mi, first=(ii == 0), last=(ii == len(bl) - 1)) for ii, (kb, mi) in enumerate(bl)]
        items = []
        for pr in range(NQT // 2):
            la, lb = sb_blocks(2 * pr), sb_blocks(2 * pr + 1)
            for ii in range(len(lb)):
                if ii < len(la):
                    items.append(la[ii])
                items.append(lb[ii])
        pXs = (pX, pY)

        def c0_of(it):
            return 128 * (it["mi"] - M_STRICT) if it["mi"] is not None else 0

        def sb0(n):
            it = items[n]
            ps = ps_ring[n % 4]
            c0 = c0_of(it)
            s.op("pe", lambda: nc.tensor.matmul(ps[:, c0:512], lhsT=KT[:, it["kb"] * 128:(it["kb"] + 1) * 128],
                                                rhs=QT[:, it["qt"] * 512 + c0:(it["qt"] + 1) * 512], start=True, stop=True),
                 reads=rK(KT, it["kb"]) + rQ(QT, it["qt"]), writes=[ps.res])

        def sb1(n):
            it = items[n]
            ps, E, L = ps_ring[n % 4], E_r[n % 4], L_r[n % 4]
            c0 = c0_of(it)
            s.op("act", lambda: nc.scalar.activation(out=E[:, c0:512], in_=ps[:, c0:512], func=AF.Exp), reads=[ps.res], writes=[E.res])
            s.op("act", lambda: nc.scalar.activation(out=L[:, c0:512], in_=E[:, c0:512], func=AF.Ln, bias=1.0, scale=1.0), reads=[E.res], writes=[L.res])
            if it["mi"] is not None:
                mi = it["mi"] - M_STRICT
                s.op("pool", lambda: nc.gpsimd.tensor_tensor(out=L[:, c0:512], in0=L[:, c0:512], in1=mt[:, mi, c0:512], op=ALU.mult),
                     reads=[L.res, mt.parts[mi]], writes=[L.res])
                s.op("dve", lambda: nc.vector.tensor_tensor(out=E[:, c0:512], in0=E[:, c0:512], in1=mt[:, mi, c0:512], op=ALU.mult),
                     reads=[E.res, mt.parts[mi]], writes=[E.res])

        def sb2(n):
            it = items[n]
            L, X = L_r[n % 4], X_r[n % 4]
            pXq = pXs[it["qt"] % 2]
            c0 = c0_of(it)
            s.op("pe", lambda: nc.tensor.matmul(pXq[:, c0:512], lhsT=nti[:], rhs=L[:, c0:512], start=it["first"], stop=False, skip_group_check=True),
                 reads=[nti.res, L.res], writes=[pXq.res])
            s.op("act", lambda: nc.scalar.activation(out=X[:, c0:512], in_=pXq[:, c0:512], func=AF.Exp), reads=[pXq.res], writes=[X.res])

        def sb3(n):
            it = items[n]
            L, X, E, A = L_r[n % 4], X_r[n % 4], E_r[n % 4], A_r[n % 4]
            pXq = pXs[it["qt"] % 2]
            c0 = c0_of(it)
            s.op("pe", lambda: nc.tensor.matmul(pXq[:, c0:512], lhsT=ntc[:], rhs=L[:, c0:512], start=False, stop=it["last"], skip_group_check=True),
                 reads=[ntc.res, L.res], writes=[pXq.res])
            s.op("dve", lambda: nc.vector.tensor_tensor(out=A[:, c0:512], in0=E[:, c0:512], in1=X[:, c0:512], op=ALU.mult), reads=[E.res, X.res], writes=[A.res])

        def sb4(n):
            it = items[n]
            A = A_r[n % 4]
            qt = it["qt"]
            po = po_ring[qt % 2]
            qb0 = c0_of(it) // 128
            for qb in range(qb0, 4):
                s.op("pe", (lambda qb=qb: nc.tensor.matmul(po[:, qb * 65:qb * 65 + 64], lhsT=A[:, qb * 128:(qb + 1) * 128],
                                                           rhs=V[:, it["kb"], 0:64], start=(it["first"] and qb == qb0), stop=it["last"],
                                                           skip_group_check=True)),
                     reads=[A.res, V.res, V.parts[it["kb"] // 16]], writes=[po.res])
            if it["last"]:
                for qb in range(4):
                    s.op("act", (lambda qb=qb: nc.scalar.copy(out=ost[:, 4 * qt + qb, :], in_=po[:, qb * 65:qb * 65 + 64])),
                         reads=[po.res], writes=[ost.res])
        K_ = len(items)
        for i in range(K_ + 4):
            if i < K_:
                sb0(i)
            if 0 <= i - 1 < K_:
                sb1(i - 1)
            if 0 <= i - 3 < K_:
                sb3(i - 3)
            if 0 <= i - 2 < K_:
                sb2(i - 2)
            if 0 <= i - 4 < K_:
                sb4(i - 4)
        s.dma(o_d[2], ost[:], reads=[ost.res], is_output=True)

    hsel_d = di("hsel", [128, 4], F32)
    with (Scope() if "nsa" in B_MIXERS else contextlib.nullcontext()):
      if "nsa" in B_MIXERS:
        QA = c.sb([128, S], BF16)
        QB = c.sb([128, S], BF16)
        oa = c.sb([128, NKB, 64], F32)
        oa.parts = [Res() for _ in range(NKB)]
        ag = c.sb([128, NKB, 3], F32)
        ident = c.sb([128, 128], F32)
        addt = c.sb([128, 254], F32)
        hsel = c.sb([128, 4], F32)
        for h in range(4):
            sl = slice(h * 2048, (h + 1) * 2048)
            s.dma(QA[0:64, sl], qr_d[:, sl], writes=[QA.res], q="act")
            s.dma(QB[0:64, sl], qr_d[:, sl], writes=[QB.res], q="act")
        for t, d in ((ag, ag_d), (ident, ident_d), (addt, add_d), (hsel, hsel_d)):
            s.dma(t[:], d, writes=[t.res])
        with Scope():
            kcT = c.sb([64, S], BF16)
            vcT = c.sb([64, S], BF16)
            for h in range(4):
                sl = slice(h * 2048, (h + 1) * 2048)
                s.dma(kcT[:, sl], kcT_d[:, sl], writes=[kcT.res])
                s.dma(vcT[:, sl], vcT_d[:, sl], writes=[vcT.res])
            kccT = c.sb([64, 512], BF16)
            Rt = c.sb([128, 4, 193], BF16)
            s.dma(Rt[:, :, 0:129], Rc_d, writes=[Rt.res])
            w1f = c.sb([64, 32, 128], F32)
            w1b = c.sb([64, 32, 128], BF16)
            w2f = c.sb([128, 64], F32)
            w2b = c.sb([128, 64], BF16)
            pef = c.sb([64, 32], F32)
            peb = c.sb([128, 1], F32)
            xg = c.sb([128, 512], F32)
            x2 = c.sb([128, 512], F32)
            gT = c.sb([128, 512], BF16)
            for which in ("k", "v"):
                src = kcT if which == "k" else vcT
                s.dma(w1f[:], w1k_d if which == "k" else w1v_d, writes=[w1f.res])
                s.dma(w2f[:], w2k_d if which == "k" else w2v_d, writes=[w2f.res])
                s.dma(pef[:], pek_d if which == "k" else pev_d, writes=[pef.res])
                s.op("pool", lambda: nc.gpsimd.tensor_copy(out=w1b[:], in_=w1f[:]), reads=[w1f.res], writes=[w1b.res])
                s.op("pool", lambda: nc.gpsimd.tensor_copy(out=w2b[:], in_=w2f[:]), reads=[w2f.res], writes=[w2b.res])
                for l in range(32):
                    s.op("pe", (lambda l=l: nc.tensor.matmul(pY[:, 0:1], lhsT=w1f[:, l, :], rhs=pef[:, l:l + 1], start=(l == 0), stop=(l == 31))),
                         reads=[w1f.res, pef.res], writes=[pY.res])
                s.op("act", lambda: nc.scalar.copy(out=peb[:], in_=pY[:, 0:1]), reads=[pY.res], writes=[peb.res])
                srcv = src[:].rearrange("p (c s) -> p c s", s=16)
                for l in range(32):
                    rhs = srcv[:, 0:511, l] if l < 16 else srcv[:, 1:512, l - 16]
                    s.op("pe", (lambda l=l, rhs=rhs: nc.tensor.matmul(pX[:, 0:511], lhsT=w1b[:, l, :], rhs=rhs, start=(l == 0), stop=(l == 31))),
                         reads=[w1b.res, src.res], writes=[pX.res])
                s.op("act", lambda: nc.scalar.activation(out=xg[:, 0:511], in_=pX[:, 0:511], func=AF.Identity, bias=peb[:], scale=1.0),
                     reads=[pX.res, peb.res], writes=[xg.res])
                s.op("dve", lambda: nc.vector.tensor_tensor(out=x2[:, 0:511], in0=xg[:, 0:511], in1=xg[:, 0:511], op=ALU.mult), reads=[xg.res], writes=[x2.res])
                s.op("dve", lambda: nc.vector.tensor_scalar(out=x2[:, 0:511], in0=x2[:, 0:511], scalar1=0.044715, scalar2=1.0, op0=ALU.mult, op1=ALU.add),
                     reads=[x2.res], writes=[x2.res])
                s.op("dve", lambda: nc.vector.tensor_tensor(out=x2[:, 0:511], in0=x2[:, 0:511], in1=xg[:, 0:511], op=ALU.mult), reads=[x2.res, xg.res], writes=[x2.res])
                s.op("act", lambda: nc.scalar.activation(out=x2[:, 0:511], in_=x2[:, 0:511], func=AF.Sigmoid, scale=1.5957691216057308),
                     reads=[x2.res], writes=[x2.res])
                s.op("pool", lambda: nc.gpsimd.memset(gT[:], 0.0), writes=[gT.res])
                s.op("dve", lambda: nc.vector.tensor_tensor(out=gT[:, 0:511], in0=xg[:, 0:511], in1=x2[:, 0:511], op=ALU.mult), reads=[xg.res, x2.res, gT.res], writes=[gT.res])
                if which == "k":
                    s.op("pe", lambda: nc.tensor.matmul(pY[0:64, :], lhsT=w2b[:], rhs=gT[:], start=True, stop=True), reads=[w2b.res, gT.res], writes=[pY.res])
                    s.op("act", lambda: nc.scalar.copy(out=kccT[:], in_=pY[0:64, :]), reads=[pY.res], writes=[kccT.res])
                else:
                    for cc in range(4):
                        s.op("pe", (lambda cc=cc: nc.tensor.matmul(pY[:, cc * 64:(cc + 1) * 64], lhsT=gT[:, cc * 128:(cc + 1) * 128], rhs=w2b[:],
                                                                   start=True, stop=True)),
                             reads=[w2b.res, gT.res], writes=[pY.res])
                    s.op("act", lambda: nc.scalar.copy(out=Rt[:, :, 129:193], in_=pY[:, 0:256].rearrange("p (a b) -> p a b", b=64)),
                         reads=[pY.res], writes=[Rt.res])
            mt = load_masks(M_CMP, 5)
            aghs = c.sb([128, 4, NKB], F32)
            for h in range(4):
                s.op("dve", (lambda h=h: nc.vector.tensor_scalar(out=aghs[:, h, :], in0=ag[:, :, 0], scalar1=hsel[:, h:h + 1], scalar2=None, op0=ALU.mult)),
                     reads=[ag.res, hsel.res], writes=[aghs.res])
            qring = [c.sb([64, 512], BF16) for _ in range(4)]
            imp = [c.sb([128, 4, 128], F32) for _ in range(2)]
            for t in imp:
                t.parts = [Res() for _ in range(4)]
            sc_r = [c.sb([128, 128], F32) for _ in range(4)]
            sc2_r = [c.sb([128, 128], F32) for _ in range(4)]
            sb_r = [c.sb([128, 128], F32) for _ in range(4)]
            sbr_r = [c.sb([128, 128], F32) for _ in range(4)]
            for t in sbr_r:
                s.op("pool", (lambda t=t: nc.gpsimd.memset(t[:], 0.0)), writes=[t.res])
            m8_r = [c.sb([128, 16], F32) for _ in range(4)]
            pT = ps_ring[3]
            usets = [(po_ring[0], po_ring[1]), (pX, pY)]
            items = []
            for qt in range(NQT):
                for h in range(4):
                    ccs = [cc for cc in range(4) if qt - 4 * cc >= 0]
                    for ii, cc in enumerate(ccs):
                        u = qt - 4 * cc
                        items.append(dict(qt=qt, h=h, kb=cc, mi=(M_CMP + u if u <= 4 else None), first=(ii == 0), last=(ii == len(ccs) - 1)))

            def qk_c(n, ps):
                it = items[n]
                qtile = qring[(it["qt"] * 4 + it["h"]) % 4]
                if it["first"]:
                    s.dma(qtile[:], qnr_d[it["h"], :, it["qt"] * 512:(it["qt"] + 1) * 512], writes=[qtile.res])
                s.op("pe", lambda: nc.tensor.matmul(ps[:], lhsT=kccT[:, it["kb"] * 128:(it["kb"] + 1) * 128], rhs=qtile[:], start=True, stop=True),
                     reads=[kccT.res, qtile.res], writes=[ps.res])

            def pv_c(n, pt):
                it = items[n]
                us = usets[(it["qt"] * 4 + it["h"]) % 2]
                for qb in range(4):
                    tl = us[qb // 2]
                    off = (qb % 2) * 193
                    s.op("pe", (lambda qb=qb, tl=tl, off=off: nc.tensor.matmul(tl[:, off:off + 193], lhsT=pt[:, qb * 128:(qb + 1) * 128], rhs=Rt[:, it["kb"], :],
                                                                             start=(it["first"] and qb % 2 == 0), stop=it["last"], skip_group_check=True)),
                         reads=[pt.res, Rt.res], writes=[tl.res])

            def fin_c(n):
                it = items[n]
                qt, h = it["qt"], it["h"]
                us = usets[(qt * 4 + h) % 2]
                rz = rz_ring[h % 2]
                fac = fac_ring[h % 2]
                imp_t = imp[qt % 2]
                for half in range(2):
                    tl = us[half]
                    s.op("dve", (lambda half=half, tl=tl: nc.vector.tensor_scalar(out=rz[:, 2 * half:2 * half + 2], in0=tl[:, 128:322:193], scalar1=1e-30,
                                                                                scalar2=None, op0=ALU.max)), reads=[tl.res], writes=[rz.res])
                s.op("dve", lambda: nc.vector.reciprocal(out=rz[:], in_=rz[:]), reads=[rz.res], writes=[rz.res])
                s.op("dve", lambda: nc.vector.tensor_tensor(out=fac[:], in0=rz[:], in1=aghs[:, h, 4 * qt:4 * qt + 4], op=ALU.mult), reads=[rz.res, aghs.res], writes=[fac.res])
                for qb in range(4):
                    tl, off = us[qb // 2], (qb % 2) * 193
                    tb = 4 * qt + qb
                    if h == 0:
                        s.op("dve", (lambda qb=qb, tl=tl, off=off: nc.vector.tensor_scalar(out=imp_t[:, qb, :], in0=tl[:, off:off + 128], scalar1=rz[:, qb:qb + 1],
                                                                                         scalar2=None, op0=ALU.mult)), reads=[tl.res, rz.res], writes=[imp_t.parts[qb]])
                        s.op("dve", (lambda qb=qb, tl=tl, off=off, tb=tb: nc.vector.tensor_scalar(out=oa[:, tb, :], in0=tl[:, off + 129:off + 193], scalar1=fac[:, qb:qb + 1],
                                                                                                scalar2=None, op0=ALU.mult)), reads=[tl.res, fac.res], writes=[oa.parts[tb]])
                    else:
                        s.op("dve", (lambda qb=qb, tl=tl, off=off: nc.vector.scalar_tensor_tensor(out=imp_t[:, qb, :], in0=tl[:, off:off + 128], scalar=rz[:, qb:qb + 1],
                                                                                                in1=imp_t[:, qb, :], op0=ALU.mult, op1=ALU.add)),
                             reads=[tl.res, rz.res, imp_t.parts[qb]], writes=[imp_t.parts[qb]])
                        s.op("dve", (lambda qb=qb, tl=tl, off=off, tb=tb: nc.vector.scalar_tensor_tensor(out=oa[:, tb, :], in0=tl[:, off + 129:off + 193], scalar=fac[:, qb:qb + 1],
                                                                                                       in1=oa[:, tb, :], op0=ALU.mult, op1=ALU.add)),
                             reads=[tl.res, fac.res, oa.parts[tb]], writes=[oa.parts[tb]])
                if h != 3:
                    return
                for qb in range(4):
                    tb = 4 * qt + qb
                    sc, sc2, sbt, m8 = sc_r[qb], sc2_r[qb], sb_r[qb], m8_r[qb]
                    s.op("dve", (lambda qb=qb, sc=sc, tb=tb: nc.vector.tensor_tensor(out=sc[:], in0=imp_t[:, qb, :], in1=addt[:, 126 - 2 * tb:254 - 2 * tb], op=ALU.add)),
                         reads=[imp_t.parts[qb], addt.res], writes=[sc.res])
                    s.op("dve", (lambda sc=sc: nc.vector.memset(sc[:, 0:1], 1e30)), reads=[sc.res], writes=[sc.res])
                    s.op("dve", (lambda sc=sc, m8=m8: nc.vector.max(out=m8[:, 0:8], in_=sc[:])), reads=[sc.res], writes=[m8.res])
                    s.op("dve", (lambda sc=sc, sc2=sc2, m8=m8: nc.vector.match_replace(out=sc2[:], in_to_replace=m8[:, 0:8], in_values=sc[:], imm_value=-3e38)),
                         reads=[sc.res, m8.res], writes=[sc2.res])
                    s.op("dve", (lambda sc2=sc2, m8=m8: nc.vector.max(out=m8[:, 8:16], in_=sc2[:])), reads=[sc2.res, m8.res], writes=[m8.res])
                    s.op("dve", (lambda sc=sc, sbt=sbt, m8=m8: nc.vector.tensor_scalar(out=sbt[:], in0=sc[:], scalar1=m8[:, 15:16], scalar2=-30000.0,
                                                                                   op0=ALU.is_lt, op1=ALU.mult)), reads=[sc.res, m8.res], writes=[sbt.res])
                    sbr = sbr_r[qb]
                    s.op("dve", (lambda sc=sc, sbr=sbr, m8=m8: nc.vector.tensor_scalar(out=sbr[:, 64:128], in0=sc[:, 0:64], scalar1=m8[:, 15:16], scalar2=-30000.0,
                                                                                   op0=ALU.is_lt, op1=ALU.mult)), reads=[sc.res, m8.res], writes=[sbr.res])
                for qb in range(4):
                    s.op("pe", (lambda qb=qb: nc.tensor.transpose(pT[:, qb * 128:(qb + 1) * 128], sb_r[qb][:], ident[:])),
                         reads=[sb_r[qb].res, ident.res], writes=[pT.res])
                s.op("act", lambda: nc.scalar.copy(out=QB[64:128, qt * 512:(qt + 1) * 512], in_=pT[64:128, :]), reads=[pT.res], writes=[QB.res])
                for qb in range(4):
                    s.op("pe", (lambda qb=qb: nc.tensor.transpose(pT[:, qb * 128:(qb + 1) * 128], sbr_r[qb][:], ident[:])),
                         reads=[sbr_r[qb].res, ident.res], writes=[pT.res])
                s.op("act", lambda: nc.scalar.copy(out=QA[64:128, qt * 512:(qt + 1) * 512], in_=pT[64:128, :]), reads=[pT.res], writes=[QA.res])
            attn(items, qk_c, pv_c, fin_c, lambda mi: (mt, mi - M_CMP), nps=3)

        with Scope():
            KS = c.sb([128, S], BF16)
            KW = c.sb([64, S], BF16)
            VS = c.sb([128, NKB, 65], BF16)
            VW = c.sb([128, NKB, 65], BF16)
            mtc = load_masks(M_CAUSAL, 4)
            for h in range(4):
                sl = slice(h * 2048, (h + 1) * 2048)
                s.dma(KS[64:128, sl], G_d[:, sl], writes=[KS.res], q="act")
            split_load(None, KS, VS, None, kslT_d, vsl_d)
            split_load(None, KW, VW, None, kwT_d, vw_d)
            mtw = load_masks(M_WIN, 8)

            def branch(KT, V, blocks_of, mt, mask_lo, br, with_sel):
                items = std_items(blocks_of)

                def qk(n, ps):
                    it = items[n]
                    kd = 128 if with_sel else 64
                    Qx = (QA if it["kb"] < 32 else QB) if with_sel else QA
                    s.op("pe", lambda: nc.tensor.matmul(ps[:], lhsT=KT[0:kd, it["kb"] * 128:(it["kb"] + 1) * 128],
                                                        rhs=Qx[0:kd, it["qt"] * 512:(it["qt"] + 1) * 512], start=True, stop=True),
                         reads=rK(KT, it["kb"]) + [Qx.res], writes=[ps.res])

                def fin(n):
                    qt = items[n]["qt"]
                    po = po_ring[qt % 2]
                    rz = rz_of(qt, po)
                    fac = fac_ring[qt % 2]
                    s.op("dve", lambda: nc.vector.tensor_tensor(out=fac[:], in0=rz[:], in1=ag[:, 4 * qt:4 * qt + 4, br], op=ALU.mult),
                         reads=[rz.res, ag.res], writes=[fac.res])
                    for qb in range(4):
                        tb = 4 * qt + qb
                        s.op("dve", (lambda qb=qb, tb=tb: nc.vector.scalar_tensor_tensor(out=oa[:, tb, :], in0=po[:, qb * 65:qb * 65 + 64], scalar=fac[:, qb:qb + 1],
                                                                                         in1=oa[:, tb, :], op0=ALU.mult, op1=ALU.add)),
                             reads=[po.res, fac.res, oa.parts[tb]], writes=[oa.parts[tb]])
                attn(items, qk, std_pv(items, V), fin, lambda mi: (mt, mi - mask_lo))
            if "nosel" not in B_MIXERS:
                branch(KS, VS, causal_blocks, mtc, M_CAUSAL, 1, True)
            if "nowin" not in B_MIXERS:
                branch(KW, VW, lambda qt: [(4 * qt - 4 + w, M_WIN + w) for w in range(8) if 4 * qt - 4 + w >= 0], mtw, M_WIN, 2, False)
        s.op("act", lambda: nc.scalar.copy(out=ost[:], in_=oa[:]), reads=[oa.res] + oa.parts, writes=[ost.res])
        s.dma(o_d[0], ost[:], reads=[ost.res], is_output=True)
    c.close()
    return c


def run_B(cB, inp, l, proj, misc):
    import ml_dtypes
    bf = ml_dtypes.bfloat16
    cst = consts_B()

    def head_rows(P, ch0, i):
        return np.ascontiguousarray(P[ch0 + i // 2][(i % 2) * 64:(i % 2) * 64 + 64])

    def vaug(vT):
        v = vT.T.reshape(NKB, 128, 64).transpose(1, 0, 2)
        return np.ascontiguousarray(np.concatenate([v, np.ones((128, NKB, 1), bf)], axis=2))
    cl = 32 * 64
    w1k = np.ascontiguousarray(inp["nsa_ck_w1"][l].reshape(32, 64, 128).transpose(1, 0, 2))
    w1v = np.ascontiguousarray(inp["nsa_cv_w1"][l].reshape(32, 64, 128).transpose(1, 0, 2))
    pek = np.ascontiguousarray(inp["nsa_pe_k"][l].T)
    pev = np.ascontiguousarray(inp["nsa_pe_v"][l].T)
    maps = []
    for core in range(8):
        b, i = core // 4, core % 4
        P = proj[b]
        m = dict(cst)
        m["qnr"] = np.ascontiguousarray(np.stack([head_rows(P, 24, h) for h in range(4)]))
        m["qr"] = head_rows(P, 0, i)
        m["kcT"] = np.ascontiguousarray(P[3][0:64]); m["vcT"] = np.ascontiguousarray(P[12][0:64])
        m["kslT"] = np.ascontiguousarray(P[2][0:64]); m["kwT"] = np.ascontiguousarray(P[2][64:128])
        m["vsl"] = vaug(P[12][64:128]); m["vw"] = vaug(P[13][0:64])
        ag = misc[b][3 * i:3 * i + 3]
        m["ag"] = np.ascontiguousarray(ag.T.reshape(NKB, 128, 3).transpose(1, 0, 2))
        m["dq"] = head_rows(P, 4, i); m["dk"] = head_rows(P, 6, i); m["dv"] = vaug(head_rows(P, 14, i))
        m["sq"] = head_rows(P, 16, i); m["sk"] = head_rows(P, 18, i); m["sv"] = vaug(head_rows(P, 20, i))
        m["fq"] = head_rows(P, 8, i); m["fk"] = head_rows(P, 10, i); m["fv"] = vaug(head_rows(P, 22, i))
        m["logf"] = np.ascontiguousarray(misc[b][32 + i:33 + i])
        m["w1k"] = w1k; m["w1v"] = w1v; m["w2k"] = inp["nsa_ck_w2"][l]; m["w2v"] = inp["nsa_cv_w2"][l]
        m["pek"] = pek; m["pev"] = pev
        hs = np.zeros((128, 4), np.float32); hs[:, i] = 1.0
        m["hsel"] = hs
        maps.append(m)
    res = run_bass_kernel_spmd(cB.nc, maps, core_ids=list(range(8)))
    outs = []
    for b in range(2):
        ob = np.zeros((4, 256, S), bf)
        for i in range(4):
            o = res.results[b * 4 + i]["o"]
            for mm in range(4):
                tok = o[mm].transpose(1, 0, 2).reshape(S, 64)
                ob[mm, i * 64:(i + 1) * 64, :] = tok.T
        outs.append(ob)
    return outs


def build_C1(c=None):
    own = c is None
    if own:
        c = Ctx()
    ph = c.begin_phase()
    nc, s = c.nc, c.s
    oT_d = c.dram("oT", [4, 2, 128, NT], BF16, "ExternalInput")
    gates_d = c.dram("gates", [32, 128, NT], F32, "ExternalInput")
    xT_d = c.dram("xT", [128, 8, NT], F32, "ExternalInput")
    wbr_d = c.dram("wbr", [128, 8, 1024], F32, "ExternalInput")
    wout_d = c.dram("wout", [128, 8, 1024], F32, "ExternalInput")
    ga_d = c.dram("ga", [128, 8], F32, "ExternalInput")
    x1_d = c.dram("x1T", [128, 8, NT], F32, "ExternalOutput")

    xT = c.sb([128, 8, NT], F32)
    wbr = c.sb([128, 8, 1024], BF16)
    wout = c.sb([128, 8, 1024], BF16)
    ga = c.sb([128, 8], F32)
    stg = [c.sb([128, 2, 1024], F32) for _ in range(2)]
    s.dma(ga[:], ga_d, writes=[ga.res])
    for j in range(8):
        s.dma(xT[:, j, :], xT_d[:, j, :], writes=[xT.res], q=("sp", "act")[j % 2])
    k = 0
    for (dst, src) in ((wbr, wbr_d), (wout, wout_d)):
        for q in range(4):
            st = stg[k % 2]
            k += 1
            s.dma(st[:], src[:, 2 * q:2 * q + 2, :], writes=[st.res])
            s.op("pool", (lambda st=st, dst=dst, q=q: nc.gpsimd.tensor_copy(out=dst[:, 2 * q:2 * q + 2, :], in_=st[:])), reads=[st.res], writes=[dst.res])
    ot_r = [c.sb([128, 8, TT], BF16) for _ in range(2)]
    gt_r = [[c.sb([128, TT], F32) for _ in range(4)] for _ in range(3)]
    zT_r = [c.sb([128, 8, TT], BF16) for _ in range(2)]
    pm_r = [c.ps([128, TT]) for _ in range(4)]
    pmix = [c.ps([128, TT]) for _ in range(2)]
    t_r = [c.sb([128, TT], F32) for _ in range(4)]
    items = [(tt, dc) for tt in range(NT // TT) for dc in range(8)]

    def c_load(n):
        tt, dc = items[n]
        sl = slice(tt * TT, (tt + 1) * TT)
        if dc == 0:
            ot = ot_r[tt % 2]
            for m in range(4):
                for kc in range(2):
                    s.dma(ot[:, m * 2 + kc, :], oT_d[m, kc, :, sl], writes=[ot.res], q="act")
        gts = gt_r[n % 3]
        for m in range(4):
            s.dma(gts[m][:], gates_d[m * 8 + dc, :, sl], writes=[gts[m].res], q=("sp", "act")[m % 2])

    def c_comp(n):
        tt, dc = items[n]
        sl = slice(tt * TT, (tt + 1) * TT)
        ot = ot_r[tt % 2]
        gts = gt_r[n % 3]
        zT = zT_r[tt % 2]
        for m in range(4):
            for kc in range(2):
                s.op("pe", (lambda m=m, kc=kc: nc.tensor.matmul(pm_r[m][:], lhsT=wbr[:, m * 2 + kc, dc * 128:(dc + 1) * 128], rhs=ot[:, m * 2 + kc, :],
                                                                start=(kc == 0), stop=(kc == 1))),
                     reads=[wbr.res, ot.res], writes=[pm_r[m].res])
        for m in range(4):
            s.op("dve", (lambda m=m: nc.vector.tensor_tensor(out=t_r[m][:], in0=pm_r[m][:], in1=gts[m][:], op=ALU.mult)),
                 reads=[pm_r[m].res, gts[m].res], writes=[t_r[m].res])
        s.op("pool", lambda: nc.gpsimd.tensor_tensor(out=t_r[0][:], in0=t_r[0][:], in1=t_r[1][:], op=ALU.add), reads=[t_r[0].res, t_r[1].res], writes=[t_r[0].res])
        s.op("pool", lambda: nc.gpsimd.tensor_tensor(out=t_r[2][:], in0=t_r[2][:], in1=t_r[3][:], op=ALU.add), reads=[t_r[2].res, t_r[3].res], writes=[t_r[2].res])
        s.op("pool", lambda: nc.gpsimd.tensor_tensor(out=zT[:, dc, :], in0=t_r[0][:], in1=t_r[2][:], op=ALU.add),
             reads=[t_r[0].res, t_r[2].res], writes=[zT.res])
        if dc != 7:
            return
        for ec in range(8):
            pmx = pmix[ec % 2]
            for j in range(8):
                s.op("pe", (lambda j=j, ec=ec, pmx=pmx: nc.tensor.matmul(pmx[:], lhsT=wout[:, j, ec * 128:(ec + 1) * 128], rhs=zT[:, j, :],
                                                                         start=(j == 0), stop=(j == 7))),
                     reads=[wout.res, zT.res], writes=[pmx.res])
            s.op("dve", (lambda ec=ec, pmx=pmx: nc.vector.scalar_tensor_tensor(out=xT[:, ec, sl], in0=pmx[:], scalar=ga[:, ec:ec + 1], in1=xT[:, ec, sl],
                                                                              op0=ALU.mult, op1=ALU.add)),
                 reads=[pmx.res, ga.res, xT.res], writes=[xT.res])
    pipeline(len(items), [c_load, c_comp])
    for j in range(8):
        s.dma(x1_d[:, j, :], xT[:, j, :], reads=[xT.res], is_output=True)
    c.end_phase(ph)
    if own:
        c.close()
    return c


def maps_C1(inp, l, o, gates, xT_all, mod):
    wbr = np.ascontiguousarray(inp["w_branch"][l].reshape(4, 2, 128, D).transpose(2, 0, 1, 3).reshape(128, 8, D))
    wout = np.ascontiguousarray(inp["w_out"][l].reshape(8, 128, D).transpose(1, 0, 2))
    maps = []
    for core in range(8):
        b, q = core // 4, core % 4
        tsl = slice(q * NT, (q + 1) * NT)
        maps.append({"oT": np.ascontiguousarray(o[b][:, :, tsl].reshape(4, 2, 128, NT)),
                     "gates": np.ascontiguousarray(gates[b][:, :, tsl]),
                     "xT": np.ascontiguousarray(xT_all[b][:, tsl].reshape(8, 128, NT).transpose(1, 0, 2)),
                     "wbr": wbr, "wout": wout, "ga": np.ascontiguousarray(mod[l, b][:, 16:24])})
    return maps


def collect_x(results, key):
    out = np.zeros((2, D, S), np.float32)
    for core in range(8):
        b, q = core // 4, core % 4
        out[b][:, q * NT:(q + 1) * NT] = results[core][key].transpose(1, 0, 2).reshape(D, NT)
    return out


def run_C1(cC1, inp, l, o, gates, xT_all, mod):
    maps = maps_C1(inp, l, o, gates, xT_all, mod)
    res = run_bass_kernel_spmd(cC1.nc, maps, core_ids=list(range(8)))
    return collect_x(res.results, "x1T")


HT = 1024
NFC = 22


def build_C2(n_exp, moe, c=None):
    own = c is None
    if own:
        c = Ctx()
    ph = c.begin_phase()
    nc, s = c.nc, c.s
    x1_d = c.dram("x1T", [128, 8, NT], F32, "ExternalInput")
    mod_d = c.dram("modF", [128, 24], F32, "ExternalInput")
    gn_d = c.dram("gn", [128, 8], F32, "ExternalInput")
    w1_d = c.dram("w1", [n_exp, NFC, 128, 8, 128], F32, "ExternalInput")
    w3_d = c.dram("w3", [n_exp, NFC, 128, 8, 128], F32, "ExternalInput")
    w2_d = c.dram("w2", [n_exp, 8, 128, NFC, 128], F32, "ExternalInput")
    if moe:
        rw_d = c.dram("rw", [128, 8, 8], F32, "ExternalInput")
        oh_d = c.dram("onehot", [8, 8, 128], F32, "ExternalInput")
        id_d = c.dram("ident", [128, 128], F32, "ExternalInput")
    x2_d = c.dram("x2T", [128, 8, NT], F32, "ExternalOutput")

    acc = c.sb([128, 8, HT], F32)
    hT = c.sb([128, 8, HT], BF16)
    act = c.sb([128, NFC, HT], BF16)
    rs = c.sb([128, HT], F32)
    ones = c.sb([128, 128], BF16)
    sqring = [c.sb([128, TT], BF16) for _ in range(2)]
    epsb = c.sb([128, 1], F32)
    mod = c.sb([128, 24], F32)
    gn = c.sb([128, 8], F32)
    Acol = c.sb([128, 8], F32)
    Bcol = c.sb([128, 8], F32)
    ring = [c.sb([128, TT], F32) for _ in range(4)]
    w1s = [c.sb([128, 8, 128], F32) for _ in range(2)]
    w3s = [c.sb([128, 8, 128], F32) for _ in range(2)]
    w1b = [c.sb([128, 8, 128], BF16) for _ in range(3)]
    w3b = [c.sb([128, 8, 128], BF16) for _ in range(3)]
    w2s = [c.sb([128, NFC, 128], F32) for _ in range(2)]
    w2b = [c.sb([128, NFC, 128], BF16) for _ in range(3)]
    sa_r = [c.sb([128, TT], F32) for _ in range(3)]
    u_r = [c.sb([128, TT], F32) for _ in range(3)]
    pa_r = [c.ps([128, TT]) for _ in range(2)]
    pg_r = [c.ps([128, TT]) for _ in range(2)]
    po_r = [c.ps([128, TT]) for _ in range(2)]
    pbank = c.ps([128, TT])
    pmisc = c.ps([128, TT])
    s.op("pool", lambda: nc.gpsimd.memset(ones[:], 1.0), writes=[ones.res])
    s.op("pool", lambda: nc.gpsimd.memset(epsb[:], EPS), writes=[epsb.res])
    s.dma(mod[:], mod_d, writes=[mod.res])
    s.dma(gn[:], gn_d, writes=[gn.res])
    emit_AB(c, gn, mod, 0, 8, Acol, Bcol)
    if moe:
        rw = c.sb([128, 8, 8], F32)
        oh = c.sb([8, 8, 128], F32)
        ident = c.sb([128, 128], F32)
        GT = c.sb([8, HT], F32)
        gb_r = [c.sb([128, HT], F32) for _ in range(2)]
        h32_r = [c.sb([128, TT], F32) for _ in range(2)]
        lg = c.sb([128, 8, 8], F32)
        m8 = c.sb([128, 8], F32)
        nt1 = c.sb([128, 1], F32)
        e2 = c.sb([128, 1], F32)
        ex = c.sb([128, 8], F32)
        selm = c.sb([128, 8], F32)
        Gt = c.sb([128, 8], F32)
        for t, d in ((rw, rw_d), (oh, oh_d), (ident, id_d)):
            s.dma(t[:], d, writes=[t.res])

    for hf in range(NT // HT):
        hsl = slice(hf * HT, (hf + 1) * HT)
        for j in range(8):
            s.dma(acc[:, j, :], x1_d[:, j, hsl], writes=[acc.res], q=("sp", "act")[j % 2])
        if not moe:
            emit_modnorm(c, acc, hT, HT, ones, Acol, Bcol, epsb, ring, pbank, rs, sq_ring=sqring)
        else:
            def after_h(tmp, j, tt, sl):
                h32 = h32_r[j % 2]
                s.op("act", lambda: nc.scalar.activation(out=h32[:], in_=tmp[:], func=AF.Identity, scale=Acol[:, j:j + 1], bias=Bcol[:, j:j + 1]),
                     reads=[tmp.res, Acol.res, Bcol.res], writes=[h32.res])
                s.op("pool", lambda: nc.gpsimd.tensor_copy(out=hT[:, j, sl], in_=h32[:]), reads=[h32.res], writes=[hT.res])
                for tb in range(4):
                    col = (tt * 4 + tb) * 8
                    s.op("pe", (lambda tb=tb, col=col: nc.tensor.matmul(pmisc[:, col:col + 8], lhsT=h32[:, tb * 128:(tb + 1) * 128], rhs=rw[:, j, :],
                                                                        start=(j == 0 and tb == 0 and tt == 0), stop=(j == 7), skip_group_check=True)),
                         reads=[h32.res, rw.res], writes=[pmisc.res])
            emit_modnorm(c, acc, hT, HT, ones, Acol, Bcol, epsb, ring, pbank, rs, after_h=after_h, sq_ring=sqring)
            s.op("act", lambda: nc.scalar.copy(out=lg[:], in_=pmisc[:, 0:64].rearrange("p (a b) -> p a b", b=8)), reads=[pmisc.res], writes=[lg.res])
            for tb in range(8):
                s.op("dve", (lambda tb=tb: nc.vector.max(out=m8[:], in_=lg[:, tb, :])), reads=[lg.res], writes=[m8.res])
                s.op("dve", lambda: nc.vector.tensor_scalar(out=nt1[:], in0=m8[:, 0:1], scalar1=-1.0, scalar2=None, op0=ALU.mult), reads=[m8.res], writes=[nt1.res])
                s.op("act", (lambda tb=tb: nc.scalar.activation(out=ex[:], in_=lg[:, tb, :], func=AF.Exp, bias=nt1[:], scale=1.0)),
                     reads=[lg.res, nt1.res], writes=[ex.res])
                s.op("act", lambda: nc.scalar.activation(out=e2[:], in_=m8[:, 1:2], func=AF.Exp, bias=nt1[:], scale=1.0), reads=[m8.res, nt1.res], writes=[e2.res])
                s.op("dve", lambda: nc.vector.tensor_scalar(out=e2[:], in0=e2[:], scalar1=1.0, scalar2=None, op0=ALU.add), reads=[e2.res], writes=[e2.res])
                s.op("dve", lambda: nc.vector.reciprocal(out=e2[:], in_=e2[:]), reads=[e2.res], writes=[e2.res])
                s.op("dve", (lambda tb=tb: nc.vector.tensor_scalar(out=selm[:], in0=lg[:, tb, :], scalar1=m8[:, 1:2], scalar2=None, op0=ALU.is_ge)),
                     reads=[lg.res, m8.res], writes=[selm.res])
                s.op("dve", lambda: nc.vector.scalar_tensor_tensor(out=Gt[:], in0=ex[:], scalar=e2[:, 0:1], in1=selm[:], op0=ALU.mult, op1=ALU.mult),
                     reads=[ex.res, e2.res, selm.res], writes=[Gt.res])
                s.op("pe", (lambda tb=tb: nc.tensor.transpose(pbank[0:8, (tb % 4) * 128:(tb % 4 + 1) * 128], Gt[:], ident[:])), reads=[Gt.res, ident.res], writes=[pbank.res])
                if tb % 4 == 3:
                    q4 = tb // 4
                    s.op("act", (lambda q4=q4: nc.scalar.copy(out=GT[:, q4 * 512:(q4 + 1) * 512], in_=pbank[0:8, 0:512])), reads=[pbank.res], writes=[GT.res])

        items = []
        kf = kg = 0
        for e in range(n_exp):
            for fc in range(NFC):
                for tt in range(2):
                    items.append(("f", e, fc, tt, kf))
                kf += 1
            for ec in range(8):
                for tt in range(2):
                    items.append(("g", e, ec, tt, kg))
                kg += 1

        def s_load(n):
            kind, e, ci, tt, k = items[n]
            if tt != 0:
                return
            if kind == "f":
                if moe and ci == 0:
                    g_b = gb_r[e % 2]
                    for t2 in range(2):
                        s.op("pe", (lambda t2=t2, e=e: nc.tensor.matmul(pmisc[:], lhsT=oh[:, e, :], rhs=GT[:, t2 * TT:(t2 + 1) * TT], start=True, stop=True)),
                             reads=[oh.res, GT.res], writes=[pmisc.res])
                        s.op("act", (lambda t2=t2, g_b=g_b: nc.scalar.copy(out=g_b[:, t2 * TT:(t2 + 1) * TT], in_=pmisc[:])), reads=[pmisc.res], writes=[g_b.res])
                s.dma(w1s[k % 2][:], w1_d[e, ci], writes=[w1s[k % 2].res], q="act")
                s.dma(w3s[k % 2][:], w3_d[e, ci], writes=[w3s[k % 2].res], q="act")
            else:
                s.dma(w2s[k % 2][:], w2_d[e, ci], writes=[w2s[k % 2].res], q="act")

        def s_cast(n):
            kind, e, ci, tt, k = items[n]
            if tt != 0:
                return
            if kind == "f":
                s.op("pool", lambda: nc.gpsimd.tensor_copy(out=w1b[k % 3][:], in_=w1s[k % 2][:]), reads=[w1s[k % 2].res], writes=[w1b[k % 3].res])
                s.op("dve", lambda: nc.vector.tensor_copy(out=w3b[k % 3][:], in_=w3s[k % 2][:]), reads=[w3s[k % 2].res], writes=[w3b[k % 3].res])
            else:
                half = NFC // 2
                s.op("pool", lambda: nc.gpsimd.tensor_copy(out=w2b[k % 3][:, 0:half, :], in_=w2s[k % 2][:, 0:half, :]), reads=[w2s[k % 2].res], writes=[w2b[k % 3].res])
                s.op("dve", lambda: nc.vector.tensor_copy(out=w2b[k % 3][:, half:NFC, :], in_=w2s[k % 2][:, half:NFC, :]), reads=[w2s[k % 2].res], writes=[w2b[k % 3].res])

        def s_mm(n):
            kind, e, ci, tt, k = items[n]
            sl = slice(tt * TT, (tt + 1) * TT)
            if kind == "f":
                pa, pg = pa_r[n % 2], pg_r[n % 2]
                for j in range(8):
                    s.op("pe", (lambda j=j: nc.tensor.matmul(pa[:], lhsT=w1b[k % 3][:, j, :], rhs=hT[:, j, sl], start=(j == 0), stop=(j == 7))),
                         reads=[w1b[k % 3].res, hT.res], writes=[pa.res])
                for j in range(8):
                    s.op("pe", (lambda j=j: nc.tensor.matmul(pg[:], lhsT=w3b[k % 3][:, j, :], rhs=hT[:, j, sl], start=(j == 0), stop=(j == 7))),
                         reads=[w3b[k % 3].res, hT.res], writes=[pg.res])
            else:
                po = po_r[n % 2]
                for fc in range(NFC):
                    s.op("pe", (lambda fc=fc: nc.tensor.matmul(po[:], lhsT=w2b[k % 3][:, fc, :], rhs=act[:, fc, sl], start=(fc == 0), stop=(fc == NFC - 1))),
                         reads=[w2b[k % 3].res, act.res], writes=[po.res])

        def s_post(n):
            kind, e, ci, tt, k = items[n]
            sl = slice(tt * TT, (tt + 1) * TT)
            if kind == "f":
                pa, pg = pa_r[n % 2], pg_r[n % 2]
                sa = sa_r[n % 3]
                s.op("act", lambda: nc.scalar.activation(out=sa[:], in_=pa[:], func=AF.Silu), reads=[pa.res], writes=[sa.res])
                if not moe:
                    s.op("dve", lambda: nc.vector.tensor_tensor(out=act[:, ci, sl], in0=sa[:], in1=pg[:], op=ALU.mult), reads=[sa.res, pg.res], writes=[act.res])
                else:
                    u = u_r[n % 3]
                    g_b = gb_r[e % 2]
                    s.op("dve", lambda: nc.vector.tensor_tensor(out=u[:], in0=pg[:], in1=g_b[:, sl], op=ALU.mult), reads=[pg.res, g_b.res], writes=[u.res])
                    s.op("pool", lambda: nc.gpsimd.tensor_tensor(out=act[:, ci, sl], in0=sa[:], in1=u[:], op=ALU.mult), reads=[sa.res, u.res], writes=[act.res])
            else:
                po = po_r[n % 2]
                s.op("dve", lambda: nc.vector.scalar_tensor_tensor(out=acc[:, ci, sl], in0=po[:], scalar=mod[:, 16 + ci:17 + ci], in1=acc[:, ci, sl],
                                                                   op0=ALU.mult, op1=ALU.add), reads=[po.res, mod.res, acc.res], writes=[acc.res])
        pipeline(len(items), [s_load, s_cast, s_mm, s_post])
        for j in range(8):
            s.dma(x2_d[:, j, hsl], acc[:, j, :], reads=[acc.res], is_output=True)
    c.end_phase(ph)
    if own:
        c.close()
    return c


def maps_C2(inp, l, x1T_all, mod, moe):
    if moe:
        w1, w3, w2 = inp["moe_w1"][l // 2], inp["moe_w3"][l // 2], inp["moe_w2"][l // 2]
    else:
        w1, w3, w2 = inp["ffn_w1"][l // 2][None], inp["ffn_w3"][l // 2][None], inp["ffn_w2"][l // 2][None]
    E = w1.shape[0]
    lay1 = lambda w: np.ascontiguousarray(w.reshape(E, 8, 128, NFC, 128).transpose(0, 3, 2, 1, 4))
    lay2 = lambda w: np.ascontiguousarray(w.reshape(E, NFC, 128, 8, 128).transpose(0, 3, 2, 1, 4))
    w1l, w3l, w2l = lay1(w1), lay1(w3), lay2(w2)
    gn = np.ascontiguousarray(inp["norm_ffn"][l].reshape(8, 128).T)
    maps = []
    for core in range(8):
        b, q = core // 4, core % 4
        m = {"modF": np.ascontiguousarray(mod[l, b][:, 24:48]), "gn": gn, "w1": w1l, "w3": w3l, "w2": w2l}
        if x1T_all is not None:
            m["x1T"] = np.ascontiguousarray(x1T_all[b][:, q * NT:(q + 1) * NT].reshape(8, 128, NT).transpose(1, 0, 2))
        if moe:
            m["rw"] = np.ascontiguousarray(inp["router_w"][l // 2].reshape(8, 128, 8).transpose(1, 0, 2))
            oh = np.zeros((8, 8, 128), np.float32)
            for e in range(8):
                oh[e, e, :] = 1.0
            m["onehot"] = oh
            m["ident"] = np.eye(128, dtype=np.float32)
        maps.append(m)
    return maps


def run_C2(cC2, inp, l, x1T_all, mod, moe):
    maps = maps_C2(inp, l, x1T_all, mod, moe)
    res = run_bass_kernel_spmd(cC2.nc, maps, core_ids=list(range(8)))
    return collect_x(res.results, "x2T")


def build_CA(moe, with_A):
    c = Ctx()
    c.alias = {}
    c.pre = "c1_"
    c.kind_override = {"c1_x1T": "Internal"}
    build_C1(c)
    c.pre = "c2_"
    c.alias["c2_x1T"] = c.made["c1_x1T"]
    build_C2(8 if moe else 1, moe, c)
    if with_A:
        c.pre = "a_"
        c.alias["a_xT"] = c.made["c2_x2T"]
        build_A(c)
    c.close()
    return c


def run_CA(cCA, inp, l, o, gates, xT_all, mod, moe, with_A):
    m1 = maps_C1(inp, l, o, gates, xT_all, mod)
    m2 = maps_C2(inp, l, None, mod, moe)
    m3 = maps_A(inp, l + 1, None, mod) if with_A else [dict() for _ in range(8)]
    maps = []
    for i in range(8):
        m = {"c1_" + k: v for k, v in m1[i].items()}
        m.update({"c2_" + k: v for k, v in m2[i].items()})
        m.update({"a_" + k: v for k, v in m3[i].items()})
        maps.append(m)
    res = run_bass_kernel_spmd(cCA.nc, maps, core_ids=list(range(8)))
    x2T = collect_x(res.results, "c2_x2T")
    nxt = collect_A(res.results, "a_") if with_A else None
    return x2T, nxt


_PROGS = {}


def _prog(name, fn):
    if name not in _PROGS:
        _PROGS[name] = fn()
    return _PROGS[name]


def kernel(**inp):
    inp = {k: np.asarray(v) for k, v in inp.items()}
    mod = run_M(inp)
    xT_all = np.ascontiguousarray(inp["x"].astype(np.float32).transpose(0, 2, 1))
    cA = _prog("A", build_A)
    proj, gates, misc = run_A(cA, inp, 0, xT_all, mod)
    cB = _prog("B", build_B)
    for l in range(2):
        o = run_B(cB, inp, l, proj, misc)
        moe = (l % 2 == 1)
        with_A = (l == 0)
        cCA = _prog("CA%d" % l, lambda: build_CA(moe, with_A))
        xT_all, nxt = run_CA(cCA, inp, l, o, gates, xT_all, mod, moe, with_A)
        if with_A:
            proj, gates, misc = nxt
    return np.ascontiguousarray(xT_all.transpose(0, 2, 1)).astype(np.float32)
```
